# Optimizing a Trainium2 kernel written in Bass

```python
import math
import jax
import jax.numpy as jnp
from jax import lax
import numpy as np

D_MODEL = 1024
BATCH = 2
SEQ = 16384
DEPTH = 2

GRID_W = 64
CTX_LEN = 256
Q_BLOCK = 128
ROPE_THETA = 10000.0
NORM_EPS = 1e-6
ALPHA = (2 * DEPTH) ** 0.25
BETA = (8 * DEPTH) ** -0.25
N_EVEN = (DEPTH + 1) // 2
N_ODD = DEPTH // 2
HEAD_DIM = 64
A_Q_HEADS = 12
A_KV_HEADS = 4
POOL_WINDOWS = (2, 4, 8, 16)
POOL_GROUP = 64
POOL_WIDTH = POOL_GROUP * len(POOL_WINDOWS)
C_HEADS = 8
C_Q_RANK = 384
C_KV_RANK = 256
C_NOPE = 64
C_ROPE = 32
C_V = 64
D_HEADS = 8
NA_WIN_H = 8
NA_WIN_W = 16
D_FF = 2816
N_EXPERTS = 8
TOP_K = 2
D_FF_EXPERT = 3584
MOE_BLOCK = 256

A_Q_W = A_Q_HEADS * HEAD_DIM
A_KV_W = A_KV_HEADS * HEAD_DIM
EVEN_IN = A_Q_W + 2 * A_KV_W + POOL_WIDTH
EVEN_MIX = A_Q_W + POOL_WIDTH
EVEN_SPLITS = (A_Q_W, A_Q_W + A_KV_W, A_Q_W + 2 * A_KV_W)
NA_W = D_HEADS * HEAD_DIM
ODD_IN = C_Q_RANK + C_KV_RANK + C_ROPE + 3 * NA_W
ODD_MIX = C_HEADS * C_V + NA_W
ODD_SPLITS = (C_Q_RANK, C_Q_RANK + C_KV_RANK, C_Q_RANK + C_KV_RANK + C_ROPE,
              C_Q_RANK + C_KV_RANK + C_ROPE + NA_W, C_Q_RANK + C_KV_RANK + C_ROPE + 2 * NA_W)

kernel_name = 'hybrid_flow_backbone'


def rms_norm(x, g):
    xf = x.astype(jnp.float32)
    y = xf * lax.rsqrt(jnp.mean(xf * xf, axis=-1, keepdims=True) + NORM_EPS)
    return (y * g.astype(jnp.float32)).astype(x.dtype)


def layer_norm(x, g, b):
    xf = x.astype(jnp.float32)
    mu = jnp.mean(xf, axis=-1, keepdims=True)
    var = jnp.mean(jnp.square(xf - mu), axis=-1, keepdims=True)
    y = (xf - mu) * lax.rsqrt(var + NORM_EPS)
    return (y * g.astype(jnp.float32) + b.astype(jnp.float32)).astype(x.dtype)


def modulate(x, shift, scale):
    return x * (1 + scale) + shift


def grid_positions(length):
    t = jnp.arange(length, dtype=jnp.int32)
    return (t // GRID_W).astype(jnp.float32), (t % GRID_W).astype(jnp.float32)


def rope_axis(x, pos):
    half = x.shape[-1] // 2
    inv = ROPE_THETA ** (-jnp.arange(half, dtype=jnp.float32) / half)
    ang = pos[:, None] * inv[None, :]
    cos = jnp.cos(ang)[None, :, None, :]
    sin = jnp.sin(ang)[None, :, None, :]
    x1, x2 = x[..., :half], x[..., half:]
    return jnp.concatenate([x1 * cos - x2 * sin, x1 * sin + x2 * cos], axis=-1).astype(x.dtype)


def rope_2d(x, row, col):
    n = x.shape[-1] // 2
    return jnp.concatenate([rope_axis(x[..., :n], row), rope_axis(x[..., n:], col)], axis=-1)


def block_attention(q, k, v):
    b, l, hq, dk = q.shape
    hkv, dv = k.shape[2], v.shape[-1]
    g = hq // hkv
    nb = l // Q_BLOCK
    scale = dk ** -0.5
    qb = q.reshape(b, nb, Q_BLOCK, hkv, g, dk).transpose(1, 0, 2, 3, 4, 5)

    def one(q_blk):
        s = jnp.einsum('bqhgd,bkhd->bhgqk', q_blk, k, preferred_element_type=jnp.float32) * scale
        p = jax.nn.softmax(s, axis=-1).astype(v.dtype)
        return jnp.einsum('bhgqk,bkhd->bqhgd', p, v)

    o = lax.map(one, qb)
    return o.transpose(1, 0, 2, 3, 4, 5).reshape(b, l, hq * dv)


def multiscale_pool(x, w_pool, pool_scale):
    b, l, _ = x.shape
    ng = len(POOL_WINDOWS)
    xg = x.reshape(b, l, ng, POOL_GROUP).astype(jnp.float32)
    csum = jnp.concatenate([jnp.zeros((b, 1, ng, POOL_GROUP), jnp.float32), jnp.cumsum(xg, axis=1)], axis=1)
    win = jnp.array(POOL_WINDOWS, dtype=jnp.int32)
    lo = win // 2
    hi = win - lo
    t = jnp.arange(l, dtype=jnp.int32)[:, None]
    start = jnp.clip(t - lo[None, :], 0, l)
    end = jnp.clip(t + hi[None, :], 0, l)
    gidx = jnp.arange(ng)[None, :]
    mean = (csum[:, end, gidx] - csum[:, start, gidx]) / (end - start).astype(jnp.float32)[None, :, :, None]
    y = (mean - xg).astype(x.dtype)
    y = jnp.einsum('blgc,gcd->blgd', y, w_pool).reshape(b, l, POOL_WIDTH)
    return y * pool_scale


def neighborhood_attention(q, k, v, k_ctx, v_ctx, rel_bias):
    b, l, h, d = q.shape
    rows = l // GRID_W
    kh = min(NA_WIN_H, rows)
    kw = NA_WIN_W
    scale = d ** -0.5
    qg = q.reshape(b, rows, GRID_W, h, d).transpose(1, 0, 2, 3, 4)
    kg = k.reshape(b, rows, GRID_W, h, d)
    vg = v.reshape(b, rows, GRID_W, h, d)
    j = jnp.arange(GRID_W, dtype=jnp.int32)
    col_start = jnp.clip(j - kw // 2, 0, GRID_W - kw)
    col_valid = (j[None, :] >= col_start[:, None]) & (j[None, :] < col_start[:, None] + kw)
    col_idx = jnp.clip(j[None, :] - j[:, None] + NA_WIN_W - 1, 0, 2 * NA_WIN_W - 2)

    def one_row(args):
        q_row, r = args
        rs = jnp.clip(r - kh // 2, 0, rows - kh)
        k_band = lax.dynamic_slice_in_dim(kg, rs, kh, axis=1)
        v_band = lax.dynamic_slice_in_dim(vg, rs, kh, axis=1)
        row_idx = rs + jnp.arange(kh, dtype=jnp.int32) - r + NA_WIN_H - 1
        bias = rel_bias[:, row_idx[:, None, None], col_idx[None, :, :]]
        s = jnp.einsum('bqhd,bikhd->bhqik', q_row, k_band, preferred_element_type=jnp.float32) * scale
        s = s + bias.transpose(0, 2, 1, 3)[None].astype(jnp.float32)
        s = jnp.where(col_valid[None, None, :, None, :], s, -jnp.inf)
        s_ctx = jnp.einsum('bqhd,bkhd->bhqk', q_row, k_ctx, preferred_element_type=jnp.float32) * scale
        p = jax.nn.softmax(jnp.concatenate([s.reshape(b, h, GRID_W, kh * GRID_W), s_ctx], axis=-1), axis=-1)
        p = p.astype(v.dtype)
        p_win = p[..., :kh * GRID_W].reshape(b, h, GRID_W, kh, GRID_W)
        p_ctx = p[..., kh * GRID_W:]
        return (jnp.einsum('bhqik,bikhd->bqhd', p_win, v_band)
                + jnp.einsum('bhqk,bkhd->bqhd', p_ctx, v_ctx))

    o = lax.map(one_row, (qg, jnp.arange(rows, dtype=jnp.int32)))
    return o.transpose(1, 0, 2, 3, 4).reshape(b, l, h * d)


def swiglu(u, w_gate, w_up, w_down):
    return (jax.nn.silu(u @ w_gate) * (u @ w_up)) @ w_down


def moe_swiglu(u, w_router, b_router, w_gate, w_up, w_down):
    shp = u.shape
    x = u.reshape(-1, shp[-1])
    t = x.shape[0]
    logits = jnp.dot(x, w_router, preferred_element_type=jnp.float32) + b_router.astype(jnp.float32)
    top_val, top_idx = lax.top_k(logits, TOP_K)
    gates = jax.nn.softmax(top_val, axis=-1)
    n_assign = t * TOP_K
    e_flat = top_idx.reshape(-1)
    order = jnp.argsort(e_flat)
    e_sorted = e_flat[order]
    tok_sorted = order // TOP_K
    g_sorted = gates.reshape(-1)[order]
    counts = jnp.bincount(e_flat, length=N_EXPERTS)
    padded = (counts + MOE_BLOCK - 1) // MOE_BLOCK * MOE_BLOCK
    start = jnp.cumsum(counts) - counts
    pad_end = jnp.cumsum(padded)
    pad_start = pad_end - padded
    dest = pad_start[e_sorted] + jnp.arange(n_assign, dtype=jnp.int32) - start[e_sorted]
    n_blocks = -(-n_assign // MOE_BLOCK) + N_EXPERTS
    x_pad = jnp.zeros((n_blocks * MOE_BLOCK, shp[-1]), x.dtype).at[dest].set(x[tok_sorted])
    blk_expert = jnp.minimum(
        jnp.searchsorted(pad_end, jnp.arange(n_blocks, dtype=jnp.int32) * MOE_BLOCK, side='right'),
        N_EXPERTS - 1)

    def one_block(args):
        xb, e = args
        return swiglu(xb, w_gate[e], w_up[e], w_down[e])

    y_pad = lax.map(one_block, (x_pad.reshape(n_blocks, MOE_BLOCK, shp[-1]), blk_expert))
    y_pad = y_pad.reshape(n_blocks * MOE_BLOCK, shp[-1])
    y = jnp.zeros_like(x).at[tok_sorted].add(y_pad[dest] * g_sorted[:, None].astype(x.dtype))
    return y.reshape(shp)


def even_mixer(u, u_ctx, w_in, w_out, q_gain, k_gain, w_pool, pool_scale, row, col, need_ctx):
    def project(h):
        b, l, _ = h.shape
        q, k, v, p = jnp.split(h, EVEN_SPLITS, axis=-1)
        q = rms_norm(q.reshape(b, l, A_Q_HEADS, HEAD_DIM), q_gain)
        k = rms_norm(k.reshape(b, l, A_KV_HEADS, HEAD_DIM), k_gain)
        return q, k, v.reshape(b, l, A_KV_HEADS, HEAD_DIM), p

    q, k, v, p = project(u @ w_in)
    qc, kc, vc, pc = project(u_ctx @ w_in)
    q = rope_2d(q, row, col)
    k = rope_2d(k, row, col)
    attn = block_attention(q, jnp.concatenate([kc, k], axis=1), jnp.concatenate([vc, v], axis=1))
    y = jnp.concatenate([attn, multiscale_pool(p, w_pool, pool_scale)], axis=-1) @ w_out
    y_ctx = None
    if need_ctx:
        attn_c = block_attention(qc, kc, vc)
        y_ctx = jnp.concatenate([attn_c, multiscale_pool(pc, w_pool, pool_scale)], axis=-1) @ w_out
    return y, y_ctx


def odd_mixer(u, u_ctx, w_in, w_out, q_lat_gain, kv_lat_gain, w_q_up, w_kv_up, na_bias, row, col, need_ctx):
    def project(h, rotate):
        b, l, _ = h.shape
        cq, ckv, kr, nq, nk, nv = jnp.split(h, ODD_SPLITS, axis=-1)
        q = (rms_norm(cq, q_lat_gain) @ w_q_up).reshape(b, l, C_HEADS, C_NOPE + C_ROPE)
        kv = (rms_norm(ckv, kv_lat_gain) @ w_kv_up).reshape(b, l, C_HEADS, C_NOPE + C_V)
        q_nope, q_rope = q[..., :C_NOPE], q[..., C_NOPE:]
        k_nope, v = kv[..., :C_NOPE], kv[..., C_NOPE:]
        kr = kr.reshape(b, l, 1, C_ROPE)
        if rotate:
            q_rope = rope_2d(q_rope, row, col)
            kr = rope_2d(kr, row, col)
        q = jnp.concatenate([q_nope, q_rope], axis=-1)
        k = jnp.concatenate([k_nope, jnp.broadcast_to(kr, (b, l, C_HEADS, C_ROPE))], axis=-1)
        shape_na = (b, l, D_HEADS, HEAD_DIM)
        return q, k, v, nq.reshape(shape_na), nk.reshape(shape_na), nv.reshape(shape_na)

    q, k, v, nq, nk, nv = project(u @ w_in, True)
    qc, kc, vc, nqc, nkc, nvc = project(u_ctx @ w_in, False)
    mla = block_attention(q, jnp.concatenate([kc, k], axis=1), jnp.concatenate([vc, v], axis=1))
    na = neighborhood_attention(nq, nk, nv, nkc, nvc, na_bias)
    y = jnp.concatenate([mla, na], axis=-1) @ w_out
    y_ctx = None
    if need_ctx:
        y_ctx = jnp.concatenate([block_attention(qc, kc, vc), block_attention(nqc, nkc, nvc)], axis=-1) @ w_out
    return y, y_ctx


def setup_inputs(seed: int = 0) -> dict:
    key = jax.random.key(seed)
    ks = iter(jax.random.split(key, 48))
    f32 = jnp.float32

    def nrm(shape, fan_in, s=1.0):
        return jax.random.normal(next(ks), shape, f32) * (s * fan_in ** -0.5)

    def gain(shape):
        return 1.0 + 0.05 * jax.random.normal(next(ks), shape, f32)

    def small(shape, s):
        return s * jax.random.normal(next(ks), shape, f32)

    d = D_MODEL
    return {
        'x': jax.random.normal(next(ks), (BATCH, SEQ, d), f32),
        'c': jax.random.normal(next(ks), (BATCH, d), f32),
        'ctx': jax.random.normal(next(ks), (BATCH, CTX_LEN, d), f32),
        'c_ctx': jax.random.normal(next(ks), (d,), f32),
        'w_ada': nrm((DEPTH, d, 6 * d), d, 0.5),
        'b_ada': small((DEPTH, 6 * d), 0.02),
        'ln1_g': gain((DEPTH, d)),
        'ln1_b': small((DEPTH, d), 0.02),
        'ln2_g': gain((DEPTH, d)),
        'ln2_b': small((DEPTH, d), 0.02),
        'ev_w_in': nrm((N_EVEN, d, EVEN_IN), d),
        'ev_w_out': nrm((N_EVEN, EVEN_MIX, d), EVEN_MIX, BETA),
        'ev_q_gain': gain((N_EVEN, HEAD_DIM)),
        'ev_k_gain': gain((N_EVEN, HEAD_DIM)),
        'ev_w_pool': nrm((N_EVEN, len(POOL_WINDOWS), POOL_GROUP, POOL_GROUP), POOL_GROUP),
        'ev_pool_scale': gain((N_EVEN, POOL_WIDTH)),
        'ev_w_gate': nrm((N_EVEN, d, D_FF), d),
        'ev_w_up': nrm((N_EVEN, d, D_FF), d),
        'ev_w_down': nrm((N_EVEN, D_FF, d), D_FF, BETA),
        'od_w_in': nrm((N_ODD, d, ODD_IN), d),
        'od_w_out': nrm((N_ODD, ODD_MIX, d), ODD_MIX, BETA),
        'od_q_lat_gain': gain((N_ODD, C_Q_RANK)),
        'od_kv_lat_gain': gain((N_ODD, C_KV_RANK)),
        'od_w_q_up': nrm((N_ODD, C_Q_RANK, C_HEADS * (C_NOPE + C_ROPE)), C_Q_RANK),
        'od_w_kv_up': nrm((N_ODD, C_KV_RANK, C_HEADS * (C_NOPE + C_V)), C_KV_RANK),
        'od_na_bias': small((N_ODD, D_HEADS, 2 * NA_WIN_H - 1, 2 * NA_WIN_W - 1), 0.2),
        'od_w_router': nrm((N_ODD, d, N_EXPERTS), d),
        'od_b_router': small((N_ODD, N_EXPERTS), 0.01),
        'od_w_gate': nrm((N_ODD, N_EXPERTS, d, D_FF_EXPERT), d),
        'od_w_up': nrm((N_ODD, N_EXPERTS, d, D_FF_EXPERT), d),
        'od_w_down': nrm((N_ODD, N_EXPERTS, D_FF_EXPERT, d), D_FF_EXPERT, BETA),
    }


def reference(x, c, ctx, c_ctx, w_ada, b_ada, ln1_g, ln1_b, ln2_g, ln2_b,
              ev_w_in, ev_w_out, ev_q_gain, ev_k_gain, ev_w_pool, ev_pool_scale,
              ev_w_gate, ev_w_up, ev_w_down,
              od_w_in, od_w_out, od_q_lat_gain, od_kv_lat_gain, od_w_q_up, od_w_kv_up, od_na_bias,
              od_w_router, od_b_router, od_w_gate, od_w_up, od_w_down):
    row, col = grid_positions(x.shape[1])
    silu_c = jax.nn.silu(c)
    silu_cc = jax.nn.silu(c_ctx)
    x_ctx = ctx
    for l in range(DEPTH):
        need_ctx = l < DEPTH - 1
        i = l // 2
        mod = jnp.split((silu_c @ w_ada[l] + b_ada[l])[:, None, :], 6, axis=-1)
        mod_c = jnp.split((silu_cc @ w_ada[l] + b_ada[l])[None, None, :], 6, axis=-1)
        u = modulate(x, mod[0], mod[1])
        u_c = modulate(x_ctx, mod_c[0], mod_c[1])
        if l % 2 == 0:
            y, y_c = even_mixer(u, u_c, ev_w_in[i], ev_w_out[i], ev_q_gain[i], ev_k_gain[i],
                                ev_w_pool[i], ev_pool_scale[i], row, col, need_ctx)
            ffn = lambda h, i=i: swiglu(h, ev_w_gate[i], ev_w_up[i], ev_w_down[i])
        else:
            y, y_c = odd_mixer(u, u_c, od_w_in[i], od_w_out[i], od_q_lat_gain[i], od_kv_lat_gain[i],
                               od_w_q_up[i], od_w_kv_up[i], od_na_bias[i], row, col, need_ctx)
            ffn = lambda h, i=i: moe_swiglu(h, od_w_router[i], od_b_router[i],
                                            od_w_gate[i], od_w_up[i], od_w_down[i])
        x = layer_norm(ALPHA * x + mod[2] * y, ln1_g[l], ln1_b[l])
        x = layer_norm(ALPHA * x + mod[5] * ffn(modulate(x, mod[3], mod[4])), ln2_g[l], ln2_b[l])
        if need_ctx:
            x_ctx = layer_norm(ALPHA * x_ctx + mod_c[2] * y_c, ln1_g[l], ln1_b[l])
            x_ctx = layer_norm(ALPHA * x_ctx + mod_c[5] * ffn(modulate(x_ctx, mod_c[3], mod_c[4])),
                               ln2_g[l], ln2_b[l])
    return x
```

```python
import contextlib
import numpy as np
import ml_dtypes
import concourse.bass as bass
import concourse.mybir as mybir
from concourse.bass_utils import run_bass_kernel_spmd

F32 = mybir.dt.float32
BF16 = mybir.dt.bfloat16
AF = mybir.ActivationFunctionType
ALU = mybir.AluOpType
NPBF = ml_dtypes.bfloat16

NCORES = 8
D = 1024
SEQ = 16384
TOWN = 4096
CTX = 256
NT = TOWN + CTX
GRID_W = 64
EPS = 1e-6
ALPHA = 4 ** 0.25
ENGS = ("pe", "act", "dve", "pool", "sp")


class Prog:
    def __init__(self, nc):
        self.nc = nc
        self.ops = []
        self.last_w = {}
        self.readers = {}
        self.dma_cnt = {}
        self.excl = set()

    def _deps(self, reads, writes):
        ex = [r for r in reads if r in self.excl]
        if ex:
            reads = [r for r in reads if r not in self.excl]
            writes = list(writes) + ex
        deps = set()
        for r in reads:
            w = self.last_w.get(r)
            if w is not None:
                deps.add(w)
        for r in writes:
            deps.update(self.readers.get(r, ()))
            w = self.last_w.get(r)
            if w is not None:
                deps.add(w)
        i = len(self.ops)
        for r in reads:
            self.readers.setdefault(r, []).append(i)
        for r in writes:
            self.last_w[r] = i
            self.readers[r] = []
        return deps

    def op(self, eng, fn, reads=(), writes=(), fence=False):
        deps = self._deps(reads, writes)
        self.ops.append(dict(eng=eng, fn=fn, deps=deps, dma=None, sig=None, need=False, fence=fence))

    def dma(self, queue, key, fn, reads=(), writes=(), n=1):
        deps = self._deps(reads, writes)
        c = self.dma_cnt.get(key, 0) + 16 * n
        self.dma_cnt[key] = c
        self.ops.append(dict(eng=queue, fn=fn, deps=deps, dma=key, sig=(("dma", key), c), need=True, fence=True))

    def emit(self):
        nc = self.nc
        ops = self.ops
        for o in ops:
            for d in o["deps"]:
                p = ops[d]
                if p["dma"] is None and (p["eng"] != o["eng"] or o["fence"]):
                    p["need"] = True
        cnt = {e: 0 for e in ENGS}
        for o in ops:
            if o["dma"] is None and o["need"]:
                cnt[o["eng"]] += 1
                o["sig"] = (("eng", o["eng"]), cnt[o["eng"]])
        with contextlib.ExitStack() as st:
            sems = {}
            for e in ENGS:
                if cnt[e]:
                    sems[("eng", e)] = st.enter_context(nc.semaphore("s_" + e))
            for k in self.dma_cnt:
                sems[("dma", k)] = st.enter_context(nc.semaphore("d_" + str(k)))
            block = st.enter_context(nc.Block())
            per = {e: [] for e in ENGS}
            for i, o in enumerate(ops):
                per[o["eng"]].append(i)
            finals = [(("dma", k), c) for k, c in self.dma_cnt.items()]

            def body(ename):
                def run(eng):
                    waited = {}
                    for i in per[ename]:
                        o = ops[i]
                        need = {}
                        for d in o["deps"]:
                            p = ops[d]
                            if p["dma"] is None and p["eng"] == ename and not o["fence"]:
                                continue
                            sk, v = p["sig"]
                            if v > need.get(sk, 0):
                                need[sk] = v
                        for sk, v in need.items():
                            if waited.get(sk, 0) >= v:
                                continue
                            eng.wait_ge(sems[sk], v)
                            waited[sk] = v
                        if o["dma"] is not None:
                            o["fn"](eng, sems[("dma", o["dma"])])
                        else:
                            ins = o["fn"](eng)
                            if o["need"]:
                                ins.then_inc(sems[o["sig"][0]], 1)
                    if ename == "sp":
                        for sk, v in finals:
                            if waited.get(sk, 0) < v:
                                eng.wait_ge(sems[sk], v)
                return run

            block.tensor(body("pe"))
            block.scalar(body("act"))
            block.vector(body("dve"))
            block.gpsimd(body("pool"))
            block.sync(body("sp"))


class Ctx:
    def __init__(self):
        self.nc = bass.Bass("TRN2", target_bir_lowering=False)
        self.st = contextlib.ExitStack()
        self.P = Prog(self.nc)
        self.out_names = []
        self._u = 0

    def inp(self, name, shape, dt=F32):
        return self.nc.dram_tensor(name, list(shape), dt, kind="ExternalInput").ap()

    def out(self, name, shape, dt=F32):
        self.out_names.append(name)
        return self.nc.dram_tensor(name, list(shape), dt, kind="ExternalOutput").ap()

    def sb(self, name, shape, dt=F32):
        return self.st.enter_context(self.nc.sbuf_tensor(name, list(shape), dt))

    def ps(self, name, shape, dt=F32):
        self.P.excl.add(name)
        return self.st.enter_context(self.nc.psum_tensor(name, list(shape), dt))

    def ring(self, name, n, shape, dt=F32):
        return Ring([(self.sb("%s%d" % (name, i), shape, dt), "%s%d" % (name, i)) for i in range(n)])

    def psring(self, name, n, shape=(128, 512), dt=F32):
        return Ring([(self.ps("%s%d" % (name, i), (128, 512), dt), "%s%d" % (name, i)) for i in range(n)])

    def load(self, q, dst, src, res, reads=()):
        self.P.dma(q, "L_" + str(res), lambda e, s: e.dma_start(out=dst, in_=src).then_inc(s, 16),
                   reads=reads, writes=[res])

    def store(self, q, dst, src, res, dres=None):
        self.P.dma(q, "S_" + str(res), lambda e, s: e.dma_start(out=dst, in_=src).then_inc(s, 16),
                   reads=[res], writes=([dres] if dres else []))

    def finish(self):
        self.P.emit()
        self.st.close()
        return self.nc


class Ring:
    def __init__(self, items):
        self.items = items
        self.i = 0

    def nxt(self):
        it = self.items[self.i % len(self.items)]
        self.i += 1
        return it


def run_spmd(cx, in_maps):
    nc = cx.finish()
    res = run_bass_kernel_spmd(nc, in_maps, core_ids=list(range(NCORES)))
    return res.results


def tiles_own_ctx(tt=512):
    t = [(i * tt, tt, 0) for i in range(TOWN // tt)]
    t.append((TOWN, CTX, 1))
    return t


def pcol(v):
    v = np.asarray(v, np.float32)
    return np.ascontiguousarray(v.reshape(-1, 128).T)


def emit_ada(cx, cs_d, wada_d, bada_d, mod_sb, modp_sb, pm=None, pmres="ps_mod"):
    P = cx.P
    cs_sb = cx.sb("cs_sb", [128, 16])
    s_sb = cx.sb("s_sb", [128, 16])
    ba_sb = cx.sb("ba_sb", [128, 48])
    if pm is None:
        pm = cx.ps("ps_mod", [128, 512])
    cx.load("sp", cs_sb[:], cs_d, "cs_sb")
    cx.load("sp", ba_sb[:], bada_d, "ba_sb")
    P.op("act", lambda e: e.activation(out=s_sb[:], in_=cs_sb[:], func=AF.Silu), reads=["cs_sb"], writes=["s_sb"])
    wr = cx.ring("wada", 2, [128, 8, 512])
    wv = wada_d.rearrange("(k p) c -> p k c", p=128)
    for piece in range(12):
        wa, wres = wr.nxt()
        cx.load("sp", wa[:], wv[:, :, piece * 512:(piece + 1) * 512], wres)

        def mm(e, wa=wa, piece=piece):
            ins = None
            for f4 in range(4):
                fc = piece * 4 + f4
                for k in range(8):
                    ins = e.matmul(pm[:, fc * 2:fc * 2 + 2], wa[:, k, f4 * 128:(f4 + 1) * 128],
                                   s_sb[:, 2 * k:2 * k + 2], start=(k == 0), stop=(k == 7))
            return ins
        P.op("pe", mm, reads=[wres, "s_sb"], writes=[pmres])
    pmv = pm[:, 0:96].rearrange("p (m c) -> p m c", c=2)
    mv = mod_sb[:].rearrange("p (m c) -> p m c", c=2)
    for c in range(2):
        P.op("dve", lambda e, c=c: e.tensor_tensor(out=mv[:, :, c], in0=pmv[:, :, c], in1=ba_sb[:], op=ALU.add),
             reads=[pmres, "ba_sb"], writes=["mod"], fence=True)
    P.op("dve", lambda e: e.tensor_copy(out=modp_sb[:], in_=mod_sb[:]), reads=["mod"], writes=["modp"], fence=True)
    for lo in (16, 64):
        P.op("dve", lambda e, lo=lo: e.tensor_scalar_add(out=modp_sb[:, lo:lo + 16], in0=mod_sb[:, lo:lo + 16], scalar1=1.0),
             reads=["mod"], writes=["modp"], fence=True)


def emit_modulate(cx, x, xres, u, ures, tw, col, mod_sb, modp_sb, piece_shift, nk=8):
    P = cx.P
    for k in range(nk):
        sc = modp_sb[:, (piece_shift + 1) * 16 + 2 * k + col:(piece_shift + 1) * 16 + 2 * k + col + 1]
        sh = mod_sb[:, piece_shift * 16 + 2 * k + col:piece_shift * 16 + 2 * k + col + 1]
        if k % 2 == 0:
            P.op("act", lambda e, k=k, sc=sc, sh=sh: e.activation(out=u[:, k, :tw], in_=x[:, k, :tw], func=AF.Identity,
                                                                 bias=sh, scale=sc),
                 reads=[xres, "mod", "modp"], writes=[(ures, k)])
        else:
            P.op("dve", lambda e, k=k, sc=sc, sh=sh: e.tensor_scalar(out=u[:, k, :tw], in0=x[:, k, :tw], scalar1=sc,
                                                                    scalar2=sh, op0=ALU.mult, op1=ALU.add),
                 reads=[xres, "mod", "modp"], writes=[(ures, k)])


def build_A0():
    cx = Ctx()
    P = cx.P
    xT = cx.inp("xT", [D, NT])
    cs_d = cx.inp("cs", [128, 16])
    wada_d = cx.inp("wada", [D, 6 * D])
    bada_d = cx.inp("bada", [128, 48])
    win_d = cx.inp("win", [D, 1536])
    gq_d = cx.inp("gq", [128, 1])
    gk_d = cx.inp("gk", [128, 1])
    cos_d = cx.inp("cosT", [128, NT])
    sin_d = cx.inp("sinT", [128, NT])
    rt_d = cx.inp("rotT", [128, 128])
    ob_d = cx.inp("oneblk", [128, 128])
    mod_o = cx.out("mod", [128, 96])
    qT_o = cx.out("qT", [768, NT], BF16)
    kT_o = cx.out("kT", [256, NT], BF16)
    v_o = cx.out("v", [NT, 256], BF16)
    pT_o = cx.out("pT", [256, NT])

    mod_sb = cx.sb("mod_sb", [128, 96])
    modp_sb = cx.sb("modp_sb", [128, 96])
    emit_ada(cx, cs_d, wada_d, bada_d, mod_sb, modp_sb)
    cx.store("sp", mod_o, mod_sb[:], "mod")

    win = cx.sb("win_sb", [128, 8, 1536], BF16)
    cx.load("pool", win[:], win_d.rearrange("(k p) c -> p k c", p=128), "win")
    oneblk = cx.sb("oneblk_sb", [128, 128], BF16)
    cx.load("pool", oneblk[:], ob_d, "oneblk")
    rt32 = cx.sb("rt32", [128, 128])
    cx.load("sp", rt32[:], rt_d, "rt32")
    g_sb = cx.sb("g_sb", [128, 2])
    cx.load("sp", g_sb[:, 0:1], gq_d, "gq")
    cx.load("sp", g_sb[:, 1:2], gk_d, "gk")
    rg = [cx.sb("rgq", [128, 128], BF16), cx.sb("rgk", [128, 128], BF16)]
    cosg = [cx.sb("cosgq", [128, NT]), cx.sb("cosgk", [128, NT])]
    sin_sb = cx.sb("sin_sb", [128, NT])
    cx.load("sp", sin_sb[:], sin_d, "sin")
    gres = ["gq", "gk"]
    for i in range(2):
        cx.load("sp", cosg[i][:], cos_d, "cosg%d" % i)
        P.op("dve", lambda e, i=i: e.tensor_scalar_mul(out=rg[i][:], in0=rt32[:], scalar1=g_sb[:, i:i + 1]),
             reads=["rt32", gres[i]], writes=["rg%d" % i])
        P.op("dve", lambda e, i=i: e.tensor_scalar_mul(out=cosg[i][:], in0=cosg[i][:], scalar1=g_sb[:, i:i + 1]),
             reads=[gres[i], "cosg%d" % i], writes=["cosg%d" % i])

    eps_sb = cx.sb("eps_sb", [128, 1])
    P.op("dve", lambda e: e.memset(eps_sb[:], EPS), writes=["eps"])
    xr = cx.ring("x", 2, [128, 8, 512])
    ur = cx.ring("u", 2, [128, 8, 512], BF16)
    pq_r = cx.psring("pq", 2)
    pms_r = cx.psring("pms", 1)
    prot_r = cx.psring("prot", 1)
    pv_r = cx.psring("pv", 2)
    sq_r = cx.ring("sq", 2, [128, 512], BF16)
    qb_r = cx.ring("qb", 2, [128, 512], BF16)
    t1_r = cx.ring("t1", 2, [128, 512])
    t2_r = cx.ring("t2", 2, [128, 512])
    rs_r = cx.ring("rs", 2, [128, 512])
    qf_r = cx.ring("qf", 3, [128, 512], BF16)
    vs_r = cx.ring("vs", 2, [128, 256], BF16)
    pp_r = cx.ring("pp", 2, [128, 512])
    xv = xT.rearrange("(k p) t -> p k t", p=128)

    def do_tile(t0, tw, col):
        x, xres = xr.nxt()
        cx.load("sp", x[:, :, :tw], xv[:, :, t0:t0 + tw], xres)
        u, ures = ur.nxt()
        emit_modulate(cx, x, xres, u, ures, tw, col, mod_sb, modp_sb, 0)
        ureads = [(ures, k) for k in range(8)]
        for cc in range(8):
            isk = cc >= 6
            c0 = cc * 128 if not isk else 768 + (cc - 6) * 128
            gi = 1 if isk else 0
            pq, pqres = pq_r.nxt()

            def mmq(e, pq=pq, c0=c0):
                ins = None
                for k in range(8):
                    ins = e.matmul(pq[:, :tw], win[:, k, c0:c0 + 128], u[:, k, :tw], start=(k == 0), stop=(k == 7))
                return ins
            P.op("pe", mmq, reads=ureads + ["win"], writes=[pqres])
            sq, sqres = sq_r.nxt()
            P.op("act", lambda e, sq=sq, pq=pq: e.activation(out=sq[:, :tw], in_=pq[:, :tw], func=AF.Square),
                 reads=[pqres], writes=[sqres])
            qb, qbres = qb_r.nxt()
            P.op("dve", lambda e, qb=qb, pq=pq: e.tensor_copy(out=qb[:, :tw], in_=pq[:, :tw]), reads=[pqres], writes=[qbres])
            pms, pmsres = pms_r.nxt()
            P.op("pe", lambda e, pms=pms, sq=sq: e.matmul(pms[:, :tw], oneblk[:], sq[:, :tw], start=True, stop=True),
                 reads=[sqres, "oneblk"], writes=[pmsres])
            prot, protres = prot_r.nxt()
            P.op("pe", lambda e, prot=prot, qb=qb, gi=gi: e.matmul(prot[:, :tw], rg[gi][:], qb[:, :tw], start=True, stop=True),
                 reads=[qbres, "rg%d" % gi], writes=[protres])
            t1, t1res = t1_r.nxt()
            P.op("dve", lambda e, t1=t1, pq=pq, gi=gi: e.tensor_tensor(out=t1[:, :tw], in0=pq[:, :tw],
                                                                     in1=cosg[gi][:, t0:t0 + tw], op=ALU.mult),
                 reads=[pqres, "cosg%d" % gi], writes=[t1res])
            t2, t2res = t2_r.nxt()
            P.op("dve", lambda e, t2=t2, prot=prot: e.tensor_tensor(out=t2[:, :tw], in0=prot[:, :tw],
                                                                  in1=sin_sb[:, t0:t0 + tw], op=ALU.mult),
                 reads=[protres, "sin"], writes=[t2res])
            rs, rsres = rs_r.nxt()
            P.op("act", lambda e, rs=rs, pms=pms: e.activation(out=rs[:, :tw], in_=pms[:, :tw], func=AF.Sqrt, bias=eps_sb[:, 0:1]),
                 reads=[pmsres, "eps"], writes=[rsres])
            P.op("dve", lambda e, rs=rs: e.reciprocal(out=rs[:, :tw], in_=rs[:, :tw]), reads=[rsres], writes=[rsres])
            P.op("dve", lambda e, t1=t1, t2=t2: e.tensor_tensor(out=t1[:, :tw], in0=t1[:, :tw], in1=t2[:, :tw], op=ALU.add),
                 reads=[t1res, t2res], writes=[t1res])
            qf, qfres = qf_r.nxt()
            P.op("dve", lambda e, qf=qf, t1=t1, rs=rs: e.tensor_tensor(out=qf[:, :tw], in0=t1[:, :tw], in1=rs[:, :tw], op=ALU.mult),
                 reads=[t1res, rsres], writes=[qfres])
            dst = (kT_o[(cc - 6) * 128:(cc - 5) * 128, t0:t0 + tw] if isk else qT_o[cc * 128:(cc + 1) * 128, t0:t0 + tw])
            cx.store("sp", dst, qf[:, :tw], qfres)
        for tb in range(tw // 128):
            pv, pvres = pv_r.nxt()

            def mmv(e, pv=pv, tb=tb):
                ins = None
                for k in range(8):
                    ins = e.matmul(pv[:, :256], u[:, k, tb * 128:(tb + 1) * 128], win[:, k, 1024:1280],
                                   start=(k == 0), stop=(k == 7))
                return ins
            P.op("pe", mmv, reads=ureads + ["win"], writes=[pvres])
            vs, vsres = vs_r.nxt()
            P.op("act", lambda e, vs=vs, pv=pv: e.activation(out=vs[:], in_=pv[:, :256], func=AF.Copy), reads=[pvres], writes=[vsres])
            cx.store("pool", v_o[t0 + tb * 128:t0 + (tb + 1) * 128, :], vs[:], vsres)
        for pc in range(2):
            pq, pqres = pq_r.nxt()

            def mmp(e, pq=pq, pc=pc):
                ins = None
                for k in range(8):
                    ins = e.matmul(pq[:, :tw], win[:, k, 1280 + pc * 128:1280 + (pc + 1) * 128], u[:, k, :tw],
                                   start=(k == 0), stop=(k == 7))
                return ins
            P.op("pe", mmp, reads=ureads + ["win"], writes=[pqres])
            pp, ppres = pp_r.nxt()
            P.op("act", lambda e, pp=pp, pq=pq: e.activation(out=pp[:, :tw], in_=pq[:, :tw], func=AF.Copy), reads=[pqres], writes=[ppres])
            cx.store("pool", pT_o[pc * 128:(pc + 1) * 128, t0:t0 + tw], pp[:, :tw], ppres)

    for (t0, tw, col) in tiles_own_ctx():
        do_tile(t0, tw, col)
    return cx


def build_attn(nheads, group, dk, nkc_full, scale):
    cx = Ctx()
    P = cx.P
    nkv = nheads // group
    nkeys = nkc_full * 128
    qT = cx.inp("qT", [nheads * dk, NT], BF16)
    kT = cx.inp("kT", [nkv, dk, nkeys], BF16)
    vv = cx.inp("v", [nkv, 128, nkc_full, 64], BF16)
    sel_d = cx.inp("sel", [65, 64])
    oT = cx.out("oT", [nheads * 64, NT], BF16)

    kg_r = cx.ring("kg", 2, [dk, nkeys], BF16)
    vg_r = cx.ring("vg", 2, [128, nkc_full, 65], BF16)
    for vg, vres in vg_r.items:
        P.op("dve", lambda e, vg=vg: e.memset(vg[:, :, 64:65], 1.0), writes=[(vres, "ones")])
    sel = cx.sb("sel_sb", [65, 64])
    cx.load("sp", sel[:], sel_d, "sel")
    q_r = cx.ring("qh", 2, [dk, NT], BF16)
    ps_r = cx.psring("ps", 4)
    po_r = cx.psring("po", 2)
    pb_r = cx.psring("pb", 1)
    pt_r = cx.ring("pt", 4, [128, 512], BF16)
    os_r = cx.ring("os", 2, [65, 512])
    at_r = cx.ring("at", 2, [64, 512], BF16)
    for g in range(nkv):
        kg, kres = kg_r.nxt()
        cx.load("sp", kg[:], kT[g], kres)
        vg, vres = vg_r.nxt()
        cx.load("sp", vg[:, :, 0:64], vv[g], vres)
        for h in range(g * group, (g + 1) * group):
            qh, qres = q_r.nxt()
            cx.load("sp", qh[:], qT[h * dk:(h + 1) * dk, :], qres)
            for (t0, tw, col) in tiles_own_ctx():
                nkc = nkc_full if col == 0 else 2
                po, pores = po_r.nxt()
                for kc in range(nkc):
                    ps, psres = ps_r.nxt()
                    P.op("pe", lambda e, ps=ps, kg=kg, qh=qh, kc=kc, t0=t0, tw=tw:
                         e.matmul(ps[:, :tw], kg[:, kc * 128:(kc + 1) * 128], qh[:, t0:t0 + tw], start=True, stop=True),
                         reads=[kres, qres], writes=[psres])
                    pt, ptres = pt_r.nxt()
                    P.op("act", lambda e, ps=ps, pt=pt, tw=tw: e.activation(out=pt[:, :tw], in_=ps[:, :tw], func=AF.Exp, scale=scale),
                         reads=[psres], writes=[ptres])
                    P.op("pe", lambda e, po=po, vg=vg, pt=pt, kc=kc, tw=tw, nkc=nkc:
                         e.matmul(po[0:65, :tw], vg[:, kc, :], pt[:, :tw], start=(kc == 0), stop=(kc == nkc - 1)),
                         reads=[ptres, vres, (vres, "ones")], writes=[pores])
                osb, osres = os_r.nxt()
                P.op("act", lambda e, osb=osb, po=po, tw=tw: e.activation(out=osb[:, :tw], in_=po[0:65, :tw], func=AF.Copy),
                     reads=[pores], writes=[osres])
                P.op("dve", lambda e, osb=osb, tw=tw: e.reciprocal(out=osb[64:65, :tw], in_=osb[64:65, :tw]),
                     reads=[osres], writes=[osres])
                pb, pbres = pb_r.nxt()
                P.op("pe", lambda e, pb=pb, osb=osb, tw=tw: e.matmul(pb[0:64, :tw], sel[:], osb[:, :tw], start=True, stop=True),
                     reads=[osres, "sel"], writes=[pbres])
                at, atres = at_r.nxt()
                P.op("dve", lambda e, at=at, osb=osb, pb=pb, tw=tw: e.tensor_tensor(out=at[:, :tw], in0=osb[0:64, :tw], in1=pb[0:64, :tw], op=ALU.mult),
                     reads=[osres, pbres], writes=[atres])
                cx.store("pool", oT[h * 64:(h + 1) * 64, t0:t0 + tw], at[:, :tw], atres)
    return cx


def rope_tables(j):
    t = np.arange(TOWN) + j * TOWN
    row = (t // GRID_W).astype(np.float64)
    colp = (t % GRID_W).astype(np.float64)
    inv = 10000.0 ** (-np.arange(16, dtype=np.float64) / 16)
    cos = np.ones((128, NT), np.float32)
    sin = np.zeros((128, NT), np.float32)
    for p in range(128):
        d = p % 64
        pos = row if d < 32 else colp
        ang = (pos.astype(np.float32) * inv[d % 16].astype(np.float32)).astype(np.float32)
        cos[p, :TOWN] = np.cos(ang)
        sin[p, :TOWN] = np.sin(ang)
    return cos, sin


def rot_matrix(n=128, blk=32):
    rt = np.zeros((n, n), np.float32)
    h = blk // 2
    for p in range(n):
        if p % blk < h:
            rt[p + h, p] = -1.0
        else:
            rt[p - h, p] = 1.0
    return rt


def blockdiag_ones(n, blk):
    m = np.zeros((n, n), np.float32)
    for i in range(0, n, blk):
        m[i:i + blk, i:i + blk] = 1.0 / blk
    return m


def core_xT(x, ctx, c):
    b, j = divmod(c, 4)
    return np.ascontiguousarray(np.concatenate([x[b, j * TOWN:(j + 1) * TOWN], ctx[b]], axis=0).T)


def core_cs(cvec, c_ctx, c):
    b = c // 4
    cs = np.empty((128, 16), np.float32)
    cs[:, 0::2] = pcol(cvec[b])
    cs[:, 1::2] = pcol(c_ctx)
    return cs


SEL = np.zeros((65, 64), np.float32)
SEL[64, :] = 1.0


def gather_kv(res, kname, vname, nkv, dk):
    out = []
    for b in range(2):
        ks = [np.asarray(res[4 * b][kname])[:, TOWN:]] + [np.asarray(res[4 * b + j][kname])[:, :TOWN] for j in range(4)]
        kf = np.concatenate(ks, axis=1).reshape(nkv, dk, -1)
        vs = [np.asarray(res[4 * b][vname])[TOWN:]] + [np.asarray(res[4 * b + j][vname])[:TOWN] for j in range(4)]
        vf = np.concatenate(vs, axis=0)
        nkc = vf.shape[0] // 128
        vf = np.ascontiguousarray(vf.reshape(nkc, 128, nkv, 64).transpose(2, 1, 0, 3))
        out.append((np.ascontiguousarray(kf), vf))
    return out


def stage_A0(inp):
    x, ctx = inp["x"], inp["ctx"]
    cx = build_A0()
    maps = []
    for c in range(NCORES):
        cos, sin = rope_tables(c % 4)
        maps.append(dict(
            xT=core_xT(x, ctx, c), cs=core_cs(inp["c"], inp["c_ctx"], c),
            wada=np.ascontiguousarray(inp["w_ada"][0]), bada=pcol(inp["b_ada"][0]),
            win=np.ascontiguousarray(inp["ev_w_in"][0]),
            gq=np.tile(inp["ev_q_gain"][0], 2)[:, None].astype(np.float32),
            gk=np.tile(inp["ev_k_gain"][0], 2)[:, None].astype(np.float32),
            cosT=cos, sinT=sin, rotT=rot_matrix(), oneblk=blockdiag_ones(128, 64)))
    return run_spmd(cx, maps)


def stage_B0(resA):
    cx = build_attn(12, 3, 64, 130, 64 ** -0.5)
    kv = gather_kv(resA, "kT", "v", 4, 64)
    maps = []
    for c in range(NCORES):
        kf, vf = kv[c // 4]
        maps.append(dict(qT=np.asarray(resA[c]["qT"]), kT=kf, v=vf, sel=SEL))
    return run_spmd(cx, maps)


class LNState:
    def __init__(self, cx, tag, width=512):
        self.ones = cx.sb(tag + "_ones", [128, 128])
        cx.P.op("dve", lambda e: e.memset(self.ones[:], 1.0 / D), writes=[tag + "_ones"])
        self.ones_res = tag + "_ones"
        self.eps = cx.sb(tag + "_eps", [128, 1])
        cx.P.op("dve", lambda e: e.memset(self.eps[:], EPS), writes=[tag + "_eps"])
        self.eps_res = tag + "_eps"
        self.pmean = cx.psring(tag + "_pmean", 1, (128, width))
        self.pmsq = cx.psring(tag + "_pmsq", 1, (128, width))
        self.rsq = cx.ring(tag + "_rsq", 2, [128, width])
        self.m = cx.ring(tag + "_m", 1, [128, width])
        self.var = cx.ring(tag + "_var", 1, [128, width])
        self.nb = cx.ring(tag + "_nb", 1, [128, width])
        self.t = cx.ring(tag + "_t", 2, [128, width])


def emit_layernorm(cx, ln, r, rres, tw, g_sb, b_sb, gres, out=None, outres=None):
    P = cx.P
    if out is None:
        out, outres = r, rres
    pmean, pmres = ln.pmean.nxt()
    pmsq, pqres = ln.pmsq.nxt()
    for k in range(8):
        rsq, rsqres = ln.rsq.nxt()
        P.op("act", lambda e, rsq=rsq, k=k: e.activation(out=rsq[:, :tw], in_=r[:, k, :tw], func=AF.Square),
             reads=[rres(k)], writes=[rsqres])
        P.op("pe", lambda e, k=k: e.matmul(pmean[:, :tw], ln.ones[:], r[:, k, :tw], start=(k == 0), stop=(k == 7)),
             reads=[rres(k), ln.ones_res], writes=[pmres])
        P.op("pe", lambda e, rsq=rsq, k=k: e.matmul(pmsq[:, :tw], ln.ones[:], rsq[:, :tw], start=(k == 0), stop=(k == 7)),
             reads=[rsqres, ln.ones_res], writes=[pqres])
    m, mres = ln.m.nxt()
    var, vres = ln.var.nxt()
    nb, nbres = ln.nb.nxt()
    P.op("act", lambda e: e.activation(out=m[:, :tw], in_=pmean[:, :tw], func=AF.Copy), reads=[pmres], writes=[mres])
    P.op("dve", lambda e: e.tensor_tensor(out=var[:, :tw], in0=m[:, :tw], in1=m[:, :tw], op=ALU.mult), reads=[mres], writes=[vres])
    P.op("dve", lambda e: e.tensor_tensor(out=var[:, :tw], in0=pmsq[:, :tw], in1=var[:, :tw], op=ALU.subtract),
         reads=[pqres, vres], writes=[vres])
    P.op("dve", lambda e: e.tensor_scalar_max(out=var[:, :tw], in0=var[:, :tw], scalar1=0.0), reads=[vres], writes=[vres])
    P.op("act", lambda e: e.activation(out=var[:, :tw], in_=var[:, :tw], func=AF.Sqrt, bias=ln.eps[:, 0:1]),
         reads=[vres, ln.eps_res], writes=[vres])
    P.op("dve", lambda e: e.reciprocal(out=var[:, :tw], in_=var[:, :tw]), reads=[vres], writes=[vres])
    P.op("dve", lambda e: e.scalar_tensor_tensor(out=nb[:, :tw], in0=m[:, :tw], scalar=-1.0, in1=var[:, :tw],
                                                 op0=ALU.mult, op1=ALU.mult), reads=[mres, vres], writes=[nbres])
    for k in range(8):
        t, tres = ln.t.nxt()
        P.op("dve", lambda e, t=t, k=k: e.tensor_tensor(out=t[:, :tw], in0=r[:, k, :tw], in1=var[:, :tw], op=ALU.mult),
             reads=[rres(k), vres], writes=[tres])
        P.op("dve", lambda e, t=t: e.tensor_tensor(out=t[:, :tw], in0=t[:, :tw], in1=nb[:, :tw], op=ALU.add),
             reads=[tres, nbres], writes=[tres])
        P.op("act", lambda e, t=t, k=k: e.activation(out=out[:, k, :tw], in_=t[:, :tw], func=AF.Identity,
                                                     bias=b_sb[:, k:k + 1], scale=g_sb[:, k:k + 1]),
             reads=[tres, gres], writes=[outres(k)])


def load_cast_w(cx, name, w_d, kch, ncol):
    w = cx.sb(name, [128, kch, ncol], BF16)
    for k in range(kch):
        cx.load("pool", w[:, k, :], w_d[k * 128:(k + 1) * 128, :], (name, k))
    return w, [(name, k) for k in range(kch)]


def build_Ca(with_pool):
    cx = Ctx()
    P = cx.P
    xT = cx.inp("xT", [D, NT])
    nat = 6 if with_pool else 8
    aT = cx.inp("aT", [nat * 128, NT], BF16)
    mod_d = cx.inp("mod", [128, 96])
    wout_d = cx.inp("wout", [D, D])
    g_d = cx.inp("lng", [128, 8])
    b_d = cx.inp("lnb", [128, 8])
    if with_pool:
        ph_d = cx.inp("ph", [256, NT + 32])
        rc_d = cx.inp("rcnt", [256, NT])
        wp_d = cx.inp("wpool", [256, 128])
        psc_d = cx.inp("pscale", [128, 2])
    xo = cx.out("xo", [D, NT])

    mod_sb = cx.sb("mod_sb", [128, 96])
    cx.load("sp", mod_sb[:], mod_d, "mod")
    g_sb = cx.sb("g_sb", [128, 8])
    b_sb = cx.sb("b_sb", [128, 8])
    cx.load("sp", g_sb[:], g_d, "lngb")
    cx.load("sp", b_sb[:], b_d, "lngb2")
    P.op("dve", lambda e: e.tensor_copy(out=b_sb[:], in_=b_sb[:]), reads=["lngb2"], writes=["lngb"])
    wout, wres = load_cast_w(cx, "wout_sb", wout_d, 8, D)
    ln = LNState(cx, "ln")
    if with_pool:
        wp = cx.sb("wp_sb", [128, 2, 128], BF16)
        for pc in range(2):
            cx.load("pool", wp[:, pc, :], wp_d[pc * 128:(pc + 1) * 128, :], ("wp", pc))
        psc = cx.sb("psc_sb", [128, 2])
        cx.load("sp", psc[:], psc_d, "psc")
        rc = cx.sb("rc_sb", [128, 2, NT])
        cx.load("sp", rc[:], rc_d.rearrange("(c p) t -> p c t", p=128), "rc")
        ph_r = cx.ring("ph", 4, [128, 528])
        s_r = [cx.ring("s%d" % i, 1, [128, 528]) for i in range(4)]
        ym_r = cx.ring("ym", 2, [128, 512], BF16)
        tmp_r = cx.ring("ptmp", 2, [128, 512])
        ppool_r = cx.psring("ppool", 1)
    x_r = cx.ring("x", 2, [128, 8, 512])
    mix_r = cx.ring("mix", 2, [128, 8, 512], BF16)
    po_r = cx.psring("po", 2)
    xv = xT.rearrange("(k p) t -> p k t", p=128)
    av = aT.rearrange("(k p) t -> p k t", p=128)
    ov = xo.rearrange("(k p) t -> p k t", p=128)
    LO = {2: 1, 4: 2, 8: 4, 16: 8}

    def do_tile(t0, tw, col):
        mix, mres = mix_r.nxt()
        cx.load("sp", mix[:, 0:nat, :tw], av[:, :, t0:t0 + tw], (mres, "a"))
        x, xres = x_r.nxt()
        cx.load("sp", x[:, :, :tw], xv[:, :, t0:t0 + tw], (xres, "ld"))
        if with_pool:
            h0 = t0 if col == 0 else t0 + 16
            L = tw + 16
            for pc in range(2):
                ph, phres = ph_r.nxt()
                cx.load("sp", ph[:, :L], ph_d[pc * 128:(pc + 1) * 128, h0:h0 + L], phres)
                srcs = [(ph, phres)]
                nlev = 2 if pc == 0 else 4
                for lev in range(nlev):
                    sh = 1 << lev
                    s, sres = s_r[lev].nxt()
                    a, ares = srcs[-1]
                    Lo = L - (2 * sh - 1)
                    P.op("dve", lambda e, s=s, a=a, sh=sh, Lo=Lo: e.tensor_tensor(out=s[:, :Lo], in0=a[:, 0:Lo], in1=a[:, sh:sh + Lo], op=ALU.add),
                         reads=[ares], writes=[sres])
                    srcs.append((s, sres))
                ym, ymres = ym_r.nxt()
                tmp, tmpres = tmp_r.nxt()
                for half in range(2):
                    w = (2, 4, 8, 16)[pc * 2 + half]
                    s, sres = srcs[{2: 1, 4: 2, 8: 3, 16: 4}[w]]
                    off = 8 - LO[w]
                    lo_p, hi_p = half * 64, half * 64 + 64
                    P.op("dve", lambda e, s=s, off=off, lo_p=lo_p, hi_p=hi_p, pc=pc, tmp=tmp:
                         e.tensor_tensor(out=tmp[lo_p:hi_p, :tw], in0=s[lo_p:hi_p, off:off + tw], in1=rc[lo_p:hi_p, pc, t0:t0 + tw], op=ALU.mult),
                         reads=[sres, "rc"], writes=[(tmpres, half)])
                    P.op("dve", lambda e, ph=ph, lo_p=lo_p, hi_p=hi_p, tmp=tmp, ym=ym:
                         e.tensor_tensor(out=ym[lo_p:hi_p, :tw], in0=tmp[lo_p:hi_p, :tw], in1=ph[lo_p:hi_p, 8:8 + tw], op=ALU.subtract),
                         reads=[(tmpres, half), phres], writes=[(ymres, half)])
                pp, ppres = ppool_r.nxt()
                P.op("pe", lambda e, pp=pp, ym=ym, pc=pc: e.matmul(pp[:, :tw], wp[:, pc, :], ym[:, :tw], start=True, stop=True),
                     reads=[(ymres, 0), (ymres, 1), ("wp", pc)], writes=[ppres])
                P.op("act", lambda e, pp=pp, pc=pc: e.activation(out=mix[:, 6 + pc, :tw], in_=pp[:, :tw], func=AF.Identity, scale=psc[:, pc:pc + 1]),
                     reads=[ppres, "psc"], writes=[(mres, "p", pc)])
        mreads = [(mres, "a")] + ([(mres, "p", 0), (mres, "p", 1)] if with_pool else [])
        for oc in range(8):
            P.op("act", lambda e, oc=oc: e.activation(out=x[:, oc, :tw], in_=x[:, oc, :tw], func=AF.Identity, scale=ALPHA),
                 reads=[(xres, "ld")], writes=[(xres, oc)])
            po, pores = po_r.nxt()

            def mm(e, po=po, oc=oc):
                ins = None
                for k in range(8):
                    ins = e.matmul(po[:, :tw], wout[:, k, oc * 128:(oc + 1) * 128], mix[:, k, :tw], start=(k == 0), stop=(k == 7))
                return ins
            P.op("pe", mm, reads=mreads + wres, writes=[pores])
            gate = mod_sb[:, 32 + 2 * oc + col:32 + 2 * oc + col + 1]
            P.op("dve", lambda e, po=po, oc=oc, gate=gate: e.scalar_tensor_tensor(out=x[:, oc, :tw], in0=po[:, :tw], scalar=gate,
                                                                               in1=x[:, oc, :tw], op0=ALU.mult, op1=ALU.add),
                 reads=[pores, (xres, oc), "mod"], writes=[(xres, oc)])
        emit_layernorm(cx, ln, x, lambda k: (xres, k), tw, g_sb, b_sb, "lngb")
        P.dma("pool", "S_" + xres, lambda e, s: e.dma_start(out=ov[:, :, t0:t0 + tw], in_=x[:, :, :tw]).then_inc(s, 16),
              reads=[(xres, k) for k in range(8)], writes=[(xres, "ld")])

    for (t0, tw, col) in tiles_own_ctx():
        do_tile(t0, tw, col)
    return cx


def build_C0b():
    cx = Ctx()
    P = cx.P
    NF = 22
    TW = 256
    xT = cx.inp("xT", [D, NT])
    mod_d = cx.inp("mod", [128, 96])
    wg_d = cx.inp("wg", [D, NF * 128])
    wu_d = cx.inp("wu", [D, NF * 128])
    wd_d = cx.inp("wd", [NF * 128, D])
    g_d = cx.inp("lng", [128, 8])
    b_d = cx.inp("lnb", [128, 8])
    xo = cx.out("xo", [D, NT])
    mod_sb = cx.sb("mod_sb", [128, 96])
    modp_sb = cx.sb("modp_sb", [128, 96])
    cx.load("sp", mod_sb[:], mod_d, "mod")
    P.op("dve", lambda e: e.tensor_scalar_add(out=modp_sb[:], in0=mod_sb[:], scalar1=1.0), reads=["mod"], writes=["modp"])
    g_sb = cx.sb("g_sb", [128, 8])
    b_sb = cx.sb("b_sb", [128, 8])
    cx.load("sp", g_sb[:], g_d, "lngb")
    cx.load("sp", b_sb[:], b_d, "lngb2")
    P.op("dve", lambda e: e.tensor_copy(out=b_sb[:], in_=b_sb[:]), reads=["lngb2"], writes=["lngb"])
    wg, wgres = load_cast_w(cx, "wg_sb", wg_d, 8, NF * 128)
    wu, wures = load_cast_w(cx, "wu_sb", wu_d, 8, NF * 128)
    wd, wdres = load_cast_w(cx, "wd_sb", wd_d, NF, D)
    ln = LNState(cx, "ln", TW)
    x_r = cx.ring("x", 1, [128, 8, TW])
    u_r = cx.ring("u", 1, [128, 8, TW], BF16)
    h_r = cx.ring("h", 1, [128, NF, TW], BF16)
    sg_r = cx.ring("sg", 2, [128, TW])
    pg_r = cx.psring("pg", 2, (128, TW))
    pu_r = cx.psring("pu", 2, (128, TW))
    pd_r = cx.psring("pd", 2, (128, TW))
    xv = xT.rearrange("(k p) t -> p k t", p=128)
    ov = xo.rearrange("(k p) t -> p k t", p=128)

    def do_tile(t0, tw, col):
        x, xres = x_r.nxt()
        cx.load("sp", x[:, :, :tw], xv[:, :, t0:t0 + tw], (xres, "ld"), reads=[(xres, k) for k in range(8)])
        u, ures = u_r.nxt()
        emit_modulate(cx, x, (xres, "ld"), u, ures, tw, col, mod_sb, modp_sb, 3)
        ureads = [(ures, k) for k in range(8)]
        h, hres = h_r.nxt()
        for fc in range(NF):
            pg, pgres = pg_r.nxt()
            pu, pures = pu_r.nxt()

            def mmg(e, pg=pg, fc=fc):
                ins = None
                for k in range(8):
                    ins = e.matmul(pg[:, :tw], wg[:, k, fc * 128:(fc + 1) * 128], u[:, k, :tw], start=(k == 0), stop=(k == 7))
                return ins

            def mmu(e, pu=pu, fc=fc):
                ins = None
                for k in range(8):
                    ins = e.matmul(pu[:, :tw], wu[:, k, fc * 128:(fc + 1) * 128], u[:, k, :tw], start=(k == 0), stop=(k == 7))
                return ins
            P.op("pe", mmg, reads=ureads + wgres, writes=[pgres])
            P.op("pe", mmu, reads=ureads + wures, writes=[pures])
            sg, sgres = sg_r.nxt()
            P.op("act", lambda e, sg=sg, pg=pg: e.activation(out=sg[:, :tw], in_=pg[:, :tw], func=AF.Silu), reads=[pgres], writes=[sgres])
            P.op("dve", lambda e, sg=sg, pu=pu, fc=fc: e.tensor_tensor(out=h[:, fc, :tw], in0=sg[:, :tw], in1=pu[:, :tw], op=ALU.mult),
                 reads=[sgres, pures], writes=[(hres, fc)])
        hreads = [(hres, fc) for fc in range(NF)]
        for oc in range(8):
            P.op("act", lambda e, oc=oc: e.activation(out=x[:, oc, :tw], in_=x[:, oc, :tw], func=AF.Identity, scale=ALPHA),
                 reads=[(xres, "ld")] + ureads, writes=[(xres, oc)])
            pd, pdres = pd_r.nxt()

            def mmd(e, pd=pd, oc=oc):
                ins = None
                for fc in range(NF):
                    ins = e.matmul(pd[:, :tw], wd[:, fc, oc * 128:(oc + 1) * 128], h[:, fc, :tw], start=(fc == 0), stop=(fc == NF - 1))
                return ins
            P.op("pe", mmd, reads=hreads + wdres, writes=[pdres])
            gate = mod_sb[:, 80 + 2 * oc + col:80 + 2 * oc + col + 1]
            P.op("dve", lambda e, pd=pd, oc=oc, gate=gate: e.scalar_tensor_tensor(out=x[:, oc, :tw], in0=pd[:, :tw], scalar=gate,
                                                                               in1=x[:, oc, :tw], op0=ALU.mult, op1=ALU.add),
                 reads=[pdres, (xres, oc), "mod"], writes=[(xres, oc)])
        emit_layernorm(cx, ln, x, lambda k: (xres, k), tw, g_sb, b_sb, "lngb")
        P.dma("pool", "S_" + xres, lambda e, s: e.dma_start(out=ov[:, :, t0:t0 + tw], in_=x[:, :, :tw]).then_inc(s, 16),
              reads=[(xres, k) for k in range(8)], writes=[(xres, "ld")])

    for (t0, tw, col) in tiles_own_ctx(TW):
        do_tile(t0, tw, col)
    return cx


POOL_W = (2, 4, 8, 16)


def pool_tables(j):
    rc = np.ones((256, NT), np.float32)
    for g, w in enumerate(POOL_W):
        lo = w // 2
        hi = w - lo
        t = np.arange(TOWN) + j * TOWN
        cnt = np.clip(t + hi, 0, SEQ) - np.clip(t - lo, 0, SEQ)
        rc[g * 64:(g + 1) * 64, :TOWN] = (1.0 / cnt.astype(np.float32))[None, :]
        t = np.arange(CTX)
        cnt = np.clip(t + hi, 0, CTX) - np.clip(t - lo, 0, CTX)
        rc[g * 64:(g + 1) * 64, TOWN:] = (1.0 / cnt.astype(np.float32))[None, :]
    return rc


def pool_halo(resA, c):
    b, j = divmod(c, 4)
    own = np.asarray(resA[c]["pT"])
    ph = np.zeros((256, NT + 32), np.float32)
    ph[:, 8:8 + TOWN] = own[:, :TOWN]
    if j > 0:
        ph[:, 0:8] = np.asarray(resA[c - 1]["pT"])[:, TOWN - 8:TOWN]
    if j < 3:
        ph[:, 8 + TOWN:16 + TOWN] = np.asarray(resA[c + 1]["pT"])[:, 0:8]
    ph[:, TOWN + 16 + 8:TOWN + 16 + 8 + CTX] = own[:, TOWN:]
    return ph


def wpool_blockdiag(w_pool):
    m = np.zeros((256, 128), np.float32)
    for g in range(4):
        pc, gl = divmod(g, 2)
        m[pc * 128 + gl * 64:pc * 128 + gl * 64 + 64, gl * 64:gl * 64 + 64] = w_pool[g]
    return m


def stage_C0a(inp, resA, resB):
    cx = build_Ca(True)
    maps = []
    for c in range(NCORES):
        maps.append(dict(
            xT=core_xT(inp["x"], inp["ctx"], c), aT=np.asarray(resB[c]["oT"]), mod=np.asarray(resA[c]["mod"]),
            wout=np.ascontiguousarray(inp["ev_w_out"][0]), lng=pcol(inp["ln1_g"][0]), lnb=pcol(inp["ln1_b"][0]),
            ph=pool_halo(resA, c), rcnt=pool_tables(c % 4), wpool=wpool_blockdiag(inp["ev_w_pool"][0]),
            pscale=pcol(inp["ev_pool_scale"][0])))
    return run_spmd(cx, maps)


def stage_C0b(inp, resA, resCa):
    cx = build_C0b()
    maps = []
    for c in range(NCORES):
        maps.append(dict(
            xT=np.asarray(resCa[c]["xo"]), mod=np.asarray(resA[c]["mod"]),
            wg=np.ascontiguousarray(inp["ev_w_gate"][0]), wu=np.ascontiguousarray(inp["ev_w_up"][0]),
            wd=np.ascontiguousarray(inp["ev_w_down"][0]), lng=pcol(inp["ln2_g"][0]), lnb=pcol(inp["ln2_b"][0])))
    return run_spmd(cx, maps)


def build_A1():
    cx = Ctx()
    P = cx.P
    xT = cx.inp("xT", [D, NT])
    cs_d = cx.inp("cs", [128, 16])
    wada_d = cx.inp("wada", [D, 6 * D])
    bada_d = cx.inp("bada", [128, 48])
    win_d = cx.inp("win", [D, 2208])
    gq_d = cx.inp("gql", [128, 3])
    gkv_d = cx.inp("gkvl", [128, 2])
    wqup_d = cx.inp("wqup", [384, 768])
    wkvk_d = cx.inp("wkvk", [256, 512])
    wkvv_d = cx.inp("wkvv", [256, 512])
    cos96_d = cx.inp("cos96", [96, NT])
    sin96_d = cx.inp("sin96", [96, NT])
    rot96_d = cx.inp("rot96", [96, 96])
    mod_o = cx.out("mod", [128, 96])
    qT_o = cx.out("qT", [768, NT], BF16)
    kT_o = cx.out("kT", [768, NT], BF16)
    v_o = cx.out("v", [NT, 512], BF16)
    nqT_o = cx.out("nqT", [512, NT], BF16)
    nkT_o = cx.out("nkT", [512, NT], BF16)
    nv_o = cx.out("nv", [NT, 512], BF16)

    pq_r = cx.psring("pq", 2)
    pms_r = cx.psring("pms", 1)
    pqh_r = cx.psring("pqh", 2)
    prot_r = cx.psring("prot", 1)
    pv_r = cx.psring("pv", 2)
    mod_sb = cx.sb("mod_sb", [128, 96])
    modp_sb = cx.sb("modp_sb", [128, 96])
    emit_ada(cx, cs_d, wada_d, bada_d, mod_sb, modp_sb, pm=pv_r.items[0][0], pmres=pv_r.items[0][1])
    cx.store("sp", mod_o, mod_sb[:], "mod")

    win, winres = load_cast_w(cx, "win_sb", win_d, 8, 2208)
    wqup, wqres = load_cast_w(cx, "wqup_sb", wqup_d, 3, 768)
    wkvk, wkres = load_cast_w(cx, "wkvk_sb", wkvk_d, 2, 512)
    wkvv, wvres = load_cast_w(cx, "wkvv_sb", wkvv_d, 2, 512)
    rot96 = cx.sb("rot96_sb", [96, 96], BF16)
    cx.load("pool", rot96[:], rot96_d, "rot96")
    cos96 = cx.sb("cos96_sb", [96, NT])
    sin96 = cx.sb("sin96_sb", [96, NT])
    cx.load("sp", cos96[:], cos96_d, "cos96")
    cx.load("sp", sin96[:], sin96_d, "sin96")
    gq = cx.sb("gq_sb", [128, 3])
    gkv = cx.sb("gkv_sb", [128, 2])
    cx.load("sp", gq[:], gq_d, "gq")
    cx.load("sp", gkv[:], gkv_d, "gkv")
    ones_q = cx.sb("ones_q", [128, 128], BF16)
    ones_kv = cx.sb("ones_kv", [128, 128], BF16)
    P.op("dve", lambda e: e.memset(ones_q[:], 1.0 / 384), writes=["ones_q"])
    P.op("dve", lambda e: e.memset(ones_kv[:], 1.0 / 256), writes=["ones_kv"])
    eps_sb = cx.sb("eps_sb", [128, 1])
    P.op("dve", lambda e: e.memset(eps_sb[:], EPS), writes=["eps"])

    xr = cx.ring("x", 2, [128, 8, 512])
    ur = cx.ring("u", 2, [128, 8, 512], BF16)
    lat_r = cx.ring("lat", 1, [128, 5, 512])
    latn_r = cx.ring("latn", 1, [128, 5, 512], BF16)
    sq_r = cx.ring("sq", 2, [128, 512], BF16)
    rs_r = cx.ring("rs", 2, [128, 512])
    qb_r = cx.ring("qb", 2, [96, 512], BF16)
    t1_r = cx.ring("t1", 2, [96, 512])
    t2_r = cx.ring("t2", 2, [96, 512])
    qf_r = cx.ring("qf", 3, [96, 512], BF16)
    ob_r = cx.ring("ob", 3, [128, 512], BF16)
    xv = xT.rearrange("(k p) t -> p k t", p=128)

    def do_tile(t0, tw, col):
        x, xres = xr.nxt()
        cx.load("sp", x[:, :, :tw], xv[:, :, t0:t0 + tw], xres)
        u, ures = ur.nxt()
        emit_modulate(cx, x, xres, u, ures, tw, col, mod_sb, modp_sb, 0)
        ureads = [(ures, k) for k in range(8)]
        lat, latres = lat_r.nxt()
        latn, latnres = latn_r.nxt()

        def proj(ps, c0, m, n0=0, n1=None):
            n1 = tw if n1 is None else n1

            def f(e):
                ins = None
                for k in range(8):
                    ins = e.matmul(ps[0:m, :n1 - n0], win[:, k, c0:c0 + m], u[:, k, n0:n1], start=(k == 0), stop=(k == 7))
                return ins
            return f
        for grp, (c_lo, nch, ones, oneres, gain, gres) in enumerate(((0, 3, ones_q, "ones_q", gq, "gq"),
                                                                       (384, 2, ones_kv, "ones_kv", gkv, "gkv"))):
            base = 0 if grp == 0 else 3
            pms, pmsres = pms_r.nxt()
            for c in range(nch):
                pq, pqres = pq_r.nxt()
                P.op("pe", proj(pq, c_lo + c * 128, 128), reads=ureads + winres, writes=[pqres])
                sq, sqres = sq_r.nxt()
                P.op("act", lambda e, sq=sq, pq=pq: e.activation(out=sq[:, :tw], in_=pq[:, :tw], func=AF.Square), reads=[pqres], writes=[sqres])
                P.op("dve", lambda e, pq=pq, c=c, base=base: e.tensor_copy(out=lat[:, base + c, :tw], in_=pq[:, :tw]),
                     reads=[pqres], writes=[(latres, base + c)])
                P.op("pe", lambda e, pms=pms, sq=sq, c=c, nch=nch, ones=ones: e.matmul(pms[:, :tw], ones[:], sq[:, :tw], start=(c == 0), stop=(c == nch - 1)),
                     reads=[sqres, oneres], writes=[pmsres])
            rs, rsres = rs_r.nxt()
            P.op("act", lambda e, rs=rs, pms=pms: e.activation(out=rs[:, :tw], in_=pms[:, :tw], func=AF.Sqrt, bias=eps_sb[:, 0:1]),
                 reads=[pmsres, "eps"], writes=[rsres])
            P.op("dve", lambda e, rs=rs: e.reciprocal(out=rs[:, :tw], in_=rs[:, :tw]), reads=[rsres], writes=[rsres])
            for c in range(nch):
                P.op("dve", lambda e, c=c, base=base, rs=rs, gain=gain: e.scalar_tensor_tensor(
                    out=latn[:, base + c, :tw], in0=lat[:, base + c, :tw], scalar=gain[:, c:c + 1], in1=rs[:, :tw], op0=ALU.mult, op1=ALU.mult),
                    reads=[(latres, base + c), rsres, gres], writes=[(latnres, base + c)])
        cqn_reads = [(latnres, c) for c in range(3)]
        ckvn_reads = [(latnres, 3 + c) for c in range(2)]
        def head_rope(pqh, pqres, nrows_lo, stores):
            qb, qbres = qb_r.nxt()
            P.op("dve", lambda e: e.tensor_copy(out=qb[:, :tw], in_=pqh[0:96, :tw]), reads=[pqres], writes=[qbres])
            prot, protres = prot_r.nxt()
            P.op("pe", lambda e: e.matmul(prot[0:96, :tw], rot96[:], qb[:, :tw], start=True, stop=True), reads=[qbres, "rot96"], writes=[protres])
            t1, t1res = t1_r.nxt()
            P.op("dve", lambda e: e.tensor_tensor(out=t1[:, :tw], in0=pqh[0:96, :tw], in1=cos96[:, t0:t0 + tw], op=ALU.mult),
                 reads=[pqres, "cos96"], writes=[t1res])
            t2, t2res = t2_r.nxt()
            P.op("dve", lambda e: e.tensor_tensor(out=t2[:, :tw], in0=prot[0:96, :tw], in1=sin96[:, t0:t0 + tw], op=ALU.mult),
                 reads=[protres, "sin96"], writes=[t2res])
            qf, qfres = qf_r.nxt()
            P.op("dve", lambda e: e.tensor_tensor(out=qf[:, :tw], in0=t1[:, :tw], in1=t2[:, :tw], op=ALU.add), reads=[t1res, t2res], writes=[qfres])
            for (dst, lo, hi) in stores:
                cx.store("sp", dst, qf[lo:hi, :tw], qfres)

        pkr, pkrres = pqh_r.nxt()

        def mmkr(e):
            ins = None
            for k in range(8):
                ins = e.matmul(pkr[64:96, :tw], win[:, k, 640:672], u[:, k, :tw], start=(k == 0), stop=(k == 7))
            return ins
        P.op("dve", lambda e: e.memset(pkr[0:64, :tw], 0.0), writes=[pkrres])
        P.op("pe", mmkr, reads=ureads + winres, writes=[pkrres])
        head_rope(pkr, pkrres, 64, [(kT_o[h * 96 + 64:h * 96 + 96, t0:t0 + tw], 64, 96) for h in range(8)])
        for h in range(8):
            pqh, pqres = pqh_r.nxt()

            def mmq(e, pqh=pqh, h=h):
                ins = None
                for k in range(3):
                    ins = e.matmul(pqh[0:96, :tw], wqup[:, k, h * 96:(h + 1) * 96], latn[:, k, :tw], start=(k == 0), stop=(k == 2))
                return ins
            P.op("pe", mmq, reads=cqn_reads + wqres, writes=[pqres])
            head_rope(pqh, pqres, 0, [(qT_o[h * 96:(h + 1) * 96, t0:t0 + tw], 0, 96)])
        for hp in range(4):
            pq, pqres = pq_r.nxt()

            def mmk(e, pq=pq, hp=hp):
                ins = None
                for k in range(2):
                    ins = e.matmul(pq[:, :tw], wkvk[:, k, hp * 128:(hp + 1) * 128], latn[:, 3 + k, :tw], start=(k == 0), stop=(k == 1))
                return ins
            P.op("pe", mmk, reads=ckvn_reads + wkres, writes=[pqres])
            ob, obres = ob_r.nxt()
            P.op("act", lambda e, ob=ob, pq=pq: e.activation(out=ob[:, :tw], in_=pq[:, :tw], func=AF.Copy), reads=[pqres], writes=[obres])
            for hh in range(2):
                h = 2 * hp + hh
                cx.store("pool", kT_o[h * 96:h * 96 + 64, t0:t0 + tw], ob[hh * 64:(hh + 1) * 64, :tw], obres)
        for (dst, c_lo) in ((nqT_o, 672), (nkT_o, 1184)):
            for c in range(4):
                pq, pqres = pq_r.nxt()
                P.op("pe", proj(pq, c_lo + c * 128, 128), reads=ureads + winres, writes=[pqres])
                ob, obres = ob_r.nxt()
                P.op("act", lambda e, ob=ob, pq=pq: e.activation(out=ob[:, :tw], in_=pq[:, :tw], func=AF.Copy), reads=[pqres], writes=[obres])
                cx.store("pool", dst[c * 128:(c + 1) * 128, t0:t0 + tw], ob[:, :tw], obres)
        for tb in range(tw // 128):
            pv, pvres = pv_r.nxt()

            def mmv(e, pv=pv, tb=tb):
                ins = None
                for k in range(2):
                    ins = e.matmul(pv[:, :512], latn[:, 3 + k, tb * 128:(tb + 1) * 128], wkvv[:, k, :], start=(k == 0), stop=(k == 1))
                return ins
            P.op("pe", mmv, reads=ckvn_reads + wvres, writes=[pvres])
            ob, obres = ob_r.nxt()
            P.op("act", lambda e, ob=ob, pv=pv: e.activation(out=ob[:, :512], in_=pv[:, :512], func=AF.Copy), reads=[pvres], writes=[obres])
            cx.store("pool", v_o[t0 + tb * 128:t0 + (tb + 1) * 128, :], ob[:, :512], obres)
            pv, pvres = pv_r.nxt()

            def mmnv(e, pv=pv, tb=tb):
                ins = None
                for k in range(8):
                    ins = e.matmul(pv[:, :512], u[:, k, tb * 128:(tb + 1) * 128], win[:, k, 1696:2208], start=(k == 0), stop=(k == 7))
                return ins
            P.op("pe", mmnv, reads=ureads + winres, writes=[pvres])
            ob, obres = ob_r.nxt()
            P.op("act", lambda e, ob=ob, pv=pv: e.activation(out=ob[:, :512], in_=pv[:, :512], func=AF.Copy), reads=[pvres], writes=[obres])
            cx.store("pool", nv_o[t0 + tb * 128:t0 + (tb + 1) * 128, :], ob[:, :512], obres)

    for (t0, tw, col) in tiles_own_ctx():
        do_tile(t0, tw, col)
    return cx


def rope_tables32(j):
    t = np.arange(TOWN) + j * TOWN
    row = (t // GRID_W).astype(np.float32)
    colp = (t % GRID_W).astype(np.float32)
    inv = (10000.0 ** (-np.arange(8, dtype=np.float64) / 8)).astype(np.float32)
    cos = np.ones((96, NT), np.float32)
    sin = np.zeros((96, NT), np.float32)
    for d in range(32):
        pos = row if d < 16 else colp
        ang = (pos * inv[d % 8]).astype(np.float32)
        cos[64 + d, :TOWN] = np.cos(ang)
        sin[64 + d, :TOWN] = np.sin(ang)
    return cos, sin


def rot96():
    m = np.zeros((96, 96), np.float32)
    m[64:, 64:] = rot_matrix(32, 16)
    return m


def stage_A1(inp, xT_list):
    cx = build_A1()
    wkv = inp["od_w_kv_up"][0].reshape(256, 8, 128)
    maps = []
    for c in range(NCORES):
        cos, sin = rope_tables32(c % 4)
        maps.append(dict(
            xT=xT_list[c], cs=core_cs(inp["c"], inp["c_ctx"], c),
            wada=np.ascontiguousarray(inp["w_ada"][1]), bada=pcol(inp["b_ada"][1]),
            win=np.ascontiguousarray(inp["od_w_in"][0]),
            gql=pcol(inp["od_q_lat_gain"][0]), gkvl=pcol(inp["od_kv_lat_gain"][0]),
            wqup=np.ascontiguousarray(inp["od_w_q_up"][0]),
            wkvk=np.ascontiguousarray(wkv[:, :, :64].reshape(256, 512)),
            wkvv=np.ascontiguousarray(wkv[:, :, 64:].reshape(256, 512)),
            cos96=cos, sin96=sin, rot96=rot96()))
    return run_spmd(cx, maps)


NA_SCALE = 0.125


def build_NA():
    cx = Ctx()
    P = cx.P
    nq_d = cx.inp("nqT", [512, TOWN], BF16)
    nk_d = cx.inp("nkTh", [512, TOWN + 512], BF16)
    nv_d = cx.inp("nvh", [128, 36 * 8 * 65], BF16)
    kc_d = cx.inp("nkc", [512, CTX], BF16)
    vc_d = cx.inp("nvc", [128, 2 * 8 * 65], BF16)
    bm_d = {n: cx.inp(n, [128, 48 * 256]) for n in ("bm_int", "bm_first", "bm_last")}
    id_d = cx.inp("ident", [128, 128])
    sel_d = cx.inp("sel", [65, 64])
    o_d = cx.out("naT", [512, TOWN], BF16)

    kh = cx.sb("kh", [128, 4, TOWN + 512], BF16)
    qs = cx.sb("qs", [128, 4, TOWN], BF16)
    kcs = cx.sb("kcs", [128, 4, CTX], BF16)
    vb = cx.sb("vb", [128, 36, 8, 65], BF16)
    vc = cx.sb("vc", [128, 2, 8, 65], BF16)
    cx.load("sp", kh[:], nk_d.rearrange("(c p) t -> p c t", p=128), "kh")
    cx.load("sp", qs[:], nq_d.rearrange("(c p) t -> p c t", p=128), "qs")
    cx.load("sp", kcs[:], kc_d.rearrange("(c p) t -> p c t", p=128), "kcs")
    cx.load("sp", vb[:].rearrange("p a h d -> p (a h d)"), nv_d, "vb")
    cx.load("sp", vc[:].rearrange("p a h d -> p (a h d)"), vc_d, "vc")
    ident = cx.sb("ident_sb", [128, 128], BF16)
    cx.load("pool", ident[:], id_d, "ident")
    sel = cx.sb("sel_sb", [65, 64])
    cx.load("sp", sel[:], sel_d, "sel")
    bmi = cx.sb("bmi", [128, 48, 256], BF16)
    bme = cx.sb("bme", [128, 48, 256], BF16)
    stg_r = cx.ring("stg", 2, [128, 6, 256])

    def load_table(dst, dres, name):
        src = bm_d[name].rearrange("p (a q) -> p a q", q=256)
        for h in range(8):
            stg, sres = stg_r.nxt()
            cx.load("sp", stg[:], src[:, h * 6:(h + 1) * 6, :], sres)
            P.op("dve", lambda e, stg=stg, h=h: e.tensor_scalar_mul(out=dst[:, h * 6:(h + 1) * 6, :], in0=stg[:], scalar1=1.0 / NA_SCALE),
                 reads=[sres], writes=[(dres, h)])
    load_table(bmi, "bmi", "bm_int")
    load_table(bme, "bme", "bm_first")

    ps_r = cx.psring("ps", 4)
    po_r = cx.psring("po", 2)
    pb_r = cx.psring("pb", 1)
    pt_r = cx.ring("pt", 2, [128, 8, 256], BF16)
    os_r = cx.ring("os", 2, [65, 256])
    at_r = cx.ring("at", 2, [64, 256], BF16)

    def unit(m, h):
        hp, pb0 = h // 2, (h % 2) * 64
        tab, tres = (bme, "bme") if m in (0, 15) else (bmi, "bmi")
        pt, ptres = pt_r.nxt()
        q0 = 256 * m
        for bk in range(4):
            ps, psres = ps_r.nxt()

            def mm(e, ps=ps, bk=bk):
                ins = None
                for cc in range(2):
                    c = 2 * bk + cc
                    o = cc * 256
                    if c < 6:
                        e.matmul(ps[:, o:o + 256], kh[pb0:pb0 + 64, hp, q0 + c * 128:q0 + (c + 1) * 128], qs[pb0:pb0 + 64, hp, q0:q0 + 256],
                                 start=True, stop=False)
                        ins = e.matmul(ps[:, o:o + 256], ident[:], tab[:, h * 6 + c, :], start=False, stop=True)
                    else:
                        ins = e.matmul(ps[:, o:o + 256], kcs[pb0:pb0 + 64, hp, (c - 6) * 128:(c - 5) * 128], qs[pb0:pb0 + 64, hp, q0:q0 + 256],
                                       start=True, stop=True)
                return ins
            P.op("pe", mm, reads=["kh", "qs", "kcs", "ident", (tres, h)], writes=[psres])
            P.op("act", lambda e, ps=ps, bk=bk: e.activation(out=pt[:, 2 * bk:2 * bk + 2, :].rearrange("p a q -> p (a q)"), in_=ps[:, :],
                                                            func=AF.Exp, scale=NA_SCALE),
                 reads=[psres], writes=[(ptres, bk)])
        po, pores = po_r.nxt()

        def pv(e):
            ins = None
            for c in range(8):
                lhs = vb[:, 2 * m + c, h, :] if c < 6 else vc[:, c - 6, h, :]
                ins = e.matmul(po[0:65, :256], lhs, pt[:, c, :], start=(c == 0), stop=(c == 7))
            return ins
        P.op("pe", pv, reads=[(ptres, bk) for bk in range(4)] + ["vb", "vc"], writes=[pores])
        osb, osres = os_r.nxt()
        P.op("act", lambda e: e.activation(out=osb[:, :], in_=po[0:65, :256], func=AF.Copy), reads=[pores], writes=[osres])
        P.op("dve", lambda e: e.reciprocal(out=osb[64:65, :], in_=osb[64:65, :]), reads=[osres], writes=[osres])
        pbk, pbres = pb_r.nxt()
        P.op("pe", lambda e: e.matmul(pbk[0:64, :256], sel[:], osb[:, :], start=True, stop=True), reads=[osres, "sel"], writes=[pbres])
        at, atres = at_r.nxt()
        P.op("dve", lambda e: e.tensor_tensor(out=at[:, :], in0=osb[0:64, :], in1=pbk[0:64, :256], op=ALU.mult), reads=[osres, pbres], writes=[atres])
        cx.store("pool", o_d[h * 64:(h + 1) * 64, q0:q0 + 256], at[:, :], atres)

    for m in range(16):
        if m == 15:
            load_table(bme, "bme", "bm_last")
        for h in range(8):
            unit(m, h)
    return cx


def na_table(bias, R0):
    kr_rel = np.arange(12)
    kr = R0 - 4 + kr_rel
    qr = R0 + np.arange(4)
    rs = np.clip(qr - 4, 0, 256 - 8)
    row_ok = (kr[:, None] >= rs[None, :]) & (kr[:, None] < rs[None, :] + 8)
    row_idx = np.clip(kr[:, None] - qr[None, :] + 7, 0, 14)
    kc = np.arange(64)
    qc = np.arange(64)
    cs0 = np.clip(qc - 8, 0, 48)
    col_ok = (kc[:, None] >= cs0[None, :]) & (kc[:, None] < cs0[None, :] + 16)
    col_idx = np.clip(kc[:, None] - qc[None, :] + 15, 0, 30)
    ok = row_ok[:, None, :, None] & col_ok[None, :, None, :]
    g = bias[:, row_idx[:, None, :, None], col_idx[None, :, None, :]]
    t = np.where(ok[None], g, np.float32(-30000.0)).astype(np.float32)
    t = t.reshape(8, 6, 2, 64, 256)
    t = t.transpose(2, 3, 0, 1, 4).reshape(128, 48 * 256)
    return np.ascontiguousarray(t)


def with_ones(v):
    o = np.ones(v.shape[:-1] + (65,), v.dtype)
    o[..., :64] = v
    return o


def stage_NA(inp, resA1):
    cx = build_NA()
    bias = inp["od_na_bias"][0]
    t_int = na_table(bias, 8)
    t_first = na_table(bias, 0)
    t_last = na_table(bias, 252)
    maps = []
    for c in range(NCORES):
        b, j = divmod(c, 4)
        nk = np.asarray(resA1[c]["nkT"])
        nv = np.asarray(resA1[c]["nv"])
        kh = np.zeros((512, TOWN + 512), NPBF)
        vh = np.zeros((TOWN + 512, 512), NPBF)
        kh[:, 256:256 + TOWN] = nk[:, :TOWN]
        vh[256:256 + TOWN] = nv[:TOWN]
        if j > 0:
            kh[:, :256] = np.asarray(resA1[c - 1]["nkT"])[:, TOWN - 256:TOWN]
            vh[:256] = np.asarray(resA1[c - 1]["nv"])[TOWN - 256:TOWN]
        if j < 3:
            kh[:, 256 + TOWN:] = np.asarray(resA1[c + 1]["nkT"])[:, :256]
            vh[256 + TOWN:] = np.asarray(resA1[c + 1]["nv"])[:256]
        vhl = with_ones(vh.reshape(36, 128, 8, 64).transpose(1, 0, 2, 3)).reshape(128, -1)
        vcl = with_ones(nv[TOWN:].reshape(2, 128, 8, 64).transpose(1, 0, 2, 3)).reshape(128, -1)
        maps.append(dict(
            nqT=np.ascontiguousarray(np.asarray(resA1[c]["nqT"])[:, :TOWN]), nkTh=kh, nvh=np.ascontiguousarray(vhl),
            nkc=np.ascontiguousarray(nk[:, TOWN:]), nvc=np.ascontiguousarray(vcl),
            bm_int=t_int, bm_first=(t_first if j == 0 else t_int), bm_last=(t_last if j == 3 else t_int),
            ident=np.eye(128, dtype=np.float32), sel=SEL))
    return run_spmd(cx, maps)


def stage_B1(resA1):
    cx = build_attn(8, 1, 96, 130, 96 ** -0.5)
    kv = gather_kv(resA1, "kT", "v", 8, 96)
    maps = []
    for c in range(NCORES):
        kf, vf = kv[c // 4]
        maps.append(dict(qT=np.asarray(resA1[c]["qT"]), kT=kf, v=vf, sel=SEL))
    return run_spmd(cx, maps)


def build_C1b():
    cx = Ctx()
    P = cx.P
    NE, NFE, FG = 8, 28, 4
    NG = NFE // FG
    ST, SUB = 1024, 256
    xT = cx.inp("xT", [D, NT])
    mod_d = cx.inp("mod", [128, 96])
    wr_d = cx.inp("wr", [D, NE])
    br_d = cx.inp("br", [NE, 1])
    wg_d = cx.inp("wg", [NE, D, NFE * 128])
    wu_d = cx.inp("wu", [NE, D, NFE * 128])
    wd_d = cx.inp("wd", [NE, NFE * 128, D])
    g_d = cx.inp("lng", [128, 8])
    b_d = cx.inp("lnb", [128, 8])
    id_d = cx.inp("ident", [128, 128])
    oh_d = cx.inp("onehot", [NE, NE * 128])
    xo = cx.out("xo", [D, TOWN])

    mod_sb = cx.sb("mod_sb", [128, 96])
    modp_sb = cx.sb("modp_sb", [128, 96])
    cx.load("sp", mod_sb[:], mod_d, "mod")
    P.op("dve", lambda e: e.tensor_scalar_add(out=modp_sb[:], in0=mod_sb[:], scalar1=1.0), reads=["mod"], writes=["modp"], fence=True)
    g_sb = cx.sb("g_sb", [128, 8])
    b_sb = cx.sb("b_sb", [128, 8])
    cx.load("sp", g_sb[:], g_d, "lngb")
    cx.load("sp", b_sb[:], b_d, "lngb2")
    P.op("dve", lambda e: e.tensor_copy(out=b_sb[:], in_=b_sb[:]), reads=["lngb2"], writes=["lngb"], fence=True)
    wr = cx.sb("wr_sb", [128, 8, NE])
    cx.load("sp", wr[:], wr_d.rearrange("(k p) e -> p k e", p=128), "wr")
    br = cx.sb("br_sb", [NE, 1])
    cx.load("sp", br[:], br_d, "br")
    ident = cx.sb("ident_sb", [128, 128])
    cx.load("sp", ident[:], id_d, "ident")
    oneh = cx.sb("oneh_sb", [NE, NE * 128])
    cx.load("sp", oneh[:], oh_d, "oneh")
    ln = LNState(cx, "ln", SUB)

    yacc = cx.sb("yacc", [128, 8, ST])
    u2 = cx.sb("u2", [128, 8, ST], BF16)
    gb = cx.sb("gb", [128, NE, ST], BF16)
    gT = cx.sb("gT", [NE, ST])
    x_r = cx.ring("x", 2, [128, 8, SUB])
    uf_r = cx.ring("uf", 1, [128, 8, SUB])
    lg_r = cx.ring("lg", 1, [NE, SUB])
    lgT_r = cx.ring("lgT", 2, [128, 8])
    mx_r = cx.ring("mx", 2, [128, 8])
    dm_r = cx.ring("dm", 2, [128, 2])
    ga_r = cx.ring("ga", 2, [128, 8])
    gq_r = cx.ring("gq", 2, [128, 8])
    wgq_r = cx.ring("wgq", 2, [128, 8, FG * 128], BF16)
    wuq_r = cx.ring("wuq", 2, [128, 8, FG * 128], BF16)
    wdq_r = cx.ring("wdq", 2, [128, FG, D], BF16)
    sg_r = cx.ring("sg", 2, [128, SUB])
    h1_r = cx.ring("h1", 2, [128, SUB])
    hq_r = cx.ring("hq", 2, [128, FG, SUB], BF16)
    pg_r = cx.psring("pg", 2)
    pu_r = cx.psring("pu", 2)
    pd_r = cx.psring("pd", 2)
    xv = xT.rearrange("(k p) t -> p k t", p=128)
    ov = xo.rearrange("(k p) t -> p k t", p=128)

    def router(s0, st):
        t0 = s0 + st * SUB
        x, xres = x_r.nxt()
        cx.load("sp", x[:, :, :], xv[:, :, t0:t0 + SUB], (xres, "ld"), reads=[(xres, k) for k in range(8)])
        uf, ufres = uf_r.nxt()
        for k in range(8):
            sc = modp_sb[:, 64 + 2 * k:64 + 2 * k + 1]
            sh = mod_sb[:, 48 + 2 * k:48 + 2 * k + 1]
            if k % 2 == 0:
                P.op("act", lambda e, k=k, sc=sc, sh=sh: e.activation(out=uf[:, k, :], in_=x[:, k, :], func=AF.Identity, bias=sh, scale=sc),
                     reads=[(xres, "ld"), "mod", "modp"], writes=[(ufres, k)])
            else:
                P.op("dve", lambda e, k=k, sc=sc, sh=sh: e.tensor_scalar(out=uf[:, k, :], in0=x[:, k, :], scalar1=sc, scalar2=sh,
                                                                        op0=ALU.mult, op1=ALU.add),
                     reads=[(xres, "ld"), "mod", "modp"], writes=[(ufres, k)])
        ufreads = [(ufres, k) for k in range(8)]
        P.op("dve", lambda e: e.tensor_copy(out=u2[:, :, st * SUB:(st + 1) * SUB], in_=uf[:, :, :]), reads=ufreads, writes=[("u2", st)])
        plg, plgres = pg_r.nxt()

        def mml(e):
            ins = None
            for k in range(8):
                ins = e.matmul(plg[0:NE, :SUB], wr[:, k, :], uf[:, k, :], start=(k == 0), stop=(k == 7))
            return ins
        P.op("pe", mml, reads=ufreads + ["wr"], writes=[plgres])
        lg, lgres = lg_r.nxt()
        P.op("act", lambda e: e.activation(out=lg[:, :], in_=plg[0:NE, :SUB], func=AF.Identity, bias=br[:, 0:1]), reads=[plgres, "br"], writes=[lgres])
        for tb in range(SUB // 128):
            pt, ptres = pu_r.nxt()
            P.op("pe", lambda e, pt=pt, tb=tb: e.transpose(pt[:, 0:NE], lg[:, tb * 128:(tb + 1) * 128], ident[0:NE, 0:NE]),
                 reads=[lgres, "ident"], writes=[ptres])
            lgT, lgTres = lgT_r.nxt()
            mx, mxres = mx_r.nxt()
            dm, dmres = dm_r.nxt()
            ga, gares = ga_r.nxt()
            gq, gqres = gq_r.nxt()
            P.op("dve", lambda e, pt=pt, lgT=lgT: e.tensor_copy(out=lgT[:], in_=pt[:, 0:NE]), reads=[ptres], writes=[lgTres], fence=True)
            P.op("dve", lambda e, mx=mx, lgT=lgT: e.max(out=mx[:], in_=lgT[:]), reads=[lgTres], writes=[mxres], fence=True)
            P.op("dve", lambda e, mx=mx, dm=dm: e.tensor_tensor(out=dm[:, 0:1], in0=mx[:, 1:2], in1=mx[:, 0:1], op=ALU.subtract),
                 reads=[mxres], writes=[(dmres, 0)], fence=True)
            P.op("act", lambda e, dm=dm: e.activation(out=dm[:, 1:2], in_=dm[:, 0:1], func=AF.Sigmoid), reads=[(dmres, 0)], writes=[(dmres, 1)], fence=True)
            P.op("dve", lambda e, dm=dm: e.tensor_scalar(out=dm[:, 0:1], in0=dm[:, 1:2], scalar1=-1.0, scalar2=1.0, op0=ALU.mult, op1=ALU.add),
                 reads=[(dmres, 1)], writes=[(dmres, 0)], fence=True)
            P.op("dve", lambda e, ga=ga, lgT=lgT, mx=mx, dm=dm: e.tensor_scalar(out=ga[:], in0=lgT[:], scalar1=mx[:, 0:1], scalar2=dm[:, 0:1],
                                                                             op0=ALU.is_equal, op1=ALU.mult),
                 reads=[lgTres, mxres, (dmres, 0)], writes=[gares], fence=True)
            P.op("dve", lambda e, gq=gq, lgT=lgT, mx=mx, dm=dm: e.tensor_scalar(out=gq[:], in0=lgT[:], scalar1=mx[:, 1:2], scalar2=dm[:, 1:2],
                                                                             op0=ALU.is_equal, op1=ALU.mult),
                 reads=[lgTres, mxres, (dmres, 1)], writes=[gqres], fence=True)
            P.op("dve", lambda e, ga=ga, gq=gq: e.tensor_tensor(out=ga[:], in0=ga[:], in1=gq[:], op=ALU.add), reads=[gares, gqres], writes=[gares], fence=True)
            pg2, pg2res = pu_r.nxt()
            P.op("pe", lambda e, pg2=pg2, ga=ga: e.transpose(pg2[0:NE, 0:128], ga[:, :], ident[:, :]), reads=[gares, "ident"], writes=[pg2res])
            c0 = st * SUB + tb * 128
            P.op("dve", lambda e, pg2=pg2, c0=c0: e.tensor_copy(out=gT[:, c0:c0 + 128], in_=pg2[0:NE, 0:128]), reads=[pg2res], writes=[("gT", c0)], fence=True)

    def gate_bcast():
        for e_ in range(NE):
            for hf in range(ST // 512):
                pgb, pgbres = pg_r.nxt()
                P.op("pe", lambda e, pgb=pgb, e_=e_, hf=hf: e.matmul(pgb[:, :512], oneh[:, e_ * 128:(e_ + 1) * 128], gT[:, hf * 512:(hf + 1) * 512],
                                                                    start=True, stop=True),
                     reads=[("gT", c0) for c0 in range(hf * 512, (hf + 1) * 512, 128)] + ["oneh"], writes=[pgbres])
                P.op("act", lambda e, pgb=pgb, e_=e_, hf=hf: e.activation(out=gb[:, e_, hf * 512:(hf + 1) * 512], in_=pgb[:, :512], func=AF.Copy),
                     reads=[pgbres], writes=[("gb", e_, hf)])

    def load_w(e_, g):
        wgq, wgres = wgq_r.nxt()
        wuq, wures = wuq_r.nxt()
        wdq, wdres = wdq_r.nxt()
        c0 = g * FG * 128
        cx.load("pool", wgq[:], wg_d[e_, :, c0:c0 + FG * 128].rearrange("(k p) c -> p k c", p=128), wgres)
        cx.load("pool", wuq[:], wu_d[e_, :, c0:c0 + FG * 128].rearrange("(k p) c -> p k c", p=128), wures)
        cx.load("pool", wdq[:], wd_d[e_, c0:c0 + FG * 128, :].rearrange("(f p) c -> p f c", p=128), wdres)
        return (wgq, wgres, wuq, wures, wdq, wdres)

    def expert_unit(e_, g, W, first):
        wgq, wgres, wuq, wures, wdq, wdres = W

        def sub(st):
            ureads = [("u2", st)]
            hq, hqres = hq_r.nxt()
            for f in range(FG):
                pg, pgres = pg_r.nxt()
                pu, pures = pu_r.nxt()

                def mmg(e, pg=pg, f=f):
                    ins = None
                    for k in range(8):
                        ins = e.matmul(pg[:, :SUB], wgq[:, k, f * 128:(f + 1) * 128], u2[:, k, st * SUB:(st + 1) * SUB], start=(k == 0), stop=(k == 7))
                    return ins

                def mmu(e, pu=pu, f=f):
                    ins = None
                    for k in range(8):
                        ins = e.matmul(pu[:, :SUB], wuq[:, k, f * 128:(f + 1) * 128], u2[:, k, st * SUB:(st + 1) * SUB], start=(k == 0), stop=(k == 7))
                    return ins
                P.op("pe", mmg, reads=ureads + [wgres], writes=[pgres])
                P.op("pe", mmu, reads=ureads + [wures], writes=[pures])
                sg, sgres = sg_r.nxt()
                P.op("act", lambda e, sg=sg, pg=pg: e.activation(out=sg[:, :], in_=pg[:, :SUB], func=AF.Silu), reads=[pgres], writes=[sgres])
                h1, h1res = h1_r.nxt()
                P.op("dve", lambda e, sg=sg, pu=pu, h1=h1: e.tensor_tensor(out=h1[:, :], in0=sg[:, :], in1=pu[:, :SUB], op=ALU.mult),
                     reads=[sgres, pures], writes=[h1res])
                P.op("dve", lambda e, h1=h1, f=f: e.tensor_tensor(out=hq[:, f, :], in0=h1[:, :], in1=gb[:, e_, st * SUB:(st + 1) * SUB], op=ALU.mult),
                     reads=[h1res, ("gb", e_, (st * SUB) // 512)], writes=[(hqres, f)])
            hreads = [(hqres, f) for f in range(FG)]
            for oc in range(8):
                pd, pdres = pd_r.nxt()

                def mmd(e, pd=pd, oc=oc):
                    ins = None
                    for f in range(FG):
                        ins = e.matmul(pd[:, :SUB], wdq[:, f, oc * 128:(oc + 1) * 128], hq[:, f, :], start=(f == 0), stop=(f == FG - 1))
                    return ins
                P.op("pe", mmd, reads=hreads + [wdres], writes=[pdres])
                ya = yacc[:, oc, st * SUB:(st + 1) * SUB]
                if first:
                    P.op("dve", lambda e, pd=pd, ya=ya: e.tensor_copy(out=ya, in_=pd[:, :SUB]), reads=[pdres], writes=[("yacc", oc, st)])
                else:
                    P.op("dve", lambda e, pd=pd, ya=ya: e.tensor_tensor(out=ya, in0=pd[:, :SUB], in1=ya, op=ALU.add),
                         reads=[pdres, ("yacc", oc, st)], writes=[("yacc", oc, st)])

        for st in range(ST // SUB):
            sub(st)

    def finish(s0, st):
        t0 = s0 + st * SUB
        x, xres = x_r.nxt()
        cx.load("sp", x[:, :, :], xv[:, :, t0:t0 + SUB], (xres, "ld"), reads=[(xres, k) for k in range(8)])
        for oc in range(8):
            P.op("act", lambda e, oc=oc: e.activation(out=x[:, oc, :], in_=x[:, oc, :], func=AF.Identity, scale=ALPHA),
                 reads=[(xres, "ld")], writes=[(xres, oc)])
            gate = mod_sb[:, 80 + 2 * oc:80 + 2 * oc + 1]
            P.op("dve", lambda e, oc=oc, gate=gate: e.scalar_tensor_tensor(out=x[:, oc, :], in0=yacc[:, oc, st * SUB:(st + 1) * SUB], scalar=gate,
                                                                        in1=x[:, oc, :], op0=ALU.mult, op1=ALU.add),
                 reads=[("yacc", oc, st), (xres, oc), "mod"], writes=[(xres, oc)])
        emit_layernorm(cx, ln, x, lambda k: (xres, k), SUB, g_sb, b_sb, "lngb")
        P.dma("sp", "S_" + xres, lambda e, s: e.dma_start(out=ov[:, :, t0:t0 + SUB], in_=x[:, :, :]).then_inc(s, 16),
              reads=[(xres, k) for k in range(8)], writes=[(xres, "ld")])

    units = [(e_, g) for e_ in range(NE) for g in range(NG)]
    for s0 in range(0, TOWN, ST):
        W = load_w(*units[0])
        for st in range(ST // SUB):
            router(s0, st)
        gate_bcast()
        for i, (e_, g) in enumerate(units):
            Wn = load_w(*units[i + 1]) if i + 1 < len(units) else None
            expert_unit(e_, g, W, first=(i == 0))
            W = Wn
        for st in range(ST // SUB):
            finish(s0, st)
    return cx


def stage_C1a(inp, resA1, resB1, resNA, xT_list):
    cx = build_Ca(False)
    maps = []
    for c in range(NCORES):
        aT = np.zeros((1024, NT), NPBF)
        aT[:512] = np.asarray(resB1[c]["oT"])
        aT[512:, :TOWN] = np.asarray(resNA[c]["naT"])
        maps.append(dict(xT=xT_list[c], aT=aT, mod=np.asarray(resA1[c]["mod"]),
                         wout=np.ascontiguousarray(inp["od_w_out"][0]), lng=pcol(inp["ln1_g"][1]), lnb=pcol(inp["ln1_b"][1])))
    return run_spmd(cx, maps)


def stage_C1b(inp, resA1, resC1a):
    cx = build_C1b()
    oh = np.zeros((8, 8 * 128), np.float32)
    for e in range(8):
        oh[e, e * 128:(e + 1) * 128] = 1.0
    shared = dict(wr=np.ascontiguousarray(inp["od_w_router"][0]), br=np.ascontiguousarray(inp["od_b_router"][0][:, None]),
                  wg=np.ascontiguousarray(inp["od_w_gate"][0]), wu=np.ascontiguousarray(inp["od_w_up"][0]),
                  wd=np.ascontiguousarray(inp["od_w_down"][0]), lng=pcol(inp["ln2_g"][1]), lnb=pcol(inp["ln2_b"][1]),
                  ident=np.eye(128, dtype=np.float32), onehot=oh)
    maps = []
    for c in range(NCORES):
        m = dict(shared)
        m.update(xT=np.asarray(resC1a[c]["xo"]), mod=np.asarray(resA1[c]["mod"]))
        maps.append(m)
    return run_spmd(cx, maps)


def kernel(**inp):
    inp = {k: np.asarray(v) for k, v in inp.items()}
    resA0 = stage_A0(inp)
    resB0 = stage_B0(resA0)
    resC0a = stage_C0a(inp, resA0, resB0)
    resC0b = stage_C0b(inp, resA0, resC0a)
    x1 = [np.asarray(resC0b[c]["xo"]) for c in range(NCORES)]
    resA1 = stage_A1(inp, x1)
    resB1 = stage_B1(resA1)
    resNA = stage_NA(inp, resA1)
    resC1a = stage_C1a(inp, resA1, resB1, resNA, x1)
    resC1b = stage_C1b(inp, resA1, resC1a)
    out = np.empty((2, SEQ, D), np.float32)
    for c in range(NCORES):
        b, j = divmod(c, 4)
        out[b, j * TOWN:(j + 1) * TOWN] = np.asarray(resC1b[c]["xo"]).T
    return out
```

```python
import contextlib
import numpy as np
import ml_dtypes
import concourse.bass as bass
import concourse.mybir as mybir
from concourse.bass_utils import run_bass_kernel_spmd

F32 = mybir.dt.float32
BF16 = mybir.dt.bfloat16
AF = mybir.ActivationFunctionType
ALU = mybir.AluOpType
NPBF = ml_dtypes.bfloat16

NCORES = 8
D = 1024
SEQ = 16384
TOWN = 4096
CTX = 256
NT = TOWN + CTX
GRID_W = 64
EPS = 1e-6
ALPHA = 4 ** 0.25
ENGS = ("pe", "act", "dve", "pool", "sp")


class Prog:
    def __init__(self, nc):
        self.nc = nc
        self.ops = []
        self.last_w = {}
        self.readers = {}
        self.dma_cnt = {}
        self.excl = set()

    def _deps(self, reads, writes):
        ex = [r for r in reads if r in self.excl]
        if ex:
            reads = [r for r in reads if r not in self.excl]
            writes = list(writes) + ex
        deps = set()
        for r in reads:
            w = self.last_w.get(r)
            if w is not None:
                deps.add(w)
        for r in writes:
            deps.update(self.readers.get(r, ()))
            w = self.last_w.get(r)
            if w is not None:
                deps.add(w)
        i = len(self.ops)
        for r in reads:
            self.readers.setdefault(r, []).append(i)
        for r in writes:
            self.last_w[r] = i
            self.readers[r] = []
        return deps

    def op(self, eng, fn, reads=(), writes=(), fence=False):
        deps = self._deps(reads, writes)
        self.ops.append(dict(eng=eng, fn=fn, deps=deps, dma=None, sig=None, need=False, fence=fence))

    def dma(self, queue, key, fn, reads=(), writes=(), n=1):
        deps = self._deps(reads, writes)
        c = self.dma_cnt.get(key, 0) + 16 * n
        self.dma_cnt[key] = c
        self.ops.append(dict(eng=queue, fn=fn, deps=deps, dma=key, sig=(("dma", key), c), need=True, fence=True))

    def emit(self):
        nc = self.nc
        ops = self.ops
        for o in ops:
            for d in o["deps"]:
                p = ops[d]
                if p["dma"] is None and (p["eng"] != o["eng"] or o["fence"]):
                    p["need"] = True
        cnt = {e: 0 for e in ENGS}
        for o in ops:
            if o["dma"] is None and o["need"]:
                cnt[o["eng"]] += 1
                o["sig"] = (("eng", o["eng"]), cnt[o["eng"]])
        with contextlib.ExitStack() as st:
            sems = {}
            for e in ENGS:
                if cnt[e]:
                    sems[("eng", e)] = st.enter_context(nc.semaphore("s_" + e))
            for k in self.dma_cnt:
                sems[("dma", k)] = st.enter_context(nc.semaphore("d_" + str(k)))
            block = st.enter_context(nc.Block())
            per = {e: [] for e in ENGS}
            for i, o in enumerate(ops):
                per[o["eng"]].append(i)
            finals = [(("dma", k), c) for k, c in self.dma_cnt.items()]

            def body(ename):
                def run(eng):
                    waited = {}
                    for i in per[ename]:
                        o = ops[i]
                        need = {}
                        for d in o["deps"]:
                            p = ops[d]
                            if p["dma"] is None and p["eng"] == ename and not o["fence"]:
                                continue
                            sk, v = p["sig"]
                            if v > need.get(sk, 0):
                                need[sk] = v
                        for sk, v in need.items():
                            if waited.get(sk, 0) >= v:
                                continue
                            eng.wait_ge(sems[sk], v)
                            waited[sk] = v
                        if o["dma"] is not None:
                            o["fn"](eng, sems[("dma", o["dma"])])
                        else:
                            ins = o["fn"](eng)
                            if o["need"]:
                                ins.then_inc(sems[o["sig"][0]], 1)
                    if ename == "sp":
                        for sk, v in finals:
                            if waited.get(sk, 0) < v:
                                eng.wait_ge(sems[sk], v)
                return run

            block.tensor(body("pe"))
            block.scalar(body("act"))
            block.vector(body("dve"))
            block.gpsimd(body("pool"))
            block.sync(body("sp"))


class Ctx:
    def __init__(self):
        self.nc = bass.Bass("TRN2", target_bir_lowering=False)
        self.st = contextlib.ExitStack()
        self.P = Prog(self.nc)
        self.out_names = []
        self._u = 0

    def inp(self, name, shape, dt=F32):
        return self.nc.dram_tensor(name, list(shape), dt, kind="ExternalInput").ap()

    def out(self, name, shape, dt=F32):
        self.out_names.append(name)
        return self.nc.dram_tensor(name, list(shape), dt, kind="ExternalOutput").ap()

    def sb(self, name, shape, dt=F32):
        return self.st.enter_context(self.nc.sbuf_tensor(name, list(shape), dt))

    def ps(self, name, shape, dt=F32):
        self.P.excl.add(name)
        return self.st.enter_context(self.nc.psum_tensor(name, list(shape), dt))

    def ring(self, name, n, shape, dt=F32):
        return Ring([(self.sb("%s%d" % (name, i), shape, dt), "%s%d" % (name, i)) for i in range(n)])

    def psring(self, name, n, shape=(128, 512), dt=F32):
        return Ring([(self.ps("%s%d" % (name, i), (128, 512), dt), "%s%d" % (name, i)) for i in range(n)])

    def load(self, q, dst, src, res, reads=()):
        self.P.dma(q, "L_" + str(res), lambda e, s: e.dma_start(out=dst, in_=src).then_inc(s, 16),
                   reads=reads, writes=[res])

    def store(self, q, dst, src, res, dres=None):
        self.P.dma(q, "S_" + str(res), lambda e, s: e.dma_start(out=dst, in_=src).then_inc(s, 16),
                   reads=[res], writes=([dres] if dres else []))

    def finish(self):
        self.P.emit()
        self.st.close()
        return self.nc


class Ring:
    def __init__(self, items):
        self.items = items
        self.i = 0

    def nxt(self):
        it = self.items[self.i % len(self.items)]
        self.i += 1
        return it


def run_spmd(cx, in_maps):
    nc = cx.finish()
    res = run_bass_kernel_spmd(nc, in_maps, core_ids=list(range(NCORES)))
    return res.results


def tiles_own_ctx(tt=512):
    t = [(i * tt, tt, 0) for i in range(TOWN // tt)]
    t.append((TOWN, CTX, 1))
    return t


def pcol(v):
    v = np.asarray(v, np.float32)
    return np.ascontiguousarray(v.reshape(-1, 128).T)


def emit_ada(cx, cs_d, wada_d, bada_d, mod_sb, modp_sb, pm=None, pmres="ps_mod"):
    P = cx.P
    cs_sb = cx.sb("cs_sb", [128, 16])
    s_sb = cx.sb("s_sb", [128, 16])
    ba_sb = cx.sb("ba_sb", [128, 48])
    if pm is None:
        pm = cx.ps("ps_mod", [128, 512])
    cx.load("sp", cs_sb[:], cs_d, "cs_sb")
    cx.load("sp", ba_sb[:], bada_d, "ba_sb")
    P.op("act", lambda e: e.activation(out=s_sb[:], in_=cs_sb[:], func=AF.Silu), reads=["cs_sb"], writes=["s_sb"])
    wr = cx.ring("wada", 2, [128, 8, 512])
    wv = wada_d.rearrange("(k p) c -> p k c", p=128)
    for piece in range(12):
        wa, wres = wr.nxt()
        cx.load("sp", wa[:], wv[:, :, piece * 512:(piece + 1) * 512], wres)

        def mm(e, wa=wa, piece=piece):
            ins = None
            for f4 in range(4):
                fc = piece * 4 + f4
                for k in range(8):
                    ins = e.matmul(pm[:, fc * 2:fc * 2 + 2], wa[:, k, f4 * 128:(f4 + 1) * 128],
                                   s_sb[:, 2 * k:2 * k + 2], start=(k == 0), stop=(k == 7))
            return ins
        P.op("pe", mm, reads=[wres, "s_sb"], writes=[pmres])
    pmv = pm[:, 0:96].rearrange("p (m c) -> p m c", c=2)
    mv = mod_sb[:].rearrange("p (m c) -> p m c", c=2)
    for c in range(2):
        P.op("dve", lambda e, c=c: e.tensor_tensor(out=mv[:, :, c], in0=pmv[:, :, c], in1=ba_sb[:], op=ALU.add),
             reads=[pmres, "ba_sb"], writes=["mod"], fence=True)
    P.op("dve", lambda e: e.tensor_copy(out=modp_sb[:], in_=mod_sb[:]), reads=["mod"], writes=["modp"], fence=True)
    for lo in (16, 64):
        P.op("dve", lambda e, lo=lo: e.tensor_scalar_add(out=modp_sb[:, lo:lo + 16], in0=mod_sb[:, lo:lo + 16], scalar1=1.0),
             reads=["mod"], writes=["modp"], fence=True)


def emit_modulate(cx, x, xres, u, ures, tw, col, mod_sb, modp_sb, piece_shift, nk=8):
    P = cx.P
    for k in range(nk):
        sc = modp_sb[:, (piece_shift + 1) * 16 + 2 * k + col:(piece_shift + 1) * 16 + 2 * k + col + 1]
        sh = mod_sb[:, piece_shift * 16 + 2 * k + col:piece_shift * 16 + 2 * k + col + 1]
        if k % 2 == 0:
            P.op("act", lambda e, k=k, sc=sc, sh=sh: e.activation(out=u[:, k, :tw], in_=x[:, k, :tw], func=AF.Identity,
                                                                 bias=sh, scale=sc),
                 reads=[xres, "mod", "modp"], writes=[(ures, k)])
        else:
            P.op("dve", lambda e, k=k, sc=sc, sh=sh: e.tensor_scalar(out=u[:, k, :tw], in0=x[:, k, :tw], scalar1=sc,
                                                                    scalar2=sh, op0=ALU.mult, op1=ALU.add),
                 reads=[xres, "mod", "modp"], writes=[(ures, k)])


def build_A0():
    cx = Ctx()
    P = cx.P
    xT = cx.inp("xT", [D, NT])
    cs_d = cx.inp("cs", [128, 16])
    wada_d = cx.inp("wada", [D, 6 * D])
    bada_d = cx.inp("bada", [128, 48])
    win_d = cx.inp("win", [D, 1536])
    gq_d = cx.inp("gq", [128, 1])
    gk_d = cx.inp("gk", [128, 1])
    cos_d = cx.inp("cosT", [128, NT])
    sin_d = cx.inp("sinT", [128, NT])
    rt_d = cx.inp("rotT", [128, 128])
    ob_d = cx.inp("oneblk", [128, 128])
    mod_o = cx.out("mod", [128, 96])
    qT_o = cx.out("qT", [768, NT], BF16)
    kT_o = cx.out("kT", [256, NT], BF16)
    v_o = cx.out("v", [NT, 256], BF16)
    pT_o = cx.out("pT", [256, NT])

    mod_sb = cx.sb("mod_sb", [128, 96])
    modp_sb = cx.sb("modp_sb", [128, 96])
    emit_ada(cx, cs_d, wada_d, bada_d, mod_sb, modp_sb)
    cx.store("sp", mod_o, mod_sb[:], "mod")

    win = cx.sb("win_sb", [128, 8, 1536], BF16)
    cx.load("pool", win[:], win_d.rearrange("(k p) c -> p k c", p=128), "win")
    oneblk = cx.sb("oneblk_sb", [128, 128], BF16)
    cx.load("pool", oneblk[:], ob_d, "oneblk")
    rt32 = cx.sb("rt32", [128, 128])
    cx.load("sp", rt32[:], rt_d, "rt32")
    g_sb = cx.sb("g_sb", [128, 2])
    cx.load("sp", g_sb[:, 0:1], gq_d, "gq")
    cx.load("sp", g_sb[:, 1:2], gk_d, "gk")
    rg = [cx.sb("rgq", [128, 128], BF16), cx.sb("rgk", [128, 128], BF16)]
    cosg = [cx.sb("cosgq", [128, NT]), cx.sb("cosgk", [128, NT])]
    sin_sb = cx.sb("sin_sb", [128, NT])
    cx.load("sp", sin_sb[:], sin_d, "sin")
    gres = ["gq", "gk"]
    for i in range(2):
        cx.load("sp", cosg[i][:], cos_d, "cosg%d" % i)
        P.op("dve", lambda e, i=i: e.tensor_scalar_mul(out=rg[i][:], in0=rt32[:], scalar1=g_sb[:, i:i + 1]),
             reads=["rt32", gres[i]], writes=["rg%d" % i])
        P.op("dve", lambda e, i=i: e.tensor_scalar_mul(out=cosg[i][:], in0=cosg[i][:], scalar1=g_sb[:, i:i + 1]),
             reads=[gres[i], "cosg%d" % i], writes=["cosg%d" % i])

    eps_sb = cx.sb("eps_sb", [128, 1])
    P.op("dve", lambda e: e.memset(eps_sb[:], EPS), writes=["eps"])
    xr = cx.ring("x", 2, [128, 8, 512])
    ur = cx.ring("u", 2, [128, 8, 512], BF16)
    pq_r = cx.psring("pq", 2)
    pms_r = cx.psring("pms", 1)
    prot_r = cx.psring("prot", 1)
    pv_r = cx.psring("pv", 2)
    sq_r = cx.ring("sq", 2, [128, 512], BF16)
    qb_r = cx.ring("qb", 2, [128, 512], BF16)
    t1_r = cx.ring("t1", 2, [128, 512])
    t2_r = cx.ring("t2", 2, [128, 512])
    rs_r = cx.ring("rs", 2, [128, 512])
    qf_r = cx.ring("qf", 3, [128, 512], BF16)
    vs_r = cx.ring("vs", 2, [128, 256], BF16)
    pp_r = cx.ring("pp", 2, [128, 512])
    xv = xT.rearrange("(k p) t -> p k t", p=128)

    def do_tile(t0, tw, col):
        x, xres = xr.nxt()
        cx.load("sp", x[:, :, :tw], xv[:, :, t0:t0 + tw], xres)
        u, ures = ur.nxt()
        emit_modulate(cx, x, xres, u, ures, tw, col, mod_sb, modp_sb, 0)
        ureads = [(ures, k) for k in range(8)]
        for cc in range(8):
            isk = cc >= 6
            c0 = cc * 128 if not isk else 768 + (cc - 6) * 128
            gi = 1 if isk else 0
            pq, pqres = pq_r.nxt()

            def mmq(e, pq=pq, c0=c0):
                ins = None
                for k in range(8):
                    ins = e.matmul(pq[:, :tw], win[:, k, c0:c0 + 128], u[:, k, :tw], start=(k == 0), stop=(k == 7))
                return ins
            P.op("pe", mmq, reads=ureads + ["win"], writes=[pqres])
            sq, sqres = sq_r.nxt()
            P.op("act", lambda e, sq=sq, pq=pq: e.activation(out=sq[:, :tw], in_=pq[:, :tw], func=AF.Square),
                 reads=[pqres], writes=[sqres])
            qb, qbres = qb_r.nxt()
            P.op("dve", lambda e, qb=qb, pq=pq: e.tensor_copy(out=qb[:, :tw], in_=pq[:, :tw]), reads=[pqres], writes=[qbres])
            pms, pmsres = pms_r.nxt()
            P.op("pe", lambda e, pms=pms, sq=sq: e.matmul(pms[:, :tw], oneblk[:], sq[:, :tw], start=True, stop=True),
                 reads=[sqres, "oneblk"], writes=[pmsres])
            prot, protres = prot_r.nxt()
            P.op("pe", lambda e, prot=prot, qb=qb, gi=gi: e.matmul(prot[:, :tw], rg[gi][:], qb[:, :tw], start=True, stop=True),
                 reads=[qbres, "rg%d" % gi], writes=[protres])
            t1, t1res = t1_r.nxt()
            P.op("dve", lambda e, t1=t1, pq=pq, gi=gi: e.tensor_tensor(out=t1[:, :tw], in0=pq[:, :tw],
                                                                     in1=cosg[gi][:, t0:t0 + tw], op=ALU.mult),
                 reads=[pqres, "cosg%d" % gi], writes=[t1res])
            t2, t2res = t2_r.nxt()
            P.op("dve", lambda e, t2=t2, prot=prot: e.tensor_tensor(out=t2[:, :tw], in0=prot[:, :tw],
                                                                  in1=sin_sb[:, t0:t0 + tw], op=ALU.mult),
                 reads=[protres, "sin"], writes=[t2res])
            rs, rsres = rs_r.nxt()
            P.op("act", lambda e, rs=rs, pms=pms: e.activation(out=rs[:, :tw], in_=pms[:, :tw], func=AF.Sqrt, bias=eps_sb[:, 0:1]),
                 reads=[pmsres, "eps"], writes=[rsres])
            P.op("dve", lambda e, rs=rs: e.reciprocal(out=rs[:, :tw], in_=rs[:, :tw]), reads=[rsres], writes=[rsres])
            P.op("dve", lambda e, t1=t1, t2=t2: e.tensor_tensor(out=t1[:, :tw], in0=t1[:, :tw], in1=t2[:, :tw], op=ALU.add),
                 reads=[t1res, t2res], writes=[t1res])
            qf, qfres = qf_r.nxt()
            P.op("dve", lambda e, qf=qf, t1=t1, rs=rs: e.tensor_tensor(out=qf[:, :tw], in0=t1[:, :tw], in1=rs[:, :tw], op=ALU.mult),
                 reads=[t1res, rsres], writes=[qfres])
            dst = (kT_o[(cc - 6) * 128:(cc - 5) * 128, t0:t0 + tw] if isk else qT_o[cc * 128:(cc + 1) * 128, t0:t0 + tw])
            cx.store("sp", dst, qf[:, :tw], qfres)
        for tb in range(tw // 128):
            pv, pvres = pv_r.nxt()

            def mmv(e, pv=pv, tb=tb):
                ins = None
                for k in range(8):
                    ins = e.matmul(pv[:, :256], u[:, k, tb * 128:(tb + 1) * 128], win[:, k, 1024:1280],
                                   start=(k == 0), stop=(k == 7))
                return ins
            P.op("pe", mmv, reads=ureads + ["win"], writes=[pvres])
            vs, vsres = vs_r.nxt()
            P.op("act", lambda e, vs=vs, pv=pv: e.activation(out=vs[:], in_=pv[:, :256], func=AF.Copy), reads=[pvres], writes=[vsres])
            cx.store("pool", v_o[t0 + tb * 128:t0 + (tb + 1) * 128, :], vs[:], vsres)
        for pc in range(2):
            pq, pqres = pq_r.nxt()

            def mmp(e, pq=pq, pc=pc):
                ins = None
                for k in range(8):
                    ins = e.matmul(pq[:, :tw], win[:, k, 1280 + pc * 128:1280 + (pc + 1) * 128], u[:, k, :tw],
                                   start=(k == 0), stop=(k == 7))
                return ins
            P.op("pe", mmp, reads=ureads + ["win"], writes=[pqres])
            pp, ppres = pp_r.nxt()
            P.op("act", lambda e, pp=pp, pq=pq: e.activation(out=pp[:, :tw], in_=pq[:, :tw], func=AF.Copy), reads=[pqres], writes=[ppres])
            cx.store("pool", pT_o[pc * 128:(pc + 1) * 128, t0:t0 + tw], pp[:, :tw], ppres)

    for (t0, tw, col) in tiles_own_ctx():
        do_tile(t0, tw, col)
    return cx


def build_attn(nheads, group, dk, nkc_full, scale):
    cx = Ctx()
    P = cx.P
    nkv = nheads // group
    nkeys = nkc_full * 128
    qT = cx.inp("qT", [nheads * dk, NT], BF16)
    kT = cx.inp("kT", [nkv, dk, nkeys], BF16)
    vv = cx.inp("v", [nkv, 128, nkc_full, 64], BF16)
    sel_d = cx.inp("sel", [65, 64])
    oT = cx.out("oT", [nheads * 64, NT], BF16)

    kg_r = cx.ring("kg", 2, [dk, nkeys], BF16)
    vg_r = cx.ring("vg", 2, [128, nkc_full, 65], BF16)
    for vg, vres in vg_r.items:
        P.op("dve", lambda e, vg=vg: e.memset(vg[:, :, 64:65], 1.0), writes=[(vres, "ones")])
    sel = cx.sb("sel_sb", [65, 64])
    cx.load("sp", sel[:], sel_d, "sel")
    q_r = cx.ring("qh", 2, [dk, NT], BF16)
    ps_r = cx.psring("ps", 4)
    po_r = cx.psring("po", 2)
    pb_r = cx.psring("pb", 1)
    pt_r = cx.ring("pt", 4, [128, 512], BF16)
    os_r = cx.ring("os", 2, [65, 512])
    at_r = cx.ring("at", 2, [64, 512], BF16)
    LOOK = 3
    for g in range(nkv):
        kg, kres = kg_r.nxt()
        cx.load("sp", kg[:], kT[g], kres)
        vg, vres = vg_r.nxt()
        cx.load("sp", vg[:, :, 0:64], vv[g], vres)
        for h in range(g * group, (g + 1) * group):
            qh, qres = q_r.nxt()
            cx.load("sp", qh[:], qT[h * dk:(h + 1) * dk, :], qres)
            steps = []
            for (t0, tw, col) in tiles_own_ctx():
                nkc = nkc_full if col == 0 else 2
                for kc in range(nkc):
                    steps.append((t0, tw, kc, nkc))
            pss = {}

            def emit_qk(i, kg=kg, kres=kres, qh=qh, qres=qres):
                t0, tw, kc, nkc = steps[i]
                ps, psres = ps_r.nxt()
                pss[i] = (ps, psres)
                P.op("pe", lambda e: e.matmul(ps[:, :tw], kg[:, kc * 128:(kc + 1) * 128], qh[:, t0:t0 + tw], start=True, stop=True),
                     reads=[kres, qres], writes=[psres])

            def emit_rest(i, po, pores, vg=vg, vres=vres):
                t0, tw, kc, nkc = steps[i]
                ps, psres = pss.pop(i)
                pt, ptres = pt_r.nxt()
                P.op("act", lambda e: e.activation(out=pt[:, :tw], in_=ps[:, :tw], func=AF.Exp, scale=scale), reads=[psres], writes=[ptres])
                P.op("pe", lambda e: e.matmul(po[0:65, :tw], vg[:, kc, :], pt[:, :tw], start=(kc == 0), stop=(kc == nkc - 1)),
                     reads=[ptres, vres, (vres, "ones")], writes=[pores])

            def emit_norm(i, po, pores, h=h):
                t0, tw, kc, nkc = steps[i]
                osb, osres = os_r.nxt()
                P.op("act", lambda e: e.activation(out=osb[:, :tw], in_=po[0:65, :tw], func=AF.Copy), reads=[pores], writes=[osres])
                P.op("dve", lambda e: e.reciprocal(out=osb[64:65, :tw], in_=osb[64:65, :tw]), reads=[osres], writes=[osres])
                pb, pbres = pb_r.nxt()
                P.op("pe", lambda e: e.matmul(pb[0:64, :tw], sel[:], osb[:, :tw], start=True, stop=True), reads=[osres, "sel"], writes=[pbres])
                at, atres = at_r.nxt()
                P.op("dve", lambda e: e.tensor_tensor(out=at[:, :tw], in0=osb[0:64, :tw], in1=pb[0:64, :tw], op=ALU.mult),
                     reads=[osres, pbres], writes=[atres])
                cx.store("pool", oT[h * 64:(h + 1) * 64, t0:t0 + tw], at[:, :tw], atres)

            for i in range(min(LOOK, len(steps))):
                emit_qk(i)
            po = pores = None
            for i in range(len(steps)):
                if steps[i][2] == 0:
                    po, pores = po_r.nxt()
                if i + LOOK < len(steps):
                    emit_qk(i + LOOK)
                emit_rest(i, po, pores)
                if steps[i][2] == steps[i][3] - 1:
                    emit_norm(i, po, pores)
    return cx


def rope_tables(j):
    t = np.arange(TOWN) + j * TOWN
    row = (t // GRID_W).astype(np.float64)
    colp = (t % GRID_W).astype(np.float64)
    inv = 10000.0 ** (-np.arange(16, dtype=np.float64) / 16)
    cos = np.ones((128, NT), np.float32)
    sin = np.zeros((128, NT), np.float32)
    for p in range(128):
        d = p % 64
        pos = row if d < 32 else colp
        ang = (pos.astype(np.float32) * inv[d % 16].astype(np.float32)).astype(np.float32)
        cos[p, :TOWN] = np.cos(ang)
        sin[p, :TOWN] = np.sin(ang)
    return cos, sin


def rot_matrix(n=128, blk=32):
    rt = np.zeros((n, n), np.float32)
    h = blk // 2
    for p in range(n):
        if p % blk < h:
            rt[p + h, p] = -1.0
        else:
            rt[p - h, p] = 1.0
    return rt


def blockdiag_ones(n, blk):
    m = np.zeros((n, n), np.float32)
    for i in range(0, n, blk):
        m[i:i + blk, i:i + blk] = 1.0 / blk
    return m


def core_xT(x, ctx, c):
    b, j = divmod(c, 4)
    return np.ascontiguousarray(np.concatenate([x[b, j * TOWN:(j + 1) * TOWN], ctx[b]], axis=0).T)


def core_cs(cvec, c_ctx, c):
    b = c // 4
    cs = np.empty((128, 16), np.float32)
    cs[:, 0::2] = pcol(cvec[b])
    cs[:, 1::2] = pcol(c_ctx)
    return cs


SEL = np.zeros((65, 64), np.float32)
SEL[64, :] = 1.0


def gather_kv(res, kname, vname, nkv, dk):
    out = []
    for b in range(2):
        ks = [np.asarray(res[4 * b][kname])[:, TOWN:]] + [np.asarray(res[4 * b + j][kname])[:, :TOWN] for j in range(4)]
        kf = np.concatenate(ks, axis=1).reshape(nkv, dk, -1)
        vs = [np.asarray(res[4 * b][vname])[TOWN:]] + [np.asarray(res[4 * b + j][vname])[:TOWN] for j in range(4)]
        vf = np.concatenate(vs, axis=0)
        nkc = vf.shape[0] // 128
        vf = np.ascontiguousarray(vf.reshape(nkc, 128, nkv, 64).transpose(2, 1, 0, 3))
        out.append((np.ascontiguousarray(kf), vf))
    return out


def stage_A0(inp):
    x, ctx = inp["x"], inp["ctx"]
    cx = build_A0()
    maps = []
    for c in range(NCORES):
        cos, sin = rope_tables(c % 4)
        maps.append(dict(
            xT=core_xT(x, ctx, c), cs=core_cs(inp["c"], inp["c_ctx"], c),
            wada=np.ascontiguousarray(inp["w_ada"][0]), bada=pcol(inp["b_ada"][0]),
            win=np.ascontiguousarray(inp["ev_w_in"][0]),
            gq=np.tile(inp["ev_q_gain"][0], 2)[:, None].astype(np.float32),
            gk=np.tile(inp["ev_k_gain"][0], 2)[:, None].astype(np.float32),
            cosT=cos, sinT=sin, rotT=rot_matrix(), oneblk=blockdiag_ones(128, 64)))
    return run_spmd(cx, maps)


def stage_B0(resA):
    cx = build_attn(12, 3, 64, 130, 64 ** -0.5)
    kv = gather_kv(resA, "kT", "v", 4, 64)
    maps = []
    for c in range(NCORES):
        kf, vf = kv[c // 4]
        maps.append(dict(qT=np.asarray(resA[c]["qT"]), kT=kf, v=vf, sel=SEL))
    return run_spmd(cx, maps)


class LNState:
    def __init__(self, cx, tag, width=512):
        self.ones = cx.sb(tag + "_ones", [128, 128])
        cx.P.op("dve", lambda e: e.memset(self.ones[:], 1.0 / D), writes=[tag + "_ones"])
        self.ones_res = tag + "_ones"
        self.eps = cx.sb(tag + "_eps", [128, 1])
        cx.P.op("dve", lambda e: e.memset(self.eps[:], EPS), writes=[tag + "_eps"])
        self.eps_res = tag + "_eps"
        self.pmean = cx.psring(tag + "_pmean", 1, (128, width))
        self.pmsq = cx.psring(tag + "_pmsq", 1, (128, width))
        self.rsq = cx.ring(tag + "_rsq", 2, [128, width])
        self.m = cx.ring(tag + "_m", 1, [128, width])
        self.var = cx.ring(tag + "_var", 1, [128, width])
        self.nb = cx.ring(tag + "_nb", 1, [128, width])
        self.t = cx.ring(tag + "_t", 2, [128, width])


def emit_layernorm(cx, ln, r, rres, tw, g_sb, b_sb, gres, out=None, outres=None):
    P = cx.P
    if out is None:
        out, outres = r, rres
    pmean, pmres = ln.pmean.nxt()
    pmsq, pqres = ln.pmsq.nxt()
    for k in range(8):
        rsq, rsqres = ln.rsq.nxt()
        P.op("act", lambda e, rsq=rsq, k=k: e.activation(out=rsq[:, :tw], in_=r[:, k, :tw], func=AF.Square),
             reads=[rres(k)], writes=[rsqres])
        P.op("pe", lambda e, k=k: e.matmul(pmean[:, :tw], ln.ones[:], r[:, k, :tw], start=(k == 0), stop=(k == 7)),
             reads=[rres(k), ln.ones_res], writes=[pmres])
        P.op("pe", lambda e, rsq=rsq, k=k: e.matmul(pmsq[:, :tw], ln.ones[:], rsq[:, :tw], start=(k == 0), stop=(k == 7)),
             reads=[rsqres, ln.ones_res], writes=[pqres])
    m, mres = ln.m.nxt()
    var, vres = ln.var.nxt()
    nb, nbres = ln.nb.nxt()
    P.op("act", lambda e: e.activation(out=m[:, :tw], in_=pmean[:, :tw], func=AF.Copy), reads=[pmres], writes=[mres])
    P.op("dve", lambda e: e.tensor_tensor(out=var[:, :tw], in0=m[:, :tw], in1=m[:, :tw], op=ALU.mult), reads=[mres], writes=[vres])
    P.op("dve", lambda e: e.tensor_tensor(out=var[:, :tw], in0=pmsq[:, :tw], in1=var[:, :tw], op=ALU.subtract),
         reads=[pqres, vres], writes=[vres])
    P.op("dve", lambda e: e.tensor_scalar_max(out=var[:, :tw], in0=var[:, :tw], scalar1=0.0), reads=[vres], writes=[vres])
    P.op("act", lambda e: e.activation(out=var[:, :tw], in_=var[:, :tw], func=AF.Sqrt, bias=ln.eps[:, 0:1]),
         reads=[vres, ln.eps_res], writes=[vres])
    P.op("dve", lambda e: e.reciprocal(out=var[:, :tw], in_=var[:, :tw]), reads=[vres], writes=[vres])
    P.op("dve", lambda e: e.scalar_tensor_tensor(out=nb[:, :tw], in0=m[:, :tw], scalar=-1.0, in1=var[:, :tw],
                                                 op0=ALU.mult, op1=ALU.mult), reads=[mres, vres], writes=[nbres])
    for k in range(8):
        t, tres = ln.t.nxt()
        P.op("dve", lambda e, t=t, k=k: e.tensor_tensor(out=t[:, :tw], in0=r[:, k, :tw], in1=var[:, :tw], op=ALU.mult),
             reads=[rres(k), vres], writes=[tres])
        P.op("dve", lambda e, t=t: e.tensor_tensor(out=t[:, :tw], in0=t[:, :tw], in1=nb[:, :tw], op=ALU.add),
             reads=[tres, nbres], writes=[tres])
        P.op("act", lambda e, t=t, k=k: e.activation(out=out[:, k, :tw], in_=t[:, :tw], func=AF.Identity,
                                                     bias=b_sb[:, k:k + 1], scale=g_sb[:, k:k + 1]),
             reads=[tres, gres], writes=[outres(k)])


def load_cast_w(cx, name, w_d, kch, ncol):
    w = cx.sb(name, [128, kch, ncol], BF16)
    for k in range(kch):
        cx.load("pool", w[:, k, :], w_d[k * 128:(k + 1) * 128, :], (name, k))
    return w, [(name, k) for k in range(kch)]


def build_Ca(with_pool):
    cx = Ctx()
    P = cx.P
    xT = cx.inp("xT", [D, NT])
    nat = 6 if with_pool else 8
    aT = cx.inp("aT", [nat * 128, NT], BF16)
    mod_d = cx.inp("mod", [128, 96])
    wout_d = cx.inp("wout", [D, D])
    g_d = cx.inp("lng", [128, 8])
    b_d = cx.inp("lnb", [128, 8])
    if with_pool:
        ph_d = cx.inp("ph", [256, NT + 32])
        rc_d = cx.inp("rcnt", [256, NT])
        wp_d = cx.inp("wpool", [256, 128])
        psc_d = cx.inp("pscale", [128, 2])
    xo = cx.out("xo", [D, NT])

    mod_sb = cx.sb("mod_sb", [128, 96])
    cx.load("sp", mod_sb[:], mod_d, "mod")
    g_sb = cx.sb("g_sb", [128, 8])
    b_sb = cx.sb("b_sb", [128, 8])
    cx.load("sp", g_sb[:], g_d, "lngb")
    cx.load("sp", b_sb[:], b_d, "lngb2")
    P.op("dve", lambda e: e.tensor_copy(out=b_sb[:], in_=b_sb[:]), reads=["lngb2"], writes=["lngb"])
    wout, wres = load_cast_w(cx, "wout_sb", wout_d, 8, D)
    ln = LNState(cx, "ln")
    if with_pool:
        wp = cx.sb("wp_sb", [128, 2, 128], BF16)
        for pc in range(2):
            cx.load("pool", wp[:, pc, :], wp_d[pc * 128:(pc + 1) * 128, :], ("wp", pc))
        psc = cx.sb("psc_sb", [128, 2])
        cx.load("sp", psc[:], psc_d, "psc")
        rc = cx.sb("rc_sb", [128, 2, NT])
        cx.load("sp", rc[:], rc_d.rearrange("(c p) t -> p c t", p=128), "rc")
        ph_r = cx.ring("ph", 4, [128, 528])
        s_r = [cx.ring("s%d" % i, 1, [128, 528]) for i in range(4)]
        ym_r = cx.ring("ym", 2, [128, 512], BF16)
        tmp_r = cx.ring("ptmp", 2, [128, 512])
        ppool_r = cx.psring("ppool", 1)
    x_r = cx.ring("x", 2, [128, 8, 512])
    mix_r = cx.ring("mix", 2, [128, 8, 512], BF16)
    po_r = cx.psring("po", 2)
    xv = xT.rearrange("(k p) t -> p k t", p=128)
    av = aT.rearrange("(k p) t -> p k t", p=128)
    ov = xo.rearrange("(k p) t -> p k t", p=128)
    LO = {2: 1, 4: 2, 8: 4, 16: 8}

    def do_tile(t0, tw, col):
        mix, mres = mix_r.nxt()
        cx.load("sp", mix[:, 0:nat, :tw], av[:, :, t0:t0 + tw], (mres, "a"))
        x, xres = x_r.nxt()
        cx.load("sp", x[:, :, :tw], xv[:, :, t0:t0 + tw], (xres, "ld"))
        if with_pool:
            h0 = t0 if col == 0 else t0 + 16
            L = tw + 16
            for pc in range(2):
                ph, phres = ph_r.nxt()
                cx.load("sp", ph[:, :L], ph_d[pc * 128:(pc + 1) * 128, h0:h0 + L], phres)
                srcs = [(ph, phres)]
                nlev = 2 if pc == 0 else 4
                for lev in range(nlev):
                    sh = 1 << lev
                    s, sres = s_r[lev].nxt()
                    a, ares = srcs[-1]
                    Lo = L - (2 * sh - 1)
                    P.op("dve", lambda e, s=s, a=a, sh=sh, Lo=Lo: e.tensor_tensor(out=s[:, :Lo], in0=a[:, 0:Lo], in1=a[:, sh:sh + Lo], op=ALU.add),
                         reads=[ares], writes=[sres])
                    srcs.append((s, sres))
                ym, ymres = ym_r.nxt()
                tmp, tmpres = tmp_r.nxt()
                for half in range(2):
                    w = (2, 4, 8, 16)[pc * 2 + half]
                    s, sres = srcs[{2: 1, 4: 2, 8: 3, 16: 4}[w]]
                    off = 8 - LO[w]
                    lo_p, hi_p = half * 64, half * 64 + 64
                    P.op("dve", lambda e, s=s, off=off, lo_p=lo_p, hi_p=hi_p, pc=pc, tmp=tmp:
                         e.tensor_tensor(out=tmp[lo_p:hi_p, :tw], in0=s[lo_p:hi_p, off:off + tw], in1=rc[lo_p:hi_p, pc, t0:t0 + tw], op=ALU.mult),
                         reads=[sres, "rc"], writes=[(tmpres, half)])
                    P.op("dve", lambda e, ph=ph, lo_p=lo_p, hi_p=hi_p, tmp=tmp, ym=ym:
                         e.tensor_tensor(out=ym[lo_p:hi_p, :tw], in0=tmp[lo_p:hi_p, :tw], in1=ph[lo_p:hi_p, 8:8 + tw], op=ALU.subtract),
                         reads=[(tmpres, half), phres], writes=[(ymres, half)])
                pp, ppres = ppool_r.nxt()
                P.op("pe", lambda e, pp=pp, ym=ym, pc=pc: e.matmul(pp[:, :tw], wp[:, pc, :], ym[:, :tw], start=True, stop=True),
                     reads=[(ymres, 0), (ymres, 1), ("wp", pc)], writes=[ppres])
                P.op("act", lambda e, pp=pp, pc=pc: e.activation(out=mix[:, 6 + pc, :tw], in_=pp[:, :tw], func=AF.Identity, scale=psc[:, pc:pc + 1]),
                     reads=[ppres, "psc"], writes=[(mres, "p", pc)])
        mreads = [(mres, "a")] + ([(mres, "p", 0), (mres, "p", 1)] if with_pool else [])
        for oc in range(8):
            P.op("act", lambda e, oc=oc: e.activation(out=x[:, oc, :tw], in_=x[:, oc, :tw], func=AF.Identity, scale=ALPHA),
                 reads=[(xres, "ld")], writes=[(xres, oc)])
            po, pores = po_r.nxt()

            def mm(e, po=po, oc=oc):
                ins = None
                for k in range(8):
                    ins = e.matmul(po[:, :tw], wout[:, k, oc * 128:(oc + 1) * 128], mix[:, k, :tw], start=(k == 0), stop=(k == 7))
                return ins
            P.op("pe", mm, reads=mreads + wres, writes=[pores])
            gate = mod_sb[:, 32 + 2 * oc + col:32 + 2 * oc + col + 1]
            P.op("dve", lambda e, po=po, oc=oc, gate=gate: e.scalar_tensor_tensor(out=x[:, oc, :tw], in0=po[:, :tw], scalar=gate,
                                                                               in1=x[:, oc, :tw], op0=ALU.mult, op1=ALU.add),
                 reads=[pores, (xres, oc), "mod"], writes=[(xres, oc)])
        emit_layernorm(cx, ln, x, lambda k: (xres, k), tw, g_sb, b_sb, "lngb")
        P.dma("pool", "S_" + xres, lambda e, s: e.dma_start(out=ov[:, :, t0:t0 + tw], in_=x[:, :, :tw]).then_inc(s, 16),
              reads=[(xres, k) for k in range(8)], writes=[(xres, "ld")])

    for (t0, tw, col) in tiles_own_ctx():
        do_tile(t0, tw, col)
    return cx


def build_C0b():
    cx = Ctx()
    P = cx.P
    NF = 22
    TW = 256
    xT = cx.inp("xT", [D, NT])
    mod_d = cx.inp("mod", [128, 96])
    wg_d = cx.inp("wg", [D, NF * 128])
    wu_d = cx.inp("wu", [D, NF * 128])
    wd_d = cx.inp("wd", [NF * 128, D])
    g_d = cx.inp("lng", [128, 8])
    b_d = cx.inp("lnb", [128, 8])
    xo = cx.out("xo", [D, NT])
    mod_sb = cx.sb("mod_sb", [128, 96])
    modp_sb = cx.sb("modp_sb", [128, 96])
    cx.load("sp", mod_sb[:], mod_d, "mod")
    P.op("dve", lambda e: e.tensor_scalar_add(out=modp_sb[:], in0=mod_sb[:], scalar1=1.0), reads=["mod"], writes=["modp"])
    g_sb = cx.sb("g_sb", [128, 8])
    b_sb = cx.sb("b_sb", [128, 8])
    cx.load("sp", g_sb[:], g_d, "lngb")
    cx.load("sp", b_sb[:], b_d, "lngb2")
    P.op("dve", lambda e: e.tensor_copy(out=b_sb[:], in_=b_sb[:]), reads=["lngb2"], writes=["lngb"])
    wg, wgres = load_cast_w(cx, "wg_sb", wg_d, 8, NF * 128)
    wu, wures = load_cast_w(cx, "wu_sb", wu_d, 8, NF * 128)
    wd, wdres = load_cast_w(cx, "wd_sb", wd_d, NF, D)
    ln = LNState(cx, "ln", TW)
    x_r = cx.ring("x", 1, [128, 8, TW])
    u_r = cx.ring("u", 1, [128, 8, TW], BF16)
    h_r = cx.ring("h", 1, [128, NF, TW], BF16)
    sg_r = cx.ring("sg", 2, [128, TW])
    pg_r = cx.psring("pg", 2, (128, TW))
    pu_r = cx.psring("pu", 2, (128, TW))
    pd_r = cx.psring("pd", 2, (128, TW))
    xv = xT.rearrange("(k p) t -> p k t", p=128)
    ov = xo.rearrange("(k p) t -> p k t", p=128)

    def do_tile(t0, tw, col):
        x, xres = x_r.nxt()
        cx.load("sp", x[:, :, :tw], xv[:, :, t0:t0 + tw], (xres, "ld"), reads=[(xres, k) for k in range(8)])
        u, ures = u_r.nxt()
        emit_modulate(cx, x, (xres, "ld"), u, ures, tw, col, mod_sb, modp_sb, 3)
        ureads = [(ures, k) for k in range(8)]
        h, hres = h_r.nxt()
        for fc in range(NF):
            pg, pgres = pg_r.nxt()
            pu, pures = pu_r.nxt()

            def mmg(e, pg=pg, fc=fc):
                ins = None
                for k in range(8):
                    ins = e.matmul(pg[:, :tw], wg[:, k, fc * 128:(fc + 1) * 128], u[:, k, :tw], start=(k == 0), stop=(k == 7))
                return ins

            def mmu(e, pu=pu, fc=fc):
                ins = None
                for k in range(8):
                    ins = e.matmul(pu[:, :tw], wu[:, k, fc * 128:(fc + 1) * 128], u[:, k, :tw], start=(k == 0), stop=(k == 7))
                return ins
            P.op("pe", mmg, reads=ureads + wgres, writes=[pgres])
            P.op("pe", mmu, reads=ureads + wures, writes=[pures])
            sg, sgres = sg_r.nxt()
            P.op("act", lambda e, sg=sg, pg=pg: e.activation(out=sg[:, :tw], in_=pg[:, :tw], func=AF.Silu), reads=[pgres], writes=[sgres])
            P.op("dve", lambda e, sg=sg, pu=pu, fc=fc: e.tensor_tensor(out=h[:, fc, :tw], in0=sg[:, :tw], in1=pu[:, :tw], op=ALU.mult),
                 reads=[sgres, pures], writes=[(hres, fc)])
        hreads = [(hres, fc) for fc in range(NF)]
        for oc in range(8):
            P.op("act", lambda e, oc=oc: e.activation(out=x[:, oc, :tw], in_=x[:, oc, :tw], func=AF.Identity, scale=ALPHA),
                 reads=[(xres, "ld")] + ureads, writes=[(xres, oc)])
            pd, pdres = pd_r.nxt()

            def mmd(e, pd=pd, oc=oc):
                ins = None
                for fc in range(NF):
                    ins = e.matmul(pd[:, :tw], wd[:, fc, oc * 128:(oc + 1) * 128], h[:, fc, :tw], start=(fc == 0), stop=(fc == NF - 1))
                return ins
            P.op("pe", mmd, reads=hreads + wdres, writes=[pdres])
            gate = mod_sb[:, 80 + 2 * oc + col:80 + 2 * oc + col + 1]
            P.op("dve", lambda e, pd=pd, oc=oc, gate=gate: e.scalar_tensor_tensor(out=x[:, oc, :tw], in0=pd[:, :tw], scalar=gate,
                                                                               in1=x[:, oc, :tw], op0=ALU.mult, op1=ALU.add),
                 reads=[pdres, (xres, oc), "mod"], writes=[(xres, oc)])
        emit_layernorm(cx, ln, x, lambda k: (xres, k), tw, g_sb, b_sb, "lngb")
        P.dma("pool", "S_" + xres, lambda e, s: e.dma_start(out=ov[:, :, t0:t0 + tw], in_=x[:, :, :tw]).then_inc(s, 16),
              reads=[(xres, k) for k in range(8)], writes=[(xres, "ld")])

    for (t0, tw, col) in tiles_own_ctx(TW):
        do_tile(t0, tw, col)
    return cx


POOL_W = (2, 4, 8, 16)


def pool_tables(j):
    rc = np.ones((256, NT), np.float32)
    for g, w in enumerate(POOL_W):
        lo = w // 2
        hi = w - lo
        t = np.arange(TOWN) + j * TOWN
        cnt = np.clip(t + hi, 0, SEQ) - np.clip(t - lo, 0, SEQ)
        rc[g * 64:(g + 1) * 64, :TOWN] = (1.0 / cnt.astype(np.float32))[None, :]
        t = np.arange(CTX)
        cnt = np.clip(t + hi, 0, CTX) - np.clip(t - lo, 0, CTX)
        rc[g * 64:(g + 1) * 64, TOWN:] = (1.0 / cnt.astype(np.float32))[None, :]
    return rc


def pool_halo(resA, c):
    b, j = divmod(c, 4)
    own = np.asarray(resA[c]["pT"])
    ph = np.zeros((256, NT + 32), np.float32)
    ph[:, 8:8 + TOWN] = own[:, :TOWN]
    if j > 0:
        ph[:, 0:8] = np.asarray(resA[c - 1]["pT"])[:, TOWN - 8:TOWN]
    if j < 3:
        ph[:, 8 + TOWN:16 + TOWN] = np.asarray(resA[c + 1]["pT"])[:, 0:8]
    ph[:, TOWN + 16 + 8:TOWN + 16 + 8 + CTX] = own[:, TOWN:]
    return ph


def wpool_blockdiag(w_pool):
    m = np.zeros((256, 128), np.float32)
    for g in range(4):
        pc, gl = divmod(g, 2)
        m[pc * 128 + gl * 64:pc * 128 + gl * 64 + 64, gl * 64:gl * 64 + 64] = w_pool[g]
    return m


def stage_C0a(inp, resA, resB):
    cx = build_Ca(True)
    maps = []
    for c in range(NCORES):
        maps.append(dict(
            xT=core_xT(inp["x"], inp["ctx"], c), aT=np.asarray(resB[c]["oT"]), mod=np.asarray(resA[c]["mod"]),
            wout=np.ascontiguousarray(inp["ev_w_out"][0]), lng=pcol(inp["ln1_g"][0]), lnb=pcol(inp["ln1_b"][0]),
            ph=pool_halo(resA, c), rcnt=pool_tables(c % 4), wpool=wpool_blockdiag(inp["ev_w_pool"][0]),
            pscale=pcol(inp["ev_pool_scale"][0])))
    return run_spmd(cx, maps)


def stage_C0b(inp, resA, resCa):
    cx = build_C0b()
    maps = []
    for c in range(NCORES):
        maps.append(dict(
            xT=np.asarray(resCa[c]["xo"]), mod=np.asarray(resA[c]["mod"]),
            wg=np.ascontiguousarray(inp["ev_w_gate"][0]), wu=np.ascontiguousarray(inp["ev_w_up"][0]),
            wd=np.ascontiguousarray(inp["ev_w_down"][0]), lng=pcol(inp["ln2_g"][0]), lnb=pcol(inp["ln2_b"][0])))
    return run_spmd(cx, maps)


def build_A1():
    cx = Ctx()
    P = cx.P
    xT = cx.inp("xT", [D, NT])
    cs_d = cx.inp("cs", [128, 16])
    wada_d = cx.inp("wada", [D, 6 * D])
    bada_d = cx.inp("bada", [128, 48])
    win_d = cx.inp("win", [D, 2208])
    gq_d = cx.inp("gql", [128, 3])
    gkv_d = cx.inp("gkvl", [128, 2])
    wqup_d = cx.inp("wqup", [384, 768])
    wkvk_d = cx.inp("wkvk", [256, 512])
    wkvv_d = cx.inp("wkvv", [256, 512])
    cos96_d = cx.inp("cos96", [96, NT])
    sin96_d = cx.inp("sin96", [96, NT])
    rot96_d = cx.inp("rot96", [96, 96])
    mod_o = cx.out("mod", [128, 96])
    qT_o = cx.out("qT", [768, NT], BF16)
    kT_o = cx.out("kT", [768, NT], BF16)
    v_o = cx.out("v", [NT, 512], BF16)
    nqT_o = cx.out("nqT", [512, NT], BF16)
    nkT_o = cx.out("nkT", [512, NT], BF16)
    nv_o = cx.out("nv", [NT, 512], BF16)

    pq_r = cx.psring("pq", 2)
    pms_r = cx.psring("pms", 1)
    pqh_r = cx.psring("pqh", 2)
    prot_r = cx.psring("prot", 1)
    pv_r = cx.psring("pv", 2)
    mod_sb = cx.sb("mod_sb", [128, 96])
    modp_sb = cx.sb("modp_sb", [128, 96])
    emit_ada(cx, cs_d, wada_d, bada_d, mod_sb, modp_sb, pm=pv_r.items[0][0], pmres=pv_r.items[0][1])
    cx.store("sp", mod_o, mod_sb[:], "mod")

    win, winres = load_cast_w(cx, "win_sb", win_d, 8, 2208)
    wqup, wqres = load_cast_w(cx, "wqup_sb", wqup_d, 3, 768)
    wkvk, wkres = load_cast_w(cx, "wkvk_sb", wkvk_d, 2, 512)
    wkvv, wvres = load_cast_w(cx, "wkvv_sb", wkvv_d, 2, 512)
    rot96 = cx.sb("rot96_sb", [96, 96], BF16)
    cx.load("pool", rot96[:], rot96_d, "rot96")
    cos96 = cx.sb("cos96_sb", [96, NT])
    sin96 = cx.sb("sin96_sb", [96, NT])
    cx.load("sp", cos96[:], cos96_d, "cos96")
    cx.load("sp", sin96[:], sin96_d, "sin96")
    gq = cx.sb("gq_sb", [128, 3])
    gkv = cx.sb("gkv_sb", [128, 2])
    cx.load("sp", gq[:], gq_d, "gq")
    cx.load("sp", gkv[:], gkv_d, "gkv")
    ones_q = cx.sb("ones_q", [128, 128], BF16)
    ones_kv = cx.sb("ones_kv", [128, 128], BF16)
    P.op("dve", lambda e: e.memset(ones_q[:], 1.0 / 384), writes=["ones_q"])
    P.op("dve", lambda e: e.memset(ones_kv[:], 1.0 / 256), writes=["ones_kv"])
    eps_sb = cx.sb("eps_sb", [128, 1])
    P.op("dve", lambda e: e.memset(eps_sb[:], EPS), writes=["eps"])

    xr = cx.ring("x", 2, [128, 8, 512])
    ur = cx.ring("u", 2, [128, 8, 512], BF16)
    lat_r = cx.ring("lat", 1, [128, 5, 512])
    latn_r = cx.ring("latn", 1, [128, 5, 512], BF16)
    sq_r = cx.ring("sq", 2, [128, 512], BF16)
    rs_r = cx.ring("rs", 2, [128, 512])
    qb_r = cx.ring("qb", 2, [96, 512], BF16)
    t1_r = cx.ring("t1", 2, [96, 512])
    t2_r = cx.ring("t2", 2, [96, 512])
    qf_r = cx.ring("qf", 3, [96, 512], BF16)
    ob_r = cx.ring("ob", 3, [128, 512], BF16)
    xv = xT.rearrange("(k p) t -> p k t", p=128)

    def do_tile(t0, tw, col):
        x, xres = xr.nxt()
        cx.load("sp", x[:, :, :tw], xv[:, :, t0:t0 + tw], xres)
        u, ures = ur.nxt()
        emit_modulate(cx, x, xres, u, ures, tw, col, mod_sb, modp_sb, 0)
        ureads = [(ures, k) for k in range(8)]
        lat, latres = lat_r.nxt()
        latn, latnres = latn_r.nxt()

        def proj(ps, c0, m, n0=0, n1=None):
            n1 = tw if n1 is None else n1

            def f(e):
                ins = None
                for k in range(8):
                    ins = e.matmul(ps[0:m, :n1 - n0], win[:, k, c0:c0 + m], u[:, k, n0:n1], start=(k == 0), stop=(k == 7))
                return ins
            return f
        for grp, (c_lo, nch, ones, oneres, gain, gres) in enumerate(((0, 3, ones_q, "ones_q", gq, "gq"),
                                                                       (384, 2, ones_kv, "ones_kv", gkv, "gkv"))):
            base = 0 if grp == 0 else 3
            pms, pmsres = pms_r.nxt()
            for c in range(nch):
                pq, pqres = pq_r.nxt()
                P.op("pe", proj(pq, c_lo + c * 128, 128), reads=ureads + winres, writes=[pqres])
                sq, sqres = sq_r.nxt()
                P.op("act", lambda e, sq=sq, pq=pq: e.activation(out=sq[:, :tw], in_=pq[:, :tw], func=AF.Square), reads=[pqres], writes=[sqres])
                P.op("dve", lambda e, pq=pq, c=c, base=base: e.tensor_copy(out=lat[:, base + c, :tw], in_=pq[:, :tw]),
                     reads=[pqres], writes=[(latres, base + c)])
                P.op("pe", lambda e, pms=pms, sq=sq, c=c, nch=nch, ones=ones: e.matmul(pms[:, :tw], ones[:], sq[:, :tw], start=(c == 0), stop=(c == nch - 1)),
                     reads=[sqres, oneres], writes=[pmsres])
            rs, rsres = rs_r.nxt()
            P.op("act", lambda e, rs=rs, pms=pms: e.activation(out=rs[:, :tw], in_=pms[:, :tw], func=AF.Sqrt, bias=eps_sb[:, 0:1]),
                 reads=[pmsres, "eps"], writes=[rsres])
            P.op("dve", lambda e, rs=rs: e.reciprocal(out=rs[:, :tw], in_=rs[:, :tw]), reads=[rsres], writes=[rsres])
            for c in range(nch):
                P.op("dve", lambda e, c=c, base=base, rs=rs, gain=gain: e.scalar_tensor_tensor(
                    out=latn[:, base + c, :tw], in0=lat[:, base + c, :tw], scalar=gain[:, c:c + 1], in1=rs[:, :tw], op0=ALU.mult, op1=ALU.mult),
                    reads=[(latres, base + c), rsres, gres], writes=[(latnres, base + c)])
        cqn_reads = [(latnres, c) for c in range(3)]
        ckvn_reads = [(latnres, 3 + c) for c in range(2)]
        def head_rope(pqh, pqres, nrows_lo, stores):
            qb, qbres = qb_r.nxt()
            P.op("dve", lambda e: e.tensor_copy(out=qb[:, :tw], in_=pqh[0:96, :tw]), reads=[pqres], writes=[qbres])
            prot, protres = prot_r.nxt()
            P.op("pe", lambda e: e.matmul(prot[0:96, :tw], rot96[:], qb[:, :tw], start=True, stop=True), reads=[qbres, "rot96"], writes=[protres])
            t1, t1res = t1_r.nxt()
            P.op("dve", lambda e: e.tensor_tensor(out=t1[:, :tw], in0=pqh[0:96, :tw], in1=cos96[:, t0:t0 + tw], op=ALU.mult),
                 reads=[pqres, "cos96"], writes=[t1res])
            t2, t2res = t2_r.nxt()
            P.op("dve", lambda e: e.tensor_tensor(out=t2[:, :tw], in0=prot[0:96, :tw], in1=sin96[:, t0:t0 + tw], op=ALU.mult),
                 reads=[protres, "sin96"], writes=[t2res])
            qf, qfres = qf_r.nxt()
            P.op("dve", lambda e: e.tensor_tensor(out=qf[:, :tw], in0=t1[:, :tw], in1=t2[:, :tw], op=ALU.add), reads=[t1res, t2res], writes=[qfres])
            for (dst, lo, hi) in stores:
                cx.store("sp", dst, qf[lo:hi, :tw], qfres)

        pkr, pkrres = pqh_r.nxt()

        def mmkr(e):
            ins = None
            for k in range(8):
                ins = e.matmul(pkr[64:96, :tw], win[:, k, 640:672], u[:, k, :tw], start=(k == 0), stop=(k == 7))
            return ins
        P.op("dve", lambda e: e.memset(pkr[0:64, :tw], 0.0), writes=[pkrres])
        P.op("pe", mmkr, reads=ureads + winres, writes=[pkrres])
        head_rope(pkr, pkrres, 64, [(kT_o[h * 96 + 64:h * 96 + 96, t0:t0 + tw], 64, 96) for h in range(8)])
        for h in range(8):
            pqh, pqres = pqh_r.nxt()

            def mmq(e, pqh=pqh, h=h):
                ins = None
                for k in range(3):
                    ins = e.matmul(pqh[0:96, :tw], wqup[:, k, h * 96:(h + 1) * 96], latn[:, k, :tw], start=(k == 0), stop=(k == 2))
                return ins
            P.op("pe", mmq, reads=cqn_reads + wqres, writes=[pqres])
            head_rope(pqh, pqres, 0, [(qT_o[h * 96:(h + 1) * 96, t0:t0 + tw], 0, 96)])
        for hp in range(4):
            pq, pqres = pq_r.nxt()

            def mmk(e, pq=pq, hp=hp):
                ins = None
                for k in range(2):
                    ins = e.matmul(pq[:, :tw], wkvk[:, k, hp * 128:(hp + 1) * 128], latn[:, 3 + k, :tw], start=(k == 0), stop=(k == 1))
                return ins
            P.op("pe", mmk, reads=ckvn_reads + wkres, writes=[pqres])
            ob, obres = ob_r.nxt()
            P.op("act", lambda e, ob=ob, pq=pq: e.activation(out=ob[:, :tw], in_=pq[:, :tw], func=AF.Copy), reads=[pqres], writes=[obres])
            for hh in range(2):
                h = 2 * hp + hh
                cx.store("pool", kT_o[h * 96:h * 96 + 64, t0:t0 + tw], ob[hh * 64:(hh + 1) * 64, :tw], obres)
        for (dst, c_lo) in ((nqT_o, 672), (nkT_o, 1184)):
            for c in range(4):
                pq, pqres = pq_r.nxt()
                P.op("pe", proj(pq, c_lo + c * 128, 128), reads=ureads + winres, writes=[pqres])
                ob, obres = ob_r.nxt()
                P.op("act", lambda e, ob=ob, pq=pq: e.activation(out=ob[:, :tw], in_=pq[:, :tw], func=AF.Copy), reads=[pqres], writes=[obres])
                cx.store("pool", dst[c * 128:(c + 1) * 128, t0:t0 + tw], ob[:, :tw], obres)
        for tb in range(tw // 128):
            pv, pvres = pv_r.nxt()

            def mmv(e, pv=pv, tb=tb):
                ins = None
                for k in range(2):
                    ins = e.matmul(pv[:, :512], latn[:, 3 + k, tb * 128:(tb + 1) * 128], wkvv[:, k, :], start=(k == 0), stop=(k == 1))
                return ins
            P.op("pe", mmv, reads=ckvn_reads + wvres, writes=[pvres])
            ob, obres = ob_r.nxt()
            P.op("act", lambda e, ob=ob, pv=pv: e.activation(out=ob[:, :512], in_=pv[:, :512], func=AF.Copy), reads=[pvres], writes=[obres])
            cx.store("pool", v_o[t0 + tb * 128:t0 + (tb + 1) * 128, :], ob[:, :512], obres)
            pv, pvres = pv_r.nxt()

            def mmnv(e, pv=pv, tb=tb):
                ins = None
                for k in range(8):
                    ins = e.matmul(pv[:, :512], u[:, k, tb * 128:(tb + 1) * 128], win[:, k, 1696:2208], start=(k == 0), stop=(k == 7))
                return ins
            P.op("pe", mmnv, reads=ureads + winres, writes=[pvres])
            ob, obres = ob_r.nxt()
            P.op("act", lambda e, ob=ob, pv=pv: e.activation(out=ob[:, :512], in_=pv[:, :512], func=AF.Copy), reads=[pvres], writes=[obres])
            cx.store("pool", nv_o[t0 + tb * 128:t0 + (tb + 1) * 128, :], ob[:, :512], obres)

    for (t0, tw, col) in tiles_own_ctx():
        do_tile(t0, tw, col)
    return cx


def rope_tables32(j):
    t = np.arange(TOWN) + j * TOWN
    row = (t // GRID_W).astype(np.float32)
    colp = (t % GRID_W).astype(np.float32)
    inv = (10000.0 ** (-np.arange(8, dtype=np.float64) / 8)).astype(np.float32)
    cos = np.ones((96, NT), np.float32)
    sin = np.zeros((96, NT), np.float32)
    for d in range(32):
        pos = row if d < 16 else colp
        ang = (pos * inv[d % 8]).astype(np.float32)
        cos[64 + d, :TOWN] = np.cos(ang)
        sin[64 + d, :TOWN] = np.sin(ang)
    return cos, sin


def rot96():
    m = np.zeros((96, 96), np.float32)
    m[64:, 64:] = rot_matrix(32, 16)
    return m


def stage_A1(inp, xT_list):
    cx = build_A1()
    wkv = inp["od_w_kv_up"][0].reshape(256, 8, 128)
    maps = []
    for c in range(NCORES):
        cos, sin = rope_tables32(c % 4)
        maps.append(dict(
            xT=xT_list[c], cs=core_cs(inp["c"], inp["c_ctx"], c),
            wada=np.ascontiguousarray(inp["w_ada"][1]), bada=pcol(inp["b_ada"][1]),
            win=np.ascontiguousarray(inp["od_w_in"][0]),
            gql=pcol(inp["od_q_lat_gain"][0]), gkvl=pcol(inp["od_kv_lat_gain"][0]),
            wqup=np.ascontiguousarray(inp["od_w_q_up"][0]),
            wkvk=np.ascontiguousarray(wkv[:, :, :64].reshape(256, 512)),
            wkvv=np.ascontiguousarray(wkv[:, :, 64:].reshape(256, 512)),
            cos96=cos, sin96=sin, rot96=rot96()))
    return run_spmd(cx, maps)


NA_SCALE = 0.125


def build_NA():
    cx = Ctx()
    P = cx.P
    nq_d = cx.inp("nqT", [512, TOWN], BF16)
    nk_d = cx.inp("nkTh", [512, TOWN + 512], BF16)
    nv_d = cx.inp("nvh", [128, 36 * 8 * 65], BF16)
    kc_d = cx.inp("nkc", [512, CTX], BF16)
    vc_d = cx.inp("nvc", [128, 2 * 8 * 65], BF16)
    bm_d = {n: cx.inp(n, [128, 48 * 256]) for n in ("bm_int", "bm_first", "bm_last")}
    id_d = cx.inp("ident", [128, 128])
    sel_d = cx.inp("sel", [65, 64])
    o_d = cx.out("naT", [512, TOWN], BF16)

    kh = cx.sb("kh", [128, 4, TOWN + 512], BF16)
    qs = cx.sb("qs", [128, 4, TOWN], BF16)
    kcs = cx.sb("kcs", [128, 4, CTX], BF16)
    vb = cx.sb("vb", [128, 36, 8, 65], BF16)
    vc = cx.sb("vc", [128, 2, 8, 65], BF16)
    cx.load("sp", kh[:], nk_d.rearrange("(c p) t -> p c t", p=128), "kh")
    cx.load("sp", qs[:], nq_d.rearrange("(c p) t -> p c t", p=128), "qs")
    cx.load("sp", kcs[:], kc_d.rearrange("(c p) t -> p c t", p=128), "kcs")
    cx.load("sp", vb[:].rearrange("p a h d -> p (a h d)"), nv_d, "vb")
    cx.load("sp", vc[:].rearrange("p a h d -> p (a h d)"), vc_d, "vc")
    ident = cx.sb("ident_sb", [128, 128], BF16)
    cx.load("pool", ident[:], id_d, "ident")
    sel = cx.sb("sel_sb", [65, 64])
    cx.load("sp", sel[:], sel_d, "sel")
    bmi = cx.sb("bmi", [128, 48, 256], BF16)
    bme = cx.sb("bme", [128, 48, 256], BF16)
    stg_r = cx.ring("stg", 2, [128, 6, 256])

    def load_table(dst, dres, name):
        src = bm_d[name].rearrange("p (a q) -> p a q", q=256)
        for h in range(8):
            stg, sres = stg_r.nxt()
            cx.load("sp", stg[:], src[:, h * 6:(h + 1) * 6, :], sres)
            P.op("dve", lambda e, stg=stg, h=h: e.tensor_scalar_mul(out=dst[:, h * 6:(h + 1) * 6, :], in0=stg[:], scalar1=1.0 / NA_SCALE),
                 reads=[sres], writes=[(dres, h)])
    load_table(bmi, "bmi", "bm_int")
    load_table(bme, "bme", "bm_first")

    ps_r = cx.psring("ps", 4)
    po_r = cx.psring("po", 2)
    pb_r = cx.psring("pb", 1)
    pt_r = cx.ring("pt", 2, [128, 8, 256], BF16)
    os_r = cx.ring("os", 2, [65, 256])
    at_r = cx.ring("at", 2, [64, 256], BF16)

    def unit(m, h):
        hp, pb0 = h // 2, (h % 2) * 64
        tab, tres = (bme, "bme") if m in (0, 15) else (bmi, "bmi")
        pt, ptres = pt_r.nxt()
        q0 = 256 * m
        for bk in range(4):
            ps, psres = ps_r.nxt()

            def mm(e, ps=ps, bk=bk):
                ins = None
                for cc in range(2):
                    c = 2 * bk + cc
                    o = cc * 256
                    if c < 6:
                        e.matmul(ps[:, o:o + 256], kh[pb0:pb0 + 64, hp, q0 + c * 128:q0 + (c + 1) * 128], qs[pb0:pb0 + 64, hp, q0:q0 + 256],
                                 start=True, stop=False)
                        ins = e.matmul(ps[:, o:o + 256], ident[:], tab[:, h * 6 + c, :], start=False, stop=True)
                    else:
                        ins = e.matmul(ps[:, o:o + 256], kcs[pb0:pb0 + 64, hp, (c - 6) * 128:(c - 5) * 128], qs[pb0:pb0 + 64, hp, q0:q0 + 256],
                                       start=True, stop=True)
                return ins
            P.op("pe", mm, reads=["kh", "qs", "kcs", "ident", (tres, h)], writes=[psres])
            P.op("act", lambda e, ps=ps, bk=bk: e.activation(out=pt[:, 2 * bk:2 * bk + 2, :].rearrange("p a q -> p (a q)"), in_=ps[:, :],
                                                            func=AF.Exp, scale=NA_SCALE),
                 reads=[psres], writes=[(ptres, bk)])
        po, pores = po_r.nxt()

        def pv(e):
            ins = None
            for c in range(8):
                lhs = vb[:, 2 * m + c, h, :] if c < 6 else vc[:, c - 6, h, :]
                ins = e.matmul(po[0:65, :256], lhs, pt[:, c, :], start=(c == 0), stop=(c == 7))
            return ins
        P.op("pe", pv, reads=[(ptres, bk) for bk in range(4)] + ["vb", "vc"], writes=[pores])
        osb, osres = os_r.nxt()
        P.op("act", lambda e: e.activation(out=osb[:, :], in_=po[0:65, :256], func=AF.Copy), reads=[pores], writes=[osres])
        P.op("dve", lambda e: e.reciprocal(out=osb[64:65, :], in_=osb[64:65, :]), reads=[osres], writes=[osres])
        pbk, pbres = pb_r.nxt()
        P.op("pe", lambda e: e.matmul(pbk[0:64, :256], sel[:], osb[:, :], start=True, stop=True), reads=[osres, "sel"], writes=[pbres])
        at, atres = at_r.nxt()
        P.op("dve", lambda e: e.tensor_tensor(out=at[:, :], in0=osb[0:64, :], in1=pbk[0:64, :256], op=ALU.mult), reads=[osres, pbres], writes=[atres])
        cx.store("pool", o_d[h * 64:(h + 1) * 64, q0:q0 + 256], at[:, :], atres)

    for m in range(16):
        if m == 15:
            load_table(bme, "bme", "bm_last")
        for h in range(8):
            unit(m, h)
    return cx


def na_table(bias, R0):
    kr_rel = np.arange(12)
    kr = R0 - 4 + kr_rel
    qr = R0 + np.arange(4)
    rs = np.clip(qr - 4, 0, 256 - 8)
    row_ok = (kr[:, None] >= rs[None, :]) & (kr[:, None] < rs[None, :] + 8)
    row_idx = np.clip(kr[:, None] - qr[None, :] + 7, 0, 14)
    kc = np.arange(64)
    qc = np.arange(64)
    cs0 = np.clip(qc - 8, 0, 48)
    col_ok = (kc[:, None] >= cs0[None, :]) & (kc[:, None] < cs0[None, :] + 16)
    col_idx = np.clip(kc[:, None] - qc[None, :] + 15, 0, 30)
    ok = row_ok[:, None, :, None] & col_ok[None, :, None, :]
    g = bias[:, row_idx[:, None, :, None], col_idx[None, :, None, :]]
    t = np.where(ok[None], g, np.float32(-30000.0)).astype(np.float32)
    t = t.reshape(8, 6, 2, 64, 256)
    t = t.transpose(2, 3, 0, 1, 4).reshape(128, 48 * 256)
    return np.ascontiguousarray(t)


def with_ones(v):
    o = np.ones(v.shape[:-1] + (65,), v.dtype)
    o[..., :64] = v
    return o


def stage_NA(inp, resA1):
    cx = build_NA()
    bias = inp["od_na_bias"][0]
    t_int = na_table(bias, 8)
    t_first = na_table(bias, 0)
    t_last = na_table(bias, 252)
    maps = []
    for c in range(NCORES):
        b, j = divmod(c, 4)
        nk = np.asarray(resA1[c]["nkT"])
        nv = np.asarray(resA1[c]["nv"])
        kh = np.zeros((512, TOWN + 512), NPBF)
        vh = np.zeros((TOWN + 512, 512), NPBF)
        kh[:, 256:256 + TOWN] = nk[:, :TOWN]
        vh[256:256 + TOWN] = nv[:TOWN]
        if j > 0:
            kh[:, :256] = np.asarray(resA1[c - 1]["nkT"])[:, TOWN - 256:TOWN]
            vh[:256] = np.asarray(resA1[c - 1]["nv"])[TOWN - 256:TOWN]
        if j < 3:
            kh[:, 256 + TOWN:] = np.asarray(resA1[c + 1]["nkT"])[:, :256]
            vh[256 + TOWN:] = np.asarray(resA1[c + 1]["nv"])[:256]
        vhl = with_ones(vh.reshape(36, 128, 8, 64).transpose(1, 0, 2, 3)).reshape(128, -1)
        vcl = with_ones(nv[TOWN:].reshape(2, 128, 8, 64).transpose(1, 0, 2, 3)).reshape(128, -1)
        maps.append(dict(
            nqT=np.ascontiguousarray(np.asarray(resA1[c]["nqT"])[:, :TOWN]), nkTh=kh, nvh=np.ascontiguousarray(vhl),
            nkc=np.ascontiguousarray(nk[:, TOWN:]), nvc=np.ascontiguousarray(vcl),
            bm_int=t_int, bm_first=(t_first if j == 0 else t_int), bm_last=(t_last if j == 3 else t_int),
            ident=np.eye(128, dtype=np.float32), sel=SEL))
    return run_spmd(cx, maps)


def stage_B1(resA1):
    cx = build_attn(8, 1, 96, 130, 96 ** -0.5)
    kv = gather_kv(resA1, "kT", "v", 8, 96)
    maps = []
    for c in range(NCORES):
        kf, vf = kv[c // 4]
        maps.append(dict(qT=np.asarray(resA1[c]["qT"]), kT=kf, v=vf, sel=SEL))
    return run_spmd(cx, maps)


def build_C1b():
    cx = Ctx()
    P = cx.P
    NE, NFE, FG = 8, 28, 4
    NG = NFE // FG
    ST, SUB = 1024, 256
    xT = cx.inp("xT", [D, NT])
    mod_d = cx.inp("mod", [128, 96])
    wr_d = cx.inp("wr", [D, NE])
    br_d = cx.inp("br", [NE, 1])
    wg_d = cx.inp("wg", [NE, D, NFE * 128])
    wu_d = cx.inp("wu", [NE, D, NFE * 128])
    wd_d = cx.inp("wd", [NE, NFE * 128, D])
    g_d = cx.inp("lng", [128, 8])
    b_d = cx.inp("lnb", [128, 8])
    id_d = cx.inp("ident", [128, 128])
    oh_d = cx.inp("onehot", [NE, NE * 128])
    xo = cx.out("xo", [D, TOWN])

    mod_sb = cx.sb("mod_sb", [128, 96])
    modp_sb = cx.sb("modp_sb", [128, 96])
    cx.load("sp", mod_sb[:], mod_d, "mod")
    P.op("dve", lambda e: e.tensor_scalar_add(out=modp_sb[:], in0=mod_sb[:], scalar1=1.0), reads=["mod"], writes=["modp"], fence=True)
    g_sb = cx.sb("g_sb", [128, 8])
    b_sb = cx.sb("b_sb", [128, 8])
    cx.load("sp", g_sb[:], g_d, "lngb")
    cx.load("sp", b_sb[:], b_d, "lngb2")
    P.op("dve", lambda e: e.tensor_copy(out=b_sb[:], in_=b_sb[:]), reads=["lngb2"], writes=["lngb"], fence=True)
    wr = cx.sb("wr_sb", [128, 8, NE])
    cx.load("sp", wr[:], wr_d.rearrange("(k p) e -> p k e", p=128), "wr")
    br = cx.sb("br_sb", [NE, 1])
    cx.load("sp", br[:], br_d, "br")
    ident = cx.sb("ident_sb", [128, 128])
    cx.load("sp", ident[:], id_d, "ident")
    oneh = cx.sb("oneh_sb", [NE, NE * 128])
    cx.load("sp", oneh[:], oh_d, "oneh")
    ln = LNState(cx, "ln", SUB)

    yacc = cx.sb("yacc", [128, 8, ST])
    u2 = cx.sb("u2", [128, 8, ST], BF16)
    gb = cx.sb("gb", [128, NE, ST], BF16)
    gT = cx.sb("gT", [NE, ST])
    x_r = cx.ring("x", 2, [128, 8, SUB])
    uf_r = cx.ring("uf", 1, [128, 8, SUB])
    lg_r = cx.ring("lg", 1, [NE, SUB])
    lgT_r = cx.ring("lgT", 2, [128, 8])
    mx_r = cx.ring("mx", 2, [128, 8])
    dm_r = cx.ring("dm", 2, [128, 2])
    ga_r = cx.ring("ga", 2, [128, 8])
    gq_r = cx.ring("gq", 2, [128, 8])
    wgq_r = cx.ring("wgq", 2, [128, 8, FG * 128], BF16)
    wuq_r = cx.ring("wuq", 2, [128, 8, FG * 128], BF16)
    wdq_r = cx.ring("wdq", 2, [128, FG, D], BF16)
    sg_r = cx.ring("sg", 2, [128, SUB])
    h1_r = cx.ring("h1", 2, [128, SUB])
    hq_r = cx.ring("hq", 2, [128, FG, SUB], BF16)
    pg_r = cx.psring("pg", 2)
    pu_r = cx.psring("pu", 2)
    pd_r = cx.psring("pd", 2)
    xv = xT.rearrange("(k p) t -> p k t", p=128)
    ov = xo.rearrange("(k p) t -> p k t", p=128)

    def router(s0, st):
        t0 = s0 + st * SUB
        x, xres = x_r.nxt()
        cx.load("sp", x[:, :, :], xv[:, :, t0:t0 + SUB], (xres, "ld"), reads=[(xres, k) for k in range(8)])
        uf, ufres = uf_r.nxt()
        for k in range(8):
            sc = modp_sb[:, 64 + 2 * k:64 + 2 * k + 1]
            sh = mod_sb[:, 48 + 2 * k:48 + 2 * k + 1]
            if k % 2 == 0:
                P.op("act", lambda e, k=k, sc=sc, sh=sh: e.activation(out=uf[:, k, :], in_=x[:, k, :], func=AF.Identity, bias=sh, scale=sc),
                     reads=[(xres, "ld"), "mod", "modp"], writes=[(ufres, k)])
            else:
                P.op("dve", lambda e, k=k, sc=sc, sh=sh: e.tensor_scalar(out=uf[:, k, :], in0=x[:, k, :], scalar1=sc, scalar2=sh,
                                                                        op0=ALU.mult, op1=ALU.add),
                     reads=[(xres, "ld"), "mod", "modp"], writes=[(ufres, k)])
        ufreads = [(ufres, k) for k in range(8)]
        P.op("dve", lambda e: e.tensor_copy(out=u2[:, :, st * SUB:(st + 1) * SUB], in_=uf[:, :, :]), reads=ufreads, writes=[("u2", st)])
        plg, plgres = pg_r.nxt()

        def mml(e):
            ins = None
            for k in range(8):
                ins = e.matmul(plg[0:NE, :SUB], wr[:, k, :], uf[:, k, :], start=(k == 0), stop=(k == 7))
            return ins
        P.op("pe", mml, reads=ufreads + ["wr"], writes=[plgres])
        lg, lgres = lg_r.nxt()
        P.op("act", lambda e: e.activation(out=lg[:, :], in_=plg[0:NE, :SUB], func=AF.Identity, bias=br[:, 0:1]), reads=[plgres, "br"], writes=[lgres])
        for tb in range(SUB // 128):
            pt, ptres = pu_r.nxt()
            P.op("pe", lambda e, pt=pt, tb=tb: e.transpose(pt[:, 0:NE], lg[:, tb * 128:(tb + 1) * 128], ident[0:NE, 0:NE]),
                 reads=[lgres, "ident"], writes=[ptres])
            lgT, lgTres = lgT_r.nxt()
            mx, mxres = mx_r.nxt()
            dm, dmres = dm_r.nxt()
            ga, gares = ga_r.nxt()
            gq, gqres = gq_r.nxt()
            P.op("dve", lambda e, pt=pt, lgT=lgT: e.tensor_copy(out=lgT[:], in_=pt[:, 0:NE]), reads=[ptres], writes=[lgTres], fence=True)
            P.op("dve", lambda e, mx=mx, lgT=lgT: e.max(out=mx[:], in_=lgT[:]), reads=[lgTres], writes=[mxres], fence=True)
            P.op("dve", lambda e, mx=mx, dm=dm: e.tensor_tensor(out=dm[:, 0:1], in0=mx[:, 1:2], in1=mx[:, 0:1], op=ALU.subtract),
                 reads=[mxres], writes=[(dmres, 0)], fence=True)
            P.op("act", lambda e, dm=dm: e.activation(out=dm[:, 1:2], in_=dm[:, 0:1], func=AF.Sigmoid), reads=[(dmres, 0)], writes=[(dmres, 1)], fence=True)
            P.op("dve", lambda e, dm=dm: e.tensor_scalar(out=dm[:, 0:1], in0=dm[:, 1:2], scalar1=-1.0, scalar2=1.0, op0=ALU.mult, op1=ALU.add),
                 reads=[(dmres, 1)], writes=[(dmres, 0)], fence=True)
            P.op("dve", lambda e, ga=ga, lgT=lgT, mx=mx, dm=dm: e.tensor_scalar(out=ga[:], in0=lgT[:], scalar1=mx[:, 0:1], scalar2=dm[:, 0:1],
                                                                             op0=ALU.is_equal, op1=ALU.mult),
                 reads=[lgTres, mxres, (dmres, 0)], writes=[gares], fence=True)
            P.op("dve", lambda e, gq=gq, lgT=lgT, mx=mx, dm=dm: e.tensor_scalar(out=gq[:], in0=lgT[:], scalar1=mx[:, 1:2], scalar2=dm[:, 1:2],
                                                                             op0=ALU.is_equal, op1=ALU.mult),
                 reads=[lgTres, mxres, (dmres, 1)], writes=[gqres], fence=True)
            P.op("dve", lambda e, ga=ga, gq=gq: e.tensor_tensor(out=ga[:], in0=ga[:], in1=gq[:], op=ALU.add), reads=[gares, gqres], writes=[gares], fence=True)
            pg2, pg2res = pu_r.nxt()
            P.op("pe", lambda e, pg2=pg2, ga=ga: e.transpose(pg2[0:NE, 0:128], ga[:, :], ident[:, :]), reads=[gares, "ident"], writes=[pg2res])
            c0 = st * SUB + tb * 128
            P.op("dve", lambda e, pg2=pg2, c0=c0: e.tensor_copy(out=gT[:, c0:c0 + 128], in_=pg2[0:NE, 0:128]), reads=[pg2res], writes=[("gT", c0)], fence=True)

    def gate_bcast():
        for e_ in range(NE):
            for hf in range(ST // 512):
                pgb, pgbres = pg_r.nxt()
                P.op("pe", lambda e, pgb=pgb, e_=e_, hf=hf: e.matmul(pgb[:, :512], oneh[:, e_ * 128:(e_ + 1) * 128], gT[:, hf * 512:(hf + 1) * 512],
                                                                    start=True, stop=True),
                     reads=[("gT", c0) for c0 in range(hf * 512, (hf + 1) * 512, 128)] + ["oneh"], writes=[pgbres])
                P.op("act", lambda e, pgb=pgb, e_=e_, hf=hf: e.activation(out=gb[:, e_, hf * 512:(hf + 1) * 512], in_=pgb[:, :512], func=AF.Copy),
                     reads=[pgbres], writes=[("gb", e_, hf)])

    def load_w(e_, g):
        wgq, wgres = wgq_r.nxt()
        wuq, wures = wuq_r.nxt()
        wdq, wdres = wdq_r.nxt()
        c0 = g * FG * 128
        cx.load("pool", wgq[:], wg_d[e_, :, c0:c0 + FG * 128].rearrange("(k p) c -> p k c", p=128), wgres)
        cx.load("pool", wuq[:], wu_d[e_, :, c0:c0 + FG * 128].rearrange("(k p) c -> p k c", p=128), wures)
        cx.load("pool", wdq[:], wd_d[e_, c0:c0 + FG * 128, :].rearrange("(f p) c -> p f c", p=128), wdres)
        return (wgq, wgres, wuq, wures, wdq, wdres)

    def expert_unit(e_, g, W, first):
        wgq, wgres, wuq, wures, wdq, wdres = W

        def sub(st):
            ureads = [("u2", st)]
            hq, hqres = hq_r.nxt()
            for f in range(FG):
                pg, pgres = pg_r.nxt()
                pu, pures = pu_r.nxt()

                def mmg(e, pg=pg, f=f):
                    ins = None
                    for k in range(8):
                        ins = e.matmul(pg[:, :SUB], wgq[:, k, f * 128:(f + 1) * 128], u2[:, k, st * SUB:(st + 1) * SUB], start=(k == 0), stop=(k == 7))
                    return ins

                def mmu(e, pu=pu, f=f):
                    ins = None
                    for k in range(8):
                        ins = e.matmul(pu[:, :SUB], wuq[:, k, f * 128:(f + 1) * 128], u2[:, k, st * SUB:(st + 1) * SUB], start=(k == 0), stop=(k == 7))
                    return ins
                P.op("pe", mmg, reads=ureads + [wgres], writes=[pgres])
                P.op("pe", mmu, reads=ureads + [wures], writes=[pures])
                sg, sgres = sg_r.nxt()
                P.op("act", lambda e, sg=sg, pg=pg: e.activation(out=sg[:, :], in_=pg[:, :SUB], func=AF.Silu), reads=[pgres], writes=[sgres])
                h1, h1res = h1_r.nxt()
                P.op("dve", lambda e, sg=sg, pu=pu, h1=h1: e.tensor_tensor(out=h1[:, :], in0=sg[:, :], in1=pu[:, :SUB], op=ALU.mult),
                     reads=[sgres, pures], writes=[h1res])
                P.op("dve", lambda e, h1=h1, f=f: e.tensor_tensor(out=hq[:, f, :], in0=h1[:, :], in1=gb[:, e_, st * SUB:(st + 1) * SUB], op=ALU.mult),
                     reads=[h1res, ("gb", e_, (st * SUB) // 512)], writes=[(hqres, f)])
            hreads = [(hqres, f) for f in range(FG)]
            for oc in range(8):
                pd, pdres = pd_r.nxt()

                def mmd(e, pd=pd, oc=oc):
                    ins = None
                    for f in range(FG):
                        ins = e.matmul(pd[:, :SUB], wdq[:, f, oc * 128:(oc + 1) * 128], hq[:, f, :], start=(f == 0), stop=(f == FG - 1))
                    return ins
                P.op("pe", mmd, reads=hreads + [wdres], writes=[pdres])
                ya = yacc[:, oc, st * SUB:(st + 1) * SUB]
                if first:
                    P.op("dve", lambda e, pd=pd, ya=ya: e.tensor_copy(out=ya, in_=pd[:, :SUB]), reads=[pdres], writes=[("yacc", oc, st)])
                else:
                    P.op("dve", lambda e, pd=pd, ya=ya: e.tensor_tensor(out=ya, in0=pd[:, :SUB], in1=ya, op=ALU.add),
                         reads=[pdres, ("yacc", oc, st)], writes=[("yacc", oc, st)])

        for st in range(ST // SUB):
            sub(st)

    def finish(s0, st):
        t0 = s0 + st * SUB
        x, xres = x_r.nxt()
        cx.load("sp", x[:, :, :], xv[:, :, t0:t0 + SUB], (xres, "ld"), reads=[(xres, k) for k in range(8)])
        for oc in range(8):
            P.op("act", lambda e, oc=oc: e.activation(out=x[:, oc, :], in_=x[:, oc, :], func=AF.Identity, scale=ALPHA),
                 reads=[(xres, "ld")], writes=[(xres, oc)])
            gate = mod_sb[:, 80 + 2 * oc:80 + 2 * oc + 1]
            P.op("dve", lambda e, oc=oc, gate=gate: e.scalar_tensor_tensor(out=x[:, oc, :], in0=yacc[:, oc, st * SUB:(st + 1) * SUB], scalar=gate,
                                                                        in1=x[:, oc, :], op0=ALU.mult, op1=ALU.add),
                 reads=[("yacc", oc, st), (xres, oc), "mod"], writes=[(xres, oc)])
        emit_layernorm(cx, ln, x, lambda k: (xres, k), SUB, g_sb, b_sb, "lngb")
        P.dma("sp", "S_" + xres, lambda e, s: e.dma_start(out=ov[:, :, t0:t0 + SUB], in_=x[:, :, :]).then_inc(s, 16),
              reads=[(xres, k) for k in range(8)], writes=[(xres, "ld")])

    units = [(e_, g) for e_ in range(NE) for g in range(NG)]
    for s0 in range(0, TOWN, ST):
        W = load_w(*units[0])
        for st in range(ST // SUB):
            router(s0, st)
        gate_bcast()
        for i, (e_, g) in enumerate(units):
            Wn = load_w(*units[i + 1]) if i + 1 < len(units) else None
            expert_unit(e_, g, W, first=(i == 0))
            W = Wn
        for st in range(ST // SUB):
            finish(s0, st)
    return cx


def stage_C1a(inp, resA1, resB1, resNA, xT_list):
    cx = build_Ca(False)
    maps = []
    for c in range(NCORES):
        aT = np.zeros((1024, NT), NPBF)
        aT[:512] = np.asarray(resB1[c]["oT"])
        aT[512:, :TOWN] = np.asarray(resNA[c]["naT"])
        maps.append(dict(xT=xT_list[c], aT=aT, mod=np.asarray(resA1[c]["mod"]),
                         wout=np.ascontiguousarray(inp["od_w_out"][0]), lng=pcol(inp["ln1_g"][1]), lnb=pcol(inp["ln1_b"][1])))
    return run_spmd(cx, maps)


def stage_C1b(inp, resA1, resC1a):
    cx = build_C1b()
    oh = np.zeros((8, 8 * 128), np.float32)
    for e in range(8):
        oh[e, e * 128:(e + 1) * 128] = 1.0
    shared = dict(wr=np.ascontiguousarray(inp["od_w_router"][0]), br=np.ascontiguousarray(inp["od_b_router"][0][:, None]),
                  wg=np.ascontiguousarray(inp["od_w_gate"][0]), wu=np.ascontiguousarray(inp["od_w_up"][0]),
                  wd=np.ascontiguousarray(inp["od_w_down"][0]), lng=pcol(inp["ln2_g"][1]), lnb=pcol(inp["ln2_b"][1]),
                  ident=np.eye(128, dtype=np.float32), onehot=oh)
    maps = []
    for c in range(NCORES):
        m = dict(shared)
        m.update(xT=np.asarray(resC1a[c]["xo"]), mod=np.asarray(resA1[c]["mod"]))
        maps.append(m)
    return run_spmd(cx, maps)


def kernel(**inp):
    inp = {k: np.asarray(v) for k, v in inp.items()}
    resA0 = stage_A0(inp)
    resB0 = stage_B0(resA0)
    resC0a = stage_C0a(inp, resA0, resB0)
    resC0b = stage_C0b(inp, resA0, resC0a)
    x1 = [np.asarray(resC0b[c]["xo"]) for c in range(NCORES)]
    resA1 = stage_A1(inp, x1)
    resB1 = stage_B1(resA1)
    resNA = stage_NA(inp, resA1)
    resC1a = stage_C1a(inp, resA1, resB1, resNA, x1)
    resC1b = stage_C1b(inp, resA1, resC1a)
    out = np.empty((2, SEQ, D), np.float32)
    for c in range(NCORES):
        b, j = divmod(c, 4)
        out[b, j * TOWN:(j + 1) * TOWN] = np.asarray(resC1b[c]["xo"]).T
    return out
```

```python
import contextlib
import numpy as np
import ml_dtypes
import concourse.bass as bass
import concourse.mybir as mybir
from concourse.bass_utils import run_bass_kernel_spmd

F32 = mybir.dt.float32
BF16 = mybir.dt.bfloat16
AF = mybir.ActivationFunctionType
ALU = mybir.AluOpType
NPBF = ml_dtypes.bfloat16

NCORES = 8
D = 1024
SEQ = 16384
TOWN = 4096
CTX = 256
NT = TOWN + CTX
GRID_W = 64
EPS = 1e-6
ALPHA = 4 ** 0.25
ENGS = ("pe", "act", "dve", "pool", "sp")


class Prog:
    def __init__(self, nc):
        self.nc = nc
        self.ops = []
        self.last_w = {}
        self.readers = {}
        self.dma_cnt = {}
        self.excl = set()

    def _deps(self, reads, writes):
        ex = [r for r in reads if r in self.excl]
        if ex:
            reads = [r for r in reads if r not in self.excl]
            writes = list(writes) + ex
        deps = set()
        for r in reads:
            w = self.last_w.get(r)
            if w is not None:
                deps.add(w)
        for r in writes:
            deps.update(self.readers.get(r, ()))
            w = self.last_w.get(r)
            if w is not None:
                deps.add(w)
        i = len(self.ops)
        for r in reads:
            self.readers.setdefault(r, []).append(i)
        for r in writes:
            self.last_w[r] = i
            self.readers[r] = []
        return deps

    def op(self, eng, fn, reads=(), writes=(), fence=False):
        deps = self._deps(reads, writes)
        self.ops.append(dict(eng=eng, fn=fn, deps=deps, dma=None, sig=None, need=False, fence=fence))

    def dma(self, queue, key, fn, reads=(), writes=(), n=1):
        deps = self._deps(reads, writes)
        c = self.dma_cnt.get(key, 0) + 16 * n
        self.dma_cnt[key] = c
        self.ops.append(dict(eng=queue, fn=fn, deps=deps, dma=key, sig=(("dma", key), c), need=True, fence=True))

    def emit(self):
        nc = self.nc
        ops = self.ops
        for o in ops:
            for d in o["deps"]:
                p = ops[d]
                if p["dma"] is None and (p["eng"] != o["eng"] or o["fence"]):
                    p["need"] = True
        cnt = {e: 0 for e in ENGS}
        for o in ops:
            if o["dma"] is None and o["need"]:
                cnt[o["eng"]] += 1
                o["sig"] = (("eng", o["eng"]), cnt[o["eng"]])
        with contextlib.ExitStack() as st:
            sems = {}
            for e in ENGS:
                if cnt[e]:
                    sems[("eng", e)] = st.enter_context(nc.semaphore("s_" + e))
            for k in self.dma_cnt:
                sems[("dma", k)] = st.enter_context(nc.semaphore("d_" + str(k)))
            block = st.enter_context(nc.Block())
            per = {e: [] for e in ENGS}
            for i, o in enumerate(ops):
                per[o["eng"]].append(i)
            finals = [(("dma", k), c) for k, c in self.dma_cnt.items()]

            def body(ename):
                def run(eng):
                    waited = {}
                    for i in per[ename]:
                        o = ops[i]
                        need = {}
                        for d in o["deps"]:
                            p = ops[d]
                            if p["dma"] is None and p["eng"] == ename and not o["fence"]:
                                continue
                            sk, v = p["sig"]
                            if v > need.get(sk, 0):
                                need[sk] = v
                        for sk, v in need.items():
                            if waited.get(sk, 0) >= v:
                                continue
                            eng.wait_ge(sems[sk], v)
                            waited[sk] = v
                        if o["dma"] is not None:
                            o["fn"](eng, sems[("dma", o["dma"])])
                        else:
                            ins = o["fn"](eng)
                            if o["need"]:
                                ins.then_inc(sems[o["sig"][0]], 1)
                    if ename == "sp":
                        for sk, v in finals:
                            if waited.get(sk, 0) < v:
                                eng.wait_ge(sems[sk], v)
                return run

            block.tensor(body("pe"))
            block.scalar(body("act"))
            block.vector(body("dve"))
            block.gpsimd(body("pool"))
            block.sync(body("sp"))


class Ctx:
    def __init__(self):
        self.nc = bass.Bass("TRN2", target_bir_lowering=False)
        self.st = contextlib.ExitStack()
        self.P = Prog(self.nc)
        self.out_names = []
        self._u = 0

    def inp(self, name, shape, dt=F32):
        return self.nc.dram_tensor(name, list(shape), dt, kind="ExternalInput").ap()

    def out(self, name, shape, dt=F32):
        self.out_names.append(name)
        return self.nc.dram_tensor(name, list(shape), dt, kind="ExternalOutput").ap()

    def sb(self, name, shape, dt=F32):
        return self.st.enter_context(self.nc.sbuf_tensor(name, list(shape), dt))

    def ps(self, name, shape, dt=F32):
        self.P.excl.add(name)
        return self.st.enter_context(self.nc.psum_tensor(name, list(shape), dt))

    def ring(self, name, n, shape, dt=F32):
        return Ring([(self.sb("%s%d" % (name, i), shape, dt), "%s%d" % (name, i)) for i in range(n)])

    def psring(self, name, n, shape=(128, 512), dt=F32):
        return Ring([(self.ps("%s%d" % (name, i), (128, 512), dt), "%s%d" % (name, i)) for i in range(n)])

    def load(self, q, dst, src, res, reads=()):
        self.P.dma(q, "L_" + str(res), lambda e, s: e.dma_start(out=dst, in_=src).then_inc(s, 16),
                   reads=reads, writes=[res])

    def store(self, q, dst, src, res, dres=None):
        self.P.dma(q, "S_" + str(res), lambda e, s: e.dma_start(out=dst, in_=src).then_inc(s, 16),
                   reads=[res], writes=([dres] if dres else []))

    def finish(self):
        self.P.emit()
        self.st.close()
        return self.nc


class Ring:
    def __init__(self, items):
        self.items = items
        self.i = 0

    def nxt(self):
        it = self.items[self.i % len(self.items)]
        self.i += 1
        return it


def run_spmd(cx, in_maps):
    nc = cx.finish()
    res = run_bass_kernel_spmd(nc, in_maps, core_ids=list(range(NCORES)))
    return res.results


def tiles_own_ctx(tt=512):
    t = [(i * tt, tt, 0) for i in range(TOWN // tt)]
    t.append((TOWN, CTX, 1))
    return t


def pcol(v):
    v = np.asarray(v, np.float32)
    return np.ascontiguousarray(v.reshape(-1, 128).T)


def emit_ada(cx, cs_d, wada_d, bada_d, mod_sb, modp_sb, pm=None, pmres="ps_mod"):
    P = cx.P
    cs_sb = cx.sb("cs_sb", [128, 16])
    s_sb = cx.sb("s_sb", [128, 16])
    ba_sb = cx.sb("ba_sb", [128, 48])
    if pm is None:
        pm = cx.ps("ps_mod", [128, 512])
    cx.load("sp", cs_sb[:], cs_d, "cs_sb")
    cx.load("sp", ba_sb[:], bada_d, "ba_sb")
    P.op("act", lambda e: e.activation(out=s_sb[:], in_=cs_sb[:], func=AF.Silu), reads=["cs_sb"], writes=["s_sb"])
    wr = cx.ring("wada", 2, [128, 8, 512])
    wv = wada_d.rearrange("(k p) c -> p k c", p=128)
    for piece in range(12):
        wa, wres = wr.nxt()
        cx.load("sp", wa[:], wv[:, :, piece * 512:(piece + 1) * 512], wres)

        def mm(e, wa=wa, piece=piece):
            ins = None
            for f4 in range(4):
                fc = piece * 4 + f4
                for k in range(8):
                    ins = e.matmul(pm[:, fc * 2:fc * 2 + 2], wa[:, k, f4 * 128:(f4 + 1) * 128],
                                   s_sb[:, 2 * k:2 * k + 2], start=(k == 0), stop=(k == 7))
            return ins
        P.op("pe", mm, reads=[wres, "s_sb"], writes=[pmres])
    pmv = pm[:, 0:96].rearrange("p (m c) -> p m c", c=2)
    mv = mod_sb[:].rearrange("p (m c) -> p m c", c=2)
    for c in range(2):
        P.op("dve", lambda e, c=c: e.tensor_tensor(out=mv[:, :, c], in0=pmv[:, :, c], in1=ba_sb[:], op=ALU.add),
             reads=[pmres, "ba_sb"], writes=["mod"], fence=True)
    P.op("dve", lambda e: e.tensor_copy(out=modp_sb[:], in_=mod_sb[:]), reads=["mod"], writes=["modp"], fence=True)
    for lo in (16, 64):
        P.op("dve", lambda e, lo=lo: e.tensor_scalar_add(out=modp_sb[:, lo:lo + 16], in0=mod_sb[:, lo:lo + 16], scalar1=1.0),
             reads=["mod"], writes=["modp"], fence=True)


def emit_modulate(cx, x, xres, u, ures, tw, col, mod_sb, modp_sb, piece_shift, nk=8):
    P = cx.P
    for k in range(nk):
        sc = modp_sb[:, (piece_shift + 1) * 16 + 2 * k + col:(piece_shift + 1) * 16 + 2 * k + col + 1]
        sh = mod_sb[:, piece_shift * 16 + 2 * k + col:piece_shift * 16 + 2 * k + col + 1]
        if k % 2 == 0:
            P.op("act", lambda e, k=k, sc=sc, sh=sh: e.activation(out=u[:, k, :tw], in_=x[:, k, :tw], func=AF.Identity,
                                                                 bias=sh, scale=sc),
                 reads=[xres, "mod", "modp"], writes=[(ures, k)])
        else:
            P.op("dve", lambda e, k=k, sc=sc, sh=sh: e.tensor_scalar(out=u[:, k, :tw], in0=x[:, k, :tw], scalar1=sc,
                                                                    scalar2=sh, op0=ALU.mult, op1=ALU.add),
                 reads=[xres, "mod", "modp"], writes=[(ures, k)])


def build_A0():
    cx = Ctx()
    P = cx.P
    xT = cx.inp("xT", [D, NT])
    cs_d = cx.inp("cs", [128, 16])
    wada_d = cx.inp("wada", [D, 6 * D])
    bada_d = cx.inp("bada", [128, 48])
    win_d = cx.inp("win", [D, 1536])
    gq_d = cx.inp("gq", [128, 1])
    gk_d = cx.inp("gk", [128, 1])
    cos_d = cx.inp("cosT", [128, NT])
    sin_d = cx.inp("sinT", [128, NT])
    rt_d = cx.inp("rotT", [128, 128])
    ob_d = cx.inp("oneblk", [128, 128])
    mod_o = cx.out("mod", [128, 96])
    qT_o = cx.out("qT", [768, NT], BF16)
    kT_o = cx.out("kT", [256, NT], BF16)
    v_o = cx.out("v", [NT, 256], BF16)
    pT_o = cx.out("pT", [256, NT])

    mod_sb = cx.sb("mod_sb", [128, 96])
    modp_sb = cx.sb("modp_sb", [128, 96])
    emit_ada(cx, cs_d, wada_d, bada_d, mod_sb, modp_sb)
    cx.store("sp", mod_o, mod_sb[:], "mod")

    win = cx.sb("win_sb", [128, 8, 1536], BF16)
    cx.load("pool", win[:], win_d.rearrange("(k p) c -> p k c", p=128), "win")
    oneblk = cx.sb("oneblk_sb", [128, 128], BF16)
    cx.load("pool", oneblk[:], ob_d, "oneblk")
    rt32 = cx.sb("rt32", [128, 128])
    cx.load("sp", rt32[:], rt_d, "rt32")
    g_sb = cx.sb("g_sb", [128, 2])
    cx.load("sp", g_sb[:, 0:1], gq_d, "gq")
    cx.load("sp", g_sb[:, 1:2], gk_d, "gk")
    rg = [cx.sb("rgq", [128, 128], BF16), cx.sb("rgk", [128, 128], BF16)]
    cosg = [cx.sb("cosgq", [128, NT]), cx.sb("cosgk", [128, NT])]
    sin_sb = cx.sb("sin_sb", [128, NT])
    cx.load("sp", sin_sb[:], sin_d, "sin")
    gres = ["gq", "gk"]
    for i in range(2):
        cx.load("sp", cosg[i][:], cos_d, "cosg%d" % i)
        P.op("dve", lambda e, i=i: e.tensor_scalar_mul(out=rg[i][:], in0=rt32[:], scalar1=g_sb[:, i:i + 1]),
             reads=["rt32", gres[i]], writes=["rg%d" % i])
        P.op("dve", lambda e, i=i: e.tensor_scalar_mul(out=cosg[i][:], in0=cosg[i][:], scalar1=g_sb[:, i:i + 1]),
             reads=[gres[i], "cosg%d" % i], writes=["cosg%d" % i])

    eps_sb = cx.sb("eps_sb", [128, 1])
    P.op("dve", lambda e: e.memset(eps_sb[:], EPS), writes=["eps"])
    xr = cx.ring("x", 2, [128, 8, 512])
    ur = cx.ring("u", 2, [128, 8, 512], BF16)
    pq_r = cx.psring("pq", 2)
    pms_r = cx.psring("pms", 1)
    prot_r = cx.psring("prot", 1)
    pv_r = cx.psring("pv", 2)
    sq_r = cx.ring("sq", 2, [128, 512], BF16)
    qb_r = cx.ring("qb", 2, [128, 512], BF16)
    t1_r = cx.ring("t1", 2, [128, 512])
    t2_r = cx.ring("t2", 2, [128, 512])
    rs_r = cx.ring("rs", 2, [128, 512])
    qf_r = cx.ring("qf", 3, [128, 512], BF16)
    vs_r = cx.ring("vs", 2, [128, 256], BF16)
    pp_r = cx.ring("pp", 2, [128, 512])
    xv = xT.rearrange("(k p) t -> p k t", p=128)

    def do_tile(t0, tw, col):
        x, xres = xr.nxt()
        cx.load("sp", x[:, :, :tw], xv[:, :, t0:t0 + tw], xres)
        u, ures = ur.nxt()
        emit_modulate(cx, x, xres, u, ures, tw, col, mod_sb, modp_sb, 0)
        ureads = [(ures, k) for k in range(8)]
        for cc in range(8):
            isk = cc >= 6
            c0 = cc * 128 if not isk else 768 + (cc - 6) * 128
            gi = 1 if isk else 0
            pq, pqres = pq_r.nxt()

            def mmq(e, pq=pq, c0=c0):
                ins = None
                for k in range(8):
                    ins = e.matmul(pq[:, :tw], win[:, k, c0:c0 + 128], u[:, k, :tw], start=(k == 0), stop=(k == 7))
                return ins
            P.op("pe", mmq, reads=ureads + ["win"], writes=[pqres])
            sq, sqres = sq_r.nxt()
            P.op("act", lambda e, sq=sq, pq=pq: e.activation(out=sq[:, :tw], in_=pq[:, :tw], func=AF.Square),
                 reads=[pqres], writes=[sqres])
            qb, qbres = qb_r.nxt()
            P.op("dve", lambda e, qb=qb, pq=pq: e.tensor_copy(out=qb[:, :tw], in_=pq[:, :tw]), reads=[pqres], writes=[qbres])
            pms, pmsres = pms_r.nxt()
            P.op("pe", lambda e, pms=pms, sq=sq: e.matmul(pms[:, :tw], oneblk[:], sq[:, :tw], start=True, stop=True),
                 reads=[sqres, "oneblk"], writes=[pmsres])
            prot, protres = prot_r.nxt()
            P.op("pe", lambda e, prot=prot, qb=qb, gi=gi: e.matmul(prot[:, :tw], rg[gi][:], qb[:, :tw], start=True, stop=True),
                 reads=[qbres, "rg%d" % gi], writes=[protres])
            t1, t1res = t1_r.nxt()
            P.op("dve", lambda e, t1=t1, pq=pq, gi=gi: e.tensor_tensor(out=t1[:, :tw], in0=pq[:, :tw],
                                                                     in1=cosg[gi][:, t0:t0 + tw], op=ALU.mult),
                 reads=[pqres, "cosg%d" % gi], writes=[t1res])
            t2, t2res = t2_r.nxt()
            P.op("dve", lambda e, t2=t2, prot=prot: e.tensor_tensor(out=t2[:, :tw], in0=prot[:, :tw],
                                                                  in1=sin_sb[:, t0:t0 + tw], op=ALU.mult),
                 reads=[protres, "sin"], writes=[t2res])
            rs, rsres = rs_r.nxt()
            P.op("act", lambda e, rs=rs, pms=pms: e.activation(out=rs[:, :tw], in_=pms[:, :tw], func=AF.Sqrt, bias=eps_sb[:, 0:1]),
                 reads=[pmsres, "eps"], writes=[rsres])
            P.op("dve", lambda e, rs=rs: e.reciprocal(out=rs[:, :tw], in_=rs[:, :tw]), reads=[rsres], writes=[rsres])
            P.op("dve", lambda e, t1=t1, t2=t2: e.tensor_tensor(out=t1[:, :tw], in0=t1[:, :tw], in1=t2[:, :tw], op=ALU.add),
                 reads=[t1res, t2res], writes=[t1res])
            qf, qfres = qf_r.nxt()
            P.op("dve", lambda e, qf=qf, t1=t1, rs=rs: e.tensor_tensor(out=qf[:, :tw], in0=t1[:, :tw], in1=rs[:, :tw], op=ALU.mult),
                 reads=[t1res, rsres], writes=[qfres])
            dst = (kT_o[(cc - 6) * 128:(cc - 5) * 128, t0:t0 + tw] if isk else qT_o[cc * 128:(cc + 1) * 128, t0:t0 + tw])
            cx.store("sp", dst, qf[:, :tw], qfres)
        for tb in range(tw // 128):
            pv, pvres = pv_r.nxt()

            def mmv(e, pv=pv, tb=tb):
                ins = None
                for k in range(8):
                    ins = e.matmul(pv[:, :256], u[:, k, tb * 128:(tb + 1) * 128], win[:, k, 1024:1280],
                                   start=(k == 0), stop=(k == 7))
                return ins
            P.op("pe", mmv, reads=ureads + ["win"], writes=[pvres])
            vs, vsres = vs_r.nxt()
            P.op("act", lambda e, vs=vs, pv=pv: e.activation(out=vs[:], in_=pv[:, :256], func=AF.Copy), reads=[pvres], writes=[vsres])
            cx.store("pool", v_o[t0 + tb * 128:t0 + (tb + 1) * 128, :], vs[:], vsres)
        for pc in range(2):
            pq, pqres = pq_r.nxt()

            def mmp(e, pq=pq, pc=pc):
                ins = None
                for k in range(8):
                    ins = e.matmul(pq[:, :tw], win[:, k, 1280 + pc * 128:1280 + (pc + 1) * 128], u[:, k, :tw],
                                   start=(k == 0), stop=(k == 7))
                return ins
            P.op("pe", mmp, reads=ureads + ["win"], writes=[pqres])
            pp, ppres = pp_r.nxt()
            P.op("act", lambda e, pp=pp, pq=pq: e.activation(out=pp[:, :tw], in_=pq[:, :tw], func=AF.Copy), reads=[pqres], writes=[ppres])
            cx.store("pool", pT_o[pc * 128:(pc + 1) * 128, t0:t0 + tw], pp[:, :tw], ppres)

    for (t0, tw, col) in tiles_own_ctx():
        do_tile(t0, tw, col)
    return cx


def build_attn(nheads, group, dk, nkc_full, scale):
    cx = Ctx()
    P = cx.P
    nkv = nheads // group
    nkeys = nkc_full * 128
    qT = cx.inp("qT", [nheads * dk, NT], BF16)
    kT = cx.inp("kT", [nkv, dk, nkeys], BF16)
    vv = cx.inp("v", [nkv, 128, nkc_full, 64], BF16)
    sel_d = cx.inp("sel", [65, 64])
    oT = cx.out("oT", [nheads * 64, NT], BF16)

    kg_r = cx.ring("kg", 2, [128, nkeys], BF16)
    vg_r = cx.ring("vg", 2, [128, nkc_full, 65], BF16)
    for vg, vres in vg_r.items:
        P.op("dve", lambda e, vg=vg: e.memset(vg[:, :, 64:65], 1.0), writes=[(vres, "ones")])
    for kg, kres in kg_r.items:
        P.op("pool", lambda e, kg=kg: e.memset(kg[:, :], 0.0), writes=[kres])
    sel = cx.sb("sel_sb", [65, 64])
    cx.load("sp", sel[:], sel_d, "sel")
    q_r = cx.ring("qh", 2, [128, NT], BF16)
    for qh, qres in q_r.items:
        P.op("pool", lambda e, qh=qh: e.memset(qh[:, :], 0.0), writes=[qres])
    ps_r = cx.psring("ps", 4)
    po_r = cx.psring("po", 2)
    pb_r = cx.psring("pb", 1)
    pt_r = cx.ring("pt", 4, [128, 512], BF16)
    os_r = cx.ring("os", 2, [65, 512])
    at_r = cx.ring("at", 2, [64, 512], BF16)
    LOOK = 3
    for g in range(nkv):
        kg, kres = kg_r.nxt()
        cx.load("sp", kg[0:dk, :], kT[g], kres)
        vg, vres = vg_r.nxt()
        cx.load("sp", vg[:, :, 0:64], vv[g], vres)
        for h in range(g * group, (g + 1) * group):
            qh, qres = q_r.nxt()
            cx.load("sp", qh[0:dk, :], qT[h * dk:(h + 1) * dk, :], qres)
            steps = []
            for (t0, tw, col) in tiles_own_ctx():
                nkc = nkc_full if col == 0 else 2
                for kc in range(nkc):
                    steps.append((t0, tw, kc, nkc))
            pss = {}

            def emit_qk(i, kg=kg, kres=kres, qh=qh, qres=qres):
                t0, tw, kc, nkc = steps[i]
                ps, psres = ps_r.nxt()
                pss[i] = (ps, psres)
                P.op("pe", lambda e: e.matmul(ps[:, :tw], kg[:, kc * 128:(kc + 1) * 128], qh[:, t0:t0 + tw], start=True, stop=True),
                     reads=[kres, qres], writes=[psres])

            def emit_rest(i, po, pores, vg=vg, vres=vres):
                t0, tw, kc, nkc = steps[i]
                ps, psres = pss.pop(i)
                pt, ptres = pt_r.nxt()
                P.op("act", lambda e: e.activation(out=pt[:, :tw], in_=ps[:, :tw], func=AF.Exp, scale=scale), reads=[psres], writes=[ptres])
                P.op("pe", lambda e: e.matmul(po[0:65, :tw], vg[:, kc, :], pt[:, :tw], start=(kc == 0), stop=(kc == nkc - 1)),
                     reads=[ptres, vres, (vres, "ones")], writes=[pores])

            def emit_norm(i, po, pores, h=h):
                t0, tw, kc, nkc = steps[i]
                osb, osres = os_r.nxt()
                P.op("act", lambda e: e.activation(out=osb[:, :tw], in_=po[0:65, :tw], func=AF.Copy), reads=[pores], writes=[osres])
                P.op("dve", lambda e: e.reciprocal(out=osb[64:65, :tw], in_=osb[64:65, :tw]), reads=[osres], writes=[osres])
                pb, pbres = pb_r.nxt()
                P.op("pe", lambda e: e.matmul(pb[0:64, :tw], sel[:], osb[:, :tw], start=True, stop=True), reads=[osres, "sel"], writes=[pbres])
                at, atres = at_r.nxt()
                P.op("dve", lambda e: e.tensor_tensor(out=at[:, :tw], in0=osb[0:64, :tw], in1=pb[0:64, :tw], op=ALU.mult),
                     reads=[osres, pbres], writes=[atres])
                cx.store("pool", oT[h * 64:(h + 1) * 64, t0:t0 + tw], at[:, :tw], atres)

            for i in range(min(LOOK, len(steps))):
                emit_qk(i)
            po = pores = None
            for i in range(len(steps)):
                if steps[i][2] == 0:
                    po, pores = po_r.nxt()
                if i + LOOK < len(steps):
                    emit_qk(i + LOOK)
                emit_rest(i, po, pores)
                if steps[i][2] == steps[i][3] - 1:
                    emit_norm(i, po, pores)
    return cx


def rope_tables(j):
    t = np.arange(TOWN) + j * TOWN
    row = (t // GRID_W).astype(np.float64)
    colp = (t % GRID_W).astype(np.float64)
    inv = 10000.0 ** (-np.arange(16, dtype=np.float64) / 16)
    cos = np.ones((128, NT), np.float32)
    sin = np.zeros((128, NT), np.float32)
    for p in range(128):
        d = p % 64
        pos = row if d < 32 else colp
        ang = (pos.astype(np.float32) * inv[d % 16].astype(np.float32)).astype(np.float32)
        cos[p, :TOWN] = np.cos(ang)
        sin[p, :TOWN] = np.sin(ang)
    return cos, sin


def rot_matrix(n=128, blk=32):
    rt = np.zeros((n, n), np.float32)
    h = blk // 2
    for p in range(n):
        if p % blk < h:
            rt[p + h, p] = -1.0
        else:
            rt[p - h, p] = 1.0
    return rt


def blockdiag_ones(n, blk):
    m = np.zeros((n, n), np.float32)
    for i in range(0, n, blk):
        m[i:i + blk, i:i + blk] = 1.0 / blk
    return m


def core_xT(x, ctx, c):
    b, j = divmod(c, 4)
    return np.ascontiguousarray(np.concatenate([x[b, j * TOWN:(j + 1) * TOWN], ctx[b]], axis=0).T)


def core_cs(cvec, c_ctx, c):
    b = c // 4
    cs = np.empty((128, 16), np.float32)
    cs[:, 0::2] = pcol(cvec[b])
    cs[:, 1::2] = pcol(c_ctx)
    return cs


SEL = np.zeros((65, 64), np.float32)
SEL[64, :] = 1.0


def gather_kv(res, kname, vname, nkv, dk):
    out = []
    for b in range(2):
        ks = [np.asarray(res[4 * b][kname])[:, TOWN:]] + [np.asarray(res[4 * b + j][kname])[:, :TOWN] for j in range(4)]
        kf = np.concatenate(ks, axis=1).reshape(nkv, dk, -1)
        vs = [np.asarray(res[4 * b][vname])[TOWN:]] + [np.asarray(res[4 * b + j][vname])[:TOWN] for j in range(4)]
        vf = np.concatenate(vs, axis=0)
        nkc = vf.shape[0] // 128
        vf = np.ascontiguousarray(vf.reshape(nkc, 128, nkv, 64).transpose(2, 1, 0, 3))
        out.append((np.ascontiguousarray(kf), vf))
    return out


def stage_A0(inp):
    x, ctx = inp["x"], inp["ctx"]
    cx = build_A0()
    maps = []
    for c in range(NCORES):
        cos, sin = rope_tables(c % 4)
        maps.append(dict(
            xT=core_xT(x, ctx, c), cs=core_cs(inp["c"], inp["c_ctx"], c),
            wada=np.ascontiguousarray(inp["w_ada"][0]), bada=pcol(inp["b_ada"][0]),
            win=np.ascontiguousarray(inp["ev_w_in"][0]),
            gq=np.tile(inp["ev_q_gain"][0], 2)[:, None].astype(np.float32),
            gk=np.tile(inp["ev_k_gain"][0], 2)[:, None].astype(np.float32),
            cosT=cos, sinT=sin, rotT=rot_matrix(), oneblk=blockdiag_ones(128, 64)))
    return run_spmd(cx, maps)


def stage_B0(resA):
    cx = build_attn(12, 3, 64, 130, 64 ** -0.5)
    kv = gather_kv(resA, "kT", "v", 4, 64)
    maps = []
    for c in range(NCORES):
        kf, vf = kv[c // 4]
        maps.append(dict(qT=np.asarray(resA[c]["qT"]), kT=kf, v=vf, sel=SEL))
    return run_spmd(cx, maps)


class LNState:
    def __init__(self, cx, tag, width=512):
        self.ones = cx.sb(tag + "_ones", [128, 128])
        cx.P.op("dve", lambda e: e.memset(self.ones[:], 1.0 / D), writes=[tag + "_ones"])
        self.ones_res = tag + "_ones"
        self.eps = cx.sb(tag + "_eps", [128, 1])
        cx.P.op("dve", lambda e: e.memset(self.eps[:], EPS), writes=[tag + "_eps"])
        self.eps_res = tag + "_eps"
        self.pmean = cx.psring(tag + "_pmean", 1, (128, width))
        self.pmsq = cx.psring(tag + "_pmsq", 1, (128, width))
        self.rsq = cx.ring(tag + "_rsq", 2, [128, width])
        self.m = cx.ring(tag + "_m", 1, [128, width])
        self.var = cx.ring(tag + "_var", 1, [128, width])
        self.nb = cx.ring(tag + "_nb", 1, [128, width])
        self.t = cx.ring(tag + "_t", 2, [128, width])


def emit_layernorm(cx, ln, r, rres, tw, g_sb, b_sb, gres, out=None, outres=None):
    P = cx.P
    if out is None:
        out, outres = r, rres
    pmean, pmres = ln.pmean.nxt()
    pmsq, pqres = ln.pmsq.nxt()
    for k in range(8):
        rsq, rsqres = ln.rsq.nxt()
        P.op("act", lambda e, rsq=rsq, k=k: e.activation(out=rsq[:, :tw], in_=r[:, k, :tw], func=AF.Square),
             reads=[rres(k)], writes=[rsqres])
        P.op("pe", lambda e, k=k: e.matmul(pmean[:, :tw], ln.ones[:], r[:, k, :tw], start=(k == 0), stop=(k == 7)),
             reads=[rres(k), ln.ones_res], writes=[pmres])
        P.op("pe", lambda e, rsq=rsq, k=k: e.matmul(pmsq[:, :tw], ln.ones[:], rsq[:, :tw], start=(k == 0), stop=(k == 7)),
             reads=[rsqres, ln.ones_res], writes=[pqres])
    m, mres = ln.m.nxt()
    var, vres = ln.var.nxt()
    nb, nbres = ln.nb.nxt()
    P.op("act", lambda e: e.activation(out=m[:, :tw], in_=pmean[:, :tw], func=AF.Copy), reads=[pmres], writes=[mres])
    P.op("dve", lambda e: e.tensor_tensor(out=var[:, :tw], in0=m[:, :tw], in1=m[:, :tw], op=ALU.mult), reads=[mres], writes=[vres])
    P.op("dve", lambda e: e.tensor_tensor(out=var[:, :tw], in0=pmsq[:, :tw], in1=var[:, :tw], op=ALU.subtract),
         reads=[pqres, vres], writes=[vres])
    P.op("dve", lambda e: e.tensor_scalar_max(out=var[:, :tw], in0=var[:, :tw], scalar1=0.0), reads=[vres], writes=[vres])
    P.op("act", lambda e: e.activation(out=var[:, :tw], in_=var[:, :tw], func=AF.Sqrt, bias=ln.eps[:, 0:1]),
         reads=[vres, ln.eps_res], writes=[vres])
    P.op("dve", lambda e: e.reciprocal(out=var[:, :tw], in_=var[:, :tw]), reads=[vres], writes=[vres])
    P.op("dve", lambda e: e.scalar_tensor_tensor(out=nb[:, :tw], in0=m[:, :tw], scalar=-1.0, in1=var[:, :tw],
                                                 op0=ALU.mult, op1=ALU.mult), reads=[mres, vres], writes=[nbres])
    for k in range(8):
        t, tres = ln.t.nxt()
        P.op("dve", lambda e, t=t, k=k: e.tensor_tensor(out=t[:, :tw], in0=r[:, k, :tw], in1=var[:, :tw], op=ALU.mult),
             reads=[rres(k), vres], writes=[tres])
        P.op("dve", lambda e, t=t: e.tensor_tensor(out=t[:, :tw], in0=t[:, :tw], in1=nb[:, :tw], op=ALU.add),
             reads=[tres, nbres], writes=[tres])
        P.op("act", lambda e, t=t, k=k: e.activation(out=out[:, k, :tw], in_=t[:, :tw], func=AF.Identity,
                                                     bias=b_sb[:, k:k + 1], scale=g_sb[:, k:k + 1]),
             reads=[tres, gres], writes=[outres(k)])


def load_cast_w(cx, name, w_d, kch, ncol):
    w = cx.sb(name, [128, kch, ncol], BF16)
    for k in range(kch):
        cx.load("pool", w[:, k, :], w_d[k * 128:(k + 1) * 128, :], (name, k))
    return w, [(name, k) for k in range(kch)]


def build_Ca(with_pool):
    cx = Ctx()
    P = cx.P
    xT = cx.inp("xT", [D, NT])
    nat = 6 if with_pool else 8
    aT = cx.inp("aT", [nat * 128, NT], BF16)
    mod_d = cx.inp("mod", [128, 96])
    wout_d = cx.inp("wout", [D, D])
    g_d = cx.inp("lng", [128, 8])
    b_d = cx.inp("lnb", [128, 8])
    if with_pool:
        ph_d = cx.inp("ph", [256, NT + 32])
        rc_d = cx.inp("rcnt", [256, NT])
        wp_d = cx.inp("wpool", [256, 128])
        psc_d = cx.inp("pscale", [128, 2])
    xo = cx.out("xo", [D, NT])

    mod_sb = cx.sb("mod_sb", [128, 96])
    cx.load("sp", mod_sb[:], mod_d, "mod")
    g_sb = cx.sb("g_sb", [128, 8])
    b_sb = cx.sb("b_sb", [128, 8])
    cx.load("sp", g_sb[:], g_d, "lngb")
    cx.load("sp", b_sb[:], b_d, "lngb2")
    P.op("dve", lambda e: e.tensor_copy(out=b_sb[:], in_=b_sb[:]), reads=["lngb2"], writes=["lngb"])
    wout, wres = load_cast_w(cx, "wout_sb", wout_d, 8, D)
    ln = LNState(cx, "ln")
    if with_pool:
        wp = cx.sb("wp_sb", [128, 2, 128], BF16)
        for pc in range(2):
            cx.load("pool", wp[:, pc, :], wp_d[pc * 128:(pc + 1) * 128, :], ("wp", pc))
        psc = cx.sb("psc_sb", [128, 2])
        cx.load("sp", psc[:], psc_d, "psc")
        rc = cx.sb("rc_sb", [128, 2, NT])
        cx.load("sp", rc[:], rc_d.rearrange("(c p) t -> p c t", p=128), "rc")
        ph_r = cx.ring("ph", 4, [128, 528])
        s_r = [cx.ring("s%d" % i, 1, [128, 528]) for i in range(4)]
        ym_r = cx.ring("ym", 2, [128, 512], BF16)
        tmp_r = cx.ring("ptmp", 2, [128, 512])
        ppool_r = cx.psring("ppool", 1)
    x_r = cx.ring("x", 2, [128, 8, 512])
    mix_r = cx.ring("mix", 2, [128, 8, 512], BF16)
    po_r = cx.psring("po", 2)
    xv = xT.rearrange("(k p) t -> p k t", p=128)
    av = aT.rearrange("(k p) t -> p k t", p=128)
    ov = xo.rearrange("(k p) t -> p k t", p=128)
    LO = {2: 1, 4: 2, 8: 4, 16: 8}

    def do_tile(t0, tw, col):
        mix, mres = mix_r.nxt()
        cx.load("sp", mix[:, 0:nat, :tw], av[:, :, t0:t0 + tw], (mres, "a"))
        x, xres = x_r.nxt()
        cx.load("sp", x[:, :, :tw], xv[:, :, t0:t0 + tw], (xres, "ld"))
        if with_pool:
            h0 = t0 if col == 0 else t0 + 16
            L = tw + 16
            for pc in range(2):
                ph, phres = ph_r.nxt()
                cx.load("sp", ph[:, :L], ph_d[pc * 128:(pc + 1) * 128, h0:h0 + L], phres)
                srcs = [(ph, phres)]
                nlev = 2 if pc == 0 else 4
                for lev in range(nlev):
                    sh = 1 << lev
                    s, sres = s_r[lev].nxt()
                    a, ares = srcs[-1]
                    Lo = L - (2 * sh - 1)
                    P.op("dve", lambda e, s=s, a=a, sh=sh, Lo=Lo: e.tensor_tensor(out=s[:, :Lo], in0=a[:, 0:Lo], in1=a[:, sh:sh + Lo], op=ALU.add),
                         reads=[ares], writes=[sres])
                    srcs.append((s, sres))
                ym, ymres = ym_r.nxt()
                tmp, tmpres = tmp_r.nxt()
                for half in range(2):
                    w = (2, 4, 8, 16)[pc * 2 + half]
                    s, sres = srcs[{2: 1, 4: 2, 8: 3, 16: 4}[w]]
                    off = 8 - LO[w]
                    lo_p, hi_p = half * 64, half * 64 + 64
                    P.op("dve", lambda e, s=s, off=off, lo_p=lo_p, hi_p=hi_p, pc=pc, tmp=tmp:
                         e.tensor_tensor(out=tmp[lo_p:hi_p, :tw], in0=s[lo_p:hi_p, off:off + tw], in1=rc[lo_p:hi_p, pc, t0:t0 + tw], op=ALU.mult),
                         reads=[sres, "rc"], writes=[(tmpres, half)])
                    P.op("dve", lambda e, ph=ph, lo_p=lo_p, hi_p=hi_p, tmp=tmp, ym=ym:
                         e.tensor_tensor(out=ym[lo_p:hi_p, :tw], in0=tmp[lo_p:hi_p, :tw], in1=ph[lo_p:hi_p, 8:8 + tw], op=ALU.subtract),
                         reads=[(tmpres, half), phres], writes=[(ymres, half)])
                pp, ppres = ppool_r.nxt()
                P.op("pe", lambda e, pp=pp, ym=ym, pc=pc: e.matmul(pp[:, :tw], wp[:, pc, :], ym[:, :tw], start=True, stop=True),
                     reads=[(ymres, 0), (ymres, 1), ("wp", pc)], writes=[ppres])
                P.op("act", lambda e, pp=pp, pc=pc: e.activation(out=mix[:, 6 + pc, :tw], in_=pp[:, :tw], func=AF.Identity, scale=psc[:, pc:pc + 1]),
                     reads=[ppres, "psc"], writes=[(mres, "p", pc)])
        mreads = [(mres, "a")] + ([(mres, "p", 0), (mres, "p", 1)] if with_pool else [])
        for oc in range(8):
            P.op("act", lambda e, oc=oc: e.activation(out=x[:, oc, :tw], in_=x[:, oc, :tw], func=AF.Identity, scale=ALPHA),
                 reads=[(xres, "ld")], writes=[(xres, oc)])
            po, pores = po_r.nxt()

            def mm(e, po=po, oc=oc):
                ins = None
                for k in range(8):
                    ins = e.matmul(po[:, :tw], wout[:, k, oc * 128:(oc + 1) * 128], mix[:, k, :tw], start=(k == 0), stop=(k == 7))
                return ins
            P.op("pe", mm, reads=mreads + wres, writes=[pores])
            gate = mod_sb[:, 32 + 2 * oc + col:32 + 2 * oc + col + 1]
            P.op("dve", lambda e, po=po, oc=oc, gate=gate: e.scalar_tensor_tensor(out=x[:, oc, :tw], in0=po[:, :tw], scalar=gate,
                                                                               in1=x[:, oc, :tw], op0=ALU.mult, op1=ALU.add),
                 reads=[pores, (xres, oc), "mod"], writes=[(xres, oc)])
        emit_layernorm(cx, ln, x, lambda k: (xres, k), tw, g_sb, b_sb, "lngb")
        P.dma("pool", "S_" + xres, lambda e, s: e.dma_start(out=ov[:, :, t0:t0 + tw], in_=x[:, :, :tw]).then_inc(s, 16),
              reads=[(xres, k) for k in range(8)], writes=[(xres, "ld")])

    for (t0, tw, col) in tiles_own_ctx():
        do_tile(t0, tw, col)
    return cx


def build_C0b():
    cx = Ctx()
    P = cx.P
    NF = 22
    TW = 256
    xT = cx.inp("xT", [D, NT])
    mod_d = cx.inp("mod", [128, 96])
    wg_d = cx.inp("wg", [D, NF * 128])
    wu_d = cx.inp("wu", [D, NF * 128])
    wd_d = cx.inp("wd", [NF * 128, D])
    g_d = cx.inp("lng", [128, 8])
    b_d = cx.inp("lnb", [128, 8])
    xo = cx.out("xo", [D, NT])
    mod_sb = cx.sb("mod_sb", [128, 96])
    modp_sb = cx.sb("modp_sb", [128, 96])
    cx.load("sp", mod_sb[:], mod_d, "mod")
    P.op("dve", lambda e: e.tensor_scalar_add(out=modp_sb[:], in0=mod_sb[:], scalar1=1.0), reads=["mod"], writes=["modp"])
    g_sb = cx.sb("g_sb", [128, 8])
    b_sb = cx.sb("b_sb", [128, 8])
    cx.load("sp", g_sb[:], g_d, "lngb")
    cx.load("sp", b_sb[:], b_d, "lngb2")
    P.op("dve", lambda e: e.tensor_copy(out=b_sb[:], in_=b_sb[:]), reads=["lngb2"], writes=["lngb"])
    wg, wgres = load_cast_w(cx, "wg_sb", wg_d, 8, NF * 128)
    wu, wures = load_cast_w(cx, "wu_sb", wu_d, 8, NF * 128)
    wd, wdres = load_cast_w(cx, "wd_sb", wd_d, NF, D)
    ln = LNState(cx, "ln", TW)
    x_r = cx.ring("x", 1, [128, 8, TW])
    u_r = cx.ring("u", 1, [128, 8, TW], BF16)
    h_r = cx.ring("h", 1, [128, NF, TW], BF16)
    sg_r = cx.ring("sg", 2, [128, TW])
    pg_r = cx.psring("pg", 2, (128, TW))
    pu_r = cx.psring("pu", 2, (128, TW))
    pd_r = cx.psring("pd", 2, (128, TW))
    xv = xT.rearrange("(k p) t -> p k t", p=128)
    ov = xo.rearrange("(k p) t -> p k t", p=128)

    def do_tile(t0, tw, col):
        x, xres = x_r.nxt()
        cx.load("sp", x[:, :, :tw], xv[:, :, t0:t0 + tw], (xres, "ld"), reads=[(xres, k) for k in range(8)])
        u, ures = u_r.nxt()
        emit_modulate(cx, x, (xres, "ld"), u, ures, tw, col, mod_sb, modp_sb, 3)
        ureads = [(ures, k) for k in range(8)]
        h, hres = h_r.nxt()
        for fc in range(NF):
            pg, pgres = pg_r.nxt()
            pu, pures = pu_r.nxt()

            def mmg(e, pg=pg, fc=fc):
                ins = None
                for k in range(8):
                    ins = e.matmul(pg[:, :tw], wg[:, k, fc * 128:(fc + 1) * 128], u[:, k, :tw], start=(k == 0), stop=(k == 7))
                return ins

            def mmu(e, pu=pu, fc=fc):
                ins = None
                for k in range(8):
                    ins = e.matmul(pu[:, :tw], wu[:, k, fc * 128:(fc + 1) * 128], u[:, k, :tw], start=(k == 0), stop=(k == 7))
                return ins
            P.op("pe", mmg, reads=ureads + wgres, writes=[pgres])
            P.op("pe", mmu, reads=ureads + wures, writes=[pures])
            sg, sgres = sg_r.nxt()
            P.op("act", lambda e, sg=sg, pg=pg: e.activation(out=sg[:, :tw], in_=pg[:, :tw], func=AF.Silu), reads=[pgres], writes=[sgres])
            P.op("dve", lambda e, sg=sg, pu=pu, fc=fc: e.tensor_tensor(out=h[:, fc, :tw], in0=sg[:, :tw], in1=pu[:, :tw], op=ALU.mult),
                 reads=[sgres, pures], writes=[(hres, fc)])
        hreads = [(hres, fc) for fc in range(NF)]
        for oc in range(8):
            P.op("act", lambda e, oc=oc: e.activation(out=x[:, oc, :tw], in_=x[:, oc, :tw], func=AF.Identity, scale=ALPHA),
                 reads=[(xres, "ld")] + ureads, writes=[(xres, oc)])
            pd, pdres = pd_r.nxt()

            def mmd(e, pd=pd, oc=oc):
                ins = None
                for fc in range(NF):
                    ins = e.matmul(pd[:, :tw], wd[:, fc, oc * 128:(oc + 1) * 128], h[:, fc, :tw], start=(fc == 0), stop=(fc == NF - 1))
                return ins
            P.op("pe", mmd, reads=hreads + wdres, writes=[pdres])
            gate = mod_sb[:, 80 + 2 * oc + col:80 + 2 * oc + col + 1]
            P.op("dve", lambda e, pd=pd, oc=oc, gate=gate: e.scalar_tensor_tensor(out=x[:, oc, :tw], in0=pd[:, :tw], scalar=gate,
                                                                               in1=x[:, oc, :tw], op0=ALU.mult, op1=ALU.add),
                 reads=[pdres, (xres, oc), "mod"], writes=[(xres, oc)])
        emit_layernorm(cx, ln, x, lambda k: (xres, k), tw, g_sb, b_sb, "lngb")
        P.dma("pool", "S_" + xres, lambda e, s: e.dma_start(out=ov[:, :, t0:t0 + tw], in_=x[:, :, :tw]).then_inc(s, 16),
              reads=[(xres, k) for k in range(8)], writes=[(xres, "ld")])

    for (t0, tw, col) in tiles_own_ctx(TW):
        do_tile(t0, tw, col)
    return cx


POOL_W = (2, 4, 8, 16)


def pool_tables(j):
    rc = np.ones((256, NT), np.float32)
    for g, w in enumerate(POOL_W):
        lo = w // 2
        hi = w - lo
        t = np.arange(TOWN) + j * TOWN
        cnt = np.clip(t + hi, 0, SEQ) - np.clip(t - lo, 0, SEQ)
        rc[g * 64:(g + 1) * 64, :TOWN] = (1.0 / cnt.astype(np.float32))[None, :]
        t = np.arange(CTX)
        cnt = np.clip(t + hi, 0, CTX) - np.clip(t - lo, 0, CTX)
        rc[g * 64:(g + 1) * 64, TOWN:] = (1.0 / cnt.astype(np.float32))[None, :]
    return rc


def pool_halo(resA, c):
    b, j = divmod(c, 4)
    own = np.asarray(resA[c]["pT"])
    ph = np.zeros((256, NT + 32), np.float32)
    ph[:, 8:8 + TOWN] = own[:, :TOWN]
    if j > 0:
        ph[:, 0:8] = np.asarray(resA[c - 1]["pT"])[:, TOWN - 8:TOWN]
    if j < 3:
        ph[:, 8 + TOWN:16 + TOWN] = np.asarray(resA[c + 1]["pT"])[:, 0:8]
    ph[:, TOWN + 16 + 8:TOWN + 16 + 8 + CTX] = own[:, TOWN:]
    return ph


def wpool_blockdiag(w_pool):
    m = np.zeros((256, 128), np.float32)
    for g in range(4):
        pc, gl = divmod(g, 2)
        m[pc * 128 + gl * 64:pc * 128 + gl * 64 + 64, gl * 64:gl * 64 + 64] = w_pool[g]
    return m


def stage_C0a(inp, resA, resB):
    cx = build_Ca(True)
    maps = []
    for c in range(NCORES):
        maps.append(dict(
            xT=core_xT(inp["x"], inp["ctx"], c), aT=np.asarray(resB[c]["oT"]), mod=np.asarray(resA[c]["mod"]),
            wout=np.ascontiguousarray(inp["ev_w_out"][0]), lng=pcol(inp["ln1_g"][0]), lnb=pcol(inp["ln1_b"][0]),
            ph=pool_halo(resA, c), rcnt=pool_tables(c % 4), wpool=wpool_blockdiag(inp["ev_w_pool"][0]),
            pscale=pcol(inp["ev_pool_scale"][0])))
    return run_spmd(cx, maps)


def stage_C0b(inp, resA, resCa):
    cx = build_C0b()
    maps = []
    for c in range(NCORES):
        maps.append(dict(
            xT=np.asarray(resCa[c]["xo"]), mod=np.asarray(resA[c]["mod"]),
            wg=np.ascontiguousarray(inp["ev_w_gate"][0]), wu=np.ascontiguousarray(inp["ev_w_up"][0]),
            wd=np.ascontiguousarray(inp["ev_w_down"][0]), lng=pcol(inp["ln2_g"][0]), lnb=pcol(inp["ln2_b"][0])))
    return run_spmd(cx, maps)


def build_A1():
    cx = Ctx()
    P = cx.P
    xT = cx.inp("xT", [D, NT])
    cs_d = cx.inp("cs", [128, 16])
    wada_d = cx.inp("wada", [D, 6 * D])
    bada_d = cx.inp("bada", [128, 48])
    win_d = cx.inp("win", [D, 2208])
    gq_d = cx.inp("gql", [128, 3])
    gkv_d = cx.inp("gkvl", [128, 2])
    wqup_d = cx.inp("wqup", [384, 768])
    wkvk_d = cx.inp("wkvk", [256, 512])
    wkvv_d = cx.inp("wkvv", [256, 512])
    cos96_d = cx.inp("cos96", [96, NT])
    sin96_d = cx.inp("sin96", [96, NT])
    rot96_d = cx.inp("rot96", [96, 96])
    mod_o = cx.out("mod", [128, 96])
    qT_o = cx.out("qT", [768, NT], BF16)
    kT_o = cx.out("kT", [768, NT], BF16)
    v_o = cx.out("v", [NT, 512], BF16)
    nqT_o = cx.out("nqT", [512, NT], BF16)
    nkT_o = cx.out("nkT", [512, NT], BF16)
    nv_o = cx.out("nv", [NT, 512], BF16)

    pq_r = cx.psring("pq", 2)
    pms_r = cx.psring("pms", 1)
    pqh_r = cx.psring("pqh", 2)
    prot_r = cx.psring("prot", 1)
    pv_r = cx.psring("pv", 2)
    mod_sb = cx.sb("mod_sb", [128, 96])
    modp_sb = cx.sb("modp_sb", [128, 96])
    emit_ada(cx, cs_d, wada_d, bada_d, mod_sb, modp_sb, pm=pv_r.items[0][0], pmres=pv_r.items[0][1])
    cx.store("sp", mod_o, mod_sb[:], "mod")

    win, winres = load_cast_w(cx, "win_sb", win_d, 8, 2208)
    wqup, wqres = load_cast_w(cx, "wqup_sb", wqup_d, 3, 768)
    wkvk, wkres = load_cast_w(cx, "wkvk_sb", wkvk_d, 2, 512)
    wkvv, wvres = load_cast_w(cx, "wkvv_sb", wkvv_d, 2, 512)
    rot96 = cx.sb("rot96_sb", [96, 96], BF16)
    cx.load("pool", rot96[:], rot96_d, "rot96")
    cos96 = cx.sb("cos96_sb", [96, NT])
    sin96 = cx.sb("sin96_sb", [96, NT])
    cx.load("sp", cos96[:], cos96_d, "cos96")
    cx.load("sp", sin96[:], sin96_d, "sin96")
    gq = cx.sb("gq_sb", [128, 3])
    gkv = cx.sb("gkv_sb", [128, 2])
    cx.load("sp", gq[:], gq_d, "gq")
    cx.load("sp", gkv[:], gkv_d, "gkv")
    ones_q = cx.sb("ones_q", [128, 128], BF16)
    ones_kv = cx.sb("ones_kv", [128, 128], BF16)
    P.op("dve", lambda e: e.memset(ones_q[:], 1.0 / 384), writes=["ones_q"])
    P.op("dve", lambda e: e.memset(ones_kv[:], 1.0 / 256), writes=["ones_kv"])
    eps_sb = cx.sb("eps_sb", [128, 1])
    P.op("dve", lambda e: e.memset(eps_sb[:], EPS), writes=["eps"])

    xr = cx.ring("x", 2, [128, 8, 512])
    ur = cx.ring("u", 2, [128, 8, 512], BF16)
    lat_r = cx.ring("lat", 1, [128, 5, 512])
    latn_r = cx.ring("latn", 1, [128, 5, 512], BF16)
    sq_r = cx.ring("sq", 2, [128, 512], BF16)
    rs_r = cx.ring("rs", 2, [128, 512])
    qb_r = cx.ring("qb", 2, [96, 512], BF16)
    t1_r = cx.ring("t1", 2, [96, 512])
    t2_r = cx.ring("t2", 2, [96, 512])
    qf_r = cx.ring("qf", 3, [96, 512], BF16)
    ob_r = cx.ring("ob", 3, [128, 512], BF16)
    xv = xT.rearrange("(k p) t -> p k t", p=128)

    def do_tile(t0, tw, col):
        x, xres = xr.nxt()
        cx.load("sp", x[:, :, :tw], xv[:, :, t0:t0 + tw], xres)
        u, ures = ur.nxt()
        emit_modulate(cx, x, xres, u, ures, tw, col, mod_sb, modp_sb, 0)
        ureads = [(ures, k) for k in range(8)]
        lat, latres = lat_r.nxt()
        latn, latnres = latn_r.nxt()

        def proj(ps, c0, m, n0=0, n1=None):
            n1 = tw if n1 is None else n1

            def f(e):
                ins = None
                for k in range(8):
                    ins = e.matmul(ps[0:m, :n1 - n0], win[:, k, c0:c0 + m], u[:, k, n0:n1], start=(k == 0), stop=(k == 7))
                return ins
            return f
        for grp, (c_lo, nch, ones, oneres, gain, gres) in enumerate(((0, 3, ones_q, "ones_q", gq, "gq"),
                                                                       (384, 2, ones_kv, "ones_kv", gkv, "gkv"))):
            base = 0 if grp == 0 else 3
            pms, pmsres = pms_r.nxt()
            for c in range(nch):
                pq, pqres = pq_r.nxt()
                P.op("pe", proj(pq, c_lo + c * 128, 128), reads=ureads + winres, writes=[pqres])
                sq, sqres = sq_r.nxt()
                P.op("act", lambda e, sq=sq, pq=pq: e.activation(out=sq[:, :tw], in_=pq[:, :tw], func=AF.Square), reads=[pqres], writes=[sqres])
                P.op("dve", lambda e, pq=pq, c=c, base=base: e.tensor_copy(out=lat[:, base + c, :tw], in_=pq[:, :tw]),
                     reads=[pqres], writes=[(latres, base + c)])
                P.op("pe", lambda e, pms=pms, sq=sq, c=c, nch=nch, ones=ones: e.matmul(pms[:, :tw], ones[:], sq[:, :tw], start=(c == 0), stop=(c == nch - 1)),
                     reads=[sqres, oneres], writes=[pmsres])
            rs, rsres = rs_r.nxt()
            P.op("act", lambda e, rs=rs, pms=pms: e.activation(out=rs[:, :tw], in_=pms[:, :tw], func=AF.Sqrt, bias=eps_sb[:, 0:1]),
                 reads=[pmsres, "eps"], writes=[rsres])
            P.op("dve", lambda e, rs=rs: e.reciprocal(out=rs[:, :tw], in_=rs[:, :tw]), reads=[rsres], writes=[rsres])
            for c in range(nch):
                P.op("dve", lambda e, c=c, base=base, rs=rs, gain=gain: e.scalar_tensor_tensor(
                    out=latn[:, base + c, :tw], in0=lat[:, base + c, :tw], scalar=gain[:, c:c + 1], in1=rs[:, :tw], op0=ALU.mult, op1=ALU.mult),
                    reads=[(latres, base + c), rsres, gres], writes=[(latnres, base + c)])
        cqn_reads = [(latnres, c) for c in range(3)]
        ckvn_reads = [(latnres, 3 + c) for c in range(2)]
        def head_rope(pqh, pqres, nrows_lo, stores):
            qb, qbres = qb_r.nxt()
            P.op("dve", lambda e: e.tensor_copy(out=qb[:, :tw], in_=pqh[0:96, :tw]), reads=[pqres], writes=[qbres])
            prot, protres = prot_r.nxt()
            P.op("pe", lambda e: e.matmul(prot[0:96, :tw], rot96[:], qb[:, :tw], start=True, stop=True), reads=[qbres, "rot96"], writes=[protres])
            t1, t1res = t1_r.nxt()
            P.op("dve", lambda e: e.tensor_tensor(out=t1[:, :tw], in0=pqh[0:96, :tw], in1=cos96[:, t0:t0 + tw], op=ALU.mult),
                 reads=[pqres, "cos96"], writes=[t1res])
            t2, t2res = t2_r.nxt()
            P.op("dve", lambda e: e.tensor_tensor(out=t2[:, :tw], in0=prot[0:96, :tw], in1=sin96[:, t0:t0 + tw], op=ALU.mult),
                 reads=[protres, "sin96"], writes=[t2res])
            qf, qfres = qf_r.nxt()
            P.op("dve", lambda e: e.tensor_tensor(out=qf[:, :tw], in0=t1[:, :tw], in1=t2[:, :tw], op=ALU.add), reads=[t1res, t2res], writes=[qfres])
            for (dst, lo, hi) in stores:
                cx.store("sp", dst, qf[lo:hi, :tw], qfres)

        pkr, pkrres = pqh_r.nxt()

        def mmkr(e):
            ins = None
            for k in range(8):
                ins = e.matmul(pkr[64:96, :tw], win[:, k, 640:672], u[:, k, :tw], start=(k == 0), stop=(k == 7))
            return ins
        P.op("dve", lambda e: e.memset(pkr[0:64, :tw], 0.0), writes=[pkrres])
        P.op("pe", mmkr, reads=ureads + winres, writes=[pkrres])
        head_rope(pkr, pkrres, 64, [(kT_o[h * 96 + 64:h * 96 + 96, t0:t0 + tw], 64, 96) for h in range(8)])
        for h in range(8):
            pqh, pqres = pqh_r.nxt()

            def mmq(e, pqh=pqh, h=h):
                ins = None
                for k in range(3):
                    ins = e.matmul(pqh[0:96, :tw], wqup[:, k, h * 96:(h + 1) * 96], latn[:, k, :tw], start=(k == 0), stop=(k == 2))
                return ins
            P.op("pe", mmq, reads=cqn_reads + wqres, writes=[pqres])
            head_rope(pqh, pqres, 0, [(qT_o[h * 96:(h + 1) * 96, t0:t0 + tw], 0, 96)])
        for hp in range(4):
            pq, pqres = pq_r.nxt()

            def mmk(e, pq=pq, hp=hp):
                ins = None
                for k in range(2):
                    ins = e.matmul(pq[:, :tw], wkvk[:, k, hp * 128:(hp + 1) * 128], latn[:, 3 + k, :tw], start=(k == 0), stop=(k == 1))
                return ins
            P.op("pe", mmk, reads=ckvn_reads + wkres, writes=[pqres])
            ob, obres = ob_r.nxt()
            P.op("act", lambda e, ob=ob, pq=pq: e.activation(out=ob[:, :tw], in_=pq[:, :tw], func=AF.Copy), reads=[pqres], writes=[obres])
            for hh in range(2):
                h = 2 * hp + hh
                cx.store("pool", kT_o[h * 96:h * 96 + 64, t0:t0 + tw], ob[hh * 64:(hh + 1) * 64, :tw], obres)
        for (dst, c_lo) in ((nqT_o, 672), (nkT_o, 1184)):
            for c in range(4):
                pq, pqres = pq_r.nxt()
                P.op("pe", proj(pq, c_lo + c * 128, 128), reads=ureads + winres, writes=[pqres])
                ob, obres = ob_r.nxt()
                P.op("act", lambda e, ob=ob, pq=pq: e.activation(out=ob[:, :tw], in_=pq[:, :tw], func=AF.Copy), reads=[pqres], writes=[obres])
                cx.store("pool", dst[c * 128:(c + 1) * 128, t0:t0 + tw], ob[:, :tw], obres)
        for tb in range(tw // 128):
            pv, pvres = pv_r.nxt()

            def mmv(e, pv=pv, tb=tb):
                ins = None
                for k in range(2):
                    ins = e.matmul(pv[:, :512], latn[:, 3 + k, tb * 128:(tb + 1) * 128], wkvv[:, k, :], start=(k == 0), stop=(k == 1))
                return ins
            P.op("pe", mmv, reads=ckvn_reads + wvres, writes=[pvres])
            ob, obres = ob_r.nxt()
            P.op("act", lambda e, ob=ob, pv=pv: e.activation(out=ob[:, :512], in_=pv[:, :512], func=AF.Copy), reads=[pvres], writes=[obres])
            cx.store("pool", v_o[t0 + tb * 128:t0 + (tb + 1) * 128, :], ob[:, :512], obres)
            pv, pvres = pv_r.nxt()

            def mmnv(e, pv=pv, tb=tb):
                ins = None
                for k in range(8):
                    ins = e.matmul(pv[:, :512], u[:, k, tb * 128:(tb + 1) * 128], win[:, k, 1696:2208], start=(k == 0), stop=(k == 7))
                return ins
            P.op("pe", mmnv, reads=ureads + winres, writes=[pvres])
            ob, obres = ob_r.nxt()
            P.op("act", lambda e, ob=ob, pv=pv: e.activation(out=ob[:, :512], in_=pv[:, :512], func=AF.Copy), reads=[pvres], writes=[obres])
            cx.store("pool", nv_o[t0 + tb * 128:t0 + (tb + 1) * 128, :], ob[:, :512], obres)

    for (t0, tw, col) in tiles_own_ctx():
        do_tile(t0, tw, col)
    return cx


def rope_tables32(j):
    t = np.arange(TOWN) + j * TOWN
    row = (t // GRID_W).astype(np.float32)
    colp = (t % GRID_W).astype(np.float32)
    inv = (10000.0 ** (-np.arange(8, dtype=np.float64) / 8)).astype(np.float32)
    cos = np.ones((96, NT), np.float32)
    sin = np.zeros((96, NT), np.float32)
    for d in range(32):
        pos = row if d < 16 else colp
        ang = (pos * inv[d % 8]).astype(np.float32)
        cos[64 + d, :TOWN] = np.cos(ang)
        sin[64 + d, :TOWN] = np.sin(ang)
    return cos, sin


def rot96():
    m = np.zeros((96, 96), np.float32)
    m[64:, 64:] = rot_matrix(32, 16)
    return m


def stage_A1(inp, xT_list):
    cx = build_A1()
    wkv = inp["od_w_kv_up"][0].reshape(256, 8, 128)
    maps = []
    for c in range(NCORES):
        cos, sin = rope_tables32(c % 4)
        maps.append(dict(
            xT=xT_list[c], cs=core_cs(inp["c"], inp["c_ctx"], c),
            wada=np.ascontiguousarray(inp["w_ada"][1]), bada=pcol(inp["b_ada"][1]),
            win=np.ascontiguousarray(inp["od_w_in"][0]),
            gql=pcol(inp["od_q_lat_gain"][0]), gkvl=pcol(inp["od_kv_lat_gain"][0]),
            wqup=np.ascontiguousarray(inp["od_w_q_up"][0]),
            wkvk=np.ascontiguousarray(wkv[:, :, :64].reshape(256, 512)),
            wkvv=np.ascontiguousarray(wkv[:, :, 64:].reshape(256, 512)),
            cos96=cos, sin96=sin, rot96=rot96()))
    return run_spmd(cx, maps)


NA_SCALE = 0.125


def build_NA():
    cx = Ctx()
    P = cx.P
    nq_d = cx.inp("nqT", [512, TOWN], BF16)
    nk_d = cx.inp("nkTh", [512, TOWN + 512], BF16)
    nv_d = cx.inp("nvh", [128, 36 * 8 * 65], BF16)
    kc_d = cx.inp("nkc", [512, CTX], BF16)
    vc_d = cx.inp("nvc", [128, 2 * 8 * 65], BF16)
    bm_d = {n: cx.inp(n, [128, 48 * 256]) for n in ("bm_int", "bm_first", "bm_last")}
    id_d = cx.inp("ident", [128, 128])
    sel_d = cx.inp("sel", [65, 64])
    o_d = cx.out("naT", [512, TOWN], BF16)

    kh = cx.sb("kh", [128, 4, TOWN + 512], BF16)
    qs = cx.sb("qs", [128, 4, TOWN], BF16)
    kcs = cx.sb("kcs", [128, 4, CTX], BF16)
    vb = cx.sb("vb", [128, 36, 8, 65], BF16)
    vc = cx.sb("vc", [128, 2, 8, 65], BF16)
    cx.load("sp", kh[:], nk_d.rearrange("(c p) t -> p c t", p=128), "kh")
    cx.load("sp", qs[:], nq_d.rearrange("(c p) t -> p c t", p=128), "qs")
    cx.load("sp", kcs[:], kc_d.rearrange("(c p) t -> p c t", p=128), "kcs")
    cx.load("sp", vb[:].rearrange("p a h d -> p (a h d)"), nv_d, "vb")
    cx.load("sp", vc[:].rearrange("p a h d -> p (a h d)"), vc_d, "vc")
    ident = cx.sb("ident_sb", [128, 128], BF16)
    cx.load("pool", ident[:], id_d, "ident")
    sel = cx.sb("sel_sb", [65, 64])
    cx.load("sp", sel[:], sel_d, "sel")
    bmi = cx.sb("bmi", [128, 48, 256], BF16)
    bme = cx.sb("bme", [128, 48, 256], BF16)
    stg_r = cx.ring("stg", 2, [128, 6, 256])

    def load_table(dst, dres, name):
        src = bm_d[name].rearrange("p (a q) -> p a q", q=256)
        for h in range(8):
            stg, sres = stg_r.nxt()
            cx.load("sp", stg[:], src[:, h * 6:(h + 1) * 6, :], sres)
            P.op("dve", lambda e, stg=stg, h=h: e.tensor_scalar_mul(out=dst[:, h * 6:(h + 1) * 6, :], in0=stg[:], scalar1=1.0 / NA_SCALE),
                 reads=[sres], writes=[(dres, h)])
    load_table(bmi, "bmi", "bm_int")
    load_table(bme, "bme", "bm_first")

    ps_r = cx.psring("ps", 4)
    po_r = cx.psring("po", 2)
    pb_r = cx.psring("pb", 1)
    pt_r = cx.ring("pt", 2, [128, 8, 256], BF16)
    os_r = cx.ring("os", 2, [65, 256])
    at_r = cx.ring("at", 2, [64, 256], BF16)

    def unit(m, h):
        hp, pb0 = h // 2, (h % 2) * 64
        tab, tres = (bme, "bme") if m in (0, 15) else (bmi, "bmi")
        pt, ptres = pt_r.nxt()
        q0 = 256 * m
        for bk in range(4):
            ps, psres = ps_r.nxt()

            def mm(e, ps=ps, bk=bk):
                ins = None
                for cc in range(2):
                    c = 2 * bk + cc
                    o = cc * 256
                    if c < 6:
                        e.matmul(ps[:, o:o + 256], kh[pb0:pb0 + 64, hp, q0 + c * 128:q0 + (c + 1) * 128], qs[pb0:pb0 + 64, hp, q0:q0 + 256],
                                 start=True, stop=False)
                        ins = e.matmul(ps[:, o:o + 256], ident[:], tab[:, h * 6 + c, :], start=False, stop=True)
                    else:
                        ins = e.matmul(ps[:, o:o + 256], kcs[pb0:pb0 + 64, hp, (c - 6) * 128:(c - 5) * 128], qs[pb0:pb0 + 64, hp, q0:q0 + 256],
                                       start=True, stop=True)
                return ins
            P.op("pe", mm, reads=["kh", "qs", "kcs", "ident", (tres, h)], writes=[psres])
            P.op("act", lambda e, ps=ps, bk=bk: e.activation(out=pt[:, 2 * bk:2 * bk + 2, :].rearrange("p a q -> p (a q)"), in_=ps[:, :],
                                                            func=AF.Exp, scale=NA_SCALE),
                 reads=[psres], writes=[(ptres, bk)])
        po, pores = po_r.nxt()

        def pv(e):
            ins = None
            for c in range(8):
                lhs = vb[:, 2 * m + c, h, :] if c < 6 else vc[:, c - 6, h, :]
                ins = e.matmul(po[0:65, :256], lhs, pt[:, c, :], start=(c == 0), stop=(c == 7))
            return ins
        P.op("pe", pv, reads=[(ptres, bk) for bk in range(4)] + ["vb", "vc"], writes=[pores])
        osb, osres = os_r.nxt()
        P.op("act", lambda e: e.activation(out=osb[:, :], in_=po[0:65, :256], func=AF.Copy), reads=[pores], writes=[osres])
        P.op("dve", lambda e: e.reciprocal(out=osb[64:65, :], in_=osb[64:65, :]), reads=[osres], writes=[osres])
        pbk, pbres = pb_r.nxt()
        P.op("pe", lambda e: e.matmul(pbk[0:64, :256], sel[:], osb[:, :], start=True, stop=True), reads=[osres, "sel"], writes=[pbres])
        at, atres = at_r.nxt()
        P.op("dve", lambda e: e.tensor_tensor(out=at[:, :], in0=osb[0:64, :], in1=pbk[0:64, :256], op=ALU.mult), reads=[osres, pbres], writes=[atres])
        cx.store("pool", o_d[h * 64:(h + 1) * 64, q0:q0 + 256], at[:, :], atres)

    for m in range(16):
        if m == 15:
            load_table(bme, "bme", "bm_last")
        for h in range(8):
            unit(m, h)
    return cx


def na_table(bias, R0):
    kr_rel = np.arange(12)
    kr = R0 - 4 + kr_rel
    qr = R0 + np.arange(4)
    rs = np.clip(qr - 4, 0, 256 - 8)
    row_ok = (kr[:, None] >= rs[None, :]) & (kr[:, None] < rs[None, :] + 8)
    row_idx = np.clip(kr[:, None] - qr[None, :] + 7, 0, 14)
    kc = np.arange(64)
    qc = np.arange(64)
    cs0 = np.clip(qc - 8, 0, 48)
    col_ok = (kc[:, None] >= cs0[None, :]) & (kc[:, None] < cs0[None, :] + 16)
    col_idx = np.clip(kc[:, None] - qc[None, :] + 15, 0, 30)
    ok = row_ok[:, None, :, None] & col_ok[None, :, None, :]
    g = bias[:, row_idx[:, None, :, None], col_idx[None, :, None, :]]
    t = np.where(ok[None], g, np.float32(-30000.0)).astype(np.float32)
    t = t.reshape(8, 6, 2, 64, 256)
    t = t.transpose(2, 3, 0, 1, 4).reshape(128, 48 * 256)
    return np.ascontiguousarray(t)


def with_ones(v):
    o = np.ones(v.shape[:-1] + (65,), v.dtype)
    o[..., :64] = v
    return o


def stage_NA(inp, resA1):
    cx = build_NA()
    bias = inp["od_na_bias"][0]
    t_int = na_table(bias, 8)
    t_first = na_table(bias, 0)
    t_last = na_table(bias, 252)
    maps = []
    for c in range(NCORES):
        b, j = divmod(c, 4)
        nk = np.asarray(resA1[c]["nkT"])
        nv = np.asarray(resA1[c]["nv"])
        kh = np.zeros((512, TOWN + 512), NPBF)
        vh = np.zeros((TOWN + 512, 512), NPBF)
        kh[:, 256:256 + TOWN] = nk[:, :TOWN]
        vh[256:256 + TOWN] = nv[:TOWN]
        if j > 0:
            kh[:, :256] = np.asarray(resA1[c - 1]["nkT"])[:, TOWN - 256:TOWN]
            vh[:256] = np.asarray(resA1[c - 1]["nv"])[TOWN - 256:TOWN]
        if j < 3:
            kh[:, 256 + TOWN:] = np.asarray(resA1[c + 1]["nkT"])[:, :256]
            vh[256 + TOWN:] = np.asarray(resA1[c + 1]["nv"])[:256]
        vhl = with_ones(vh.reshape(36, 128, 8, 64).transpose(1, 0, 2, 3)).reshape(128, -1)
        vcl = with_ones(nv[TOWN:].reshape(2, 128, 8, 64).transpose(1, 0, 2, 3)).reshape(128, -1)
        maps.append(dict(
            nqT=np.ascontiguousarray(np.asarray(resA1[c]["nqT"])[:, :TOWN]), nkTh=kh, nvh=np.ascontiguousarray(vhl),
            nkc=np.ascontiguousarray(nk[:, TOWN:]), nvc=np.ascontiguousarray(vcl),
            bm_int=t_int, bm_first=(t_first if j == 0 else t_int), bm_last=(t_last if j == 3 else t_int),
            ident=np.eye(128, dtype=np.float32), sel=SEL))
    return run_spmd(cx, maps)


def stage_B1(resA1):
    cx = build_attn(8, 1, 96, 130, 96 ** -0.5)
    kv = gather_kv(resA1, "kT", "v", 8, 96)
    maps = []
    for c in range(NCORES):
        kf, vf = kv[c // 4]
        maps.append(dict(qT=np.asarray(resA1[c]["qT"]), kT=kf, v=vf, sel=SEL))
    return run_spmd(cx, maps)


def build_C1b():
    cx = Ctx()
    P = cx.P
    NE, NFE, FG = 8, 28, 4
    NG = NFE // FG
    ST, SUB = 1024, 256
    xT = cx.inp("xT", [D, NT])
    mod_d = cx.inp("mod", [128, 96])
    wr_d = cx.inp("wr", [D, NE])
    br_d = cx.inp("br", [NE, 1])
    wg_d = cx.inp("wg", [NE, D, NFE * 128])
    wu_d = cx.inp("wu", [NE, D, NFE * 128])
    wd_d = cx.inp("wd", [NE, NFE * 128, D])
    g_d = cx.inp("lng", [128, 8])
    b_d = cx.inp("lnb", [128, 8])
    id_d = cx.inp("ident", [128, 128])
    oh_d = cx.inp("onehot", [NE, NE * 128])
    xo = cx.out("xo", [D, TOWN])

    mod_sb = cx.sb("mod_sb", [128, 96])
    modp_sb = cx.sb("modp_sb", [128, 96])
    cx.load("sp", mod_sb[:], mod_d, "mod")
    P.op("dve", lambda e: e.tensor_scalar_add(out=modp_sb[:], in0=mod_sb[:], scalar1=1.0), reads=["mod"], writes=["modp"], fence=True)
    g_sb = cx.sb("g_sb", [128, 8])
    b_sb = cx.sb("b_sb", [128, 8])
    cx.load("sp", g_sb[:], g_d, "lngb")
    cx.load("sp", b_sb[:], b_d, "lngb2")
    P.op("dve", lambda e: e.tensor_copy(out=b_sb[:], in_=b_sb[:]), reads=["lngb2"], writes=["lngb"], fence=True)
    wr = cx.sb("wr_sb", [128, 8, NE])
    cx.load("sp", wr[:], wr_d.rearrange("(k p) e -> p k e", p=128), "wr")
    br = cx.sb("br_sb", [NE, 1])
    cx.load("sp", br[:], br_d, "br")
    ident = cx.sb("ident_sb", [128, 128])
    cx.load("sp", ident[:], id_d, "ident")
    oneh = cx.sb("oneh_sb", [NE, NE * 128])
    cx.load("sp", oneh[:], oh_d, "oneh")
    ln = LNState(cx, "ln", SUB)

    yacc = cx.sb("yacc", [128, 8, ST])
    u2 = cx.sb("u2", [128, 8, ST], BF16)
    gb = cx.sb("gb", [128, NE, ST], BF16)
    gT = cx.sb("gT", [NE, ST])
    x_r = cx.ring("x", 2, [128, 8, SUB])
    uf_r = cx.ring("uf", 1, [128, 8, SUB])
    lg_r = cx.ring("lg", 1, [NE, SUB])
    lgT_r = cx.ring("lgT", 2, [128, 8])
    mx_r = cx.ring("mx", 2, [128, 8])
    dm_r = cx.ring("dm", 2, [128, 2])
    ga_r = cx.ring("ga", 2, [128, 8])
    gq_r = cx.ring("gq", 2, [128, 8])
    wgq_r = cx.ring("wgq", 2, [128, 8, FG * 128], BF16)
    wuq_r = cx.ring("wuq", 2, [128, 8, FG * 128], BF16)
    wdq_r = cx.ring("wdq", 2, [128, FG, D], BF16)
    sg_r = cx.ring("sg", 2, [128, SUB])
    h1_r = cx.ring("h1", 2, [128, SUB])
    hq_r = cx.ring("hq", 2, [128, FG, SUB], BF16)
    pg_r = cx.psring("pg", 2)
    pu_r = cx.psring("pu", 2)
    pd_r = cx.psring("pd", 2)
    xv = xT.rearrange("(k p) t -> p k t", p=128)
    ov = xo.rearrange("(k p) t -> p k t", p=128)

    def router(s0, st):
        t0 = s0 + st * SUB
        x, xres = x_r.nxt()
        cx.load("sp", x[:, :, :], xv[:, :, t0:t0 + SUB], (xres, "ld"), reads=[(xres, k) for k in range(8)])
        uf, ufres = uf_r.nxt()
        for k in range(8):
            sc = modp_sb[:, 64 + 2 * k:64 + 2 * k + 1]
            sh = mod_sb[:, 48 + 2 * k:48 + 2 * k + 1]
            if k % 2 == 0:
                P.op("act", lambda e, k=k, sc=sc, sh=sh: e.activation(out=uf[:, k, :], in_=x[:, k, :], func=AF.Identity, bias=sh, scale=sc),
                     reads=[(xres, "ld"), "mod", "modp"], writes=[(ufres, k)])
            else:
                P.op("dve", lambda e, k=k, sc=sc, sh=sh: e.tensor_scalar(out=uf[:, k, :], in0=x[:, k, :], scalar1=sc, scalar2=sh,
                                                                        op0=ALU.mult, op1=ALU.add),
                     reads=[(xres, "ld"), "mod", "modp"], writes=[(ufres, k)])
        ufreads = [(ufres, k) for k in range(8)]
        P.op("dve", lambda e: e.tensor_copy(out=u2[:, :, st * SUB:(st + 1) * SUB], in_=uf[:, :, :]), reads=ufreads, writes=[("u2", st)])
        plg, plgres = pg_r.nxt()

        def mml(e):
            ins = None
            for k in range(8):
                ins = e.matmul(plg[0:NE, :SUB], wr[:, k, :], uf[:, k, :], start=(k == 0), stop=(k == 7))
            return ins
        P.op("pe", mml, reads=ufreads + ["wr"], writes=[plgres])
        lg, lgres = lg_r.nxt()
        P.op("act", lambda e: e.activation(out=lg[:, :], in_=plg[0:NE, :SUB], func=AF.Identity, bias=br[:, 0:1]), reads=[plgres, "br"], writes=[lgres])
        for tb in range(SUB // 128):
            pt, ptres = pu_r.nxt()
            P.op("pe", lambda e, pt=pt, tb=tb: e.transpose(pt[:, 0:NE], lg[:, tb * 128:(tb + 1) * 128], ident[0:NE, 0:NE]),
                 reads=[lgres, "ident"], writes=[ptres])
            lgT, lgTres = lgT_r.nxt()
            mx, mxres = mx_r.nxt()
            dm, dmres = dm_r.nxt()
            ga, gares = ga_r.nxt()
            gq, gqres = gq_r.nxt()
            P.op("dve", lambda e, pt=pt, lgT=lgT: e.tensor_copy(out=lgT[:], in_=pt[:, 0:NE]), reads=[ptres], writes=[lgTres], fence=True)
            P.op("dve", lambda e, mx=mx, lgT=lgT: e.max(out=mx[:], in_=lgT[:]), reads=[lgTres], writes=[mxres], fence=True)
            P.op("dve", lambda e, mx=mx, dm=dm: e.tensor_tensor(out=dm[:, 0:1], in0=mx[:, 1:2], in1=mx[:, 0:1], op=ALU.subtract),
                 reads=[mxres], writes=[(dmres, 0)], fence=True)
            P.op("act", lambda e, dm=dm: e.activation(out=dm[:, 1:2], in_=dm[:, 0:1], func=AF.Sigmoid), reads=[(dmres, 0)], writes=[(dmres, 1)], fence=True)
            P.op("dve", lambda e, dm=dm: e.tensor_scalar(out=dm[:, 0:1], in0=dm[:, 1:2], scalar1=-1.0, scalar2=1.0, op0=ALU.mult, op1=ALU.add),
                 reads=[(dmres, 1)], writes=[(dmres, 0)], fence=True)
            P.op("dve", lambda e, ga=ga, lgT=lgT, mx=mx, dm=dm: e.tensor_scalar(out=ga[:], in0=lgT[:], scalar1=mx[:, 0:1], scalar2=dm[:, 0:1],
                                                                             op0=ALU.is_equal, op1=ALU.mult),
                 reads=[lgTres, mxres, (dmres, 0)], writes=[gares], fence=True)
            P.op("dve", lambda e, gq=gq, lgT=lgT, mx=mx, dm=dm: e.tensor_scalar(out=gq[:], in0=lgT[:], scalar1=mx[:, 1:2], scalar2=dm[:, 1:2],
                                                                             op0=ALU.is_equal, op1=ALU.mult),
                 reads=[lgTres, mxres, (dmres, 1)], writes=[gqres], fence=True)
            P.op("dve", lambda e, ga=ga, gq=gq: e.tensor_tensor(out=ga[:], in0=ga[:], in1=gq[:], op=ALU.add), reads=[gares, gqres], writes=[gares], fence=True)
            pg2, pg2res = pu_r.nxt()
            P.op("pe", lambda e, pg2=pg2, ga=ga: e.transpose(pg2[0:NE, 0:128], ga[:, :], ident[:, :]), reads=[gares, "ident"], writes=[pg2res])
            c0 = st * SUB + tb * 128
            P.op("dve", lambda e, pg2=pg2, c0=c0: e.tensor_copy(out=gT[:, c0:c0 + 128], in_=pg2[0:NE, 0:128]), reads=[pg2res], writes=[("gT", c0)], fence=True)

    def gate_bcast():
        for e_ in range(NE):
            for hf in range(ST // 512):
                pgb, pgbres = pg_r.nxt()
                P.op("pe", lambda e, pgb=pgb, e_=e_, hf=hf: e.matmul(pgb[:, :512], oneh[:, e_ * 128:(e_ + 1) * 128], gT[:, hf * 512:(hf + 1) * 512],
                                                                    start=True, stop=True),
                     reads=[("gT", c0) for c0 in range(hf * 512, (hf + 1) * 512, 128)] + ["oneh"], writes=[pgbres])
                P.op("act", lambda e, pgb=pgb, e_=e_, hf=hf: e.activation(out=gb[:, e_, hf * 512:(hf + 1) * 512], in_=pgb[:, :512], func=AF.Copy),
                     reads=[pgbres], writes=[("gb", e_, hf)])

    def load_w(e_, g):
        wgq, wgres = wgq_r.nxt()
        wuq, wures = wuq_r.nxt()
        wdq, wdres = wdq_r.nxt()
        c0 = g * FG * 128
        cx.load("pool", wgq[:], wg_d[e_, :, c0:c0 + FG * 128].rearrange("(k p) c -> p k c", p=128), wgres)
        cx.load("pool", wuq[:], wu_d[e_, :, c0:c0 + FG * 128].rearrange("(k p) c -> p k c", p=128), wures)
        cx.load("pool", wdq[:], wd_d[e_, c0:c0 + FG * 128, :].rearrange("(f p) c -> p f c", p=128), wdres)
        return (wgq, wgres, wuq, wures, wdq, wdres)

    def expert_unit(e_, g, W, first):
        wgq, wgres, wuq, wures, wdq, wdres = W

        def sub(st):
            ureads = [("u2", st)]
            hq, hqres = hq_r.nxt()
            for f in range(FG):
                pg, pgres = pg_r.nxt()
                pu, pures = pu_r.nxt()

                def mmg(e, pg=pg, f=f):
                    ins = None
                    for k in range(8):
                        ins = e.matmul(pg[:, :SUB], wgq[:, k, f * 128:(f + 1) * 128], u2[:, k, st * SUB:(st + 1) * SUB], start=(k == 0), stop=(k == 7))
                    return ins

                def mmu(e, pu=pu, f=f):
                    ins = None
                    for k in range(8):
                        ins = e.matmul(pu[:, :SUB], wuq[:, k, f * 128:(f + 1) * 128], u2[:, k, st * SUB:(st + 1) * SUB], start=(k == 0), stop=(k == 7))
                    return ins
                P.op("pe", mmg, reads=ureads + [wgres], writes=[pgres])
                P.op("pe", mmu, reads=ureads + [wures], writes=[pures])
                sg, sgres = sg_r.nxt()
                P.op("act", lambda e, sg=sg, pg=pg: e.activation(out=sg[:, :], in_=pg[:, :SUB], func=AF.Silu), reads=[pgres], writes=[sgres])
                h1, h1res = h1_r.nxt()
                P.op("dve", lambda e, sg=sg, pu=pu, h1=h1: e.tensor_tensor(out=h1[:, :], in0=sg[:, :], in1=pu[:, :SUB], op=ALU.mult),
                     reads=[sgres, pures], writes=[h1res])
                P.op("dve", lambda e, h1=h1, f=f: e.tensor_tensor(out=hq[:, f, :], in0=h1[:, :], in1=gb[:, e_, st * SUB:(st + 1) * SUB], op=ALU.mult),
                     reads=[h1res, ("gb", e_, (st * SUB) // 512)], writes=[(hqres, f)])
            hreads = [(hqres, f) for f in range(FG)]
            for oc in range(8):
                pd, pdres = pd_r.nxt()

                def mmd(e, pd=pd, oc=oc):
                    ins = None
                    for f in range(FG):
                        ins = e.matmul(pd[:, :SUB], wdq[:, f, oc * 128:(oc + 1) * 128], hq[:, f, :], start=(f == 0), stop=(f == FG - 1))
                    return ins
                P.op("pe", mmd, reads=hreads + [wdres], writes=[pdres])
                ya = yacc[:, oc, st * SUB:(st + 1) * SUB]
                if first:
                    P.op("dve", lambda e, pd=pd, ya=ya: e.tensor_copy(out=ya, in_=pd[:, :SUB]), reads=[pdres], writes=[("yacc", oc, st)])
                else:
                    P.op("dve", lambda e, pd=pd, ya=ya: e.tensor_tensor(out=ya, in0=pd[:, :SUB], in1=ya, op=ALU.add),
                         reads=[pdres, ("yacc", oc, st)], writes=[("yacc", oc, st)])

        for st in range(ST // SUB):
            sub(st)

    def finish(s0, st):
        t0 = s0 + st * SUB
        x, xres = x_r.nxt()
        cx.load("sp", x[:, :, :], xv[:, :, t0:t0 + SUB], (xres, "ld"), reads=[(xres, k) for k in range(8)])
        for oc in range(8):
            P.op("act", lambda e, oc=oc: e.activation(out=x[:, oc, :], in_=x[:, oc, :], func=AF.Identity, scale=ALPHA),
                 reads=[(xres, "ld")], writes=[(xres, oc)])
            gate = mod_sb[:, 80 + 2 * oc:80 + 2 * oc + 1]
            P.op("dve", lambda e, oc=oc, gate=gate: e.scalar_tensor_tensor(out=x[:, oc, :], in0=yacc[:, oc, st * SUB:(st + 1) * SUB], scalar=gate,
                                                                        in1=x[:, oc, :], op0=ALU.mult, op1=ALU.add),
                 reads=[("yacc", oc, st), (xres, oc), "mod"], writes=[(xres, oc)])
        emit_layernorm(cx, ln, x, lambda k: (xres, k), SUB, g_sb, b_sb, "lngb")
        P.dma("sp", "S_" + xres, lambda e, s: e.dma_start(out=ov[:, :, t0:t0 + SUB], in_=x[:, :, :]).then_inc(s, 16),
              reads=[(xres, k) for k in range(8)], writes=[(xres, "ld")])

    units = [(e_, g) for e_ in range(NE) for g in range(NG)]
    for s0 in range(0, TOWN, ST):
        W = load_w(*units[0])
        for st in range(ST // SUB):
            router(s0, st)
        gate_bcast()
        for i, (e_, g) in enumerate(units):
            Wn = load_w(*units[i + 1]) if i + 1 < len(units) else None
            expert_unit(e_, g, W, first=(i == 0))
            W = Wn
        for st in range(ST // SUB):
            finish(s0, st)
    return cx


def stage_C1a(inp, resA1, resB1, resNA, xT_list):
    cx = build_Ca(False)
    maps = []
    for c in range(NCORES):
        aT = np.zeros((1024, NT), NPBF)
        aT[:512] = np.asarray(resB1[c]["oT"])
        aT[512:, :TOWN] = np.asarray(resNA[c]["naT"])
        maps.append(dict(xT=xT_list[c], aT=aT, mod=np.asarray(resA1[c]["mod"]),
                         wout=np.ascontiguousarray(inp["od_w_out"][0]), lng=pcol(inp["ln1_g"][1]), lnb=pcol(inp["ln1_b"][1])))
    return run_spmd(cx, maps)


def stage_C1b(inp, resA1, resC1a):
    cx = build_C1b()
    oh = np.zeros((8, 8 * 128), np.float32)
    for e in range(8):
        oh[e, e * 128:(e + 1) * 128] = 1.0
    shared = dict(wr=np.ascontiguousarray(inp["od_w_router"][0]), br=np.ascontiguousarray(inp["od_b_router"][0][:, None]),
                  wg=np.ascontiguousarray(inp["od_w_gate"][0]), wu=np.ascontiguousarray(inp["od_w_up"][0]),
                  wd=np.ascontiguousarray(inp["od_w_down"][0]), lng=pcol(inp["ln2_g"][1]), lnb=pcol(inp["ln2_b"][1]),
                  ident=np.eye(128, dtype=np.float32), onehot=oh)
    maps = []
    for c in range(NCORES):
        m = dict(shared)
        m.update(xT=np.asarray(resC1a[c]["xo"]), mod=np.asarray(resA1[c]["mod"]))
        maps.append(m)
    return run_spmd(cx, maps)


def kernel(**inp):
    inp = {k: np.asarray(v) for k, v in inp.items()}
    resA0 = stage_A0(inp)
    resB0 = stage_B0(resA0)
    resC0a = stage_C0a(inp, resA0, resB0)
    resC0b = stage_C0b(inp, resA0, resC0a)
    x1 = [np.asarray(resC0b[c]["xo"]) for c in range(NCORES)]
    resA1 = stage_A1(inp, x1)
    resB1 = stage_B1(resA1)
    resNA = stage_NA(inp, resA1)
    resC1a = stage_C1a(inp, resA1, resB1, resNA, x1)
    resC1b = stage_C1b(inp, resA1, resC1a)
    out = np.empty((2, SEQ, D), np.float32)
    for c in range(NCORES):
        b, j = divmod(c, 4)
        out[b, j * TOWN:(j + 1) * TOWN] = np.asarray(resC1b[c]["xo"]).T
    return out
```

```python
import contextlib
import numpy as np
import ml_dtypes
import concourse.bass as bass
import concourse.mybir as mybir
from concourse.bass_utils import run_bass_kernel_spmd

F32 = mybir.dt.float32
BF16 = mybir.dt.bfloat16
AF = mybir.ActivationFunctionType
ALU = mybir.AluOpType
NPBF = ml_dtypes.bfloat16

NCORES = 8
D = 1024
SEQ = 16384
TOWN = 4096
CTX = 256
NT = TOWN + CTX
GRID_W = 64
EPS = 1e-6
ALPHA = 4 ** 0.25
ENGS = ("pe", "act", "dve", "pool", "sp")


class Prog:
    def __init__(self, nc):
        self.nc = nc
        self.ops = []
        self.last_w = {}
        self.readers = {}
        self.dma_cnt = {}
        self.excl = set()

    def _deps(self, reads, writes):
        ex = [r for r in reads if r in self.excl]
        if ex:
            reads = [r for r in reads if r not in self.excl]
            writes = list(writes) + ex
        deps = set()
        for r in reads:
            w = self.last_w.get(r)
            if w is not None:
                deps.add(w)
        for r in writes:
            deps.update(self.readers.get(r, ()))
            w = self.last_w.get(r)
            if w is not None:
                deps.add(w)
        i = len(self.ops)
        for r in reads:
            self.readers.setdefault(r, []).append(i)
        for r in writes:
            self.last_w[r] = i
            self.readers[r] = []
        return deps

    def op(self, eng, fn, reads=(), writes=(), fence=False):
        deps = self._deps(reads, writes)
        self.ops.append(dict(eng=eng, fn=fn, deps=deps, dma=None, sig=None, need=False, fence=fence))

    def dma(self, queue, key, fn, reads=(), writes=(), n=1):
        deps = self._deps(reads, writes)
        c = self.dma_cnt.get(key, 0) + 16 * n
        self.dma_cnt[key] = c
        self.ops.append(dict(eng=queue, fn=fn, deps=deps, dma=key, sig=(("dma", key), c), need=True, fence=True))

    def emit(self):
        nc = self.nc
        ops = self.ops
        for o in ops:
            for d in o["deps"]:
                p = ops[d]
                if p["dma"] is None and (p["eng"] != o["eng"] or o["fence"]):
                    p["need"] = True
        cnt = {e: 0 for e in ENGS}
        for o in ops:
            if o["dma"] is None and o["need"]:
                cnt[o["eng"]] += 1
                o["sig"] = (("eng", o["eng"]), cnt[o["eng"]])
        with contextlib.ExitStack() as st:
            sems = {}
            for e in ENGS:
                if cnt[e]:
                    sems[("eng", e)] = st.enter_context(nc.semaphore("s_" + e))
            for k in self.dma_cnt:
                sems[("dma", k)] = st.enter_context(nc.semaphore("d_" + str(k)))
            block = st.enter_context(nc.Block())
            per = {e: [] for e in ENGS}
            for i, o in enumerate(ops):
                per[o["eng"]].append(i)
            finals = [(("dma", k), c) for k, c in self.dma_cnt.items()]

            def body(ename):
                def run(eng):
                    waited = {}
                    for i in per[ename]:
                        o = ops[i]
                        need = {}
                        for d in o["deps"]:
                            p = ops[d]
                            if p["dma"] is None and p["eng"] == ename and not o["fence"]:
                                continue
                            sk, v = p["sig"]
                            if v > need.get(sk, 0):
                                need[sk] = v
                        for sk, v in need.items():
                            if waited.get(sk, 0) >= v:
                                continue
                            eng.wait_ge(sems[sk], v)
                            waited[sk] = v
                        if o["dma"] is not None:
                            o["fn"](eng, sems[("dma", o["dma"])])
                        else:
                            ins = o["fn"](eng)
                            if o["need"]:
                                ins.then_inc(sems[o["sig"][0]], 1)
                    if ename == "sp":
                        for sk, v in finals:
                            if waited.get(sk, 0) < v:
                                eng.wait_ge(sems[sk], v)
                return run

            block.tensor(body("pe"))
            block.scalar(body("act"))
            block.vector(body("dve"))
            block.gpsimd(body("pool"))
            block.sync(body("sp"))


class Ctx:
    def __init__(self):
        self.nc = bass.Bass("TRN2", target_bir_lowering=False)
        self.st = contextlib.ExitStack()
        self.P = Prog(self.nc)
        self.out_names = []
        self._u = 0

    def inp(self, name, shape, dt=F32):
        return self.nc.dram_tensor(name, list(shape), dt, kind="ExternalInput").ap()

    def out(self, name, shape, dt=F32):
        self.out_names.append(name)
        return self.nc.dram_tensor(name, list(shape), dt, kind="ExternalOutput").ap()

    def sb(self, name, shape, dt=F32):
        return self.st.enter_context(self.nc.sbuf_tensor(name, list(shape), dt))

    def ps(self, name, shape, dt=F32):
        self.P.excl.add(name)
        return self.st.enter_context(self.nc.psum_tensor(name, list(shape), dt))

    def ring(self, name, n, shape, dt=F32):
        return Ring([(self.sb("%s%d" % (name, i), shape, dt), "%s%d" % (name, i)) for i in range(n)])

    def psring(self, name, n, shape=(128, 512), dt=F32):
        return Ring([(self.ps("%s%d" % (name, i), (128, 512), dt), "%s%d" % (name, i)) for i in range(n)])

    def load(self, q, dst, src, res, reads=()):
        self.P.dma(q, "L_" + str(res), lambda e, s: e.dma_start(out=dst, in_=src).then_inc(s, 16),
                   reads=reads, writes=[res])

    def store(self, q, dst, src, res, dres=None):
        self.P.dma(q, "S_" + str(res), lambda e, s: e.dma_start(out=dst, in_=src).then_inc(s, 16),
                   reads=[res], writes=([dres] if dres else []))

    def finish(self):
        self.P.emit()
        self.st.close()
        return self.nc


class Ring:
    def __init__(self, items):
        self.items = items
        self.i = 0

    def nxt(self):
        it = self.items[self.i % len(self.items)]
        self.i += 1
        return it


def run_spmd(cx, in_maps):
    nc = cx.finish()
    res = run_bass_kernel_spmd(nc, in_maps, core_ids=list(range(NCORES)))
    return res.results


def tiles_own_ctx(tt=512):
    t = [(i * tt, tt, 0) for i in range(TOWN // tt)]
    t.append((TOWN, CTX, 1))
    return t


def pcol(v):
    v = np.asarray(v, np.float32)
    return np.ascontiguousarray(v.reshape(-1, 128).T)


def emit_ada(cx, cs_d, wada_d, bada_d, mod_sb, modp_sb, pm=None, pmres="ps_mod"):
    P = cx.P
    cs_sb = cx.sb("cs_sb", [128, 16])
    s_sb = cx.sb("s_sb", [128, 16])
    ba_sb = cx.sb("ba_sb", [128, 48])
    if pm is None:
        pm = cx.ps("ps_mod", [128, 512])
    cx.load("sp", cs_sb[:], cs_d, "cs_sb")
    cx.load("sp", ba_sb[:], bada_d, "ba_sb")
    P.op("act", lambda e: e.activation(out=s_sb[:], in_=cs_sb[:], func=AF.Silu), reads=["cs_sb"], writes=["s_sb"])
    wr = cx.ring("wada", 2, [128, 8, 512])
    wv = wada_d.rearrange("(k p) c -> p k c", p=128)
    for piece in range(12):
        wa, wres = wr.nxt()
        cx.load("sp", wa[:], wv[:, :, piece * 512:(piece + 1) * 512], wres)

        def mm(e, wa=wa, piece=piece):
            ins = None
            for f4 in range(4):
                fc = piece * 4 + f4
                for k in range(8):
                    ins = e.matmul(pm[:, fc * 2:fc * 2 + 2], wa[:, k, f4 * 128:(f4 + 1) * 128],
                                   s_sb[:, 2 * k:2 * k + 2], start=(k == 0), stop=(k == 7))
            return ins
        P.op("pe", mm, reads=[wres, "s_sb"], writes=[pmres])
    pmv = pm[:, 0:96].rearrange("p (m c) -> p m c", c=2)
    mv = mod_sb[:].rearrange("p (m c) -> p m c", c=2)
    for c in range(2):
        P.op("dve", lambda e, c=c: e.tensor_tensor(out=mv[:, :, c], in0=pmv[:, :, c], in1=ba_sb[:], op=ALU.add),
             reads=[pmres, "ba_sb"], writes=["mod"], fence=True)
    P.op("dve", lambda e: e.tensor_copy(out=modp_sb[:], in_=mod_sb[:]), reads=["mod"], writes=["modp"], fence=True)
    for lo in (16, 64):
        P.op("dve", lambda e, lo=lo: e.tensor_scalar_add(out=modp_sb[:, lo:lo + 16], in0=mod_sb[:, lo:lo + 16], scalar1=1.0),
             reads=["mod"], writes=["modp"], fence=True)


def emit_modulate(cx, x, xres, u, ures, tw, col, mod_sb, modp_sb, piece_shift, nk=8):
    P = cx.P
    for k in range(nk):
        sc = modp_sb[:, (piece_shift + 1) * 16 + 2 * k + col:(piece_shift + 1) * 16 + 2 * k + col + 1]
        sh = mod_sb[:, piece_shift * 16 + 2 * k + col:piece_shift * 16 + 2 * k + col + 1]
        if k % 2 == 0:
            P.op("act", lambda e, k=k, sc=sc, sh=sh: e.activation(out=u[:, k, :tw], in_=x[:, k, :tw], func=AF.Identity,
                                                                 bias=sh, scale=sc),
                 reads=[xres, "mod", "modp"], writes=[(ures, k)])
        else:
            P.op("dve", lambda e, k=k, sc=sc, sh=sh: e.tensor_scalar(out=u[:, k, :tw], in0=x[:, k, :tw], scalar1=sc,
                                                                    scalar2=sh, op0=ALU.mult, op1=ALU.add),
                 reads=[xres, "mod", "modp"], writes=[(ures, k)])


def build_A0():
    cx = Ctx()
    P = cx.P
    xT = cx.inp("xT", [D, NT])
    cs_d = cx.inp("cs", [128, 16])
    wada_d = cx.inp("wada", [D, 6 * D])
    bada_d = cx.inp("bada", [128, 48])
    win_d = cx.inp("win", [D, 1536])
    gq_d = cx.inp("gq", [128, 1])
    gk_d = cx.inp("gk", [128, 1])
    cos_d = cx.inp("cosT", [128, NT])
    sin_d = cx.inp("sinT", [128, NT])
    rt_d = cx.inp("rotT", [128, 128])
    ob_d = cx.inp("oneblk", [128, 128])
    mod_o = cx.out("mod", [128, 96])
    qT_o = cx.out("qT", [768, NT], BF16)
    kT_o = cx.out("kT", [256, NT], BF16)
    v_o = cx.out("v", [NT, 256], BF16)
    pT_o = cx.out("pT", [256, NT])

    mod_sb = cx.sb("mod_sb", [128, 96])
    modp_sb = cx.sb("modp_sb", [128, 96])
    emit_ada(cx, cs_d, wada_d, bada_d, mod_sb, modp_sb)
    cx.store("sp", mod_o, mod_sb[:], "mod")

    win = cx.sb("win_sb", [128, 8, 1536], BF16)
    cx.load("pool", win[:], win_d.rearrange("(k p) c -> p k c", p=128), "win")
    oneblk = cx.sb("oneblk_sb", [128, 128], BF16)
    cx.load("pool", oneblk[:], ob_d, "oneblk")
    rt32 = cx.sb("rt32", [128, 128])
    cx.load("sp", rt32[:], rt_d, "rt32")
    g_sb = cx.sb("g_sb", [128, 2])
    cx.load("sp", g_sb[:, 0:1], gq_d, "gq")
    cx.load("sp", g_sb[:, 1:2], gk_d, "gk")
    rg = [cx.sb("rgq", [128, 128], BF16), cx.sb("rgk", [128, 128], BF16)]
    cosg = [cx.sb("cosgq", [128, NT]), cx.sb("cosgk", [128, NT])]
    sin_sb = cx.sb("sin_sb", [128, NT])
    cx.load("sp", sin_sb[:], sin_d, "sin")
    gres = ["gq", "gk"]
    for i in range(2):
        cx.load("sp", cosg[i][:], cos_d, "cosg%d" % i)
        P.op("dve", lambda e, i=i: e.tensor_scalar_mul(out=rg[i][:], in0=rt32[:], scalar1=g_sb[:, i:i + 1]),
             reads=["rt32", gres[i]], writes=["rg%d" % i])
        P.op("dve", lambda e, i=i: e.tensor_scalar_mul(out=cosg[i][:], in0=cosg[i][:], scalar1=g_sb[:, i:i + 1]),
             reads=[gres[i], "cosg%d" % i], writes=["cosg%d" % i])

    eps_sb = cx.sb("eps_sb", [128, 1])
    P.op("dve", lambda e: e.memset(eps_sb[:], EPS), writes=["eps"])
    xr = cx.ring("x", 2, [128, 8, 512])
    ur = cx.ring("u", 2, [128, 8, 512], BF16)
    pq_r = cx.psring("pq", 2)
    pms_r = cx.psring("pms", 1)
    prot_r = cx.psring("prot", 1)
    pv_r = cx.psring("pv", 2)
    sq_r = cx.ring("sq", 2, [128, 512], BF16)
    qb_r = cx.ring("qb", 2, [128, 512], BF16)
    t1_r = cx.ring("t1", 2, [128, 512])
    t2_r = cx.ring("t2", 2, [128, 512])
    rs_r = cx.ring("rs", 2, [128, 512])
    qf_r = cx.ring("qf", 3, [128, 512], BF16)
    vs_r = cx.ring("vs", 2, [128, 256], BF16)
    pp_r = cx.ring("pp", 2, [128, 512])
    xv = xT.rearrange("(k p) t -> p k t", p=128)

    def do_tile(t0, tw, col):
        x, xres = xr.nxt()
        cx.load("sp", x[:, :, :tw], xv[:, :, t0:t0 + tw], xres)
        u, ures = ur.nxt()
        emit_modulate(cx, x, xres, u, ures, tw, col, mod_sb, modp_sb, 0)
        ureads = [(ures, k) for k in range(8)]
        for cc in range(8):
            isk = cc >= 6
            c0 = cc * 128 if not isk else 768 + (cc - 6) * 128
            gi = 1 if isk else 0
            pq, pqres = pq_r.nxt()

            def mmq(e, pq=pq, c0=c0):
                ins = None
                for k in range(8):
                    ins = e.matmul(pq[:, :tw], win[:, k, c0:c0 + 128], u[:, k, :tw], start=(k == 0), stop=(k == 7))
                return ins
            P.op("pe", mmq, reads=ureads + ["win"], writes=[pqres])
            sq, sqres = sq_r.nxt()
            P.op("act", lambda e, sq=sq, pq=pq: e.activation(out=sq[:, :tw], in_=pq[:, :tw], func=AF.Square),
                 reads=[pqres], writes=[sqres])
            qb, qbres = qb_r.nxt()
            P.op("dve", lambda e, qb=qb, pq=pq: e.tensor_copy(out=qb[:, :tw], in_=pq[:, :tw]), reads=[pqres], writes=[qbres])
            pms, pmsres = pms_r.nxt()
            P.op("pe", lambda e, pms=pms, sq=sq: e.matmul(pms[:, :tw], oneblk[:], sq[:, :tw], start=True, stop=True),
                 reads=[sqres, "oneblk"], writes=[pmsres])
            prot, protres = prot_r.nxt()
            P.op("pe", lambda e, prot=prot, qb=qb, gi=gi: e.matmul(prot[:, :tw], rg[gi][:], qb[:, :tw], start=True, stop=True),
                 reads=[qbres, "rg%d" % gi], writes=[protres])
            t1, t1res = t1_r.nxt()
            P.op("dve", lambda e, t1=t1, pq=pq, gi=gi: e.tensor_tensor(out=t1[:, :tw], in0=pq[:, :tw],
                                                                     in1=cosg[gi][:, t0:t0 + tw], op=ALU.mult),
                 reads=[pqres, "cosg%d" % gi], writes=[t1res])
            t2, t2res = t2_r.nxt()
            P.op("dve", lambda e, t2=t2, prot=prot: e.tensor_tensor(out=t2[:, :tw], in0=prot[:, :tw],
                                                                  in1=sin_sb[:, t0:t0 + tw], op=ALU.mult),
                 reads=[protres, "sin"], writes=[t2res])
            rs, rsres = rs_r.nxt()
            P.op("act", lambda e, rs=rs, pms=pms: e.activation(out=rs[:, :tw], in_=pms[:, :tw], func=AF.Sqrt, bias=eps_sb[:, 0:1]),
                 reads=[pmsres, "eps"], writes=[rsres])
            P.op("dve", lambda e, rs=rs: e.reciprocal(out=rs[:, :tw], in_=rs[:, :tw]), reads=[rsres], writes=[rsres])
            P.op("dve", lambda e, t1=t1, t2=t2: e.tensor_tensor(out=t1[:, :tw], in0=t1[:, :tw], in1=t2[:, :tw], op=ALU.add),
                 reads=[t1res, t2res], writes=[t1res])
            qf, qfres = qf_r.nxt()
            P.op("dve", lambda e, qf=qf, t1=t1, rs=rs: e.tensor_tensor(out=qf[:, :tw], in0=t1[:, :tw], in1=rs[:, :tw], op=ALU.mult),
                 reads=[t1res, rsres], writes=[qfres])
            dst = (kT_o[(cc - 6) * 128:(cc - 5) * 128, t0:t0 + tw] if isk else qT_o[cc * 128:(cc + 1) * 128, t0:t0 + tw])
            cx.store("sp", dst, qf[:, :tw], qfres)
        for tb in range(tw // 128):
            pv, pvres = pv_r.nxt()

            def mmv(e, pv=pv, tb=tb):
                ins = None
                for k in range(8):
                    ins = e.matmul(pv[:, :256], u[:, k, tb * 128:(tb + 1) * 128], win[:, k, 1024:1280],
                                   start=(k == 0), stop=(k == 7))
                return ins
            P.op("pe", mmv, reads=ureads + ["win"], writes=[pvres])
            vs, vsres = vs_r.nxt()
            P.op("act", lambda e, vs=vs, pv=pv: e.activation(out=vs[:], in_=pv[:, :256], func=AF.Copy), reads=[pvres], writes=[vsres])
            cx.store("pool", v_o[t0 + tb * 128:t0 + (tb + 1) * 128, :], vs[:], vsres)
        for pc in range(2):
            pq, pqres = pq_r.nxt()

            def mmp(e, pq=pq, pc=pc):
                ins = None
                for k in range(8):
                    ins = e.matmul(pq[:, :tw], win[:, k, 1280 + pc * 128:1280 + (pc + 1) * 128], u[:, k, :tw],
                                   start=(k == 0), stop=(k == 7))
                return ins
            P.op("pe", mmp, reads=ureads + ["win"], writes=[pqres])
            pp, ppres = pp_r.nxt()
            P.op("act", lambda e, pp=pp, pq=pq: e.activation(out=pp[:, :tw], in_=pq[:, :tw], func=AF.Copy), reads=[pqres], writes=[ppres])
            cx.store("pool", pT_o[pc * 128:(pc + 1) * 128, t0:t0 + tw], pp[:, :tw], ppres)

    for (t0, tw, col) in tiles_own_ctx():
        do_tile(t0, tw, col)
    return cx


def build_attn(nheads, group, dk, nkc_full, scale):
    cx = Ctx()
    P = cx.P
    nkv = nheads // group
    nkeys = nkc_full * 128
    qT = cx.inp("qT", [nheads * dk, NT], BF16)
    kT = cx.inp("kT", [nkv, dk, nkeys], BF16)
    vv = cx.inp("v", [nkv, 128, nkc_full, 64], BF16)
    sel_d = cx.inp("sel", [65, 64])
    oT = cx.out("oT", [nheads * 64, NT], BF16)

    kg_r = cx.ring("kg", 2, [128, nkeys], BF16)
    vg_r = cx.ring("vg", 2, [128, nkc_full, 65], BF16)
    for vg, vres in vg_r.items:
        P.op("dve", lambda e, vg=vg: e.memset(vg[:, :, 64:65], 1.0), writes=[(vres, "ones")])
    for kg, kres in kg_r.items:
        P.op("pool", lambda e, kg=kg: e.memset(kg[:, :], 0.0), writes=[kres])
    sel = cx.sb("sel_sb", [65, 64])
    cx.load("sp", sel[:], sel_d, "sel")
    q_r = cx.ring("qh", 2, [128, NT], BF16)
    for qh, qres in q_r.items:
        P.op("pool", lambda e, qh=qh: e.memset(qh[:, :], 0.0), writes=[qres])
    ps_r = cx.psring("ps", 4)
    po_r = cx.psring("po", 2)
    pb_r = cx.psring("pb", 1)
    pt_r = cx.ring("pt", 4, [128, 512], BF16)
    os_r = cx.ring("os", 2, [65, 512])
    at_r = cx.ring("at", 2, [64, 512], BF16)
    LOOK = 3
    for g in range(nkv):
        kg, kres = kg_r.nxt()
        cx.load("sp", kg[0:dk, :], kT[g], kres)
        vg, vres = vg_r.nxt()
        cx.load("sp", vg[:, :, 0:64], vv[g], vres)
        for h in range(g * group, (g + 1) * group):
            qh, qres = q_r.nxt()
            cx.load("sp", qh[0:dk, :], qT[h * dk:(h + 1) * dk, :], qres)
            steps = []
            for (t0, tw, col) in tiles_own_ctx():
                nkc = nkc_full if col == 0 else 2
                for kc in range(nkc):
                    steps.append((t0, tw, kc, nkc))
            pss = {}

            def emit_qk(i, kg=kg, kres=kres, qh=qh, qres=qres):
                t0, tw, kc, nkc = steps[i]
                ps, psres = ps_r.nxt()
                pss[i] = (ps, psres)
                P.op("pe", lambda e: e.matmul(ps[:, :tw], kg[:, kc * 128:(kc + 1) * 128], qh[:, t0:t0 + tw], start=True, stop=True),
                     reads=[kres, qres], writes=[psres])

            def emit_rest(i, po, pores, vg=vg, vres=vres):
                t0, tw, kc, nkc = steps[i]
                ps, psres = pss.pop(i)
                pt, ptres = pt_r.nxt()
                P.op("act", lambda e: e.activation(out=pt[:, :tw], in_=ps[:, :tw], func=AF.Exp, scale=scale), reads=[psres], writes=[ptres])
                P.op("pe", lambda e: e.matmul(po[0:65, :tw], vg[:, kc, :], pt[:, :tw], start=(kc == 0), stop=(kc == nkc - 1)),
                     reads=[ptres, vres, (vres, "ones")], writes=[pores])

            def emit_norm(i, po, pores, h=h):
                t0, tw, kc, nkc = steps[i]
                osb, osres = os_r.nxt()
                P.op("act", lambda e: e.activation(out=osb[:, :tw], in_=po[0:65, :tw], func=AF.Copy), reads=[pores], writes=[osres])
                P.op("dve", lambda e: e.reciprocal(out=osb[64:65, :tw], in_=osb[64:65, :tw]), reads=[osres], writes=[osres])
                pb, pbres = pb_r.nxt()
                P.op("pe", lambda e: e.matmul(pb[0:64, :tw], sel[:], osb[:, :tw], start=True, stop=True), reads=[osres, "sel"], writes=[pbres])
                at, atres = at_r.nxt()
                P.op("dve", lambda e: e.tensor_tensor(out=at[:, :tw], in0=osb[0:64, :tw], in1=pb[0:64, :tw], op=ALU.mult),
                     reads=[osres, pbres], writes=[atres])
                cx.store("pool", oT[h * 64:(h + 1) * 64, t0:t0 + tw], at[:, :tw], atres)

            for i in range(min(LOOK, len(steps))):
                emit_qk(i)
            po = pores = None
            for i in range(len(steps)):
                if steps[i][2] == 0:
                    po, pores = po_r.nxt()
                if i + LOOK < len(steps):
                    emit_qk(i + LOOK)
                emit_rest(i, po, pores)
                if steps[i][2] == steps[i][3] - 1:
                    emit_norm(i, po, pores)
    return cx


def rope_tables(j):
    t = np.arange(TOWN) + j * TOWN
    row = (t // GRID_W).astype(np.float64)
    colp = (t % GRID_W).astype(np.float64)
    inv = 10000.0 ** (-np.arange(16, dtype=np.float64) / 16)
    cos = np.ones((128, NT), np.float32)
    sin = np.zeros((128, NT), np.float32)
    for p in range(128):
        d = p % 64
        pos = row if d < 32 else colp
        ang = (pos.astype(np.float32) * inv[d % 16].astype(np.float32)).astype(np.float32)
        cos[p, :TOWN] = np.cos(ang)
        sin[p, :TOWN] = np.sin(ang)
    return cos, sin


def rot_matrix(n=128, blk=32):
    rt = np.zeros((n, n), np.float32)
    h = blk // 2
    for p in range(n):
        if p % blk < h:
            rt[p + h, p] = -1.0
        else:
            rt[p - h, p] = 1.0
    return rt


def blockdiag_ones(n, blk):
    m = np.zeros((n, n), np.float32)
    for i in range(0, n, blk):
        m[i:i + blk, i:i + blk] = 1.0 / blk
    return m


def core_xT(x, ctx, c):
    b, j = divmod(c, 4)
    return np.ascontiguousarray(np.concatenate([x[b, j * TOWN:(j + 1) * TOWN], ctx[b]], axis=0).T)


def core_cs(cvec, c_ctx, c):
    b = c // 4
    cs = np.empty((128, 16), np.float32)
    cs[:, 0::2] = pcol(cvec[b])
    cs[:, 1::2] = pcol(c_ctx)
    return cs


SEL = np.zeros((65, 64), np.float32)
SEL[64, :] = 1.0


def gather_kv(res, kname, vname, nkv, dk):
    out = []
    for b in range(2):
        ks = [np.asarray(res[4 * b][kname])[:, TOWN:]] + [np.asarray(res[4 * b + j][kname])[:, :TOWN] for j in range(4)]
        kf = np.concatenate(ks, axis=1).reshape(nkv, dk, -1)
        vs = [np.asarray(res[4 * b][vname])[TOWN:]] + [np.asarray(res[4 * b + j][vname])[:TOWN] for j in range(4)]
        vf = np.concatenate(vs, axis=0)
        nkc = vf.shape[0] // 128
        vf = np.ascontiguousarray(vf.reshape(nkc, 128, nkv, 64).transpose(2, 1, 0, 3))
        out.append((np.ascontiguousarray(kf), vf))
    return out


def stage_A0(inp):
    x, ctx = inp["x"], inp["ctx"]
    cx = build_A0()
    maps = []
    for c in range(NCORES):
        cos, sin = rope_tables(c % 4)
        maps.append(dict(
            xT=core_xT(x, ctx, c), cs=core_cs(inp["c"], inp["c_ctx"], c),
            wada=np.ascontiguousarray(inp["w_ada"][0]), bada=pcol(inp["b_ada"][0]),
            win=np.ascontiguousarray(inp["ev_w_in"][0]),
            gq=np.tile(inp["ev_q_gain"][0], 2)[:, None].astype(np.float32),
            gk=np.tile(inp["ev_k_gain"][0], 2)[:, None].astype(np.float32),
            cosT=cos, sinT=sin, rotT=rot_matrix(), oneblk=blockdiag_ones(128, 64)))
    return run_spmd(cx, maps)


def stage_B0(resA):
    cx = build_attn(12, 3, 64, 130, 64 ** -0.5)
    kv = gather_kv(resA, "kT", "v", 4, 64)
    maps = []
    for c in range(NCORES):
        kf, vf = kv[c // 4]
        maps.append(dict(qT=np.asarray(resA[c]["qT"]), kT=kf, v=vf, sel=SEL))
    return run_spmd(cx, maps)


class LNState:
    def __init__(self, cx, tag, width=512):
        self.ones = cx.sb(tag + "_ones", [128, 128])
        cx.P.op("dve", lambda e: e.memset(self.ones[:], 1.0 / D), writes=[tag + "_ones"])
        self.ones_res = tag + "_ones"
        self.eps = cx.sb(tag + "_eps", [128, 1])
        cx.P.op("dve", lambda e: e.memset(self.eps[:], EPS), writes=[tag + "_eps"])
        self.eps_res = tag + "_eps"
        self.pmean = cx.psring(tag + "_pmean", 1, (128, width))
        self.pmsq = cx.psring(tag + "_pmsq", 1, (128, width))
        self.rsq = cx.ring(tag + "_rsq", 2, [128, width])
        self.m = cx.ring(tag + "_m", 1, [128, width])
        self.var = cx.ring(tag + "_var", 1, [128, width])
        self.nb = cx.ring(tag + "_nb", 1, [128, width])
        self.t = cx.ring(tag + "_t", 2, [128, width])


def emit_layernorm(cx, ln, r, rres, tw, g_sb, b_sb, gres, out=None, outres=None):
    P = cx.P
    if out is None:
        out, outres = r, rres
    pmean, pmres = ln.pmean.nxt()
    pmsq, pqres = ln.pmsq.nxt()
    for k in range(8):
        rsq, rsqres = ln.rsq.nxt()
        P.op("act", lambda e, rsq=rsq, k=k: e.activation(out=rsq[:, :tw], in_=r[:, k, :tw], func=AF.Square),
             reads=[rres(k)], writes=[rsqres])
        P.op("pe", lambda e, k=k: e.matmul(pmean[:, :tw], ln.ones[:], r[:, k, :tw], start=(k == 0), stop=(k == 7)),
             reads=[rres(k), ln.ones_res], writes=[pmres])
        P.op("pe", lambda e, rsq=rsq, k=k: e.matmul(pmsq[:, :tw], ln.ones[:], rsq[:, :tw], start=(k == 0), stop=(k == 7)),
             reads=[rsqres, ln.ones_res], writes=[pqres])
    m, mres = ln.m.nxt()
    var, vres = ln.var.nxt()
    nb, nbres = ln.nb.nxt()
    P.op("act", lambda e: e.activation(out=m[:, :tw], in_=pmean[:, :tw], func=AF.Copy), reads=[pmres], writes=[mres])
    P.op("dve", lambda e: e.tensor_tensor(out=var[:, :tw], in0=m[:, :tw], in1=m[:, :tw], op=ALU.mult), reads=[mres], writes=[vres])
    P.op("dve", lambda e: e.tensor_tensor(out=var[:, :tw], in0=pmsq[:, :tw], in1=var[:, :tw], op=ALU.subtract),
         reads=[pqres, vres], writes=[vres])
    P.op("dve", lambda e: e.tensor_scalar_max(out=var[:, :tw], in0=var[:, :tw], scalar1=0.0), reads=[vres], writes=[vres])
    P.op("act", lambda e: e.activation(out=var[:, :tw], in_=var[:, :tw], func=AF.Sqrt, bias=ln.eps[:, 0:1]),
         reads=[vres, ln.eps_res], writes=[vres])
    P.op("dve", lambda e: e.reciprocal(out=var[:, :tw], in_=var[:, :tw]), reads=[vres], writes=[vres])
    P.op("dve", lambda e: e.scalar_tensor_tensor(out=nb[:, :tw], in0=m[:, :tw], scalar=-1.0, in1=var[:, :tw],
                                                 op0=ALU.mult, op1=ALU.mult), reads=[mres, vres], writes=[nbres])
    for k in range(8):
        t, tres = ln.t.nxt()
        P.op("dve", lambda e, t=t, k=k: e.tensor_tensor(out=t[:, :tw], in0=r[:, k, :tw], in1=var[:, :tw], op=ALU.mult),
             reads=[rres(k), vres], writes=[tres])
        P.op("dve", lambda e, t=t: e.tensor_tensor(out=t[:, :tw], in0=t[:, :tw], in1=nb[:, :tw], op=ALU.add),
             reads=[tres, nbres], writes=[tres])
        P.op("act", lambda e, t=t, k=k: e.activation(out=out[:, k, :tw], in_=t[:, :tw], func=AF.Identity,
                                                     bias=b_sb[:, k:k + 1], scale=g_sb[:, k:k + 1]),
             reads=[tres, gres], writes=[outres(k)])


def load_cast_w(cx, name, w_d, kch, ncol):
    w = cx.sb(name, [128, kch, ncol], BF16)
    for k in range(kch):
        cx.load("pool", w[:, k, :], w_d[k * 128:(k + 1) * 128, :], (name, k))
    return w, [(name, k) for k in range(kch)]


def build_Ca(with_pool):
    cx = Ctx()
    P = cx.P
    xT = cx.inp("xT", [D, NT])
    nat = 6 if with_pool else 8
    aT = cx.inp("aT", [nat * 128, NT], BF16)
    mod_d = cx.inp("mod", [128, 96])
    wout_d = cx.inp("wout", [D, D])
    g_d = cx.inp("lng", [128, 8])
    b_d = cx.inp("lnb", [128, 8])
    if with_pool:
        ph_d = cx.inp("ph", [256, NT + 32])
        rc_d = cx.inp("rcnt", [256, NT])
        wp_d = cx.inp("wpool", [256, 128])
        psc_d = cx.inp("pscale", [128, 2])
    xo = cx.out("xo", [D, NT])

    mod_sb = cx.sb("mod_sb", [128, 96])
    cx.load("sp", mod_sb[:], mod_d, "mod")
    g_sb = cx.sb("g_sb", [128, 8])
    b_sb = cx.sb("b_sb", [128, 8])
    cx.load("sp", g_sb[:], g_d, "lngb")
    cx.load("sp", b_sb[:], b_d, "lngb2")
    P.op("dve", lambda e: e.tensor_copy(out=b_sb[:], in_=b_sb[:]), reads=["lngb2"], writes=["lngb"])
    wout, wres = load_cast_w(cx, "wout_sb", wout_d, 8, D)
    ln = LNState(cx, "ln")
    if with_pool:
        wp = cx.sb("wp_sb", [128, 2, 128], BF16)
        for pc in range(2):
            cx.load("pool", wp[:, pc, :], wp_d[pc * 128:(pc + 1) * 128, :], ("wp", pc))
        psc = cx.sb("psc_sb", [128, 2])
        cx.load("sp", psc[:], psc_d, "psc")
        rc = cx.sb("rc_sb", [128, 2, NT])
        cx.load("sp", rc[:], rc_d.rearrange("(c p) t -> p c t", p=128), "rc")
        ph_r = cx.ring("ph", 4, [128, 528])
        s_r = [cx.ring("s%d" % i, 1, [128, 528]) for i in range(4)]
        ym_r = cx.ring("ym", 2, [128, 512], BF16)
        tmp_r = cx.ring("ptmp", 2, [128, 512])
        ppool_r = cx.psring("ppool", 1)
    x_r = cx.ring("x", 2, [128, 8, 512])
    mix_r = cx.ring("mix", 2, [128, 8, 512], BF16)
    po_r = cx.psring("po", 2)
    xv = xT.rearrange("(k p) t -> p k t", p=128)
    av = aT.rearrange("(k p) t -> p k t", p=128)
    ov = xo.rearrange("(k p) t -> p k t", p=128)
    LO = {2: 1, 4: 2, 8: 4, 16: 8}

    def do_tile(t0, tw, col):
        mix, mres = mix_r.nxt()
        cx.load("sp", mix[:, 0:nat, :tw], av[:, :, t0:t0 + tw], (mres, "a"))
        x, xres = x_r.nxt()
        cx.load("sp", x[:, :, :tw], xv[:, :, t0:t0 + tw], (xres, "ld"))
        if with_pool:
            h0 = t0 if col == 0 else t0 + 16
            L = tw + 16
            for pc in range(2):
                ph, phres = ph_r.nxt()
                cx.load("sp", ph[:, :L], ph_d[pc * 128:(pc + 1) * 128, h0:h0 + L], phres)
                srcs = [(ph, phres)]
                nlev = 2 if pc == 0 else 4
                for lev in range(nlev):
                    sh = 1 << lev
                    s, sres = s_r[lev].nxt()
                    a, ares = srcs[-1]
                    Lo = L - (2 * sh - 1)
                    P.op("dve", lambda e, s=s, a=a, sh=sh, Lo=Lo: e.tensor_tensor(out=s[:, :Lo], in0=a[:, 0:Lo], in1=a[:, sh:sh + Lo], op=ALU.add),
                         reads=[ares], writes=[sres])
                    srcs.append((s, sres))
                ym, ymres = ym_r.nxt()
                tmp, tmpres = tmp_r.nxt()
                for half in range(2):
                    w = (2, 4, 8, 16)[pc * 2 + half]
                    s, sres = srcs[{2: 1, 4: 2, 8: 3, 16: 4}[w]]
                    off = 8 - LO[w]
                    lo_p, hi_p = half * 64, half * 64 + 64
                    P.op("dve", lambda e, s=s, off=off, lo_p=lo_p, hi_p=hi_p, pc=pc, tmp=tmp:
                         e.tensor_tensor(out=tmp[lo_p:hi_p, :tw], in0=s[lo_p:hi_p, off:off + tw], in1=rc[lo_p:hi_p, pc, t0:t0 + tw], op=ALU.mult),
                         reads=[sres, "rc"], writes=[(tmpres, half)])
                    P.op("dve", lambda e, ph=ph, lo_p=lo_p, hi_p=hi_p, tmp=tmp, ym=ym:
                         e.tensor_tensor(out=ym[lo_p:hi_p, :tw], in0=tmp[lo_p:hi_p, :tw], in1=ph[lo_p:hi_p, 8:8 + tw], op=ALU.subtract),
                         reads=[(tmpres, half), phres], writes=[(ymres, half)])
                pp, ppres = ppool_r.nxt()
                P.op("pe", lambda e, pp=pp, ym=ym, pc=pc: e.matmul(pp[:, :tw], wp[:, pc, :], ym[:, :tw], start=True, stop=True),
                     reads=[(ymres, 0), (ymres, 1), ("wp", pc)], writes=[ppres])
                P.op("act", lambda e, pp=pp, pc=pc: e.activation(out=mix[:, 6 + pc, :tw], in_=pp[:, :tw], func=AF.Identity, scale=psc[:, pc:pc + 1]),
                     reads=[ppres, "psc"], writes=[(mres, "p", pc)])
        mreads = [(mres, "a")] + ([(mres, "p", 0), (mres, "p", 1)] if with_pool else [])
        for oc in range(8):
            P.op("act", lambda e, oc=oc: e.activation(out=x[:, oc, :tw], in_=x[:, oc, :tw], func=AF.Identity, scale=ALPHA),
                 reads=[(xres, "ld")], writes=[(xres, oc)])
            po, pores = po_r.nxt()

            def mm(e, po=po, oc=oc):
                ins = None
                for k in range(8):
                    ins = e.matmul(po[:, :tw], wout[:, k, oc * 128:(oc + 1) * 128], mix[:, k, :tw], start=(k == 0), stop=(k == 7))
                return ins
            P.op("pe", mm, reads=mreads + wres, writes=[pores])
            gate = mod_sb[:, 32 + 2 * oc + col:32 + 2 * oc + col + 1]
            P.op("dve", lambda e, po=po, oc=oc, gate=gate: e.scalar_tensor_tensor(out=x[:, oc, :tw], in0=po[:, :tw], scalar=gate,
                                                                               in1=x[:, oc, :tw], op0=ALU.mult, op1=ALU.add),
                 reads=[pores, (xres, oc), "mod"], writes=[(xres, oc)])
        emit_layernorm(cx, ln, x, lambda k: (xres, k), tw, g_sb, b_sb, "lngb")
        P.dma("pool", "S_" + xres, lambda e, s: e.dma_start(out=ov[:, :, t0:t0 + tw], in_=x[:, :, :tw]).then_inc(s, 16),
              reads=[(xres, k) for k in range(8)], writes=[(xres, "ld")])

    for (t0, tw, col) in tiles_own_ctx():
        do_tile(t0, tw, col)
    return cx


def build_C0b():
    cx = Ctx()
    P = cx.P
    NF = 22
    TW = 256
    xT = cx.inp("xT", [D, NT])
    mod_d = cx.inp("mod", [128, 96])
    wg_d = cx.inp("wg", [D, NF * 128])
    wu_d = cx.inp("wu", [D, NF * 128])
    wd_d = cx.inp("wd", [NF * 128, D])
    g_d = cx.inp("lng", [128, 8])
    b_d = cx.inp("lnb", [128, 8])
    xo = cx.out("xo", [D, NT])
    mod_sb = cx.sb("mod_sb", [128, 96])
    modp_sb = cx.sb("modp_sb", [128, 96])
    cx.load("sp", mod_sb[:], mod_d, "mod")
    P.op("dve", lambda e: e.tensor_scalar_add(out=modp_sb[:], in0=mod_sb[:], scalar1=1.0), reads=["mod"], writes=["modp"])
    g_sb = cx.sb("g_sb", [128, 8])
    b_sb = cx.sb("b_sb", [128, 8])
    cx.load("sp", g_sb[:], g_d, "lngb")
    cx.load("sp", b_sb[:], b_d, "lngb2")
    P.op("dve", lambda e: e.tensor_copy(out=b_sb[:], in_=b_sb[:]), reads=["lngb2"], writes=["lngb"])
    wg, wgres = load_cast_w(cx, "wg_sb", wg_d, 8, NF * 128)
    wu, wures = load_cast_w(cx, "wu_sb", wu_d, 8, NF * 128)
    wd, wdres = load_cast_w(cx, "wd_sb", wd_d, NF, D)
    ln = LNState(cx, "ln", TW)
    x_r = cx.ring("x", 1, [128, 8, TW])
    u_r = cx.ring("u", 1, [128, 8, TW], BF16)
    h_r = cx.ring("h", 1, [128, NF, TW], BF16)
    sg_r = cx.ring("sg", 2, [128, TW])
    pg_r = cx.psring("pg", 2, (128, TW))
    pu_r = cx.psring("pu", 2, (128, TW))
    pd_r = cx.psring("pd", 2, (128, TW))
    xv = xT.rearrange("(k p) t -> p k t", p=128)
    ov = xo.rearrange("(k p) t -> p k t", p=128)

    def do_tile(t0, tw, col):
        x, xres = x_r.nxt()
        cx.load("sp", x[:, :, :tw], xv[:, :, t0:t0 + tw], (xres, "ld"), reads=[(xres, k) for k in range(8)])
        u, ures = u_r.nxt()
        emit_modulate(cx, x, (xres, "ld"), u, ures, tw, col, mod_sb, modp_sb, 3)
        ureads = [(ures, k) for k in range(8)]
        h, hres = h_r.nxt()
        for fc in range(NF):
            pg, pgres = pg_r.nxt()
            pu, pures = pu_r.nxt()

            def mmg(e, pg=pg, fc=fc):
                ins = None
                for k in range(8):
                    ins = e.matmul(pg[:, :tw], wg[:, k, fc * 128:(fc + 1) * 128], u[:, k, :tw], start=(k == 0), stop=(k == 7))
                return ins

            def mmu(e, pu=pu, fc=fc):
                ins = None
                for k in range(8):
                    ins = e.matmul(pu[:, :tw], wu[:, k, fc * 128:(fc + 1) * 128], u[:, k, :tw], start=(k == 0), stop=(k == 7))
                return ins
            P.op("pe", mmg, reads=ureads + wgres, writes=[pgres])
            P.op("pe", mmu, reads=ureads + wures, writes=[pures])
            sg, sgres = sg_r.nxt()
            P.op("act", lambda e, sg=sg, pg=pg: e.activation(out=sg[:, :tw], in_=pg[:, :tw], func=AF.Silu), reads=[pgres], writes=[sgres])
            P.op("dve", lambda e, sg=sg, pu=pu, fc=fc: e.tensor_tensor(out=h[:, fc, :tw], in0=sg[:, :tw], in1=pu[:, :tw], op=ALU.mult),
                 reads=[sgres, pures], writes=[(hres, fc)])
        hreads = [(hres, fc) for fc in range(NF)]
        for oc in range(8):
            P.op("act", lambda e, oc=oc: e.activation(out=x[:, oc, :tw], in_=x[:, oc, :tw], func=AF.Identity, scale=ALPHA),
                 reads=[(xres, "ld")] + ureads, writes=[(xres, oc)])
            pd, pdres = pd_r.nxt()

            def mmd(e, pd=pd, oc=oc):
                ins = None
                for fc in range(NF):
                    ins = e.matmul(pd[:, :tw], wd[:, fc, oc * 128:(oc + 1) * 128], h[:, fc, :tw], start=(fc == 0), stop=(fc == NF - 1))
                return ins
            P.op("pe", mmd, reads=hreads + wdres, writes=[pdres])
            gate = mod_sb[:, 80 + 2 * oc + col:80 + 2 * oc + col + 1]
            P.op("dve", lambda e, pd=pd, oc=oc, gate=gate: e.scalar_tensor_tensor(out=x[:, oc, :tw], in0=pd[:, :tw], scalar=gate,
                                                                               in1=x[:, oc, :tw], op0=ALU.mult, op1=ALU.add),
                 reads=[pdres, (xres, oc), "mod"], writes=[(xres, oc)])
        emit_layernorm(cx, ln, x, lambda k: (xres, k), tw, g_sb, b_sb, "lngb")
        P.dma("pool", "S_" + xres, lambda e, s: e.dma_start(out=ov[:, :, t0:t0 + tw], in_=x[:, :, :tw]).then_inc(s, 16),
              reads=[(xres, k) for k in range(8)], writes=[(xres, "ld")])

    for (t0, tw, col) in tiles_own_ctx(TW):
        do_tile(t0, tw, col)
    return cx


POOL_W = (2, 4, 8, 16)


def pool_tables(j):
    rc = np.ones((256, NT), np.float32)
    for g, w in enumerate(POOL_W):
        lo = w // 2
        hi = w - lo
        t = np.arange(TOWN) + j * TOWN
        cnt = np.clip(t + hi, 0, SEQ) - np.clip(t - lo, 0, SEQ)
        rc[g * 64:(g + 1) * 64, :TOWN] = (1.0 / cnt.astype(np.float32))[None, :]
        t = np.arange(CTX)
        cnt = np.clip(t + hi, 0, CTX) - np.clip(t - lo, 0, CTX)
        rc[g * 64:(g + 1) * 64, TOWN:] = (1.0 / cnt.astype(np.float32))[None, :]
    return rc


def pool_halo(resA, c):
    b, j = divmod(c, 4)
    own = np.asarray(resA[c]["pT"])
    ph = np.zeros((256, NT + 32), np.float32)
    ph[:, 8:8 + TOWN] = own[:, :TOWN]
    if j > 0:
        ph[:, 0:8] = np.asarray(resA[c - 1]["pT"])[:, TOWN - 8:TOWN]
    if j < 3:
        ph[:, 8 + TOWN:16 + TOWN] = np.asarray(resA[c + 1]["pT"])[:, 0:8]
    ph[:, TOWN + 16 + 8:TOWN + 16 + 8 + CTX] = own[:, TOWN:]
    return ph


def wpool_blockdiag(w_pool):
    m = np.zeros((256, 128), np.float32)
    for g in range(4):
        pc, gl = divmod(g, 2)
        m[pc * 128 + gl * 64:pc * 128 + gl * 64 + 64, gl * 64:gl * 64 + 64] = w_pool[g]
    return m


def stage_C0a(inp, resA, resB):
    cx = build_Ca(True)
    maps = []
    for c in range(NCORES):
        maps.append(dict(
            xT=core_xT(inp["x"], inp["ctx"], c), aT=np.asarray(resB[c]["oT"]), mod=np.asarray(resA[c]["mod"]),
            wout=np.ascontiguousarray(inp["ev_w_out"][0]), lng=pcol(inp["ln1_g"][0]), lnb=pcol(inp["ln1_b"][0]),
            ph=pool_halo(resA, c), rcnt=pool_tables(c % 4), wpool=wpool_blockdiag(inp["ev_w_pool"][0]),
            pscale=pcol(inp["ev_pool_scale"][0])))
    return run_spmd(cx, maps)


def stage_C0b(inp, resA, resCa):
    cx = build_C0b()
    maps = []
    for c in range(NCORES):
        maps.append(dict(
            xT=np.asarray(resCa[c]["xo"]), mod=np.asarray(resA[c]["mod"]),
            wg=np.ascontiguousarray(inp["ev_w_gate"][0]), wu=np.ascontiguousarray(inp["ev_w_up"][0]),
            wd=np.ascontiguousarray(inp["ev_w_down"][0]), lng=pcol(inp["ln2_g"][0]), lnb=pcol(inp["ln2_b"][0])))
    return run_spmd(cx, maps)


def build_A1():
    cx = Ctx()
    P = cx.P
    xT = cx.inp("xT", [D, NT])
    cs_d = cx.inp("cs", [128, 16])
    wada_d = cx.inp("wada", [D, 6 * D])
    bada_d = cx.inp("bada", [128, 48])
    win_d = cx.inp("win", [D, 2208])
    gq_d = cx.inp("gql", [128, 3])
    gkv_d = cx.inp("gkvl", [128, 2])
    wqup_d = cx.inp("wqup", [384, 768])
    wkvk_d = cx.inp("wkvk", [256, 512])
    wkvv_d = cx.inp("wkvv", [256, 512])
    cos96_d = cx.inp("cos96", [96, NT])
    sin96_d = cx.inp("sin96", [96, NT])
    rot96_d = cx.inp("rot96", [96, 96])
    mod_o = cx.out("mod", [128, 96])
    qT_o = cx.out("qT", [768, NT], BF16)
    kT_o = cx.out("kT", [768, NT], BF16)
    v_o = cx.out("v", [NT, 512], BF16)
    nqT_o = cx.out("nqT", [512, NT], BF16)
    nkT_o = cx.out("nkT", [512, NT], BF16)
    nv_o = cx.out("nv", [NT, 512], BF16)

    pq_r = cx.psring("pq", 2)
    pms_r = cx.psring("pms", 1)
    pqh_r = cx.psring("pqh", 2)
    prot_r = cx.psring("prot", 1)
    pv_r = cx.psring("pv", 2)
    mod_sb = cx.sb("mod_sb", [128, 96])
    modp_sb = cx.sb("modp_sb", [128, 96])
    emit_ada(cx, cs_d, wada_d, bada_d, mod_sb, modp_sb, pm=pv_r.items[0][0], pmres=pv_r.items[0][1])
    cx.store("sp", mod_o, mod_sb[:], "mod")

    win, winres = load_cast_w(cx, "win_sb", win_d, 8, 2208)
    wqup, wqres = load_cast_w(cx, "wqup_sb", wqup_d, 3, 768)
    wkvk, wkres = load_cast_w(cx, "wkvk_sb", wkvk_d, 2, 512)
    wkvv, wvres = load_cast_w(cx, "wkvv_sb", wkvv_d, 2, 512)
    rot96 = cx.sb("rot96_sb", [96, 96], BF16)
    cx.load("pool", rot96[:], rot96_d, "rot96")
    cos96 = cx.sb("cos96_sb", [96, NT])
    sin96 = cx.sb("sin96_sb", [96, NT])
    cx.load("sp", cos96[:], cos96_d, "cos96")
    cx.load("sp", sin96[:], sin96_d, "sin96")
    gq = cx.sb("gq_sb", [128, 3])
    gkv = cx.sb("gkv_sb", [128, 2])
    cx.load("sp", gq[:], gq_d, "gq")
    cx.load("sp", gkv[:], gkv_d, "gkv")
    ones_q = cx.sb("ones_q", [128, 128], BF16)
    ones_kv = cx.sb("ones_kv", [128, 128], BF16)
    P.op("dve", lambda e: e.memset(ones_q[:], 1.0 / 384), writes=["ones_q"])
    P.op("dve", lambda e: e.memset(ones_kv[:], 1.0 / 256), writes=["ones_kv"])
    eps_sb = cx.sb("eps_sb", [128, 1])
    P.op("dve", lambda e: e.memset(eps_sb[:], EPS), writes=["eps"])

    xr = cx.ring("x", 2, [128, 8, 512])
    ur = cx.ring("u", 2, [128, 8, 512], BF16)
    lat_r = cx.ring("lat", 1, [128, 5, 512])
    latn_r = cx.ring("latn", 1, [128, 5, 512], BF16)
    sq_r = cx.ring("sq", 2, [128, 512], BF16)
    rs_r = cx.ring("rs", 2, [128, 512])
    qb_r = cx.ring("qb", 2, [96, 512], BF16)
    t1_r = cx.ring("t1", 2, [96, 512])
    t2_r = cx.ring("t2", 2, [96, 512])
    qf_r = cx.ring("qf", 3, [96, 512], BF16)
    ob_r = cx.ring("ob", 3, [128, 512], BF16)
    xv = xT.rearrange("(k p) t -> p k t", p=128)

    def do_tile(t0, tw, col):
        x, xres = xr.nxt()
        cx.load("sp", x[:, :, :tw], xv[:, :, t0:t0 + tw], xres)
        u, ures = ur.nxt()
        emit_modulate(cx, x, xres, u, ures, tw, col, mod_sb, modp_sb, 0)
        ureads = [(ures, k) for k in range(8)]
        lat, latres = lat_r.nxt()
        latn, latnres = latn_r.nxt()

        def proj(ps, c0, m, n0=0, n1=None):
            n1 = tw if n1 is None else n1

            def f(e):
                ins = None
                for k in range(8):
                    ins = e.matmul(ps[0:m, :n1 - n0], win[:, k, c0:c0 + m], u[:, k, n0:n1], start=(k == 0), stop=(k == 7))
                return ins
            return f
        for grp, (c_lo, nch, ones, oneres, gain, gres) in enumerate(((0, 3, ones_q, "ones_q", gq, "gq"),
                                                                       (384, 2, ones_kv, "ones_kv", gkv, "gkv"))):
            base = 0 if grp == 0 else 3
            pms, pmsres = pms_r.nxt()
            for c in range(nch):
                pq, pqres = pq_r.nxt()
                P.op("pe", proj(pq, c_lo + c * 128, 128), reads=ureads + winres, writes=[pqres])
                sq, sqres = sq_r.nxt()
                P.op("act", lambda e, sq=sq, pq=pq: e.activation(out=sq[:, :tw], in_=pq[:, :tw], func=AF.Square), reads=[pqres], writes=[sqres])
                P.op("dve", lambda e, pq=pq, c=c, base=base: e.tensor_copy(out=lat[:, base + c, :tw], in_=pq[:, :tw]),
                     reads=[pqres], writes=[(latres, base + c)])
                P.op("pe", lambda e, pms=pms, sq=sq, c=c, nch=nch, ones=ones: e.matmul(pms[:, :tw], ones[:], sq[:, :tw], start=(c == 0), stop=(c == nch - 1)),
                     reads=[sqres, oneres], writes=[pmsres])
            rs, rsres = rs_r.nxt()
            P.op("act", lambda e, rs=rs, pms=pms: e.activation(out=rs[:, :tw], in_=pms[:, :tw], func=AF.Sqrt, bias=eps_sb[:, 0:1]),
                 reads=[pmsres, "eps"], writes=[rsres])
            P.op("dve", lambda e, rs=rs: e.reciprocal(out=rs[:, :tw], in_=rs[:, :tw]), reads=[rsres], writes=[rsres])
            for c in range(nch):
                P.op("dve", lambda e, c=c, base=base, rs=rs, gain=gain: e.scalar_tensor_tensor(
                    out=latn[:, base + c, :tw], in0=lat[:, base + c, :tw], scalar=gain[:, c:c + 1], in1=rs[:, :tw], op0=ALU.mult, op1=ALU.mult),
                    reads=[(latres, base + c), rsres, gres], writes=[(latnres, base + c)])
        cqn_reads = [(latnres, c) for c in range(3)]
        ckvn_reads = [(latnres, 3 + c) for c in range(2)]
        def head_rope(pqh, pqres, nrows_lo, stores):
            qb, qbres = qb_r.nxt()
            P.op("dve", lambda e: e.tensor_copy(out=qb[:, :tw], in_=pqh[0:96, :tw]), reads=[pqres], writes=[qbres])
            prot, protres = prot_r.nxt()
            P.op("pe", lambda e: e.matmul(prot[0:96, :tw], rot96[:], qb[:, :tw], start=True, stop=True), reads=[qbres, "rot96"], writes=[protres])
            t1, t1res = t1_r.nxt()
            P.op("dve", lambda e: e.tensor_tensor(out=t1[:, :tw], in0=pqh[0:96, :tw], in1=cos96[:, t0:t0 + tw], op=ALU.mult),
                 reads=[pqres, "cos96"], writes=[t1res])
            t2, t2res = t2_r.nxt()
            P.op("dve", lambda e: e.tensor_tensor(out=t2[:, :tw], in0=prot[0:96, :tw], in1=sin96[:, t0:t0 + tw], op=ALU.mult),
                 reads=[protres, "sin96"], writes=[t2res])
            qf, qfres = qf_r.nxt()
            P.op("dve", lambda e: e.tensor_tensor(out=qf[:, :tw], in0=t1[:, :tw], in1=t2[:, :tw], op=ALU.add), reads=[t1res, t2res], writes=[qfres])
            for (dst, lo, hi) in stores:
                cx.store("sp", dst, qf[lo:hi, :tw], qfres)

        pkr, pkrres = pqh_r.nxt()

        def mmkr(e):
            ins = None
            for k in range(8):
                ins = e.matmul(pkr[64:96, :tw], win[:, k, 640:672], u[:, k, :tw], start=(k == 0), stop=(k == 7))
            return ins
        P.op("dve", lambda e: e.memset(pkr[0:64, :tw], 0.0), writes=[pkrres])
        P.op("pe", mmkr, reads=ureads + winres, writes=[pkrres])
        head_rope(pkr, pkrres, 64, [(kT_o[h * 96 + 64:h * 96 + 96, t0:t0 + tw], 64, 96) for h in range(8)])
        for h in range(8):
            pqh, pqres = pqh_r.nxt()

            def mmq(e, pqh=pqh, h=h):
                ins = None
                for k in range(3):
                    ins = e.matmul(pqh[0:96, :tw], wqup[:, k, h * 96:(h + 1) * 96], latn[:, k, :tw], start=(k == 0), stop=(k == 2))
                return ins
            P.op("pe", mmq, reads=cqn_reads + wqres, writes=[pqres])
            head_rope(pqh, pqres, 0, [(qT_o[h * 96:(h + 1) * 96, t0:t0 + tw], 0, 96)])
        for hp in range(4):
            pq, pqres = pq_r.nxt()

            def mmk(e, pq=pq, hp=hp):
                ins = None
                for k in range(2):
                    ins = e.matmul(pq[:, :tw], wkvk[:, k, hp * 128:(hp + 1) * 128], latn[:, 3 + k, :tw], start=(k == 0), stop=(k == 1))
                return ins
            P.op("pe", mmk, reads=ckvn_reads + wkres, writes=[pqres])
            ob, obres = ob_r.nxt()
            P.op("act", lambda e, ob=ob, pq=pq: e.activation(out=ob[:, :tw], in_=pq[:, :tw], func=AF.Copy), reads=[pqres], writes=[obres])
            for hh in range(2):
                h = 2 * hp + hh
                cx.store("pool", kT_o[h * 96:h * 96 + 64, t0:t0 + tw], ob[hh * 64:(hh + 1) * 64, :tw], obres)
        for (dst, c_lo) in ((nqT_o, 672), (nkT_o, 1184)):
            for c in range(4):
                pq, pqres = pq_r.nxt()
                P.op("pe", proj(pq, c_lo + c * 128, 128), reads=ureads + winres, writes=[pqres])
                ob, obres = ob_r.nxt()
                P.op("act", lambda e, ob=ob, pq=pq: e.activation(out=ob[:, :tw], in_=pq[:, :tw], func=AF.Copy), reads=[pqres], writes=[obres])
                cx.store("pool", dst[c * 128:(c + 1) * 128, t0:t0 + tw], ob[:, :tw], obres)
        for tb in range(tw // 128):
            pv, pvres = pv_r.nxt()

            def mmv(e, pv=pv, tb=tb):
                ins = None
                for k in range(2):
                    ins = e.matmul(pv[:, :512], latn[:, 3 + k, tb * 128:(tb + 1) * 128], wkvv[:, k, :], start=(k == 0), stop=(k == 1))
                return ins
            P.op("pe", mmv, reads=ckvn_reads + wvres, writes=[pvres])
            ob, obres = ob_r.nxt()
            P.op("act", lambda e, ob=ob, pv=pv: e.activation(out=ob[:, :512], in_=pv[:, :512], func=AF.Copy), reads=[pvres], writes=[obres])
            cx.store("pool", v_o[t0 + tb * 128:t0 + (tb + 1) * 128, :], ob[:, :512], obres)
            pv, pvres = pv_r.nxt()

            def mmnv(e, pv=pv, tb=tb):
                ins = None
                for k in range(8):
                    ins = e.matmul(pv[:, :512], u[:, k, tb * 128:(tb + 1) * 128], win[:, k, 1696:2208], start=(k == 0), stop=(k == 7))
                return ins
            P.op("pe", mmnv, reads=ureads + winres, writes=[pvres])
            ob, obres = ob_r.nxt()
            P.op("act", lambda e, ob=ob, pv=pv: e.activation(out=ob[:, :512], in_=pv[:, :512], func=AF.Copy), reads=[pvres], writes=[obres])
            cx.store("pool", nv_o[t0 + tb * 128:t0 + (tb + 1) * 128, :], ob[:, :512], obres)

    for (t0, tw, col) in tiles_own_ctx():
        do_tile(t0, tw, col)
    return cx


def rope_tables32(j):
    t = np.arange(TOWN) + j * TOWN
    row = (t // GRID_W).astype(np.float32)
    colp = (t % GRID_W).astype(np.float32)
    inv = (10000.0 ** (-np.arange(8, dtype=np.float64) / 8)).astype(np.float32)
    cos = np.ones((96, NT), np.float32)
    sin = np.zeros((96, NT), np.float32)
    for d in range(32):
        pos = row if d < 16 else colp
        ang = (pos * inv[d % 8]).astype(np.float32)
        cos[64 + d, :TOWN] = np.cos(ang)
        sin[64 + d, :TOWN] = np.sin(ang)
    return cos, sin


def rot96():
    m = np.zeros((96, 96), np.float32)
    m[64:, 64:] = rot_matrix(32, 16)
    return m


def stage_A1(inp, xT_list):
    cx = build_A1()
    wkv = inp["od_w_kv_up"][0].reshape(256, 8, 128)
    maps = []
    for c in range(NCORES):
        cos, sin = rope_tables32(c % 4)
        maps.append(dict(
            xT=xT_list[c], cs=core_cs(inp["c"], inp["c_ctx"], c),
            wada=np.ascontiguousarray(inp["w_ada"][1]), bada=pcol(inp["b_ada"][1]),
            win=np.ascontiguousarray(inp["od_w_in"][0]),
            gql=pcol(inp["od_q_lat_gain"][0]), gkvl=pcol(inp["od_kv_lat_gain"][0]),
            wqup=np.ascontiguousarray(inp["od_w_q_up"][0]),
            wkvk=np.ascontiguousarray(wkv[:, :, :64].reshape(256, 512)),
            wkvv=np.ascontiguousarray(wkv[:, :, 64:].reshape(256, 512)),
            cos96=cos, sin96=sin, rot96=rot96()))
    return run_spmd(cx, maps)


NA_SCALE = 0.125


def build_NA():
    cx = Ctx()
    P = cx.P
    nq_d = cx.inp("nqT", [512, TOWN], BF16)
    nk_d = cx.inp("nkTh", [512, TOWN + 512], BF16)
    nv_d = cx.inp("nvh", [128, 36 * 8 * 65], BF16)
    kc_d = cx.inp("nkc", [512, CTX], BF16)
    vc_d = cx.inp("nvc", [128, 2 * 8 * 65], BF16)
    bm_d = {n: cx.inp(n, [128, 48 * 256]) for n in ("bm_int", "bm_first", "bm_last")}
    id_d = cx.inp("ident", [128, 128])
    sel_d = cx.inp("sel", [65, 64])
    o_d = cx.out("naT", [512, TOWN], BF16)

    kh = cx.sb("kh", [128, 4, TOWN + 512], BF16)
    qs = cx.sb("qs", [128, 4, TOWN], BF16)
    kcs = cx.sb("kcs", [128, 4, CTX], BF16)
    vb = cx.sb("vb", [128, 36, 8, 65], BF16)
    vc = cx.sb("vc", [128, 2, 8, 65], BF16)
    cx.load("sp", kh[:], nk_d.rearrange("(c p) t -> p c t", p=128), "kh")
    cx.load("sp", qs[:], nq_d.rearrange("(c p) t -> p c t", p=128), "qs")
    cx.load("sp", kcs[:], kc_d.rearrange("(c p) t -> p c t", p=128), "kcs")
    cx.load("sp", vb[:].rearrange("p a h d -> p (a h d)"), nv_d, "vb")
    cx.load("sp", vc[:].rearrange("p a h d -> p (a h d)"), vc_d, "vc")
    ident = cx.sb("ident_sb", [128, 128], BF16)
    cx.load("pool", ident[:], id_d, "ident")
    sel = cx.sb("sel_sb", [65, 64])
    cx.load("sp", sel[:], sel_d, "sel")
    bmi = cx.sb("bmi", [128, 48, 256], BF16)
    bme = cx.sb("bme", [128, 48, 256], BF16)
    stg_r = cx.ring("stg", 2, [128, 6, 256])

    def load_table(dst, dres, name):
        src = bm_d[name].rearrange("p (a q) -> p a q", q=256)
        for h in range(8):
            stg, sres = stg_r.nxt()
            cx.load("sp", stg[:], src[:, h * 6:(h + 1) * 6, :], sres)
            P.op("dve", lambda e, stg=stg, h=h: e.tensor_scalar_mul(out=dst[:, h * 6:(h + 1) * 6, :], in0=stg[:], scalar1=1.0 / NA_SCALE),
                 reads=[sres], writes=[(dres, h)])
    load_table(bmi, "bmi", "bm_int")
    load_table(bme, "bme", "bm_first")

    ps_r = cx.psring("ps", 4)
    po_r = cx.psring("po", 2)
    pb_r = cx.psring("pb", 1)
    pt_r = cx.ring("pt", 2, [128, 8, 256], BF16)
    os_r = cx.ring("os", 2, [65, 256])
    at_r = cx.ring("at", 2, [64, 256], BF16)

    def unit(m, h):
        hp, pb0 = h // 2, (h % 2) * 64
        tab, tres = (bme, "bme") if m in (0, 15) else (bmi, "bmi")
        pt, ptres = pt_r.nxt()
        q0 = 256 * m
        for bk in range(4):
            ps, psres = ps_r.nxt()

            def mm(e, ps=ps, bk=bk):
                ins = None
                for cc in range(2):
                    c = 2 * bk + cc
                    o = cc * 256
                    if c < 6:
                        e.matmul(ps[:, o:o + 256], kh[pb0:pb0 + 64, hp, q0 + c * 128:q0 + (c + 1) * 128], qs[pb0:pb0 + 64, hp, q0:q0 + 256],
                                 start=True, stop=False)
                        ins = e.matmul(ps[:, o:o + 256], ident[:], tab[:, h * 6 + c, :], start=False, stop=True)
                    else:
                        ins = e.matmul(ps[:, o:o + 256], kcs[pb0:pb0 + 64, hp, (c - 6) * 128:(c - 5) * 128], qs[pb0:pb0 + 64, hp, q0:q0 + 256],
                                       start=True, stop=True)
                return ins
            P.op("pe", mm, reads=["kh", "qs", "kcs", "ident", (tres, h)], writes=[psres])
            P.op("act", lambda e, ps=ps, bk=bk: e.activation(out=pt[:, 2 * bk:2 * bk + 2, :].rearrange("p a q -> p (a q)"), in_=ps[:, :],
                                                            func=AF.Exp, scale=NA_SCALE),
                 reads=[psres], writes=[(ptres, bk)])
        po, pores = po_r.nxt()

        def pv(e):
            ins = None
            for c in range(8):
                lhs = vb[:, 2 * m + c, h, :] if c < 6 else vc[:, c - 6, h, :]
                ins = e.matmul(po[0:65, :256], lhs, pt[:, c, :], start=(c == 0), stop=(c == 7))
            return ins
        P.op("pe", pv, reads=[(ptres, bk) for bk in range(4)] + ["vb", "vc"], writes=[pores])
        osb, osres = os_r.nxt()
        P.op("act", lambda e: e.activation(out=osb[:, :], in_=po[0:65, :256], func=AF.Copy), reads=[pores], writes=[osres])
        P.op("dve", lambda e: e.reciprocal(out=osb[64:65, :], in_=osb[64:65, :]), reads=[osres], writes=[osres])
        pbk, pbres = pb_r.nxt()
        P.op("pe", lambda e: e.matmul(pbk[0:64, :256], sel[:], osb[:, :], start=True, stop=True), reads=[osres, "sel"], writes=[pbres])
        at, atres = at_r.nxt()
        P.op("dve", lambda e: e.tensor_tensor(out=at[:, :], in0=osb[0:64, :], in1=pbk[0:64, :256], op=ALU.mult), reads=[osres, pbres], writes=[atres])
        cx.store("pool", o_d[h * 64:(h + 1) * 64, q0:q0 + 256], at[:, :], atres)

    for m in range(16):
        if m == 15:
            load_table(bme, "bme", "bm_last")
        for h in range(8):
            unit(m, h)
    return cx


def na_table(bias, R0):
    kr_rel = np.arange(12)
    kr = R0 - 4 + kr_rel
    qr = R0 + np.arange(4)
    rs = np.clip(qr - 4, 0, 256 - 8)
    row_ok = (kr[:, None] >= rs[None, :]) & (kr[:, None] < rs[None, :] + 8)
    row_idx = np.clip(kr[:, None] - qr[None, :] + 7, 0, 14)
    kc = np.arange(64)
    qc = np.arange(64)
    cs0 = np.clip(qc - 8, 0, 48)
    col_ok = (kc[:, None] >= cs0[None, :]) & (kc[:, None] < cs0[None, :] + 16)
    col_idx = np.clip(kc[:, None] - qc[None, :] + 15, 0, 30)
    ok = row_ok[:, None, :, None] & col_ok[None, :, None, :]
    g = bias[:, row_idx[:, None, :, None], col_idx[None, :, None, :]]
    t = np.where(ok[None], g, np.float32(-30000.0)).astype(np.float32)
    t = t.reshape(8, 6, 2, 64, 256)
    t = t.transpose(2, 3, 0, 1, 4).reshape(128, 48 * 256)
    return np.ascontiguousarray(t)


def with_ones(v):
    o = np.ones(v.shape[:-1] + (65,), v.dtype)
    o[..., :64] = v
    return o


def stage_NA(inp, resA1):
    cx = build_NA()
    bias = inp["od_na_bias"][0]
    t_int = na_table(bias, 8)
    t_first = na_table(bias, 0)
    t_last = na_table(bias, 252)
    maps = []
    for c in range(NCORES):
        b, j = divmod(c, 4)
        nk = np.asarray(resA1[c]["nkT"])
        nv = np.asarray(resA1[c]["nv"])
        kh = np.zeros((512, TOWN + 512), NPBF)
        vh = np.zeros((TOWN + 512, 512), NPBF)
        kh[:, 256:256 + TOWN] = nk[:, :TOWN]
        vh[256:256 + TOWN] = nv[:TOWN]
        if j > 0:
            kh[:, :256] = np.asarray(resA1[c - 1]["nkT"])[:, TOWN - 256:TOWN]
            vh[:256] = np.asarray(resA1[c - 1]["nv"])[TOWN - 256:TOWN]
        if j < 3:
            kh[:, 256 + TOWN:] = np.asarray(resA1[c + 1]["nkT"])[:, :256]
            vh[256 + TOWN:] = np.asarray(resA1[c + 1]["nv"])[:256]
        vhl = with_ones(vh.reshape(36, 128, 8, 64).transpose(1, 0, 2, 3)).reshape(128, -1)
        vcl = with_ones(nv[TOWN:].reshape(2, 128, 8, 64).transpose(1, 0, 2, 3)).reshape(128, -1)
        maps.append(dict(
            nqT=np.ascontiguousarray(np.asarray(resA1[c]["nqT"])[:, :TOWN]), nkTh=kh, nvh=np.ascontiguousarray(vhl),
            nkc=np.ascontiguousarray(nk[:, TOWN:]), nvc=np.ascontiguousarray(vcl),
            bm_int=t_int, bm_first=(t_first if j == 0 else t_int), bm_last=(t_last if j == 3 else t_int),
            ident=np.eye(128, dtype=np.float32), sel=SEL))
    return run_spmd(cx, maps)


def stage_B1(resA1):
    cx = build_attn(8, 1, 96, 130, 96 ** -0.5)
    kv = gather_kv(resA1, "kT", "v", 8, 96)
    maps = []
    for c in range(NCORES):
        kf, vf = kv[c // 4]
        maps.append(dict(qT=np.asarray(resA1[c]["qT"]), kT=kf, v=vf, sel=SEL))
    return run_spmd(cx, maps)


def build_C1b():
    cx = Ctx()
    P = cx.P
    NE, NFE, FG = 8, 28, 4
    NG = NFE // FG
    ST, SUB = 1024, 512
    xT = cx.inp("xT", [D, NT])
    mod_d = cx.inp("mod", [128, 96])
    wr_d = cx.inp("wr", [D, NE])
    br_d = cx.inp("br", [NE, 1])
    wg_d = cx.inp("wg", [NE, D, NFE * 128])
    wu_d = cx.inp("wu", [NE, D, NFE * 128])
    wd_d = cx.inp("wd", [NE, NFE * 128, D])
    g_d = cx.inp("lng", [128, 8])
    b_d = cx.inp("lnb", [128, 8])
    id_d = cx.inp("ident", [128, 128])
    oh_d = cx.inp("onehot", [NE, NE * 128])
    xo = cx.out("xo", [D, TOWN])

    mod_sb = cx.sb("mod_sb", [128, 96])
    modp_sb = cx.sb("modp_sb", [128, 96])
    cx.load("sp", mod_sb[:], mod_d, "mod")
    P.op("dve", lambda e: e.tensor_scalar_add(out=modp_sb[:], in0=mod_sb[:], scalar1=1.0), reads=["mod"], writes=["modp"], fence=True)
    g_sb = cx.sb("g_sb", [128, 8])
    b_sb = cx.sb("b_sb", [128, 8])
    cx.load("sp", g_sb[:], g_d, "lngb")
    cx.load("sp", b_sb[:], b_d, "lngb2")
    P.op("dve", lambda e: e.tensor_copy(out=b_sb[:], in_=b_sb[:]), reads=["lngb2"], writes=["lngb"], fence=True)
    wr = cx.sb("wr_sb", [128, 8, NE])
    cx.load("sp", wr[:], wr_d.rearrange("(k p) e -> p k e", p=128), "wr")
    br = cx.sb("br_sb", [NE, 1])
    cx.load("sp", br[:], br_d, "br")
    ident = cx.sb("ident_sb", [128, 128])
    cx.load("sp", ident[:], id_d, "ident")
    oneh = cx.sb("oneh_sb", [NE, NE * 128])
    cx.load("sp", oneh[:], oh_d, "oneh")
    ln = LNState(cx, "ln", SUB)

    yacc = cx.sb("yacc", [128, 8, ST])
    u2 = cx.sb("u2", [128, 8, ST], BF16)
    gb = cx.sb("gb", [128, NE, ST], BF16)
    gT = cx.sb("gT", [NE, ST])
    x_r = cx.ring("x", 1, [128, 8, SUB])
    uf_r = cx.ring("uf", 1, [128, 8, SUB])
    lg_r = cx.ring("lg", 1, [NE, SUB])
    lgT_r = cx.ring("lgT", 2, [128, 8])
    mx_r = cx.ring("mx", 2, [128, 8])
    dm_r = cx.ring("dm", 2, [128, 2])
    ga_r = cx.ring("ga", 2, [128, 8])
    gq_r = cx.ring("gq", 2, [128, 8])
    wgq_r = cx.ring("wgq", 2, [128, 8, FG * 128], BF16)
    wuq_r = cx.ring("wuq", 2, [128, 8, FG * 128], BF16)
    wdq_r = cx.ring("wdq", 2, [128, FG, D], BF16)
    sg_r = cx.ring("sg", 2, [128, SUB])
    h1_r = cx.ring("h1", 2, [128, SUB])
    hq_r = cx.ring("hq", 2, [128, FG, SUB], BF16)
    pg_r = cx.psring("pg", 2)
    pu_r = cx.psring("pu", 2)
    pd_r = cx.psring("pd", 2)
    xv = xT.rearrange("(k p) t -> p k t", p=128)
    ov = xo.rearrange("(k p) t -> p k t", p=128)

    def router(s0, st):
        t0 = s0 + st * SUB
        x, xres = x_r.nxt()
        cx.load("sp", x[:, :, :], xv[:, :, t0:t0 + SUB], (xres, "ld"), reads=[(xres, k) for k in range(8)])
        uf, ufres = uf_r.nxt()
        for k in range(8):
            sc = modp_sb[:, 64 + 2 * k:64 + 2 * k + 1]
            sh = mod_sb[:, 48 + 2 * k:48 + 2 * k + 1]
            if k % 2 == 0:
                P.op("act", lambda e, k=k, sc=sc, sh=sh: e.activation(out=uf[:, k, :], in_=x[:, k, :], func=AF.Identity, bias=sh, scale=sc),
                     reads=[(xres, "ld"), "mod", "modp"], writes=[(ufres, k)])
            else:
                P.op("dve", lambda e, k=k, sc=sc, sh=sh: e.tensor_scalar(out=uf[:, k, :], in0=x[:, k, :], scalar1=sc, scalar2=sh,
                                                                        op0=ALU.mult, op1=ALU.add),
                     reads=[(xres, "ld"), "mod", "modp"], writes=[(ufres, k)])
        ufreads = [(ufres, k) for k in range(8)]
        P.op("dve", lambda e: e.tensor_copy(out=u2[:, :, st * SUB:(st + 1) * SUB], in_=uf[:, :, :]), reads=ufreads, writes=[("u2", st)])
        plg, plgres = pg_r.nxt()

        def mml(e):
            ins = None
            for k in range(8):
                ins = e.matmul(plg[0:NE, :SUB], wr[:, k, :], uf[:, k, :], start=(k == 0), stop=(k == 7))
            return ins
        P.op("pe", mml, reads=ufreads + ["wr"], writes=[plgres])
        lg, lgres = lg_r.nxt()
        P.op("act", lambda e: e.activation(out=lg[:, :], in_=plg[0:NE, :SUB], func=AF.Identity, bias=br[:, 0:1]), reads=[plgres, "br"], writes=[lgres])
        for tb in range(SUB // 128):
            pt, ptres = pu_r.nxt()
            P.op("pe", lambda e, pt=pt, tb=tb: e.transpose(pt[:, 0:NE], lg[:, tb * 128:(tb + 1) * 128], ident[0:NE, 0:NE]),
                 reads=[lgres, "ident"], writes=[ptres])
            lgT, lgTres = lgT_r.nxt()
            mx, mxres = mx_r.nxt()
            dm, dmres = dm_r.nxt()
            ga, gares = ga_r.nxt()
            gq, gqres = gq_r.nxt()
            P.op("dve", lambda e, pt=pt, lgT=lgT: e.tensor_copy(out=lgT[:], in_=pt[:, 0:NE]), reads=[ptres], writes=[lgTres], fence=True)
            P.op("dve", lambda e, mx=mx, lgT=lgT: e.max(out=mx[:], in_=lgT[:]), reads=[lgTres], writes=[mxres], fence=True)
            P.op("dve", lambda e, mx=mx, dm=dm: e.tensor_tensor(out=dm[:, 0:1], in0=mx[:, 1:2], in1=mx[:, 0:1], op=ALU.subtract),
                 reads=[mxres], writes=[(dmres, 0)], fence=True)
            P.op("act", lambda e, dm=dm: e.activation(out=dm[:, 1:2], in_=dm[:, 0:1], func=AF.Sigmoid), reads=[(dmres, 0)], writes=[(dmres, 1)], fence=True)
            P.op("dve", lambda e, dm=dm: e.tensor_scalar(out=dm[:, 0:1], in0=dm[:, 1:2], scalar1=-1.0, scalar2=1.0, op0=ALU.mult, op1=ALU.add),
                 reads=[(dmres, 1)], writes=[(dmres, 0)], fence=True)
            P.op("dve", lambda e, ga=ga, lgT=lgT, mx=mx, dm=dm: e.tensor_scalar(out=ga[:], in0=lgT[:], scalar1=mx[:, 0:1], scalar2=dm[:, 0:1],
                                                                             op0=ALU.is_equal, op1=ALU.mult),
                 reads=[lgTres, mxres, (dmres, 0)], writes=[gares], fence=True)
            P.op("dve", lambda e, gq=gq, lgT=lgT, mx=mx, dm=dm: e.tensor_scalar(out=gq[:], in0=lgT[:], scalar1=mx[:, 1:2], scalar2=dm[:, 1:2],
                                                                             op0=ALU.is_equal, op1=ALU.mult),
                 reads=[lgTres, mxres, (dmres, 1)], writes=[gqres], fence=True)
            P.op("dve", lambda e, ga=ga, gq=gq: e.tensor_tensor(out=ga[:], in0=ga[:], in1=gq[:], op=ALU.add), reads=[gares, gqres], writes=[gares], fence=True)
            pg2, pg2res = pu_r.nxt()
            P.op("pe", lambda e, pg2=pg2, ga=ga: e.transpose(pg2[0:NE, 0:128], ga[:, :], ident[:, :]), reads=[gares, "ident"], writes=[pg2res])
            c0 = st * SUB + tb * 128
            P.op("dve", lambda e, pg2=pg2, c0=c0: e.tensor_copy(out=gT[:, c0:c0 + 128], in_=pg2[0:NE, 0:128]), reads=[pg2res], writes=[("gT", c0)], fence=True)

    def gate_bcast():
        for e_ in range(NE):
            for hf in range(ST // 512):
                pgb, pgbres = pg_r.nxt()
                P.op("pe", lambda e, pgb=pgb, e_=e_, hf=hf: e.matmul(pgb[:, :512], oneh[:, e_ * 128:(e_ + 1) * 128], gT[:, hf * 512:(hf + 1) * 512],
                                                                    start=True, stop=True),
                     reads=[("gT", c0) for c0 in range(hf * 512, (hf + 1) * 512, 128)] + ["oneh"], writes=[pgbres])
                P.op("act", lambda e, pgb=pgb, e_=e_, hf=hf: e.activation(out=gb[:, e_, hf * 512:(hf + 1) * 512], in_=pgb[:, :512], func=AF.Copy),
                     reads=[pgbres], writes=[("gb", e_, hf)])

    def load_w(e_, g):
        wgq, wgres = wgq_r.nxt()
        wuq, wures = wuq_r.nxt()
        wdq, wdres = wdq_r.nxt()
        c0 = g * FG * 128
        cx.load("pool", wgq[:], wg_d[e_, :, c0:c0 + FG * 128].rearrange("(k p) c -> p k c", p=128), wgres)
        cx.load("pool", wuq[:], wu_d[e_, :, c0:c0 + FG * 128].rearrange("(k p) c -> p k c", p=128), wures)
        cx.load("pool", wdq[:], wd_d[e_, c0:c0 + FG * 128, :].rearrange("(f p) c -> p f c", p=128), wdres)
        return (wgq, wgres, wuq, wures, wdq, wdres)

    def expert_unit(e_, g, W, first):
        wgq, wgres, wuq, wures, wdq, wdres = W

        def sub(st):
            ureads = [("u2", st)]
            hq, hqres = hq_r.nxt()
            for f in range(FG):
                pg, pgres = pg_r.nxt()
                pu, pures = pu_r.nxt()

                def mmg(e, pg=pg, f=f):
                    ins = None
                    for k in range(8):
                        ins = e.matmul(pg[:, :SUB], wgq[:, k, f * 128:(f + 1) * 128], u2[:, k, st * SUB:(st + 1) * SUB], start=(k == 0), stop=(k == 7))
                    return ins

                def mmu(e, pu=pu, f=f):
                    ins = None
                    for k in range(8):
                        ins = e.matmul(pu[:, :SUB], wuq[:, k, f * 128:(f + 1) * 128], u2[:, k, st * SUB:(st + 1) * SUB], start=(k == 0), stop=(k == 7))
                    return ins
                P.op("pe", mmg, reads=ureads + [wgres], writes=[pgres])
                P.op("pe", mmu, reads=ureads + [wures], writes=[pures])
                sg, sgres = sg_r.nxt()
                P.op("act", lambda e, sg=sg, pg=pg: e.activation(out=sg[:, :], in_=pg[:, :SUB], func=AF.Silu), reads=[pgres], writes=[sgres])
                h1, h1res = h1_r.nxt()
                P.op("dve", lambda e, sg=sg, pu=pu, h1=h1: e.tensor_tensor(out=h1[:, :], in0=sg[:, :], in1=pu[:, :SUB], op=ALU.mult),
                     reads=[sgres, pures], writes=[h1res])
                P.op("dve", lambda e, h1=h1, f=f: e.tensor_tensor(out=hq[:, f, :], in0=h1[:, :], in1=gb[:, e_, st * SUB:(st + 1) * SUB], op=ALU.mult),
                     reads=[h1res, ("gb", e_, (st * SUB) // 512)], writes=[(hqres, f)])
            hreads = [(hqres, f) for f in range(FG)]
            for oc in range(8):
                pd, pdres = pd_r.nxt()

                def mmd(e, pd=pd, oc=oc):
                    ins = None
                    for f in range(FG):
                        ins = e.matmul(pd[:, :SUB], wdq[:, f, oc * 128:(oc + 1) * 128], hq[:, f, :], start=(f == 0), stop=(f == FG - 1))
                    return ins
                P.op("pe", mmd, reads=hreads + [wdres], writes=[pdres])
                ya = yacc[:, oc, st * SUB:(st + 1) * SUB]
                if first:
                    P.op("dve", lambda e, pd=pd, ya=ya: e.tensor_copy(out=ya, in_=pd[:, :SUB]), reads=[pdres], writes=[("yacc", oc, st)])
                else:
                    P.op("dve", lambda e, pd=pd, ya=ya: e.tensor_tensor(out=ya, in0=pd[:, :SUB], in1=ya, op=ALU.add),
                         reads=[pdres, ("yacc", oc, st)], writes=[("yacc", oc, st)])

        for st in range(ST // SUB):
            sub(st)

    def finish(s0, st):
        t0 = s0 + st * SUB
        x, xres = x_r.nxt()
        cx.load("sp", x[:, :, :], xv[:, :, t0:t0 + SUB], (xres, "ld"), reads=[(xres, k) for k in range(8)])
        for oc in range(8):
            P.op("act", lambda e, oc=oc: e.activation(out=x[:, oc, :], in_=x[:, oc, :], func=AF.Identity, scale=ALPHA),
                 reads=[(xres, "ld")], writes=[(xres, oc)])
            gate = mod_sb[:, 80 + 2 * oc:80 + 2 * oc + 1]
            P.op("dve", lambda e, oc=oc, gate=gate: e.scalar_tensor_tensor(out=x[:, oc, :], in0=yacc[:, oc, st * SUB:(st + 1) * SUB], scalar=gate,
                                                                        in1=x[:, oc, :], op0=ALU.mult, op1=ALU.add),
                 reads=[("yacc", oc, st), (xres, oc), "mod"], writes=[(xres, oc)])
        emit_layernorm(cx, ln, x, lambda k: (xres, k), SUB, g_sb, b_sb, "lngb")
        P.dma("sp", "S_" + xres, lambda e, s: e.dma_start(out=ov[:, :, t0:t0 + SUB], in_=x[:, :, :]).then_inc(s, 16),
              reads=[(xres, k) for k in range(8)], writes=[(xres, "ld")])

    units = [(e_, g) for e_ in range(NE) for g in range(NG)]
    for s0 in range(0, TOWN, ST):
        W = load_w(*units[0])
        for st in range(ST // SUB):
            router(s0, st)
        gate_bcast()
        for i, (e_, g) in enumerate(units):
            Wn = load_w(*units[i + 1]) if i + 1 < len(units) else None
            expert_unit(e_, g, W, first=(i == 0))
            W = Wn
        for st in range(ST // SUB):
            finish(s0, st)
    return cx


def stage_C1a(inp, resA1, resB1, resNA, xT_list):
    cx = build_Ca(False)
    maps = []
    for c in range(NCORES):
        aT = np.zeros((1024, NT), NPBF)
        aT[:512] = np.asarray(resB1[c]["oT"])
        aT[512:, :TOWN] = np.asarray(resNA[c]["naT"])
        maps.append(dict(xT=xT_list[c], aT=aT, mod=np.asarray(resA1[c]["mod"]),
                         wout=np.ascontiguousarray(inp["od_w_out"][0]), lng=pcol(inp["ln1_g"][1]), lnb=pcol(inp["ln1_b"][1])))
    return run_spmd(cx, maps)


def stage_C1b(inp, resA1, resC1a):
    cx = build_C1b()
    oh = np.zeros((8, 8 * 128), np.float32)
    for e in range(8):
        oh[e, e * 128:(e + 1) * 128] = 1.0
    shared = dict(wr=np.ascontiguousarray(inp["od_w_router"][0]), br=np.ascontiguousarray(inp["od_b_router"][0][:, None]),
                  wg=np.ascontiguousarray(inp["od_w_gate"][0]), wu=np.ascontiguousarray(inp["od_w_up"][0]),
                  wd=np.ascontiguousarray(inp["od_w_down"][0]), lng=pcol(inp["ln2_g"][1]), lnb=pcol(inp["ln2_b"][1]),
                  ident=np.eye(128, dtype=np.float32), onehot=oh)
    maps = []
    for c in range(NCORES):
        m = dict(shared)
        m.update(xT=np.asarray(resC1a[c]["xo"]), mod=np.asarray(resA1[c]["mod"]))
        maps.append(m)
    return run_spmd(cx, maps)


def kernel(**inp):
    inp = {k: np.asarray(v) for k, v in inp.items()}
    resA0 = stage_A0(inp)
    resB0 = stage_B0(resA0)
    resC0a = stage_C0a(inp, resA0, resB0)
    resC0b = stage_C0b(inp, resA0, resC0a)
    x1 = [np.asarray(resC0b[c]["xo"]) for c in range(NCORES)]
    resA1 = stage_A1(inp, x1)
    resB1 = stage_B1(resA1)
    resNA = stage_NA(inp, resA1)
    resC1a = stage_C1a(inp, resA1, resB1, resNA, x1)
    resC1b = stage_C1b(inp, resA1, resC1a)
    out = np.empty((2, SEQ, D), np.float32)
    for c in range(NCORES):
        b, j = divmod(c, 4)
        out[b, j * TOWN:(j + 1) * TOWN] = np.asarray(resC1b[c]["xo"]).T
    return out
```

```python
import contextlib
import numpy as np
import ml_dtypes
import concourse.bass as bass
import concourse.mybir as mybir
from concourse.bass_utils import run_bass_kernel_spmd

F32 = mybir.dt.float32
BF16 = mybir.dt.bfloat16
AF = mybir.ActivationFunctionType
ALU = mybir.AluOpType
NPBF = ml_dtypes.bfloat16

NCORES = 8
D = 1024
SEQ = 16384
TOWN = 4096
CTX = 256
NT = TOWN + CTX
GRID_W = 64
EPS = 1e-6
ALPHA = 4 ** 0.25
ENGS = ("pe", "act", "dve", "pool", "sp")


class Prog:
    def __init__(self, nc):
        self.nc = nc
        self.ops = []
        self.last_w = {}
        self.readers = {}
        self.dma_cnt = {}
        self.excl = set()

    def _deps(self, reads, writes):
        ex = [r for r in reads if r in self.excl]
        if ex:
            reads = [r for r in reads if r not in self.excl]
            writes = list(writes) + ex
        deps = set()
        for r in reads:
            w = self.last_w.get(r)
            if w is not None:
                deps.add(w)
        for r in writes:
            deps.update(self.readers.get(r, ()))
            w = self.last_w.get(r)
            if w is not None:
                deps.add(w)
        i = len(self.ops)
        for r in reads:
            self.readers.setdefault(r, []).append(i)
        for r in writes:
            self.last_w[r] = i
            self.readers[r] = []
        return deps

    def op(self, eng, fn, reads=(), writes=(), fence=True):
        deps = self._deps(reads, writes)
        if eng == "pe":
            fence = False
        self.ops.append(dict(eng=eng, fn=fn, deps=deps, dma=None, sig=None, need=False, fence=fence))

    def dma(self, queue, key, fn, reads=(), writes=(), n=1):
        deps = self._deps(reads, writes)
        c = self.dma_cnt.get(key, 0) + 16 * n
        self.dma_cnt[key] = c
        self.ops.append(dict(eng=queue, fn=fn, deps=deps, dma=key, sig=(("dma", key), c), need=True, fence=True))

    def emit(self):
        nc = self.nc
        ops = self.ops
        for o in ops:
            for d in o["deps"]:
                p = ops[d]
                if p["dma"] is None and (p["eng"] != o["eng"] or o["fence"]):
                    p["need"] = True
        cnt = {e: 0 for e in ENGS}
        for o in ops:
            if o["dma"] is None and o["need"]:
                cnt[o["eng"]] += 1
                o["sig"] = (("eng", o["eng"]), cnt[o["eng"]])
        with contextlib.ExitStack() as st:
            sems = {}
            for e in ENGS:
                if cnt[e]:
                    sems[("eng", e)] = st.enter_context(nc.semaphore("s_" + e))
            for k in self.dma_cnt:
                sems[("dma", k)] = st.enter_context(nc.semaphore("d_" + str(k)))
            block = st.enter_context(nc.Block())
            per = {e: [] for e in ENGS}
            for i, o in enumerate(ops):
                per[o["eng"]].append(i)
            finals = [(("dma", k), c) for k, c in self.dma_cnt.items()]

            def body(ename):
                def run(eng):
                    waited = {}
                    for i in per[ename]:
                        o = ops[i]
                        need = {}
                        for d in o["deps"]:
                            p = ops[d]
                            if p["dma"] is None and p["eng"] == ename and not o["fence"]:
                                continue
                            sk, v = p["sig"]
                            if v > need.get(sk, 0):
                                need[sk] = v
                        for sk, v in need.items():
                            if waited.get(sk, 0) >= v:
                                continue
                            eng.wait_ge(sems[sk], v)
                            waited[sk] = v
                        if o["dma"] is not None:
                            o["fn"](eng, sems[("dma", o["dma"])])
                        else:
                            ins = o["fn"](eng)
                            if o["need"]:
                                ins.then_inc(sems[o["sig"][0]], 1)
                    if ename == "sp":
                        for sk, v in finals:
                            if waited.get(sk, 0) < v:
                                eng.wait_ge(sems[sk], v)
                return run

            block.tensor(body("pe"))
            block.scalar(body("act"))
            block.vector(body("dve"))
            block.gpsimd(body("pool"))
            block.sync(body("sp"))


class Ctx:
    def __init__(self):
        self.nc = bass.Bass("TRN2", target_bir_lowering=False)
        self.st = contextlib.ExitStack()
        self.P = Prog(self.nc)
        self.out_names = []
        self._u = 0

    def inp(self, name, shape, dt=F32):
        return self.nc.dram_tensor(name, list(shape), dt, kind="ExternalInput").ap()

    def out(self, name, shape, dt=F32):
        self.out_names.append(name)
        return self.nc.dram_tensor(name, list(shape), dt, kind="ExternalOutput").ap()

    def sb(self, name, shape, dt=F32):
        return self.st.enter_context(self.nc.sbuf_tensor(name, list(shape), dt))

    def ps(self, name, shape, dt=F32):
        self.P.excl.add(name)
        return self.st.enter_context(self.nc.psum_tensor(name, list(shape), dt))

    def ring(self, name, n, shape, dt=F32):
        return Ring([(self.sb("%s%d" % (name, i), shape, dt), "%s%d" % (name, i)) for i in range(n)])

    def psring(self, name, n, shape=(128, 512), dt=F32):
        return Ring([(self.ps("%s%d" % (name, i), (128, 512), dt), "%s%d" % (name, i)) for i in range(n)])

    def load(self, q, dst, src, res, reads=()):
        self.P.dma(q, "L_" + str(res), lambda e, s: e.dma_start(out=dst, in_=src).then_inc(s, 16),
                   reads=reads, writes=[res])

    def store(self, q, dst, src, res, dres=None):
        self.P.dma(q, "S_" + str(res), lambda e, s: e.dma_start(out=dst, in_=src).then_inc(s, 16),
                   reads=[res], writes=([dres] if dres else []))

    def finish(self):
        self.P.emit()
        self.st.close()
        return self.nc


class Ring:
    def __init__(self, items):
        self.items = items
        self.i = 0

    def nxt(self):
        it = self.items[self.i % len(self.items)]
        self.i += 1
        return it


def run_spmd(cx, in_maps):
    nc = cx.finish()
    res = run_bass_kernel_spmd(nc, in_maps, core_ids=list(range(NCORES)))
    return res.results


def tiles_own_ctx(tt=512):
    t = [(i * tt, tt, 0) for i in range(TOWN // tt)]
    t.append((TOWN, CTX, 1))
    return t


def pcol(v):
    v = np.asarray(v, np.float32)
    return np.ascontiguousarray(v.reshape(-1, 128).T)


def emit_ada(cx, cs_d, wada_d, bada_d, mod_sb, modp_sb, pm=None, pmres="ps_mod"):
    P = cx.P
    cs_sb = cx.sb("cs_sb", [128, 16])
    s_sb = cx.sb("s_sb", [128, 16])
    ba_sb = cx.sb("ba_sb", [128, 48])
    if pm is None:
        pm = cx.ps("ps_mod", [128, 512])
    cx.load("sp", cs_sb[:], cs_d, "cs_sb")
    cx.load("sp", ba_sb[:], bada_d, "ba_sb")
    P.op("act", lambda e: e.activation(out=s_sb[:], in_=cs_sb[:], func=AF.Silu), reads=["cs_sb"], writes=["s_sb"])
    wr = cx.ring("wada", 2, [128, 8, 512])
    wv = wada_d.rearrange("(k p) c -> p k c", p=128)
    for piece in range(12):
        wa, wres = wr.nxt()
        cx.load("sp", wa[:], wv[:, :, piece * 512:(piece + 1) * 512], wres)

        def mm(e, wa=wa, piece=piece):
            ins = None
            for f4 in range(4):
                fc = piece * 4 + f4
                for k in range(8):
                    ins = e.matmul(pm[:, fc * 2:fc * 2 + 2], wa[:, k, f4 * 128:(f4 + 1) * 128],
                                   s_sb[:, 2 * k:2 * k + 2], start=(k == 0), stop=(k == 7))
            return ins
        P.op("pe", mm, reads=[wres, "s_sb"], writes=[pmres])
    pmv = pm[:, 0:96].rearrange("p (m c) -> p m c", c=2)
    mv = mod_sb[:].rearrange("p (m c) -> p m c", c=2)
    for c in range(2):
        P.op("dve", lambda e, c=c: e.tensor_tensor(out=mv[:, :, c], in0=pmv[:, :, c], in1=ba_sb[:], op=ALU.add),
             reads=[pmres, "ba_sb"], writes=["mod"], fence=True)
    P.op("dve", lambda e: e.tensor_copy(out=modp_sb[:], in_=mod_sb[:]), reads=["mod"], writes=["modp"], fence=True)
    for lo in (16, 64):
        P.op("dve", lambda e, lo=lo: e.tensor_scalar_add(out=modp_sb[:, lo:lo + 16], in0=mod_sb[:, lo:lo + 16], scalar1=1.0),
             reads=["mod"], writes=["modp"], fence=True)


def emit_modulate(cx, x, xres, u, ures, tw, col, mod_sb, modp_sb, piece_shift, nk=8):
    P = cx.P
    for k in range(nk):
        sc = modp_sb[:, (piece_shift + 1) * 16 + 2 * k + col:(piece_shift + 1) * 16 + 2 * k + col + 1]
        sh = mod_sb[:, piece_shift * 16 + 2 * k + col:piece_shift * 16 + 2 * k + col + 1]
        if k % 2 == 0:
            P.op("act", lambda e, k=k, sc=sc, sh=sh: e.activation(out=u[:, k, :tw], in_=x[:, k, :tw], func=AF.Identity,
                                                                 bias=sh, scale=sc),
                 reads=[xres, "mod", "modp"], writes=[(ures, k)])
        else:
            P.op("dve", lambda e, k=k, sc=sc, sh=sh: e.tensor_scalar(out=u[:, k, :tw], in0=x[:, k, :tw], scalar1=sc,
                                                                    scalar2=sh, op0=ALU.mult, op1=ALU.add),
                 reads=[xres, "mod", "modp"], writes=[(ures, k)])


def build_A0():
    cx = Ctx()
    P = cx.P
    xT = cx.inp("xT", [D, NT])
    cs_d = cx.inp("cs", [128, 16])
    wada_d = cx.inp("wada", [D, 6 * D])
    bada_d = cx.inp("bada", [128, 48])
    win_d = cx.inp("win", [D, 1536])
    gq_d = cx.inp("gq", [128, 1])
    gk_d = cx.inp("gk", [128, 1])
    cos_d = cx.inp("cosT", [128, NT])
    sin_d = cx.inp("sinT", [128, NT])
    rt_d = cx.inp("rotT", [128, 128])
    ob_d = cx.inp("oneblk", [128, 128])
    mod_o = cx.out("mod", [128, 96])
    qT_o = cx.out("qT", [768, NT], BF16)
    kT_o = cx.out("kT", [256, NT], BF16)
    v_o = cx.out("v", [NT, 256], BF16)
    pT_o = cx.out("pT", [256, NT])

    mod_sb = cx.sb("mod_sb", [128, 96])
    modp_sb = cx.sb("modp_sb", [128, 96])
    emit_ada(cx, cs_d, wada_d, bada_d, mod_sb, modp_sb)
    cx.store("sp", mod_o, mod_sb[:], "mod")

    win = cx.sb("win_sb", [128, 8, 1536], BF16)
    cx.load("pool", win[:], win_d.rearrange("(k p) c -> p k c", p=128), "win")
    oneblk = cx.sb("oneblk_sb", [128, 128], BF16)
    cx.load("pool", oneblk[:], ob_d, "oneblk")
    rt32 = cx.sb("rt32", [128, 128])
    cx.load("sp", rt32[:], rt_d, "rt32")
    g_sb = cx.sb("g_sb", [128, 2])
    cx.load("sp", g_sb[:, 0:1], gq_d, "gq")
    cx.load("sp", g_sb[:, 1:2], gk_d, "gk")
    rg = [cx.sb("rgq", [128, 128], BF16), cx.sb("rgk", [128, 128], BF16)]
    cosg = [cx.sb("cosgq", [128, NT]), cx.sb("cosgk", [128, NT])]
    sin_sb = cx.sb("sin_sb", [128, NT])
    cx.load("sp", sin_sb[:], sin_d, "sin")
    gres = ["gq", "gk"]
    for i in range(2):
        cx.load("sp", cosg[i][:], cos_d, "cosg%d" % i)
        P.op("dve", lambda e, i=i: e.tensor_scalar_mul(out=rg[i][:], in0=rt32[:], scalar1=g_sb[:, i:i + 1]),
             reads=["rt32", gres[i]], writes=["rg%d" % i])
        P.op("dve", lambda e, i=i: e.tensor_scalar_mul(out=cosg[i][:], in0=cosg[i][:], scalar1=g_sb[:, i:i + 1]),
             reads=[gres[i], "cosg%d" % i], writes=["cosg%d" % i])

    eps_sb = cx.sb("eps_sb", [128, 1])
    P.op("dve", lambda e: e.memset(eps_sb[:], EPS), writes=["eps"])
    xr = cx.ring("x", 2, [128, 8, 512])
    ur = cx.ring("u", 2, [128, 8, 512], BF16)
    pq_r = cx.psring("pq", 2)
    pms_r = cx.psring("pms", 1)
    prot_r = cx.psring("prot", 1)
    pv_r = cx.psring("pv", 2)
    sq_r = cx.ring("sq", 2, [128, 512], BF16)
    qb_r = cx.ring("qb", 2, [128, 512], BF16)
    t1_r = cx.ring("t1", 2, [128, 512])
    t2_r = cx.ring("t2", 2, [128, 512])
    rs_r = cx.ring("rs", 2, [128, 512])
    qf_r = cx.ring("qf", 3, [128, 512], BF16)
    vs_r = cx.ring("vs", 2, [128, 256], BF16)
    pp_r = cx.ring("pp", 2, [128, 512])
    xv = xT.rearrange("(k p) t -> p k t", p=128)

    def do_tile(t0, tw, col):
        x, xres = xr.nxt()
        cx.load("sp", x[:, :, :tw], xv[:, :, t0:t0 + tw], xres)
        u, ures = ur.nxt()
        emit_modulate(cx, x, xres, u, ures, tw, col, mod_sb, modp_sb, 0)
        ureads = [(ures, k) for k in range(8)]
        for cc in range(8):
            isk = cc >= 6
            c0 = cc * 128 if not isk else 768 + (cc - 6) * 128
            gi = 1 if isk else 0
            pq, pqres = pq_r.nxt()

            def mmq(e, pq=pq, c0=c0):
                ins = None
                for k in range(8):
                    ins = e.matmul(pq[:, :tw], win[:, k, c0:c0 + 128], u[:, k, :tw], start=(k == 0), stop=(k == 7))
                return ins
            P.op("pe", mmq, reads=ureads + ["win"], writes=[pqres])
            sq, sqres = sq_r.nxt()
            P.op("act", lambda e, sq=sq, pq=pq: e.activation(out=sq[:, :tw], in_=pq[:, :tw], func=AF.Square),
                 reads=[pqres], writes=[sqres])
            qb, qbres = qb_r.nxt()
            P.op("dve", lambda e, qb=qb, pq=pq: e.tensor_copy(out=qb[:, :tw], in_=pq[:, :tw]), reads=[pqres], writes=[qbres])
            pms, pmsres = pms_r.nxt()
            P.op("pe", lambda e, pms=pms, sq=sq: e.matmul(pms[:, :tw], oneblk[:], sq[:, :tw], start=True, stop=True),
                 reads=[sqres, "oneblk"], writes=[pmsres])
            prot, protres = prot_r.nxt()
            P.op("pe", lambda e, prot=prot, qb=qb, gi=gi: e.matmul(prot[:, :tw], rg[gi][:], qb[:, :tw], start=True, stop=True),
                 reads=[qbres, "rg%d" % gi], writes=[protres])
            t1, t1res = t1_r.nxt()
            P.op("dve", lambda e, t1=t1, pq=pq, gi=gi: e.tensor_tensor(out=t1[:, :tw], in0=pq[:, :tw],
                                                                     in1=cosg[gi][:, t0:t0 + tw], op=ALU.mult),
                 reads=[pqres, "cosg%d" % gi], writes=[t1res])
            t2, t2res = t2_r.nxt()
            P.op("dve", lambda e, t2=t2, prot=prot: e.tensor_tensor(out=t2[:, :tw], in0=prot[:, :tw],
                                                                  in1=sin_sb[:, t0:t0 + tw], op=ALU.mult),
                 reads=[protres, "sin"], writes=[t2res])
            rs, rsres = rs_r.nxt()
            P.op("act", lambda e, rs=rs, pms=pms: e.activation(out=rs[:, :tw], in_=pms[:, :tw], func=AF.Sqrt, bias=eps_sb[:, 0:1]),
                 reads=[pmsres, "eps"], writes=[rsres])
            P.op("dve", lambda e, rs=rs: e.reciprocal(out=rs[:, :tw], in_=rs[:, :tw]), reads=[rsres], writes=[rsres])
            P.op("dve", lambda e, t1=t1, t2=t2: e.tensor_tensor(out=t1[:, :tw], in0=t1[:, :tw], in1=t2[:, :tw], op=ALU.add),
                 reads=[t1res, t2res], writes=[t1res])
            qf, qfres = qf_r.nxt()
            P.op("dve", lambda e, qf=qf, t1=t1, rs=rs: e.tensor_tensor(out=qf[:, :tw], in0=t1[:, :tw], in1=rs[:, :tw], op=ALU.mult),
                 reads=[t1res, rsres], writes=[qfres])
            dst = (kT_o[(cc - 6) * 128:(cc - 5) * 128, t0:t0 + tw] if isk else qT_o[cc * 128:(cc + 1) * 128, t0:t0 + tw])
            cx.store("sp", dst, qf[:, :tw], qfres)
        for tb in range(tw // 128):
            pv, pvres = pv_r.nxt()

            def mmv(e, pv=pv, tb=tb):
                ins = None
                for k in range(8):
                    ins = e.matmul(pv[:, :256], u[:, k, tb * 128:(tb + 1) * 128], win[:, k, 1024:1280],
                                   start=(k == 0), stop=(k == 7))
                return ins
            P.op("pe", mmv, reads=ureads + ["win"], writes=[pvres])
            vs, vsres = vs_r.nxt()
            P.op("act", lambda e, vs=vs, pv=pv: e.activation(out=vs[:], in_=pv[:, :256], func=AF.Copy), reads=[pvres], writes=[vsres])
            cx.store("pool", v_o[t0 + tb * 128:t0 + (tb + 1) * 128, :], vs[:], vsres)
        for pc in range(2):
            pq, pqres = pq_r.nxt()

            def mmp(e, pq=pq, pc=pc):
                ins = None
                for k in range(8):
                    ins = e.matmul(pq[:, :tw], win[:, k, 1280 + pc * 128:1280 + (pc + 1) * 128], u[:, k, :tw],
                                   start=(k == 0), stop=(k == 7))
                return ins
            P.op("pe", mmp, reads=ureads + ["win"], writes=[pqres])
            pp, ppres = pp_r.nxt()
            P.op("act", lambda e, pp=pp, pq=pq: e.activation(out=pp[:, :tw], in_=pq[:, :tw], func=AF.Copy), reads=[pqres], writes=[ppres])
            cx.store("pool", pT_o[pc * 128:(pc + 1) * 128, t0:t0 + tw], pp[:, :tw], ppres)

    for (t0, tw, col) in tiles_own_ctx():
        do_tile(t0, tw, col)
    return cx


def build_attn(nheads, group, dk, nkc_full, scale):
    cx = Ctx()
    P = cx.P
    nkv = nheads // group
    nkeys = nkc_full * 128
    qT = cx.inp("qT", [nheads * dk, NT], BF16)
    kT = cx.inp("kT", [nkv, dk, nkeys], BF16)
    vv = cx.inp("v", [nkv, 128, nkc_full, 64], BF16)
    sel_d = cx.inp("sel", [65, 64])
    oT = cx.out("oT", [nheads * 64, NT], BF16)

    kg_r = cx.ring("kg", 2, [128, nkeys], BF16)
    vg_r = cx.ring("vg", 2, [128, nkc_full, 65], BF16)
    for vg, vres in vg_r.items:
        P.op("dve", lambda e, vg=vg: e.memset(vg[:, :, 64:65], 1.0), writes=[(vres, "ones")])
    for kg, kres in kg_r.items:
        P.op("pool", lambda e, kg=kg: e.memset(kg[:, :], 0.0), writes=[kres])
    sel = cx.sb("sel_sb", [65, 64])
    cx.load("sp", sel[:], sel_d, "sel")
    q_r = cx.ring("qh", 2, [128, NT], BF16)
    for qh, qres in q_r.items:
        P.op("pool", lambda e, qh=qh: e.memset(qh[:, :], 0.0), writes=[qres])
    ps_r = cx.psring("ps", 4)
    po_r = cx.psring("po", 2)
    pb_r = cx.psring("pb", 1)
    pt_r = cx.ring("pt", 4, [128, 512], BF16)
    os_r = cx.ring("os", 2, [65, 512])
    at_r = cx.ring("at", 2, [64, 512], BF16)
    LOOK = 3
    for g in range(nkv):
        kg, kres = kg_r.nxt()
        cx.load("sp", kg[0:dk, :], kT[g], kres)
        vg, vres = vg_r.nxt()
        cx.load("sp", vg[:, :, 0:64], vv[g], vres)
        for h in range(g * group, (g + 1) * group):
            qh, qres = q_r.nxt()
            cx.load("sp", qh[0:dk, :], qT[h * dk:(h + 1) * dk, :], qres)
            steps = []
            for (t0, tw, col) in tiles_own_ctx():
                nkc = nkc_full if col == 0 else 2
                for kc in range(nkc):
                    steps.append((t0, tw, kc, nkc))
            pss = {}

            def emit_qk(i, kg=kg, kres=kres, qh=qh, qres=qres):
                t0, tw, kc, nkc = steps[i]
                ps, psres = ps_r.nxt()
                pss[i] = (ps, psres)
                P.op("pe", lambda e: e.matmul(ps[:, :tw], kg[:, kc * 128:(kc + 1) * 128], qh[:, t0:t0 + tw], start=True, stop=True),
                     reads=[kres, qres], writes=[psres])

            def emit_rest(i, po, pores, vg=vg, vres=vres):
                t0, tw, kc, nkc = steps[i]
                ps, psres = pss.pop(i)
                pt, ptres = pt_r.nxt()
                P.op("act", lambda e: e.activation(out=pt[:, :tw], in_=ps[:, :tw], func=AF.Exp, scale=scale), reads=[psres], writes=[ptres])
                P.op("pe", lambda e: e.matmul(po[0:65, :tw], vg[:, kc, :], pt[:, :tw], start=(kc == 0), stop=(kc == nkc - 1)),
                     reads=[ptres, vres, (vres, "ones")], writes=[pores])

            def emit_norm(i, po, pores, h=h):
                t0, tw, kc, nkc = steps[i]
                osb, osres = os_r.nxt()
                P.op("act", lambda e: e.activation(out=osb[:, :tw], in_=po[0:65, :tw], func=AF.Copy), reads=[pores], writes=[osres])
                P.op("dve", lambda e: e.reciprocal(out=osb[64:65, :tw], in_=osb[64:65, :tw]), reads=[osres], writes=[osres])
                pb, pbres = pb_r.nxt()
                P.op("pe", lambda e: e.matmul(pb[0:64, :tw], sel[:], osb[:, :tw], start=True, stop=True), reads=[osres, "sel"], writes=[pbres])
                at, atres = at_r.nxt()
                P.op("dve", lambda e: e.tensor_tensor(out=at[:, :tw], in0=osb[0:64, :tw], in1=pb[0:64, :tw], op=ALU.mult),
                     reads=[osres, pbres], writes=[atres])
                cx.store("pool", oT[h * 64:(h + 1) * 64, t0:t0 + tw], at[:, :tw], atres)

            for i in range(min(LOOK, len(steps))):
                emit_qk(i)
            po = pores = None
            for i in range(len(steps)):
                if steps[i][2] == 0:
                    po, pores = po_r.nxt()
                if i + LOOK < len(steps):
                    emit_qk(i + LOOK)
                emit_rest(i, po, pores)
                if steps[i][2] == steps[i][3] - 1:
                    emit_norm(i, po, pores)
    return cx


def rope_tables(j):
    t = np.arange(TOWN) + j * TOWN
    row = (t // GRID_W).astype(np.float64)
    colp = (t % GRID_W).astype(np.float64)
    inv = 10000.0 ** (-np.arange(16, dtype=np.float64) / 16)
    cos = np.ones((128, NT), np.float32)
    sin = np.zeros((128, NT), np.float32)
    for p in range(128):
        d = p % 64
        pos = row if d < 32 else colp
        ang = (pos.astype(np.float32) * inv[d % 16].astype(np.float32)).astype(np.float32)
        cos[p, :TOWN] = np.cos(ang)
        sin[p, :TOWN] = np.sin(ang)
    return cos, sin


def rot_matrix(n=128, blk=32):
    rt = np.zeros((n, n), np.float32)
    h = blk // 2
    for p in range(n):
        if p % blk < h:
            rt[p + h, p] = -1.0
        else:
            rt[p - h, p] = 1.0
    return rt


def blockdiag_ones(n, blk):
    m = np.zeros((n, n), np.float32)
    for i in range(0, n, blk):
        m[i:i + blk, i:i + blk] = 1.0 / blk
    return m


def core_xT(x, ctx, c):
    b, j = divmod(c, 4)
    return np.ascontiguousarray(np.concatenate([x[b, j * TOWN:(j + 1) * TOWN], ctx[b]], axis=0).T)


def core_cs(cvec, c_ctx, c):
    b = c // 4
    cs = np.empty((128, 16), np.float32)
    cs[:, 0::2] = pcol(cvec[b])
    cs[:, 1::2] = pcol(c_ctx)
    return cs


SEL = np.zeros((65, 64), np.float32)
SEL[64, :] = 1.0


def gather_kv(res, kname, vname, nkv, dk):
    out = []
    for b in range(2):
        ks = [np.asarray(res[4 * b][kname])[:, TOWN:]] + [np.asarray(res[4 * b + j][kname])[:, :TOWN] for j in range(4)]
        kf = np.concatenate(ks, axis=1).reshape(nkv, dk, -1)
        vs = [np.asarray(res[4 * b][vname])[TOWN:]] + [np.asarray(res[4 * b + j][vname])[:TOWN] for j in range(4)]
        vf = np.concatenate(vs, axis=0)
        nkc = vf.shape[0] // 128
        vf = np.ascontiguousarray(vf.reshape(nkc, 128, nkv, 64).transpose(2, 1, 0, 3))
        out.append((np.ascontiguousarray(kf), vf))
    return out


def stage_A0(inp):
    x, ctx = inp["x"], inp["ctx"]
    cx = build_A0()
    maps = []
    for c in range(NCORES):
        cos, sin = rope_tables(c % 4)
        maps.append(dict(
            xT=core_xT(x, ctx, c), cs=core_cs(inp["c"], inp["c_ctx"], c),
            wada=np.ascontiguousarray(inp["w_ada"][0]), bada=pcol(inp["b_ada"][0]),
            win=np.ascontiguousarray(inp["ev_w_in"][0]),
            gq=np.tile(inp["ev_q_gain"][0], 2)[:, None].astype(np.float32),
            gk=np.tile(inp["ev_k_gain"][0], 2)[:, None].astype(np.float32),
            cosT=cos, sinT=sin, rotT=rot_matrix(), oneblk=blockdiag_ones(128, 64)))
    return run_spmd(cx, maps)


def stage_B0(resA):
    cx = build_attn(12, 3, 64, 130, 64 ** -0.5)
    kv = gather_kv(resA, "kT", "v", 4, 64)
    maps = []
    for c in range(NCORES):
        kf, vf = kv[c // 4]
        maps.append(dict(qT=np.asarray(resA[c]["qT"]), kT=kf, v=vf, sel=SEL))
    return run_spmd(cx, maps)


class LNState:
    def __init__(self, cx, tag, width=512):
        self.ones = cx.sb(tag + "_ones", [128, 128])
        cx.P.op("dve", lambda e: e.memset(self.ones[:], 1.0 / D), writes=[tag + "_ones"])
        self.ones_res = tag + "_ones"
        self.eps = cx.sb(tag + "_eps", [128, 1])
        cx.P.op("dve", lambda e: e.memset(self.eps[:], EPS), writes=[tag + "_eps"])
        self.eps_res = tag + "_eps"
        self.pmean = cx.psring(tag + "_pmean", 1, (128, width))
        self.pmsq = cx.psring(tag + "_pmsq", 1, (128, width))
        self.rsq = cx.ring(tag + "_rsq", 2, [128, width])
        self.m = cx.ring(tag + "_m", 1, [128, width])
        self.var = cx.ring(tag + "_var", 1, [128, width])
        self.nb = cx.ring(tag + "_nb", 1, [128, width])
        self.t = cx.ring(tag + "_t", 2, [128, width])


def emit_layernorm(cx, ln, r, rres, tw, g_sb, b_sb, gres, out=None, outres=None):
    P = cx.P
    if out is None:
        out, outres = r, rres
    pmean, pmres = ln.pmean.nxt()
    pmsq, pqres = ln.pmsq.nxt()
    for k in range(8):
        rsq, rsqres = ln.rsq.nxt()
        P.op("act", lambda e, rsq=rsq, k=k: e.activation(out=rsq[:, :tw], in_=r[:, k, :tw], func=AF.Square),
             reads=[rres(k)], writes=[rsqres])
        P.op("pe", lambda e, k=k: e.matmul(pmean[:, :tw], ln.ones[:], r[:, k, :tw], start=(k == 0), stop=(k == 7)),
             reads=[rres(k), ln.ones_res], writes=[pmres])
        P.op("pe", lambda e, rsq=rsq, k=k: e.matmul(pmsq[:, :tw], ln.ones[:], rsq[:, :tw], start=(k == 0), stop=(k == 7)),
             reads=[rsqres, ln.ones_res], writes=[pqres])
    m, mres = ln.m.nxt()
    var, vres = ln.var.nxt()
    nb, nbres = ln.nb.nxt()
    P.op("act", lambda e: e.activation(out=m[:, :tw], in_=pmean[:, :tw], func=AF.Copy), reads=[pmres], writes=[mres])
    P.op("dve", lambda e: e.tensor_tensor(out=var[:, :tw], in0=m[:, :tw], in1=m[:, :tw], op=ALU.mult), reads=[mres], writes=[vres])
    P.op("dve", lambda e: e.tensor_tensor(out=var[:, :tw], in0=pmsq[:, :tw], in1=var[:, :tw], op=ALU.subtract),
         reads=[pqres, vres], writes=[vres])
    P.op("dve", lambda e: e.tensor_scalar_max(out=var[:, :tw], in0=var[:, :tw], scalar1=0.0), reads=[vres], writes=[vres])
    P.op("act", lambda e: e.activation(out=var[:, :tw], in_=var[:, :tw], func=AF.Sqrt, bias=ln.eps[:, 0:1]),
         reads=[vres, ln.eps_res], writes=[vres])
    P.op("dve", lambda e: e.reciprocal(out=var[:, :tw], in_=var[:, :tw]), reads=[vres], writes=[vres])
    P.op("dve", lambda e: e.scalar_tensor_tensor(out=nb[:, :tw], in0=m[:, :tw], scalar=-1.0, in1=var[:, :tw],
                                                 op0=ALU.mult, op1=ALU.mult), reads=[mres, vres], writes=[nbres])
    for k in range(8):
        t, tres = ln.t.nxt()
        P.op("dve", lambda e, t=t, k=k: e.tensor_tensor(out=t[:, :tw], in0=r[:, k, :tw], in1=var[:, :tw], op=ALU.mult),
             reads=[rres(k), vres], writes=[tres])
        P.op("dve", lambda e, t=t: e.tensor_tensor(out=t[:, :tw], in0=t[:, :tw], in1=nb[:, :tw], op=ALU.add),
             reads=[tres, nbres], writes=[tres])
        P.op("act", lambda e, t=t, k=k: e.activation(out=out[:, k, :tw], in_=t[:, :tw], func=AF.Identity,
                                                     bias=b_sb[:, k:k + 1], scale=g_sb[:, k:k + 1]),
             reads=[tres, gres], writes=[outres(k)])


def load_cast_w(cx, name, w_d, kch, ncol):
    w = cx.sb(name, [128, kch, ncol], BF16)
    for k in range(kch):
        cx.load("pool", w[:, k, :], w_d[k * 128:(k + 1) * 128, :], (name, k))
    return w, [(name, k) for k in range(kch)]


def build_Ca(with_pool):
    cx = Ctx()
    P = cx.P
    xT = cx.inp("xT", [D, NT])
    nat = 6 if with_pool else 8
    aT = cx.inp("aT", [nat * 128, NT], BF16)
    mod_d = cx.inp("mod", [128, 96])
    wout_d = cx.inp("wout", [D, D])
    g_d = cx.inp("lng", [128, 8])
    b_d = cx.inp("lnb", [128, 8])
    if with_pool:
        ph_d = cx.inp("ph", [256, NT + 32])
        rc_d = cx.inp("rcnt", [256, NT])
        wp_d = cx.inp("wpool", [256, 128])
        psc_d = cx.inp("pscale", [128, 2])
    xo = cx.out("xo", [D, NT])

    mod_sb = cx.sb("mod_sb", [128, 96])
    cx.load("sp", mod_sb[:], mod_d, "mod")
    g_sb = cx.sb("g_sb", [128, 8])
    b_sb = cx.sb("b_sb", [128, 8])
    cx.load("sp", g_sb[:], g_d, "lngb")
    cx.load("sp", b_sb[:], b_d, "lngb2")
    P.op("dve", lambda e: e.tensor_copy(out=b_sb[:], in_=b_sb[:]), reads=["lngb2"], writes=["lngb"])
    wout, wres = load_cast_w(cx, "wout_sb", wout_d, 8, D)
    ln = LNState(cx, "ln")
    if with_pool:
        wp = cx.sb("wp_sb", [128, 2, 128], BF16)
        for pc in range(2):
            cx.load("pool", wp[:, pc, :], wp_d[pc * 128:(pc + 1) * 128, :], ("wp", pc))
        psc = cx.sb("psc_sb", [128, 2])
        cx.load("sp", psc[:], psc_d, "psc")
        rc = cx.sb("rc_sb", [128, 2, NT])
        cx.load("sp", rc[:], rc_d.rearrange("(c p) t -> p c t", p=128), "rc")
        ph_r = cx.ring("ph", 4, [128, 528])
        s_r = [cx.ring("s%d" % i, 1, [128, 528]) for i in range(4)]
        ym_r = cx.ring("ym", 2, [128, 512], BF16)
        tmp_r = cx.ring("ptmp", 2, [128, 512])
        ppool_r = cx.psring("ppool", 1)
    x_r = cx.ring("x", 2, [128, 8, 512])
    mix_r = cx.ring("mix", 2, [128, 8, 512], BF16)
    po_r = cx.psring("po", 2)
    xv = xT.rearrange("(k p) t -> p k t", p=128)
    av = aT.rearrange("(k p) t -> p k t", p=128)
    ov = xo.rearrange("(k p) t -> p k t", p=128)
    LO = {2: 1, 4: 2, 8: 4, 16: 8}

    def do_tile(t0, tw, col):
        mix, mres = mix_r.nxt()
        cx.load("sp", mix[:, 0:nat, :tw], av[:, :, t0:t0 + tw], (mres, "a"))
        x, xres = x_r.nxt()
        cx.load("sp", x[:, :, :tw], xv[:, :, t0:t0 + tw], (xres, "ld"))
        if with_pool:
            h0 = t0 if col == 0 else t0 + 16
            L = tw + 16
            for pc in range(2):
                ph, phres = ph_r.nxt()
                cx.load("sp", ph[:, :L], ph_d[pc * 128:(pc + 1) * 128, h0:h0 + L], phres)
                srcs = [(ph, phres)]
                nlev = 2 if pc == 0 else 4
                for lev in range(nlev):
                    sh = 1 << lev
                    s, sres = s_r[lev].nxt()
                    a, ares = srcs[-1]
                    Lo = L - (2 * sh - 1)
                    P.op("dve", lambda e, s=s, a=a, sh=sh, Lo=Lo: e.tensor_tensor(out=s[:, :Lo], in0=a[:, 0:Lo], in1=a[:, sh:sh + Lo], op=ALU.add),
                         reads=[ares], writes=[sres])
                    srcs.append((s, sres))
                ym, ymres = ym_r.nxt()
                tmp, tmpres = tmp_r.nxt()
                for half in range(2):
                    w = (2, 4, 8, 16)[pc * 2 + half]
                    s, sres = srcs[{2: 1, 4: 2, 8: 3, 16: 4}[w]]
                    off = 8 - LO[w]
                    lo_p, hi_p = half * 64, half * 64 + 64
                    P.op("dve", lambda e, s=s, off=off, lo_p=lo_p, hi_p=hi_p, pc=pc, tmp=tmp:
                         e.tensor_tensor(out=tmp[lo_p:hi_p, :tw], in0=s[lo_p:hi_p, off:off + tw], in1=rc[lo_p:hi_p, pc, t0:t0 + tw], op=ALU.mult),
                         reads=[sres, "rc"], writes=[(tmpres, half)])
                    P.op("dve", lambda e, ph=ph, lo_p=lo_p, hi_p=hi_p, tmp=tmp, ym=ym:
                         e.tensor_tensor(out=ym[lo_p:hi_p, :tw], in0=tmp[lo_p:hi_p, :tw], in1=ph[lo_p:hi_p, 8:8 + tw], op=ALU.subtract),
                         reads=[(tmpres, half), phres], writes=[(ymres, half)])
                pp, ppres = ppool_r.nxt()
                P.op("pe", lambda e, pp=pp, ym=ym, pc=pc: e.matmul(pp[:, :tw], wp[:, pc, :], ym[:, :tw], start=True, stop=True),
                     reads=[(ymres, 0), (ymres, 1), ("wp", pc)], writes=[ppres])
                P.op("act", lambda e, pp=pp, pc=pc: e.activation(out=mix[:, 6 + pc, :tw], in_=pp[:, :tw], func=AF.Identity, scale=psc[:, pc:pc + 1]),
                     reads=[ppres, "psc"], writes=[(mres, "p", pc)])
        mreads = [(mres, "a")] + ([(mres, "p", 0), (mres, "p", 1)] if with_pool else [])
        for oc in range(8):
            P.op("act", lambda e, oc=oc: e.activation(out=x[:, oc, :tw], in_=x[:, oc, :tw], func=AF.Identity, scale=ALPHA),
                 reads=[(xres, "ld")], writes=[(xres, oc)])
            po, pores = po_r.nxt()

            def mm(e, po=po, oc=oc):
                ins = None
                for k in range(8):
                    ins = e.matmul(po[:, :tw], wout[:, k, oc * 128:(oc + 1) * 128], mix[:, k, :tw], start=(k == 0), stop=(k == 7))
                return ins
            P.op("pe", mm, reads=mreads + wres, writes=[pores])
            gate = mod_sb[:, 32 + 2 * oc + col:32 + 2 * oc + col + 1]
            P.op("dve", lambda e, po=po, oc=oc, gate=gate: e.scalar_tensor_tensor(out=x[:, oc, :tw], in0=po[:, :tw], scalar=gate,
                                                                               in1=x[:, oc, :tw], op0=ALU.mult, op1=ALU.add),
                 reads=[pores, (xres, oc), "mod"], writes=[(xres, oc)])
        emit_layernorm(cx, ln, x, lambda k: (xres, k), tw, g_sb, b_sb, "lngb")
        P.dma("pool", "S_" + xres, lambda e, s: e.dma_start(out=ov[:, :, t0:t0 + tw], in_=x[:, :, :tw]).then_inc(s, 16),
              reads=[(xres, k) for k in range(8)], writes=[(xres, "ld")])

    for (t0, tw, col) in tiles_own_ctx():
        do_tile(t0, tw, col)
    return cx


def build_C0b():
    cx = Ctx()
    P = cx.P
    NF = 22
    TW = 256
    xT = cx.inp("xT", [D, NT])
    mod_d = cx.inp("mod", [128, 96])
    wg_d = cx.inp("wg", [D, NF * 128])
    wu_d = cx.inp("wu", [D, NF * 128])
    wd_d = cx.inp("wd", [NF * 128, D])
    g_d = cx.inp("lng", [128, 8])
    b_d = cx.inp("lnb", [128, 8])
    xo = cx.out("xo", [D, NT])
    mod_sb = cx.sb("mod_sb", [128, 96])
    modp_sb = cx.sb("modp_sb", [128, 96])
    cx.load("sp", mod_sb[:], mod_d, "mod")
    P.op("dve", lambda e: e.tensor_scalar_add(out=modp_sb[:], in0=mod_sb[:], scalar1=1.0), reads=["mod"], writes=["modp"])
    g_sb = cx.sb("g_sb", [128, 8])
    b_sb = cx.sb("b_sb", [128, 8])
    cx.load("sp", g_sb[:], g_d, "lngb")
    cx.load("sp", b_sb[:], b_d, "lngb2")
    P.op("dve", lambda e: e.tensor_copy(out=b_sb[:], in_=b_sb[:]), reads=["lngb2"], writes=["lngb"])
    wg, wgres = load_cast_w(cx, "wg_sb", wg_d, 8, NF * 128)
    wu, wures = load_cast_w(cx, "wu_sb", wu_d, 8, NF * 128)
    wd, wdres = load_cast_w(cx, "wd_sb", wd_d, NF, D)
    ln = LNState(cx, "ln", TW)
    x_r = cx.ring("x", 1, [128, 8, TW])
    u_r = cx.ring("u", 1, [128, 8, TW], BF16)
    h_r = cx.ring("h", 1, [128, NF, TW], BF16)
    sg_r = cx.ring("sg", 2, [128, TW])
    pg_r = cx.psring("pg", 2, (128, TW))
    pu_r = cx.psring("pu", 2, (128, TW))
    pd_r = cx.psring("pd", 2, (128, TW))
    xv = xT.rearrange("(k p) t -> p k t", p=128)
    ov = xo.rearrange("(k p) t -> p k t", p=128)

    def do_tile(t0, tw, col):
        x, xres = x_r.nxt()
        cx.load("sp", x[:, :, :tw], xv[:, :, t0:t0 + tw], (xres, "ld"), reads=[(xres, k) for k in range(8)])
        u, ures = u_r.nxt()
        emit_modulate(cx, x, (xres, "ld"), u, ures, tw, col, mod_sb, modp_sb, 3)
        ureads = [(ures, k) for k in range(8)]
        h, hres = h_r.nxt()
        for fc in range(NF):
            pg, pgres = pg_r.nxt()
            pu, pures = pu_r.nxt()

            def mmg(e, pg=pg, fc=fc):
                ins = None
                for k in range(8):
                    ins = e.matmul(pg[:, :tw], wg[:, k, fc * 128:(fc + 1) * 128], u[:, k, :tw], start=(k == 0), stop=(k == 7))
                return ins

            def mmu(e, pu=pu, fc=fc):
                ins = None
                for k in range(8):
                    ins = e.matmul(pu[:, :tw], wu[:, k, fc * 128:(fc + 1) * 128], u[:, k, :tw], start=(k == 0), stop=(k == 7))
                return ins
            P.op("pe", mmg, reads=ureads + wgres, writes=[pgres])
            P.op("pe", mmu, reads=ureads + wures, writes=[pures])
            sg, sgres = sg_r.nxt()
            P.op("act", lambda e, sg=sg, pg=pg: e.activation(out=sg[:, :tw], in_=pg[:, :tw], func=AF.Silu), reads=[pgres], writes=[sgres])
            P.op("dve", lambda e, sg=sg, pu=pu, fc=fc: e.tensor_tensor(out=h[:, fc, :tw], in0=sg[:, :tw], in1=pu[:, :tw], op=ALU.mult),
                 reads=[sgres, pures], writes=[(hres, fc)])
        hreads = [(hres, fc) for fc in range(NF)]
        for oc in range(8):
            P.op("act", lambda e, oc=oc: e.activation(out=x[:, oc, :tw], in_=x[:, oc, :tw], func=AF.Identity, scale=ALPHA),
                 reads=[(xres, "ld")] + ureads, writes=[(xres, oc)])
            pd, pdres = pd_r.nxt()

            def mmd(e, pd=pd, oc=oc):
                ins = None
                for fc in range(NF):
                    ins = e.matmul(pd[:, :tw], wd[:, fc, oc * 128:(oc + 1) * 128], h[:, fc, :tw], start=(fc == 0), stop=(fc == NF - 1))
                return ins
            P.op("pe", mmd, reads=hreads + wdres, writes=[pdres])
            gate = mod_sb[:, 80 + 2 * oc + col:80 + 2 * oc + col + 1]
            P.op("dve", lambda e, pd=pd, oc=oc, gate=gate: e.scalar_tensor_tensor(out=x[:, oc, :tw], in0=pd[:, :tw], scalar=gate,
                                                                               in1=x[:, oc, :tw], op0=ALU.mult, op1=ALU.add),
                 reads=[pdres, (xres, oc), "mod"], writes=[(xres, oc)])
        emit_layernorm(cx, ln, x, lambda k: (xres, k), tw, g_sb, b_sb, "lngb")
        P.dma("pool", "S_" + xres, lambda e, s: e.dma_start(out=ov[:, :, t0:t0 + tw], in_=x[:, :, :tw]).then_inc(s, 16),
              reads=[(xres, k) for k in range(8)], writes=[(xres, "ld")])

    for (t0, tw, col) in tiles_own_ctx(TW):
        do_tile(t0, tw, col)
    return cx


POOL_W = (2, 4, 8, 16)


def pool_tables(j):
    rc = np.ones((256, NT), np.float32)
    for g, w in enumerate(POOL_W):
        lo = w // 2
        hi = w - lo
        t = np.arange(TOWN) + j * TOWN
        cnt = np.clip(t + hi, 0, SEQ) - np.clip(t - lo, 0, SEQ)
        rc[g * 64:(g + 1) * 64, :TOWN] = (1.0 / cnt.astype(np.float32))[None, :]
        t = np.arange(CTX)
        cnt = np.clip(t + hi, 0, CTX) - np.clip(t - lo, 0, CTX)
        rc[g * 64:(g + 1) * 64, TOWN:] = (1.0 / cnt.astype(np.float32))[None, :]
    return rc


def pool_halo(resA, c):
    b, j = divmod(c, 4)
    own = np.asarray(resA[c]["pT"])
    ph = np.zeros((256, NT + 32), np.float32)
    ph[:, 8:8 + TOWN] = own[:, :TOWN]
    if j > 0:
        ph[:, 0:8] = np.asarray(resA[c - 1]["pT"])[:, TOWN - 8:TOWN]
    if j < 3:
        ph[:, 8 + TOWN:16 + TOWN] = np.asarray(resA[c + 1]["pT"])[:, 0:8]
    ph[:, TOWN + 16 + 8:TOWN + 16 + 8 + CTX] = own[:, TOWN:]
    return ph


def wpool_blockdiag(w_pool):
    m = np.zeros((256, 128), np.float32)
    for g in range(4):
        pc, gl = divmod(g, 2)
        m[pc * 128 + gl * 64:pc * 128 + gl * 64 + 64, gl * 64:gl * 64 + 64] = w_pool[g]
    return m


def stage_C0a(inp, resA, resB):
    cx = build_Ca(True)
    maps = []
    for c in range(NCORES):
        maps.append(dict(
            xT=core_xT(inp["x"], inp["ctx"], c), aT=np.asarray(resB[c]["oT"]), mod=np.asarray(resA[c]["mod"]),
            wout=np.ascontiguousarray(inp["ev_w_out"][0]), lng=pcol(inp["ln1_g"][0]), lnb=pcol(inp["ln1_b"][0]),
            ph=pool_halo(resA, c), rcnt=pool_tables(c % 4), wpool=wpool_blockdiag(inp["ev_w_pool"][0]),
            pscale=pcol(inp["ev_pool_scale"][0])))
    return run_spmd(cx, maps)


def stage_C0b(inp, resA, resCa):
    cx = build_C0b()
    maps = []
    for c in range(NCORES):
        maps.append(dict(
            xT=np.asarray(resCa[c]["xo"]), mod=np.asarray(resA[c]["mod"]),
            wg=np.ascontiguousarray(inp["ev_w_gate"][0]), wu=np.ascontiguousarray(inp["ev_w_up"][0]),
            wd=np.ascontiguousarray(inp["ev_w_down"][0]), lng=pcol(inp["ln2_g"][0]), lnb=pcol(inp["ln2_b"][0])))
    return run_spmd(cx, maps)


def build_A1():
    cx = Ctx()
    P = cx.P
    xT = cx.inp("xT", [D, NT])
    cs_d = cx.inp("cs", [128, 16])
    wada_d = cx.inp("wada", [D, 6 * D])
    bada_d = cx.inp("bada", [128, 48])
    win_d = cx.inp("win", [D, 2208])
    gq_d = cx.inp("gql", [128, 3])
    gkv_d = cx.inp("gkvl", [128, 2])
    wqup_d = cx.inp("wqup", [384, 768])
    wkvk_d = cx.inp("wkvk", [256, 512])
    wkvv_d = cx.inp("wkvv", [256, 512])
    cos96_d = cx.inp("cos96", [96, NT])
    sin96_d = cx.inp("sin96", [96, NT])
    rot96_d = cx.inp("rot96", [96, 96])
    mod_o = cx.out("mod", [128, 96])
    qT_o = cx.out("qT", [768, NT], BF16)
    kT_o = cx.out("kT", [768, NT], BF16)
    v_o = cx.out("v", [NT, 512], BF16)
    nqT_o = cx.out("nqT", [512, NT], BF16)
    nkT_o = cx.out("nkT", [512, NT], BF16)
    nv_o = cx.out("nv", [NT, 512], BF16)

    pq_r = cx.psring("pq", 2)
    pms_r = cx.psring("pms", 1)
    pqh_r = cx.psring("pqh", 2)
    prot_r = cx.psring("prot", 1)
    pv_r = cx.psring("pv", 2)
    mod_sb = cx.sb("mod_sb", [128, 96])
    modp_sb = cx.sb("modp_sb", [128, 96])
    emit_ada(cx, cs_d, wada_d, bada_d, mod_sb, modp_sb, pm=pv_r.items[0][0], pmres=pv_r.items[0][1])
    cx.store("sp", mod_o, mod_sb[:], "mod")

    win, winres = load_cast_w(cx, "win_sb", win_d, 8, 2208)
    wqup, wqres = load_cast_w(cx, "wqup_sb", wqup_d, 3, 768)
    wkvk, wkres = load_cast_w(cx, "wkvk_sb", wkvk_d, 2, 512)
    wkvv, wvres = load_cast_w(cx, "wkvv_sb", wkvv_d, 2, 512)
    rot96 = cx.sb("rot96_sb", [96, 96], BF16)
    cx.load("pool", rot96[:], rot96_d, "rot96")
    cos96 = cx.sb("cos96_sb", [96, NT])
    sin96 = cx.sb("sin96_sb", [96, NT])
    cx.load("sp", cos96[:], cos96_d, "cos96")
    cx.load("sp", sin96[:], sin96_d, "sin96")
    gq = cx.sb("gq_sb", [128, 3])
    gkv = cx.sb("gkv_sb", [128, 2])
    cx.load("sp", gq[:], gq_d, "gq")
    cx.load("sp", gkv[:], gkv_d, "gkv")
    ones_q = cx.sb("ones_q", [128, 128], BF16)
    ones_kv = cx.sb("ones_kv", [128, 128], BF16)
    P.op("dve", lambda e: e.memset(ones_q[:], 1.0 / 384), writes=["ones_q"])
    P.op("dve", lambda e: e.memset(ones_kv[:], 1.0 / 256), writes=["ones_kv"])
    eps_sb = cx.sb("eps_sb", [128, 1])
    P.op("dve", lambda e: e.memset(eps_sb[:], EPS), writes=["eps"])

    xr = cx.ring("x", 2, [128, 8, 512])
    ur = cx.ring("u", 2, [128, 8, 512], BF16)
    lat_r = cx.ring("lat", 1, [128, 5, 512])
    latn_r = cx.ring("latn", 1, [128, 5, 512], BF16)
    sq_r = cx.ring("sq", 2, [128, 512], BF16)
    rs_r = cx.ring("rs", 2, [128, 512])
    qb_r = cx.ring("qb", 2, [96, 512], BF16)
    t1_r = cx.ring("t1", 2, [96, 512])
    t2_r = cx.ring("t2", 2, [96, 512])
    qf_r = cx.ring("qf", 3, [96, 512], BF16)
    ob_r = cx.ring("ob", 3, [128, 512], BF16)
    xv = xT.rearrange("(k p) t -> p k t", p=128)

    def do_tile(t0, tw, col):
        x, xres = xr.nxt()
        cx.load("sp", x[:, :, :tw], xv[:, :, t0:t0 + tw], xres)
        u, ures = ur.nxt()
        emit_modulate(cx, x, xres, u, ures, tw, col, mod_sb, modp_sb, 0)
        ureads = [(ures, k) for k in range(8)]
        lat, latres = lat_r.nxt()
        latn, latnres = latn_r.nxt()

        def proj(ps, c0, m, n0=0, n1=None):
            n1 = tw if n1 is None else n1

            def f(e):
                ins = None
                for k in range(8):
                    ins = e.matmul(ps[0:m, :n1 - n0], win[:, k, c0:c0 + m], u[:, k, n0:n1], start=(k == 0), stop=(k == 7))
                return ins
            return f
        for grp, (c_lo, nch, ones, oneres, gain, gres) in enumerate(((0, 3, ones_q, "ones_q", gq, "gq"),
                                                                       (384, 2, ones_kv, "ones_kv", gkv, "gkv"))):
            base = 0 if grp == 0 else 3
            pms, pmsres = pms_r.nxt()
            for c in range(nch):
                pq, pqres = pq_r.nxt()
                P.op("pe", proj(pq, c_lo + c * 128, 128), reads=ureads + winres, writes=[pqres])
                sq, sqres = sq_r.nxt()
                P.op("act", lambda e, sq=sq, pq=pq: e.activation(out=sq[:, :tw], in_=pq[:, :tw], func=AF.Square), reads=[pqres], writes=[sqres])
                P.op("dve", lambda e, pq=pq, c=c, base=base: e.tensor_copy(out=lat[:, base + c, :tw], in_=pq[:, :tw]),
                     reads=[pqres], writes=[(latres, base + c)])
                P.op("pe", lambda e, pms=pms, sq=sq, c=c, nch=nch, ones=ones: e.matmul(pms[:, :tw], ones[:], sq[:, :tw], start=(c == 0), stop=(c == nch - 1)),
                     reads=[sqres, oneres], writes=[pmsres])
            rs, rsres = rs_r.nxt()
            P.op("act", lambda e, rs=rs, pms=pms: e.activation(out=rs[:, :tw], in_=pms[:, :tw], func=AF.Sqrt, bias=eps_sb[:, 0:1]),
                 reads=[pmsres, "eps"], writes=[rsres])
            P.op("dve", lambda e, rs=rs: e.reciprocal(out=rs[:, :tw], in_=rs[:, :tw]), reads=[rsres], writes=[rsres])
            for c in range(nch):
                P.op("dve", lambda e, c=c, base=base, rs=rs, gain=gain: e.scalar_tensor_tensor(
                    out=latn[:, base + c, :tw], in0=lat[:, base + c, :tw], scalar=gain[:, c:c + 1], in1=rs[:, :tw], op0=ALU.mult, op1=ALU.mult),
                    reads=[(latres, base + c), rsres, gres], writes=[(latnres, base + c)])
        cqn_reads = [(latnres, c) for c in range(3)]
        ckvn_reads = [(latnres, 3 + c) for c in range(2)]
        def head_rope(pqh, pqres, nrows_lo, stores):
            qb, qbres = qb_r.nxt()
            P.op("dve", lambda e: e.tensor_copy(out=qb[:, :tw], in_=pqh[0:96, :tw]), reads=[pqres], writes=[qbres])
            prot, protres = prot_r.nxt()
            P.op("pe", lambda e: e.matmul(prot[0:96, :tw], rot96[:], qb[:, :tw], start=True, stop=True), reads=[qbres, "rot96"], writes=[protres])
            t1, t1res = t1_r.nxt()
            P.op("dve", lambda e: e.tensor_tensor(out=t1[:, :tw], in0=pqh[0:96, :tw], in1=cos96[:, t0:t0 + tw], op=ALU.mult),
                 reads=[pqres, "cos96"], writes=[t1res])
            t2, t2res = t2_r.nxt()
            P.op("dve", lambda e: e.tensor_tensor(out=t2[:, :tw], in0=prot[0:96, :tw], in1=sin96[:, t0:t0 + tw], op=ALU.mult),
                 reads=[protres, "sin96"], writes=[t2res])
            qf, qfres = qf_r.nxt()
            P.op("dve", lambda e: e.tensor_tensor(out=qf[:, :tw], in0=t1[:, :tw], in1=t2[:, :tw], op=ALU.add), reads=[t1res, t2res], writes=[qfres])
            for (dst, lo, hi) in stores:
                cx.store("sp", dst, qf[lo:hi, :tw], qfres)

        pkr, pkrres = pqh_r.nxt()

        def mmkr(e):
            ins = None
            for k in range(8):
                ins = e.matmul(pkr[64:96, :tw], win[:, k, 640:672], u[:, k, :tw], start=(k == 0), stop=(k == 7))
            return ins
        P.op("dve", lambda e: e.memset(pkr[0:64, :tw], 0.0), writes=[pkrres])
        P.op("pe", mmkr, reads=ureads + winres, writes=[pkrres])
        head_rope(pkr, pkrres, 64, [(kT_o[h * 96 + 64:h * 96 + 96, t0:t0 + tw], 64, 96) for h in range(8)])
        for h in range(8):
            pqh, pqres = pqh_r.nxt()

            def mmq(e, pqh=pqh, h=h):
                ins = None
                for k in range(3):
                    ins = e.matmul(pqh[0:96, :tw], wqup[:, k, h * 96:(h + 1) * 96], latn[:, k, :tw], start=(k == 0), stop=(k == 2))
                return ins
            P.op("pe", mmq, reads=cqn_reads + wqres, writes=[pqres])
            head_rope(pqh, pqres, 0, [(qT_o[h * 96:(h + 1) * 96, t0:t0 + tw], 0, 96)])
        for hp in range(4):
            pq, pqres = pq_r.nxt()

            def mmk(e, pq=pq, hp=hp):
                ins = None
                for k in range(2):
                    ins = e.matmul(pq[:, :tw], wkvk[:, k, hp * 128:(hp + 1) * 128], latn[:, 3 + k, :tw], start=(k == 0), stop=(k == 1))
                return ins
            P.op("pe", mmk, reads=ckvn_reads + wkres, writes=[pqres])
            ob, obres = ob_r.nxt()
            P.op("act", lambda e, ob=ob, pq=pq: e.activation(out=ob[:, :tw], in_=pq[:, :tw], func=AF.Copy), reads=[pqres], writes=[obres])
            for hh in range(2):
                h = 2 * hp + hh
                cx.store("pool", kT_o[h * 96:h * 96 + 64, t0:t0 + tw], ob[hh * 64:(hh + 1) * 64, :tw], obres)
        for (dst, c_lo) in ((nqT_o, 672), (nkT_o, 1184)):
            for c in range(4):
                pq, pqres = pq_r.nxt()
                P.op("pe", proj(pq, c_lo + c * 128, 128), reads=ureads + winres, writes=[pqres])
                ob, obres = ob_r.nxt()
                P.op("act", lambda e, ob=ob, pq=pq: e.activation(out=ob[:, :tw], in_=pq[:, :tw], func=AF.Copy), reads=[pqres], writes=[obres])
                cx.store("pool", dst[c * 128:(c + 1) * 128, t0:t0 + tw], ob[:, :tw], obres)
        for tb in range(tw // 128):
            pv, pvres = pv_r.nxt()

            def mmv(e, pv=pv, tb=tb):
                ins = None
                for k in range(2):
                    ins = e.matmul(pv[:, :512], latn[:, 3 + k, tb * 128:(tb + 1) * 128], wkvv[:, k, :], start=(k == 0), stop=(k == 1))
                return ins
            P.op("pe", mmv, reads=ckvn_reads + wvres, writes=[pvres])
            ob, obres = ob_r.nxt()
            P.op("act", lambda e, ob=ob, pv=pv: e.activation(out=ob[:, :512], in_=pv[:, :512], func=AF.Copy), reads=[pvres], writes=[obres])
            cx.store("pool", v_o[t0 + tb * 128:t0 + (tb + 1) * 128, :], ob[:, :512], obres)
            pv, pvres = pv_r.nxt()

            def mmnv(e, pv=pv, tb=tb):
                ins = None
                for k in range(8):
                    ins = e.matmul(pv[:, :512], u[:, k, tb * 128:(tb + 1) * 128], win[:, k, 1696:2208], start=(k == 0), stop=(k == 7))
                return ins
            P.op("pe", mmnv, reads=ureads + winres, writes=[pvres])
            ob, obres = ob_r.nxt()
            P.op("act", lambda e, ob=ob, pv=pv: e.activation(out=ob[:, :512], in_=pv[:, :512], func=AF.Copy), reads=[pvres], writes=[obres])
            cx.store("pool", nv_o[t0 + tb * 128:t0 + (tb + 1) * 128, :], ob[:, :512], obres)

    for (t0, tw, col) in tiles_own_ctx():
        do_tile(t0, tw, col)
    return cx


def rope_tables32(j):
    t = np.arange(TOWN) + j * TOWN
    row = (t // GRID_W).astype(np.float32)
    colp = (t % GRID_W).astype(np.float32)
    inv = (10000.0 ** (-np.arange(8, dtype=np.float64) / 8)).astype(np.float32)
    cos = np.ones((96, NT), np.float32)
    sin = np.zeros((96, NT), np.float32)
    for d in range(32):
        pos = row if d < 16 else colp
        ang = (pos * inv[d % 8]).astype(np.float32)
        cos[64 + d, :TOWN] = np.cos(ang)
        sin[64 + d, :TOWN] = np.sin(ang)
    return cos, sin


def rot96():
    m = np.zeros((96, 96), np.float32)
    m[64:, 64:] = rot_matrix(32, 16)
    return m


def stage_A1(inp, xT_list):
    cx = build_A1()
    wkv = inp["od_w_kv_up"][0].reshape(256, 8, 128)
    maps = []
    for c in range(NCORES):
        cos, sin = rope_tables32(c % 4)
        maps.append(dict(
            xT=xT_list[c], cs=core_cs(inp["c"], inp["c_ctx"], c),
            wada=np.ascontiguousarray(inp["w_ada"][1]), bada=pcol(inp["b_ada"][1]),
            win=np.ascontiguousarray(inp["od_w_in"][0]),
            gql=pcol(inp["od_q_lat_gain"][0]), gkvl=pcol(inp["od_kv_lat_gain"][0]),
            wqup=np.ascontiguousarray(inp["od_w_q_up"][0]),
            wkvk=np.ascontiguousarray(wkv[:, :, :64].reshape(256, 512)),
            wkvv=np.ascontiguousarray(wkv[:, :, 64:].reshape(256, 512)),
            cos96=cos, sin96=sin, rot96=rot96()))
    return run_spmd(cx, maps)


NA_SCALE = 0.125


def build_NA():
    cx = Ctx()
    P = cx.P
    nq_d = cx.inp("nqT", [512, TOWN], BF16)
    nk_d = cx.inp("nkTh", [512, TOWN + 512], BF16)
    nv_d = cx.inp("nvh", [128, 36 * 8 * 65], BF16)
    kc_d = cx.inp("nkc", [512, CTX], BF16)
    vc_d = cx.inp("nvc", [128, 2 * 8 * 65], BF16)
    bm_d = {n: cx.inp(n, [128, 48 * 256]) for n in ("bm_int", "bm_first", "bm_last")}
    id_d = cx.inp("ident", [128, 128])
    sel_d = cx.inp("sel", [65, 64])
    o_d = cx.out("naT", [512, TOWN], BF16)

    kh = cx.sb("kh", [128, 4, TOWN + 512], BF16)
    qs = cx.sb("qs", [128, 4, TOWN], BF16)
    kcs = cx.sb("kcs", [128, 4, CTX], BF16)
    vb = cx.sb("vb", [128, 36, 8, 65], BF16)
    vc = cx.sb("vc", [128, 2, 8, 65], BF16)
    cx.load("sp", kh[:], nk_d.rearrange("(c p) t -> p c t", p=128), "kh")
    cx.load("sp", qs[:], nq_d.rearrange("(c p) t -> p c t", p=128), "qs")
    cx.load("sp", kcs[:], kc_d.rearrange("(c p) t -> p c t", p=128), "kcs")
    cx.load("sp", vb[:].rearrange("p a h d -> p (a h d)"), nv_d, "vb")
    cx.load("sp", vc[:].rearrange("p a h d -> p (a h d)"), vc_d, "vc")
    ident = cx.sb("ident_sb", [128, 128], BF16)
    cx.load("pool", ident[:], id_d, "ident")
    sel = cx.sb("sel_sb", [65, 64])
    cx.load("sp", sel[:], sel_d, "sel")
    bmi = cx.sb("bmi", [128, 48, 256], BF16)
    bme = cx.sb("bme", [128, 48, 256], BF16)
    stg_r = cx.ring("stg", 2, [128, 6, 256])

    def load_table(dst, dres, name):
        src = bm_d[name].rearrange("p (a q) -> p a q", q=256)
        for h in range(8):
            stg, sres = stg_r.nxt()
            cx.load("sp", stg[:], src[:, h * 6:(h + 1) * 6, :], sres)
            P.op("dve", lambda e, stg=stg, h=h: e.tensor_scalar_mul(out=dst[:, h * 6:(h + 1) * 6, :], in0=stg[:], scalar1=1.0 / NA_SCALE),
                 reads=[sres], writes=[(dres, h)])
    load_table(bmi, "bmi", "bm_int")
    load_table(bme, "bme", "bm_first")

    ps_r = cx.psring("ps", 4)
    po_r = cx.psring("po", 2)
    pb_r = cx.psring("pb", 1)
    pt_r = cx.ring("pt", 2, [128, 8, 256], BF16)
    os_r = cx.ring("os", 2, [65, 256])
    at_r = cx.ring("at", 2, [64, 256], BF16)

    LOOK = 2
    steps = [(m, h, bk) for m in range(16) for h in range(8) for bk in range(4)]
    state = {}

    def emit_qk(i):
        m, h, bk = steps[i]
        hp, pb0 = h // 2, (h % 2) * 64
        tab, tres = (bme, "bme") if m in (0, 15) else (bmi, "bmi")
        q0 = 256 * m
        ps, psres = ps_r.nxt()
        state[i] = (ps, psres)

        def mm(e):
            ins = None
            for cc in range(2):
                c = 2 * bk + cc
                o = cc * 256
                if c < 6:
                    e.matmul(ps[:, o:o + 256], kh[pb0:pb0 + 64, hp, q0 + c * 128:q0 + (c + 1) * 128], qs[pb0:pb0 + 64, hp, q0:q0 + 256],
                             start=True, stop=False)
                    ins = e.matmul(ps[:, o:o + 256], ident[:], tab[:, h * 6 + c, :], start=False, stop=True)
                else:
                    ins = e.matmul(ps[:, o:o + 256], kcs[pb0:pb0 + 64, hp, (c - 6) * 128:(c - 5) * 128], qs[pb0:pb0 + 64, hp, q0:q0 + 256],
                                   start=True, stop=True)
            return ins
        P.op("pe", mm, reads=["kh", "qs", "kcs", "ident", (tres, h)], writes=[psres])

    def emit_rest(i, pt, ptres, po, pores):
        m, h, bk = steps[i]
        ps, psres = state.pop(i)
        P.op("act", lambda e: e.activation(out=pt[:, 2 * bk:2 * bk + 2, :].rearrange("p a q -> p (a q)"), in_=ps[:, :],
                                           func=AF.Exp, scale=NA_SCALE),
             reads=[psres], writes=[(ptres, bk)])

        def pv(e):
            ins = None
            for cc in range(2):
                c = 2 * bk + cc
                lhs = vb[:, 2 * m + c, h, :] if c < 6 else vc[:, c - 6, h, :]
                ins = e.matmul(po[0:65, :256], lhs, pt[:, c, :], start=(c == 0), stop=(c == 7))
            return ins
        P.op("pe", pv, reads=[(ptres, bk), "vb", "vc"], writes=[pores])

    def emit_norm(i, po, pores):
        m, h, bk = steps[i]
        q0 = 256 * m
        osb, osres = os_r.nxt()
        P.op("act", lambda e: e.activation(out=osb[:, :], in_=po[0:65, :256], func=AF.Copy), reads=[pores], writes=[osres])
        P.op("dve", lambda e: e.reciprocal(out=osb[64:65, :], in_=osb[64:65, :]), reads=[osres], writes=[osres])
        pbk, pbres = pb_r.nxt()
        P.op("pe", lambda e: e.matmul(pbk[0:64, :256], sel[:], osb[:, :], start=True, stop=True), reads=[osres, "sel"], writes=[pbres])
        at, atres = at_r.nxt()
        P.op("dve", lambda e: e.tensor_tensor(out=at[:, :], in0=osb[0:64, :], in1=pbk[0:64, :256], op=ALU.mult), reads=[osres, pbres], writes=[atres])
        cx.store("pool", o_d[h * 64:(h + 1) * 64, q0:q0 + 256], at[:, :], atres)

    def qk_with_table(j):
        if steps[j] == (15, 0, 0):
            load_table(bme, "bme", "bm_last")
        emit_qk(j)

    for j in range(LOOK):
        qk_with_table(j)
    pt = ptres = po = pores = None
    for i in range(len(steps)):
        if steps[i][2] == 0:
            pt, ptres = pt_r.nxt()
            po, pores = po_r.nxt()
        if i + LOOK < len(steps):
            qk_with_table(i + LOOK)
        emit_rest(i, pt, ptres, po, pores)
        if steps[i][2] == 3:
            emit_norm(i, po, pores)
    return cx


def na_table(bias, R0):
    kr_rel = np.arange(12)
    kr = R0 - 4 + kr_rel
    qr = R0 + np.arange(4)
    rs = np.clip(qr - 4, 0, 256 - 8)
    row_ok = (kr[:, None] >= rs[None, :]) & (kr[:, None] < rs[None, :] + 8)
    row_idx = np.clip(kr[:, None] - qr[None, :] + 7, 0, 14)
    kc = np.arange(64)
    qc = np.arange(64)
    cs0 = np.clip(qc - 8, 0, 48)
    col_ok = (kc[:, None] >= cs0[None, :]) & (kc[:, None] < cs0[None, :] + 16)
    col_idx = np.clip(kc[:, None] - qc[None, :] + 15, 0, 30)
    ok = row_ok[:, None, :, None] & col_ok[None, :, None, :]
    g = bias[:, row_idx[:, None, :, None], col_idx[None, :, None, :]]
    t = np.where(ok[None], g, np.float32(-30000.0)).astype(np.float32)
    t = t.reshape(8, 6, 2, 64, 256)
    t = t.transpose(2, 3, 0, 1, 4).reshape(128, 48 * 256)
    return np.ascontiguousarray(t)


def with_ones(v):
    o = np.ones(v.shape[:-1] + (65,), v.dtype)
    o[..., :64] = v
    return o


def stage_NA(inp, resA1):
    cx = build_NA()
    bias = inp["od_na_bias"][0]
    t_int = na_table(bias, 8)
    t_first = na_table(bias, 0)
    t_last = na_table(bias, 252)
    maps = []
    for c in range(NCORES):
        b, j = divmod(c, 4)
        nk = np.asarray(resA1[c]["nkT"])
        nv = np.asarray(resA1[c]["nv"])
        kh = np.zeros((512, TOWN + 512), NPBF)
        vh = np.zeros((TOWN + 512, 512), NPBF)
        kh[:, 256:256 + TOWN] = nk[:, :TOWN]
        vh[256:256 + TOWN] = nv[:TOWN]
        if j > 0:
            kh[:, :256] = np.asarray(resA1[c - 1]["nkT"])[:, TOWN - 256:TOWN]
            vh[:256] = np.asarray(resA1[c - 1]["nv"])[TOWN - 256:TOWN]
        if j < 3:
            kh[:, 256 + TOWN:] = np.asarray(resA1[c + 1]["nkT"])[:, :256]
            vh[256 + TOWN:] = np.asarray(resA1[c + 1]["nv"])[:256]
        vhl = with_ones(vh.reshape(36, 128, 8, 64).transpose(1, 0, 2, 3)).reshape(128, -1)
        vcl = with_ones(nv[TOWN:].reshape(2, 128, 8, 64).transpose(1, 0, 2, 3)).reshape(128, -1)
        maps.append(dict(
            nqT=np.ascontiguousarray(np.asarray(resA1[c]["nqT"])[:, :TOWN]), nkTh=kh, nvh=np.ascontiguousarray(vhl),
            nkc=np.ascontiguousarray(nk[:, TOWN:]), nvc=np.ascontiguousarray(vcl),
            bm_int=t_int, bm_first=(t_first if j == 0 else t_int), bm_last=(t_last if j == 3 else t_int),
            ident=np.eye(128, dtype=np.float32), sel=SEL))
    return run_spmd(cx, maps)


def stage_B1(resA1):
    cx = build_attn(8, 1, 96, 130, 96 ** -0.5)
    kv = gather_kv(resA1, "kT", "v", 8, 96)
    maps = []
    for c in range(NCORES):
        kf, vf = kv[c // 4]
        maps.append(dict(qT=np.asarray(resA1[c]["qT"]), kT=kf, v=vf, sel=SEL))
    return run_spmd(cx, maps)


def build_C1b():
    cx = Ctx()
    P = cx.P
    NE, NFE, FG = 8, 28, 4
    NG = NFE // FG
    ST, SUB = 1024, 512
    xT = cx.inp("xT", [D, NT])
    mod_d = cx.inp("mod", [128, 96])
    wr_d = cx.inp("wr", [D, NE])
    br_d = cx.inp("br", [NE, 1])
    wg_d = cx.inp("wg", [NE, D, NFE * 128])
    wu_d = cx.inp("wu", [NE, D, NFE * 128])
    wd_d = cx.inp("wd", [NE, NFE * 128, D])
    g_d = cx.inp("lng", [128, 8])
    b_d = cx.inp("lnb", [128, 8])
    id_d = cx.inp("ident", [128, 128])
    oh_d = cx.inp("onehot", [NE, NE * 128])
    xo = cx.out("xo", [D, TOWN])

    mod_sb = cx.sb("mod_sb", [128, 96])
    modp_sb = cx.sb("modp_sb", [128, 96])
    cx.load("sp", mod_sb[:], mod_d, "mod")
    P.op("dve", lambda e: e.tensor_scalar_add(out=modp_sb[:], in0=mod_sb[:], scalar1=1.0), reads=["mod"], writes=["modp"], fence=True)
    g_sb = cx.sb("g_sb", [128, 8])
    b_sb = cx.sb("b_sb", [128, 8])
    cx.load("sp", g_sb[:], g_d, "lngb")
    cx.load("sp", b_sb[:], b_d, "lngb2")
    P.op("dve", lambda e: e.tensor_copy(out=b_sb[:], in_=b_sb[:]), reads=["lngb2"], writes=["lngb"], fence=True)
    wr = cx.sb("wr_sb", [128, 8, NE])
    cx.load("sp", wr[:], wr_d.rearrange("(k p) e -> p k e", p=128), "wr")
    br = cx.sb("br_sb", [NE, 1])
    cx.load("sp", br[:], br_d, "br")
    ident = cx.sb("ident_sb", [128, 128])
    cx.load("sp", ident[:], id_d, "ident")
    oneh = cx.sb("oneh_sb", [NE, NE * 128])
    cx.load("sp", oneh[:], oh_d, "oneh")
    ln = LNState(cx, "ln", SUB)

    yacc = cx.sb("yacc", [128, 8, ST])
    u2 = cx.sb("u2", [128, 8, ST], BF16)
    gb = cx.sb("gb", [128, NE, ST], BF16)
    gT = cx.sb("gT", [NE, ST])
    x_r = cx.ring("x", 1, [128, 8, SUB])
    uf_r = cx.ring("uf", 1, [128, 8, SUB])
    lg_r = cx.ring("lg", 1, [NE, SUB])
    lgT_r = cx.ring("lgT", 2, [128, 8])
    mx_r = cx.ring("mx", 2, [128, 8])
    dm_r = cx.ring("dm", 2, [128, 2])
    ga_r = cx.ring("ga", 2, [128, 8])
    gq_r = cx.ring("gq", 2, [128, 8])
    wgq_r = cx.ring("wgq", 2, [128, 8, FG * 128], BF16)
    wuq_r = cx.ring("wuq", 2, [128, 8, FG * 128], BF16)
    wdq_r = cx.ring("wdq", 2, [128, FG, D], BF16)
    sg_r = cx.ring("sg", 2, [128, SUB])
    h1_r = cx.ring("h1", 2, [128, SUB])
    hq_r = cx.ring("hq", 2, [128, FG, SUB], BF16)
    pg_r = cx.psring("pg", 2)
    pu_r = cx.psring("pu", 2)
    pd_r = cx.psring("pd", 2)
    xv = xT.rearrange("(k p) t -> p k t", p=128)
    ov = xo.rearrange("(k p) t -> p k t", p=128)

    def router(s0, st):
        t0 = s0 + st * SUB
        x, xres = x_r.nxt()
        cx.load("sp", x[:, :, :], xv[:, :, t0:t0 + SUB], (xres, "ld"), reads=[(xres, k) for k in range(8)])
        uf, ufres = uf_r.nxt()
        for k in range(8):
            sc = modp_sb[:, 64 + 2 * k:64 + 2 * k + 1]
            sh = mod_sb[:, 48 + 2 * k:48 + 2 * k + 1]
            if k % 2 == 0:
                P.op("act", lambda e, k=k, sc=sc, sh=sh: e.activation(out=uf[:, k, :], in_=x[:, k, :], func=AF.Identity, bias=sh, scale=sc),
                     reads=[(xres, "ld"), "mod", "modp"], writes=[(ufres, k)])
            else:
                P.op("dve", lambda e, k=k, sc=sc, sh=sh: e.tensor_scalar(out=uf[:, k, :], in0=x[:, k, :], scalar1=sc, scalar2=sh,
                                                                        op0=ALU.mult, op1=ALU.add),
                     reads=[(xres, "ld"), "mod", "modp"], writes=[(ufres, k)])
        ufreads = [(ufres, k) for k in range(8)]
        P.op("dve", lambda e: e.tensor_copy(out=u2[:, :, st * SUB:(st + 1) * SUB], in_=uf[:, :, :]), reads=ufreads, writes=[("u2", st)])
        plg, plgres = pg_r.nxt()

        def mml(e):
            ins = None
            for k in range(8):
                ins = e.matmul(plg[0:NE, :SUB], wr[:, k, :], uf[:, k, :], start=(k == 0), stop=(k == 7))
            return ins
        P.op("pe", mml, reads=ufreads + ["wr"], writes=[plgres])
        lg, lgres = lg_r.nxt()
        P.op("act", lambda e: e.activation(out=lg[:, :], in_=plg[0:NE, :SUB], func=AF.Identity, bias=br[:, 0:1]), reads=[plgres, "br"], writes=[lgres])
        for tb in range(SUB // 128):
            pt, ptres = pu_r.nxt()
            P.op("pe", lambda e, pt=pt, tb=tb: e.transpose(pt[:, 0:NE], lg[:, tb * 128:(tb + 1) * 128], ident[0:NE, 0:NE]),
                 reads=[lgres, "ident"], writes=[ptres])
            lgT, lgTres = lgT_r.nxt()
            mx, mxres = mx_r.nxt()
            dm, dmres = dm_r.nxt()
            ga, gares = ga_r.nxt()
            gq, gqres = gq_r.nxt()
            P.op("dve", lambda e, pt=pt, lgT=lgT: e.tensor_copy(out=lgT[:], in_=pt[:, 0:NE]), reads=[ptres], writes=[lgTres], fence=True)
            P.op("dve", lambda e, mx=mx, lgT=lgT: e.max(out=mx[:], in_=lgT[:]), reads=[lgTres], writes=[mxres], fence=True)
            P.op("dve", lambda e, mx=mx, dm=dm: e.tensor_tensor(out=dm[:, 0:1], in0=mx[:, 1:2], in1=mx[:, 0:1], op=ALU.subtract),
                 reads=[mxres], writes=[(dmres, 0)], fence=True)
            P.op("act", lambda e, dm=dm: e.activation(out=dm[:, 1:2], in_=dm[:, 0:1], func=AF.Sigmoid), reads=[(dmres, 0)], writes=[(dmres, 1)], fence=True)
            P.op("dve", lambda e, dm=dm: e.tensor_scalar(out=dm[:, 0:1], in0=dm[:, 1:2], scalar1=-1.0, scalar2=1.0, op0=ALU.mult, op1=ALU.add),
                 reads=[(dmres, 1)], writes=[(dmres, 0)], fence=True)
            P.op("dve", lambda e, ga=ga, lgT=lgT, mx=mx, dm=dm: e.tensor_scalar(out=ga[:], in0=lgT[:], scalar1=mx[:, 0:1], scalar2=dm[:, 0:1],
                                                                             op0=ALU.is_equal, op1=ALU.mult),
                 reads=[lgTres, mxres, (dmres, 0)], writes=[gares], fence=True)
            P.op("dve", lambda e, gq=gq, lgT=lgT, mx=mx, dm=dm: e.tensor_scalar(out=gq[:], in0=lgT[:], scalar1=mx[:, 1:2], scalar2=dm[:, 1:2],
                                                                             op0=ALU.is_equal, op1=ALU.mult),
                 reads=[lgTres, mxres, (dmres, 1)], writes=[gqres], fence=True)
            P.op("dve", lambda e, ga=ga, gq=gq: e.tensor_tensor(out=ga[:], in0=ga[:], in1=gq[:], op=ALU.add), reads=[gares, gqres], writes=[gares], fence=True)
            pg2, pg2res = pu_r.nxt()
            P.op("pe", lambda e, pg2=pg2, ga=ga: e.transpose(pg2[0:NE, 0:128], ga[:, :], ident[:, :]), reads=[gares, "ident"], writes=[pg2res])
            c0 = st * SUB + tb * 128
            P.op("dve", lambda e, pg2=pg2, c0=c0: e.tensor_copy(out=gT[:, c0:c0 + 128], in_=pg2[0:NE, 0:128]), reads=[pg2res], writes=[("gT", c0)], fence=True)

    def gate_bcast():
        for e_ in range(NE):
            for hf in range(ST // 512):
                pgb, pgbres = pg_r.nxt()
                P.op("pe", lambda e, pgb=pgb, e_=e_, hf=hf: e.matmul(pgb[:, :512], oneh[:, e_ * 128:(e_ + 1) * 128], gT[:, hf * 512:(hf + 1) * 512],
                                                                    start=True, stop=True),
                     reads=[("gT", c0) for c0 in range(hf * 512, (hf + 1) * 512, 128)] + ["oneh"], writes=[pgbres])
                P.op("act", lambda e, pgb=pgb, e_=e_, hf=hf: e.activation(out=gb[:, e_, hf * 512:(hf + 1) * 512], in_=pgb[:, :512], func=AF.Copy),
                     reads=[pgbres], writes=[("gb", e_, hf)])

    def load_w(e_, g):
        wgq, wgres = wgq_r.nxt()
        wuq, wures = wuq_r.nxt()
        wdq, wdres = wdq_r.nxt()
        c0 = g * FG * 128
        cx.load("pool", wgq[:], wg_d[e_, :, c0:c0 + FG * 128].rearrange("(k p) c -> p k c", p=128), wgres)
        cx.load("pool", wuq[:], wu_d[e_, :, c0:c0 + FG * 128].rearrange("(k p) c -> p k c", p=128), wures)
        cx.load("pool", wdq[:], wd_d[e_, c0:c0 + FG * 128, :].rearrange("(f p) c -> p f c", p=128), wdres)
        return (wgq, wgres, wuq, wures, wdq, wdres)

    def sub_gu(e_, W, st):
        wgq, wgres, wuq, wures, wdq, wdres = W
        ureads = [("u2", st)]
        hq, hqres = hq_r.nxt()
        for f in range(FG):
            pg, pgres = pg_r.nxt()
            pu, pures = pu_r.nxt()

            def mmg(e, pg=pg, f=f):
                ins = None
                for k in range(8):
                    ins = e.matmul(pg[:, :SUB], wgq[:, k, f * 128:(f + 1) * 128], u2[:, k, st * SUB:(st + 1) * SUB], start=(k == 0), stop=(k == 7))
                return ins

            def mmu(e, pu=pu, f=f):
                ins = None
                for k in range(8):
                    ins = e.matmul(pu[:, :SUB], wuq[:, k, f * 128:(f + 1) * 128], u2[:, k, st * SUB:(st + 1) * SUB], start=(k == 0), stop=(k == 7))
                return ins
            P.op("pe", mmg, reads=ureads + [wgres], writes=[pgres])
            P.op("pe", mmu, reads=ureads + [wures], writes=[pures])
            sg, sgres = sg_r.nxt()
            P.op("act", lambda e, sg=sg, pg=pg: e.activation(out=sg[:, :], in_=pg[:, :SUB], func=AF.Silu), reads=[pgres], writes=[sgres])
            h1, h1res = h1_r.nxt()
            P.op("dve", lambda e, sg=sg, pu=pu, h1=h1: e.tensor_tensor(out=h1[:, :], in0=sg[:, :], in1=pu[:, :SUB], op=ALU.mult),
                 reads=[sgres, pures], writes=[h1res])
            P.op("dve", lambda e, h1=h1, f=f: e.tensor_tensor(out=hq[:, f, :], in0=h1[:, :], in1=gb[:, e_, st * SUB:(st + 1) * SUB], op=ALU.mult),
                 reads=[h1res, ("gb", e_, (st * SUB) // 512)], writes=[(hqres, f)])
        return hq, hqres

    def sub_down(W, st, hq, hqres, first):
        wgq, wgres, wuq, wures, wdq, wdres = W
        hreads = [(hqres, f) for f in range(FG)]
        for oc in range(8):
            pd, pdres = pd_r.nxt()

            def mmd(e, pd=pd, oc=oc):
                ins = None
                for f in range(FG):
                    ins = e.matmul(pd[:, :SUB], wdq[:, f, oc * 128:(oc + 1) * 128], hq[:, f, :], start=(f == 0), stop=(f == FG - 1))
                return ins
            P.op("pe", mmd, reads=hreads + [wdres], writes=[pdres])
            ya = yacc[:, oc, st * SUB:(st + 1) * SUB]
            if first:
                P.op("dve", lambda e, pd=pd, ya=ya: e.tensor_copy(out=ya, in_=pd[:, :SUB]), reads=[pdres], writes=[("yacc", oc, st)])
            else:
                P.op("dve", lambda e, pd=pd, ya=ya: e.tensor_tensor(out=ya, in0=pd[:, :SUB], in1=ya, op=ALU.add),
                     reads=[pdres, ("yacc", oc, st)], writes=[("yacc", oc, st)])

    def finish(s0, st):
        t0 = s0 + st * SUB
        x, xres = x_r.nxt()
        cx.load("sp", x[:, :, :], xv[:, :, t0:t0 + SUB], (xres, "ld"), reads=[(xres, k) for k in range(8)])
        for oc in range(8):
            P.op("act", lambda e, oc=oc: e.activation(out=x[:, oc, :], in_=x[:, oc, :], func=AF.Identity, scale=ALPHA),
                 reads=[(xres, "ld")], writes=[(xres, oc)])
            gate = mod_sb[:, 80 + 2 * oc:80 + 2 * oc + 1]
            P.op("dve", lambda e, oc=oc, gate=gate: e.scalar_tensor_tensor(out=x[:, oc, :], in0=yacc[:, oc, st * SUB:(st + 1) * SUB], scalar=gate,
                                                                        in1=x[:, oc, :], op0=ALU.mult, op1=ALU.add),
                 reads=[("yacc", oc, st), (xres, oc), "mod"], writes=[(xres, oc)])
        emit_layernorm(cx, ln, x, lambda k: (xres, k), SUB, g_sb, b_sb, "lngb")
        P.dma("sp", "S_" + xres, lambda e, s: e.dma_start(out=ov[:, :, t0:t0 + SUB], in_=x[:, :, :]).then_inc(s, 16),
              reads=[(xres, k) for k in range(8)], writes=[(xres, "ld")])

    units = [(e_, g) for e_ in range(NE) for g in range(NG)]
    for s0 in range(0, TOWN, ST):
        Ws = {0: load_w(*units[0])}
        for st in range(ST // SUB):
            router(s0, st)
        gate_bcast()
        pending = None
        for i, (e_, g) in enumerate(units):
            for st in range(ST // SUB):
                hq, hqres = sub_gu(e_, Ws[i], st)
                if pending is not None:
                    sub_down(*pending)
                pending = (Ws[i], st, hq, hqres, i == 0)
                if st == 0:
                    Ws.pop(i - 1, None)
                    if i + 1 < len(units):
                        Ws[i + 1] = load_w(*units[i + 1])
        sub_down(*pending)
        for st in range(ST // SUB):
            finish(s0, st)
    return cx


def stage_C1a(inp, resA1, resB1, resNA, xT_list):
    cx = build_Ca(False)
    maps = []
    for c in range(NCORES):
        aT = np.zeros((1024, NT), NPBF)
        aT[:512] = np.asarray(resB1[c]["oT"])
        aT[512:, :TOWN] = np.asarray(resNA[c]["naT"])
        maps.append(dict(xT=xT_list[c], aT=aT, mod=np.asarray(resA1[c]["mod"]),
                         wout=np.ascontiguousarray(inp["od_w_out"][0]), lng=pcol(inp["ln1_g"][1]), lnb=pcol(inp["ln1_b"][1])))
    return run_spmd(cx, maps)


def stage_C1b(inp, resA1, resC1a):
    cx = build_C1b()
    oh = np.zeros((8, 8 * 128), np.float32)
    for e in range(8):
        oh[e, e * 128:(e + 1) * 128] = 1.0
    shared = dict(wr=np.ascontiguousarray(inp["od_w_router"][0]), br=np.ascontiguousarray(inp["od_b_router"][0][:, None]),
                  wg=np.ascontiguousarray(inp["od_w_gate"][0]), wu=np.ascontiguousarray(inp["od_w_up"][0]),
                  wd=np.ascontiguousarray(inp["od_w_down"][0]), lng=pcol(inp["ln2_g"][1]), lnb=pcol(inp["ln2_b"][1]),
                  ident=np.eye(128, dtype=np.float32), onehot=oh)
    maps = []
    for c in range(NCORES):
        m = dict(shared)
        m.update(xT=np.asarray(resC1a[c]["xo"]), mod=np.asarray(resA1[c]["mod"]))
        maps.append(m)
    return run_spmd(cx, maps)


def kernel(**inp):
    inp = {k: np.asarray(v) for k, v in inp.items()}
    resA0 = stage_A0(inp)
    resB0 = stage_B0(resA0)
    resC0a = stage_C0a(inp, resA0, resB0)
    resC0b = stage_C0b(inp, resA0, resC0a)
    x1 = [np.asarray(resC0b[c]["xo"]) for c in range(NCORES)]
    resA1 = stage_A1(inp, x1)
    resB1 = stage_B1(resA1)
    resNA = stage_NA(inp, resA1)
    resC1a = stage_C1a(inp, resA1, resB1, resNA, x1)
    resC1b = stage_C1b(inp, resA1, resC1a)
    out = np.empty((2, SEQ, D), np.float32)
    for c in range(NCORES):
        b, j = divmod(c, 4)
        out[b, j * TOWN:(j + 1) * TOWN] = np.asarray(resC1b[c]["xo"]).T
    return out
```
